# Optimizing a Trainium2 kernel written in Bass

```python
import math
import jax, jax.numpy as jnp
from jax import lax
import numpy as np

D_MODEL = 1024
BATCH = 4
SEQ = 8192
DEPTH = 1

CHUNK = 64
Q_BLOCK = 128
EPS = 1e-6

FOX_HEAD_DIM = 64
FOX_WIDTH = D_MODEL // 2
FOX_HEADS = FOX_WIDTH // FOX_HEAD_DIM

SSM_WIDTH = D_MODEL // 2
SSM_GROUP = 16
SSM_GROUPS = SSM_WIDTH // SSM_GROUP
SSM_STATE = 64
DT_MIN = 1e-3
DT_MAX = 1e-1

N_BRANCHES = 2

COL_Q = FOX_WIDTH
COL_K = COL_Q + FOX_WIDTH
COL_V = COL_K + FOX_WIDTH
COL_F = COL_V + FOX_HEADS
COL_U = COL_F + SSM_WIDTH
IN_COLS = COL_U + N_BRANCHES * D_MODEL

N_GROUPS = 4
EXPERTS_PER_GROUP = 4
N_EXPERTS = N_GROUPS * EXPERTS_PER_GROUP
TOP_K_IN_GROUP = 2
EXPERT_FF = D_MODEL // 4

kernel_name = "hybrid_fox_s5_hiermoe_block"


def rms_norm(x, g):
    xf = x.astype(jnp.float32)
    y = xf * lax.rsqrt(jnp.mean(xf * xf, axis=-1, keepdims=True) + EPS)
    return (y * g.astype(jnp.float32)).astype(x.dtype)


def forgetting_attention(q, k, v, cum):
    seq = q.shape[1]
    scale = 1.0 / math.sqrt(FOX_HEAD_DIM)
    cum_h = jnp.transpose(cum, (0, 2, 1))
    outs = []
    for blk in range(seq // Q_BLOCK):
        lo, hi = blk * Q_BLOCK, (blk + 1) * Q_BLOCK
        qb, kb, vb = q[:, lo:hi], k[:, :hi], v[:, :hi]
        s = jnp.einsum('bqhd,bkhd->bhqk', qb, kb).astype(jnp.float32) * scale
        s = s + cum_h[:, :, lo:hi, None] - cum_h[:, :, None, :hi]
        q_pos = lo + jnp.arange(Q_BLOCK)
        k_pos = jnp.arange(hi)
        mask = k_pos[None, :] <= q_pos[:, None]
        s = jnp.where(mask[None, None], s, -jnp.inf)
        p = jax.nn.softmax(s, axis=-1).astype(vb.dtype)
        outs.append(jnp.einsum('bhqk,bkhd->bqhd', p, vb))
    return jnp.concatenate(outs, axis=1)


def s5_ssm(u, lambda_re, lambda_im, log_step, b_re, b_im, c_re, c_im, d_skip):
    bsz, seq, _ = u.shape
    f32 = jnp.float32
    ug = u.reshape(bsz, seq, SSM_GROUPS, SSM_GROUP).astype(f32)
    dt = jnp.exp(log_step.astype(f32))[:, None]
    lr = lambda_re.astype(f32)
    li = lambda_im.astype(f32)
    mag = jnp.exp(lr * dt)
    abar_re = mag * jnp.cos(li * dt)
    abar_im = mag * jnp.sin(li * dt)
    den = lr * lr + li * li
    num_re = abar_re - 1.0
    z_re = (num_re * lr + abar_im * li) / den
    z_im = (abar_im * lr - num_re * li) / den
    br = b_re.astype(f32)
    bi = b_im.astype(f32)
    bb_re = z_re[..., None] * br - z_im[..., None] * bi
    bb_im = z_re[..., None] * bi + z_im[..., None] * br
    bu_re = jnp.einsum('bsgc,gpc->bsgp', ug, bb_re)
    bu_im = jnp.einsum('bsgc,gpc->bsgp', ug, bb_im)
    a_re = jnp.broadcast_to(abar_re, (1, seq, SSM_GROUPS, SSM_STATE))
    a_im = jnp.broadcast_to(abar_im, (1, seq, SSM_GROUPS, SSM_STATE))

    def combine(left, right):
        ar1, ai1, br1, bi1 = left
        ar2, ai2, br2, bi2 = right
        return (ar2 * ar1 - ai2 * ai1,
                ar2 * ai1 + ai2 * ar1,
                ar2 * br1 - ai2 * bi1 + br2,
                ar2 * bi1 + ai2 * br1 + bi2)

    _, _, s_re, s_im = lax.associative_scan(combine, (a_re, a_im, bu_re, bu_im), axis=1)
    y = (jnp.einsum('bsgp,gcp->bsgc', s_re, c_re.astype(f32))
         - jnp.einsum('bsgp,gcp->bsgc', s_im, c_im.astype(f32)))
    y = y + d_skip.astype(f32).reshape(SSM_GROUPS, SSM_GROUP) * ug
    return y.reshape(bsz, seq, SSM_WIDTH)


def hierarchical_moe(h, w_rg, b_rg, w_re, b_re, w_gate, w_up, w_down):
    f32 = jnp.float32
    logits_g = (h @ w_rg).astype(f32) + b_rg.astype(f32)
    p_g = jax.nn.softmax(logits_g, axis=-1)
    g_idx = jnp.argmax(logits_g, axis=-1)
    p_sel = jnp.max(p_g, axis=-1)
    logits_e = ((h @ w_re).astype(f32) + b_re.astype(f32)).reshape(
        h.shape[0], h.shape[1], N_GROUPS, EXPERTS_PER_GROUP)
    le_sel = jnp.einsum('bsg,bsge->bse', jax.nn.one_hot(g_idx, N_GROUPS, dtype=f32), logits_e)
    top_v, top_i = lax.top_k(le_sel, TOP_K_IN_GROUP)
    w = jax.nn.softmax(top_v, axis=-1) * p_sel[..., None]
    expert_id = g_idx[..., None] * EXPERTS_PER_GROUP + top_i
    combine_w = jnp.einsum('bsk,bske->bse', w,
                           jax.nn.one_hot(expert_id, N_EXPERTS, dtype=f32)).astype(h.dtype)
    hg = jnp.einsum('bsd,edf->bsef', h, w_gate)
    hu = jnp.einsum('bsd,edf->bsef', h, w_up)
    act = jax.nn.silu(hg) * hu * combine_w[..., None]
    return jnp.einsum('bsef,efd->bsd', act, w_down)


def setup_inputs(seed: int = 0) -> dict:
    key = jax.random.key(seed)
    ks = jax.random.split(key, 32)
    f32 = jnp.float32
    L = DEPTH

    def nrm(k, shape, scale):
        return jax.random.normal(k, shape, f32) * scale

    x = nrm(ks[0], (BATCH, SEQ, D_MODEL), 1.0)
    g_mix = 1.0 + nrm(ks[1], (L, D_MODEL), 0.02)
    w_in = nrm(ks[2], (L, D_MODEL, IN_COLS), D_MODEL ** -0.5)
    b_forget = 2.0 + nrm(ks[3], (L, FOX_HEADS), 0.1)
    b_gate = nrm(ks[4], (L, N_BRANCHES * D_MODEL), 0.02)
    w_out_a = nrm(ks[5], (L, FOX_WIDTH, D_MODEL), FOX_WIDTH ** -0.5)
    lambda_re = -0.5 + nrm(ks[6], (L, SSM_GROUPS, SSM_STATE), 0.01)
    lambda_im = (jnp.pi * jnp.arange(SSM_STATE, dtype=f32))[None, None, :] \
        + nrm(ks[7], (L, SSM_GROUPS, SSM_STATE), 0.01)
    log_step = jax.random.uniform(ks[8], (L, SSM_GROUPS), f32,
                                  math.log(DT_MIN), math.log(DT_MAX))
    ssm_b_re = nrm(ks[9], (L, SSM_GROUPS, SSM_STATE, SSM_GROUP), (2 * SSM_GROUP) ** -0.5)
    ssm_b_im = nrm(ks[10], (L, SSM_GROUPS, SSM_STATE, SSM_GROUP), (2 * SSM_GROUP) ** -0.5)
    ssm_c_re = nrm(ks[11], (L, SSM_GROUPS, SSM_GROUP, SSM_STATE), SSM_STATE ** -0.5)
    ssm_c_im = nrm(ks[12], (L, SSM_GROUPS, SSM_GROUP, SSM_STATE), SSM_STATE ** -0.5)
    ssm_d = nrm(ks[13], (L, SSM_WIDTH), 1.0)
    w_glu = nrm(ks[14], (L, SSM_WIDTH, SSM_WIDTH), SSM_WIDTH ** -0.5)
    b_glu = nrm(ks[15], (L, SSM_WIDTH), 0.02)
    w_out_b = nrm(ks[16], (L, SSM_WIDTH, D_MODEL), SSM_WIDTH ** -0.5)
    w_out = nrm(ks[17], (L, D_MODEL, D_MODEL), D_MODEL ** -0.5)
    g_ffn = 1.0 + nrm(ks[18], (L, D_MODEL), 0.02)
    w_router_group = nrm(ks[19], (L, D_MODEL, N_GROUPS), D_MODEL ** -0.5)
    b_router_group = nrm(ks[20], (L, N_GROUPS), 0.01)
    w_router_expert = nrm(ks[21], (L, D_MODEL, N_EXPERTS), D_MODEL ** -0.5)
    b_router_expert = nrm(ks[22], (L, N_EXPERTS), 0.01)
    w_exp_gate = nrm(ks[23], (L, N_EXPERTS, D_MODEL, EXPERT_FF), D_MODEL ** -0.5)
    w_exp_up = nrm(ks[24], (L, N_EXPERTS, D_MODEL, EXPERT_FF), D_MODEL ** -0.5)
    w_exp_down = nrm(ks[25], (L, N_EXPERTS, EXPERT_FF, D_MODEL), EXPERT_FF ** -0.5)
    g_final = 1.0 + nrm(ks[26], (D_MODEL,), 0.02)
    return {
        "x": x, "g_mix": g_mix, "w_in": w_in, "b_forget": b_forget, "b_gate": b_gate,
        "w_out_a": w_out_a, "lambda_re": lambda_re, "lambda_im": lambda_im,
        "log_step": log_step, "ssm_b_re": ssm_b_re, "ssm_b_im": ssm_b_im,
        "ssm_c_re": ssm_c_re, "ssm_c_im": ssm_c_im, "ssm_d": ssm_d,
        "w_glu": w_glu, "b_glu": b_glu, "w_out_b": w_out_b, "w_out": w_out,
        "g_ffn": g_ffn, "w_router_group": w_router_group, "b_router_group": b_router_group,
        "w_router_expert": w_router_expert, "b_router_expert": b_router_expert,
        "w_exp_gate": w_exp_gate, "w_exp_up": w_exp_up, "w_exp_down": w_exp_down,
        "g_final": g_final,
    }


def reference(x, g_mix, w_in, b_forget, b_gate, w_out_a, lambda_re, lambda_im, log_step,
              ssm_b_re, ssm_b_im, ssm_c_re, ssm_c_im, ssm_d, w_glu, b_glu, w_out_b, w_out,
              g_ffn, w_router_group, b_router_group, w_router_expert, b_router_expert,
              w_exp_gate, w_exp_up, w_exp_down, g_final):
    bsz, seq, _ = x.shape
    for layer in range(DEPTH):
        h = rms_norm(x, g_mix[layer])
        proj = h @ w_in[layer]
        q, k, v, f_logit, u, gate_logit = jnp.split(
            proj, [COL_Q, COL_K, COL_V, COL_F, COL_U], axis=-1)

        q = q.reshape(bsz, seq, FOX_HEADS, FOX_HEAD_DIM)
        k = k.reshape(bsz, seq, FOX_HEADS, FOX_HEAD_DIM)
        v = v.reshape(bsz, seq, FOX_HEADS, FOX_HEAD_DIM)
        log_f = jax.nn.log_sigmoid(f_logit.astype(jnp.float32)
                                   + b_forget[layer].astype(jnp.float32))
        cum = jnp.cumsum(log_f, axis=1)
        o_a = forgetting_attention(q, k, v, cum).reshape(bsz, seq, FOX_WIDTH)
        y_a = o_a @ w_out_a[layer]

        y_s = s5_ssm(u, lambda_re[layer], lambda_im[layer], log_step[layer],
                     ssm_b_re[layer], ssm_b_im[layer], ssm_c_re[layer], ssm_c_im[layer],
                     ssm_d[layer]).astype(x.dtype)
        z = jax.nn.gelu(y_s)
        z = z * jax.nn.sigmoid(z @ w_glu[layer] + b_glu[layer])
        y_b = z @ w_out_b[layer]

        gates = jax.nn.sigmoid(gate_logit + b_gate[layer]).reshape(
            bsz, seq, N_BRANCHES, D_MODEL)
        merged = gates[:, :, 0, :] * y_a + gates[:, :, 1, :] * y_b
        x = x + merged @ w_out[layer]

        h2 = rms_norm(x, g_ffn[layer])
        x = x + hierarchical_moe(h2, w_router_group[layer], b_router_group[layer],
                                 w_router_expert[layer], b_router_expert[layer],
                                 w_exp_gate[layer], w_exp_up[layer], w_exp_down[layer])
    return rms_norm(x, g_final)
```

```python
import contextlib
import numpy as np
import concourse.bass as bass
import concourse.mybir as mybir
from concourse.bass_utils import run_bass_kernel_spmd

F32 = mybir.dt.float32
BF16 = mybir.dt.bfloat16
I32 = mybir.dt.int32
AF = mybir.ActivationFunctionType
ALU = mybir.AluOpType

NDSEM = 48
SAME_SYNC = {'pe': False, 'act': True, 'dve': True, 'pool': True, 'sp': False}
EPS = 1e-6
L = 16
TWO_PI = 6.283185307179586


class Ctx:
    def __init__(self, nc):
        self.nc = nc
        self.names = ['pe', 'act', 'dve', 'pool', 'sp']
        self.ops = {e: [] for e in self.names}
        self.cnt = {e: 0 for e in self.names}
        self.seen = {e: {} for e in self.names}
        self.pending = {e: {} for e in self.names}
        self.lastw = {}
        self.readers = {}
        self.dval = [0] * NDSEM
        self.dnext = 0
        self.sb_off = 16640
        self.uid = 0
        self.sb_max = 0

    def sb(self, shape, dtype, name="t"):
        esz = {F32: 4, BF16: 2, I32: 4}[dtype]
        n = 1
        for s in shape[1:]:
            n *= s
        off = (self.sb_off + 63) // 64 * 64
        self.sb_off = off + n * esz
        self.sb_max = max(self.sb_max, self.sb_off)
        assert self.sb_off <= 229376, f"SBUF overflow {self.sb_off} ({name})"
        self.uid += 1
        return self.nc.alloc_sbuf_tensor_at(f"{name}_{self.uid}", list(shape), dtype, offset=off)

    def mark(self):
        return self.sb_off

    def release(self, m):
        self.sb_off = m

    def _deps(self, reads, writes):
        deps = {}
        for k in reads:
            t = self.lastw.get(k)
            if t and deps.get(t[0], 0) < t[1]:
                deps[t[0]] = t[1]
        for k in writes:
            t = self.lastw.get(k)
            if t and deps.get(t[0], 0) < t[1]:
                deps[t[0]] = t[1]
            for s, v in self.readers.get(k, {}).items():
                if deps.get(s, 0) < v:
                    deps[s] = v
        return deps

    def _waits(self, e, deps):
        for s, v in self.pending[e].items():
            if deps.get(s, 0) < v:
                deps[s] = v
        self.pending[e] = {}
        waits = []
        for s, v in deps.items():
            if s == e and not SAME_SYNC[e]:
                continue
            if self.seen[e].get(s, 0) >= v:
                continue
            self.seen[e][s] = v
            waits.append((s, v))
        return waits

    def _commit(self, tok, reads, writes):
        for k in reads:
            r = self.readers.setdefault(k, {})
            if r.get(tok[0], 0) < tok[1]:
                r[tok[0]] = tok[1]
        for k in writes:
            self.lastw[k] = tok
            self.readers[k] = {}

    def op(self, e, fn, reads=(), writes=()):
        deps = self._deps(reads, writes)
        waits = self._waits(e, deps)
        self.cnt[e] += 1
        tok = (e, self.cnt[e])
        self.ops[e].append((waits, fn, e))
        self._commit(tok, reads, writes)
        return tok

    def dma(self, e, out, in_, reads=(), writes=()):
        i = self.dnext
        self.dnext = (self.dnext + 1) % NDSEM
        deps = self._deps(reads, writes)
        if self.dval[i] > 0:
            s = ('d', i)
            if deps.get(s, 0) < self.dval[i]:
                deps[s] = self.dval[i]
        waits = self._waits(e, deps)
        self.dval[i] += 16
        tok = (('d', i), self.dval[i])
        self.ops[e].append((waits, lambda eng: eng.dma_start(out=out, in_=in_), ('d', i)))
        self._commit(tok, reads, writes)
        return tok

    def barrier(self):
        allt = {e: self.cnt[e] for e in self.names if self.cnt[e] > 0}
        for i in range(NDSEM):
            if self.dval[i] > 0:
                allt[('d', i)] = self.dval[i]
        for e in self.names:
            for s, v in allt.items():
                if self.pending[e].get(s, 0) < v:
                    self.pending[e][s] = v

    def emit(self):
        nc = self.nc
        self.barrier()
        self.op('sp', lambda eng: eng.nop(), (), ())
        with contextlib.ExitStack() as st:
            sems = {e: st.enter_context(nc.semaphore(f"s_{e}")) for e in self.names}
            for i in range(NDSEM):
                sems[('d', i)] = st.enter_context(nc.semaphore(f"d_{i}"))
            block = st.enter_context(nc.Block())

            def run(e, eng):
                for waits, fn, inc in self.ops[e]:
                    for s, v in waits:
                        eng.wait_ge(sems[s], v)
                    ins = fn(eng)
                    if isinstance(inc, tuple):
                        ins.then_inc(sems[inc], 16)
                    else:
                        ins.then_inc(sems[inc], 1)

            @block.sync
            def _(eng):
                run('sp', eng)

            @block.tensor
            def _(eng):
                run('pe', eng)

            @block.scalar
            def _(eng):
                run('act', eng)

            @block.vector
            def _(eng):
                run('dve', eng)

            @block.gpsimd
            def _(eng):
                run('pool', eng)


def build_program(TH, dbg=(), stop=None):
    nc = bass.Bass("TRN2", target_bir_lowering=False)
    T2 = 2 * TH
    NT = TH // 512
    NBH = TH // 128
    NCH = TH // L
    c = Ctx(nc)

    def din(name, shape, dt=F32):
        return nc.dram_tensor(name, list(shape), dt, kind="ExternalInput").ap()

    def dscr(name, shape, dt):
        return nc.dram_tensor(name, list(shape), dt).ap()

    xin = din("xin", [T2, 1024])
    flag_d = din("flag", [128, 1])
    w_in = din("w_in", [1024, 4104])
    gmixT = din("gmixT", [128, 8])
    bforget = din("bforget", [8, 1])
    bgate = din("bgate", [128, 16])
    w_out_a = din("w_out_a", [512, 1024])
    w_glu = din("w_glu", [512, 512])
    bglu = din("bglu", [128, 4])
    w_out_b = din("w_out_b", [512, 1024])
    w_out = din("w_out", [1024, 1024])
    gffnT = din("gffnT", [128, 8])
    wr = din("wr", [1024, 20])
    br = din("br", [128, 20])
    w_eg = din("w_eg", [16, 1024, 256])
    w_eu = din("w_eu", [16, 1024, 256])
    w_ed = din("w_ed", [16, 256, 1024])
    gfin = din("gfin", [128, 1024])
    ssm_names = ["LRX", "LIX", "LSX", "BRX", "BIX", "LRY", "LIY", "LSY", "CRY", "CIY", "BRY", "BIY"]
    ssm_in = {n: din(n, [128, 512]) for n in ssm_names}
    ddg = din("DDG", [128, 512])
    ident_d = din("ident", [128, 128])
    tri_d = din("tri", [128, 128])
    sel_d = din("sel", [16, 2048])
    out_d = nc.dram_tensor("out", [TH, 1024], F32, kind="ExternalOutput").ap()
    dbg_out = {}
    for name, shape in dbg:
        dbg_out[name] = nc.dram_tensor(name, list(shape), F32, kind="ExternalOutput").ap()

    kT_d = dscr("kT_d", [8, 70, T2], BF16)
    qT_d = dscr("qT_d", [8, 70, TH], BF16)
    v_d = dscr("v_d", [T2, 520], BF16)
    uT_d = dscr("uT_d", [512, T2], BF16)
    gT_d = dscr("gT_d", [2048, TH], BF16)
    oT_d = dscr("oT_d", [8, 64, TH], BF16)
    zT_d = dscr("zT_d", [512, TH], BF16)
    x1_d = dscr("x1_d", [TH, 1024], F32)
    h2T_d = dscr("h2T_d", [1024, TH], BF16)
    cwT_d = dscr("cwT_d", [16, TH], BF16)

    ps = [nc.alloc_psum_tensor(f"ps{i}", [128, 512], F32) for i in range(6)]
    pb = [nc.alloc_psum_tensor(f"pb{i}", [128, 1024], BF16) for i in range(2)]

    def V(fn, r=(), w=()):
        return c.op('dve', fn, r, w)

    def A(fn, r=(), w=()):
        return c.op('act', fn, r, w)

    def G(fn, r=(), w=()):
        return c.op('pool', fn, r, w)

    def MM(out, lhsT, rhs, st, sp_, r, w, tp=None):
        kw = dict(start=st, stop=sp_)
        if tp is not None:
            kw['tile_position'] = tp
        return c.op('pe', lambda e: e.matmul(out, lhsT=lhsT, rhs=rhs, **kw), r, w)

    def TR(out, in_, ident, r, w):
        return c.op('pe', lambda e: e.transpose(out, in_, ident), r, w)

    def D(out, in_, r=(), w=(), q='sp'):
        return c.dma(q, out, in_, r, w)


    dump_i = [0]
    dump_st = []

    def dump(name, ap2d, ncols):
        if name not in dbg_out:
            return
        for c0 in range(0, ncols, 2048):
            n = min(2048, ncols - c0)
            dump_i[0] += 1
            kx = 'dump0'
            if not dump_st:
                dump_st.append(c.sb([128, 2048], F32, "dumpst"))
            stt = dump_st[0]
            V(lambda e, stt=stt, c0=c0, n=n: e.tensor_copy(out=stt[:, 0:n], in_=ap2d[:, c0:c0 + n]), [], [kx])
            D(dbg_out[name][:, c0:c0 + n], stt[:, 0:n], r=[kx])
    identf = c.sb([128, 128], F32, "identf")
    identb = c.sb([128, 128], BF16, "identb")
    trib = c.sb([128, 128], BF16, "trib")
    onesf = c.sb([128, 512], F32, "onesf")
    flag = c.sb([128, 1], F32, "flag")
    stg = c.sb([128, 128], F32, "stg")
    D(identf[:], ident_d, w=['identf'])
    D(stg[:], tri_d, w=['stg'])
    D(flag[:], flag_d, w=['flag'])
    V(lambda e: e.tensor_copy(out=identb[:], in_=identf[:]), ['identf'], ['identb'])
    V(lambda e: e.tensor_copy(out=trib[:], in_=stg[:]), ['stg'], ['trib'])
    V(lambda e: e.memset(onesf[:], 1.0), (), ['onesf'])
    base_mark = c.mark()

    def rstd_of(ss, n, tag):
        ms = c.sb([128, n], F32, "ms")
        rs = c.sb([128, n], F32, "rs")
        V(lambda e: e.tensor_scalar(out=ms[:], in0=ss[:], scalar1=1.0 / 1024, scalar2=EPS, op0=ALU.mult, op1=ALU.add), [tag + 'ss'], [tag + 'ms'])
        A(lambda e: e.activation(out=ms[:], in_=ms[:], func=AF.Sqrt), [tag + 'ms'], [tag + 'ms'])
        V(lambda e: e.reciprocal(out=rs[:], in_=ms[:]), [tag + 'ms'], [tag + 'rs'])
        return rs

    Win = c.sb([128, 8, 4104], BF16, "Win")
    gm = c.sb([128, 8], F32, "gm")
    negb = c.sb([8, 1], F32, "negb")
    bg = c.sb([128, 16], F32, "bg")
    D(gm[:], gmixT, w=['gm'])
    D(negb[:], bforget, w=['negb'])
    D(bg[:], bgate, w=['bg'])
    V(lambda e: e.tensor_scalar(out=negb[:], in0=negb[:], scalar1=-1.0, scalar2=None, op0=ALU.mult), ['negb'], ['negb'])
    wst = [c.sb([128, 8, 256], F32, "wst")] * 2
    w_in_v = w_in.rearrange("(kc p) n -> p kc n", p=128)
    ei = 0
    for cc in range(17):
        c0 = cc * 256
        ncol = min(256, 4104 - c0)
        st = wst[cc % 2]
        sk = 'wst'
        D(st[:, :, 0:ncol], w_in_v[:, :, c0:c0 + ncol], w=[sk])
        for kc in range(8):
            if ei % 2 == 0:
                A(lambda e, st=st, kc=kc, c0=c0, ncol=ncol: e.activation(out=Win[:, kc, c0:c0 + ncol], in_=st[:, kc, 0:ncol], func=AF.Copy, scale=gm[:, kc:kc + 1]), [sk, 'gm'], ['Win'])
            else:
                V(lambda e, st=st, kc=kc, c0=c0, ncol=ncol: e.tensor_scalar(out=Win[:, kc, c0:c0 + ncol], in0=st[:, kc, 0:ncol], scalar1=gm[:, kc:kc + 1], scalar2=None, op0=ALU.mult), [sk, 'gm'], ['Win'])
            ei += 1

    CQ, CK, CV, CF, CU, CG = 0, 512, 1024, 1536, 1544, 2056
    xt_b = [c.sb([128, 4, 1024], F32, "xt") for _ in range(2)]
    xs = c.sb([128, 4, 1024], BF16, "xs")
    hT_b = [c.sb([128, 8, 512], BF16, "hT") for _ in range(2)]
    junk = c.sb([128, 1024], BF16, "junk")
    junk_b = [junk, c.sb([128, 1024], BF16, "junkb")]
    ss = c.sb([128, 4], F32, "ss")
    kT_s = [c.sb([64, 8, 512], BF16, "kTs")] * 2
    qT_s = [c.sb([64, 8, 512], BF16, "qTs")] * 2
    v_s = [c.sb([128, 4, 8, 65], BF16, "vs")] * 2
    uT_s = [c.sb([128, 4, 512], BF16, "uTs")] * 2
    gT_s = [c.sb([128, 16, 512], BF16, "gTs")] * 2
    CPK = c.sb([8, 6, 512], BF16, "CPK")
    CPQ = c.sb([8, 6, 512], BF16, "CPQ")
    e1 = c.sb([8, 512], F32, "e1")
    negc = c.sb([8, 512], F32, "negc")
    r1 = c.sb([8, 512], F32, "r1")
    carry = c.sb([8, 1], F32, "carry")
    ones_own = c.sb([128, 32], BF16, "ones_own")
    ones_ctx = c.sb([128, 32], BF16, "ones_ctx")
    V(lambda e: e.memset(ones_own[:], 1.0), (), ['ones_own'])
    V(lambda e: e.tensor_scalar(out=ones_ctx[:], in0=onesf[:, 0:32], scalar1=flag[:, 0:1], scalar2=None, op0=ALU.mult), ['onesf', 'flag'], ['ones_ctx'])
    V(lambda e: e.memset(CPK[:, 0:3, :], 1.0), (), ['CPK'])
    V(lambda e: e.memset(CPQ[:, 3:6, :], 1.0), (), ['CPQ'])
    V(lambda e: e.memset(carry[:], 0.0), (), ['carry'])

    xin_v = xin.rearrange("(t j p) d -> t p j d", j=4, p=128)
    D(xt_b[0][:], xin_v[0], w=['xt0'])
    pi = [0]

    def nps():
        pi[0] = (pi[0] + 1) % 6
        return pi[0]

    for i in range(2 * NT):
        own = i >= NT
        b = i % 2
        xt, xk = xt_b[b], f'xt{b}'
        hT, hk = hT_b[b], f'hT{b}'
        if i + 1 < 2 * NT:
            D(xt_b[1 - b][:], xin_v[i + 1], w=[f'xt{1 - b}'])
        for j in range(4):
            jk_, jkk_ = junk_b[j % 2], f'junk{j % 2}'
            A(lambda e, j=j, xt=xt, jk_=jk_: e.activation(out=jk_[:], in_=xt[:, j, :], func=AF.Square), [xk], [jkk_])
            V(lambda e, j=j, jk_=jk_: e.tensor_reduce(out=ss[:, j:j + 1], in_=jk_[:], axis=mybir.AxisListType.X, op=ALU.add), [jkk_], ['Ass'])
        m0 = c.mark()
        rs = rstd_of(ss, 4, 'A')
        for j in range(4):
            V(lambda e, j=j, xt=xt, rs=rs: e.tensor_scalar(out=xs[:, j, :], in0=xt[:, j, :], scalar1=rs[:, j:j + 1], scalar2=None, op0=ALU.mult), [xk, 'Ars'], [f'xs{j}'])
        c.release(m0)
        for j in range(4):
            pbk = j % 2
            for kc in range(8):
                TR(pb[pbk][:, kc * 128:(kc + 1) * 128], xs[:, j, kc * 128:(kc + 1) * 128], identb[:], [f'xs{j}', 'identb'], [f'pb{pbk}'])
            src = pb[pbk][:, :].rearrange("p (k t) -> p k t", k=8)
            if j % 2 == 0:
                A(lambda e, j=j, hT=hT, src=src: e.activation(out=hT[:, :, j * 128:(j + 1) * 128], in_=src, func=AF.Copy), [f'pb{pbk}'], [hk])
            else:
                V(lambda e, j=j, hT=hT, src=src: e.tensor_copy(out=hT[:, :, j * 128:(j + 1) * 128], in_=src), [f'pb{pbk}'], [hk])
        tok0 = i * 512
        p = nps()
        for kc in range(8):
            MM(ps[p][0:8, :], Win[:, kc, CF:CF + 8], hT[:, kc, :], kc == 0, kc == 7, ['Win', hk], [f'ps{p}'])
        A(lambda e, p=p: e.activation(out=e1[:], in_=ps[p][0:8, :], func=AF.Exp, scale=-1.0, bias=negb[:, 0:1]), [f'ps{p}', 'negb'], ['e1'])
        A(lambda e: e.activation(out=e1[:], in_=e1[:], func=AF.Ln, bias=1.0), ['e1'], ['e1'])
        V(lambda e: e.tensor_tensor_scan(out=negc[:], data0=onesf[0:8, 0:512], data1=e1[:], initial=carry[:, 0:1], op0=ALU.mult, op1=ALU.add), ['e1', 'carry', 'onesf'], ['negc'])
        V(lambda e: e.tensor_copy(out=carry[:], in_=negc[:, 511:512]), ['negc'], ['carry'])
        V(lambda e: e.tensor_copy(out=CPK[:, 3, :], in_=negc[:]), ['negc'], ['CPK'])
        V(lambda e: e.tensor_tensor(out=r1[:], in0=negc[:], in1=CPK[:, 3, :], op=ALU.subtract), ['negc', 'CPK'], ['r1'])
        V(lambda e: e.tensor_copy(out=CPK[:, 4, :], in_=r1[:]), ['r1'], ['CPK'])
        V(lambda e: e.tensor_tensor(out=r1[:], in0=r1[:], in1=CPK[:, 4, :], op=ALU.subtract), ['r1', 'CPK'], ['r1'])
        V(lambda e: e.tensor_copy(out=CPK[:, 5, :], in_=r1[:]), ['r1'], ['CPK'])
        D(kT_d[:, 64:70, tok0:tok0 + 512], CPK[:, :, :], r=['CPK'], w=['kT_d'])
        if own:
            V(lambda e: e.tensor_scalar(out=CPQ[:, 0:3, :], in0=CPK[:, 3:6, :], scalar1=-1.0, scalar2=None, op0=ALU.mult), ['CPK'], ['CPQ'])
            D(qT_d[:, 64:70, tok0 - TH:tok0 - TH + 512], CPQ[:, :, :], r=['CPQ'], w=['qT_d'])
        kts, ktk = kT_s[b], 'kTs'
        for h in range(8):
            p = nps()
            for kc in range(8):
                MM(ps[p][0:64, :], Win[:, kc, CK + h * 64:CK + (h + 1) * 64], hT[:, kc, :], kc == 0, kc == 7, ['Win', hk], [f'ps{p}'])
            if h % 2 == 0:
                A(lambda e, p=p, h=h, kts=kts: e.activation(out=kts[:, h, :], in_=ps[p][0:64, :], func=AF.Copy), [f'ps{p}'], [ktk])
            else:
                V(lambda e, p=p, h=h, kts=kts: e.tensor_copy(out=kts[:, h, :], in_=ps[p][0:64, :]), [f'ps{p}'], [ktk])
        D(kT_d[:, 0:64, tok0:tok0 + 512].rearrange("h r t -> r h t"), kts[:, :, :], r=[ktk], w=['kT_d'])
        vs, vk = v_s[b], 'vs'
        V(lambda e, vs=vs, own=own: e.tensor_copy(out=vs[:, :, :, 64:65], in_=(ones_own if own else ones_ctx)[:, :].rearrange("p (j h o) -> p j h o", j=4, o=1)), ['ones_own', 'ones_ctx'], [vk])
        for j in range(4):
            p = nps()
            for kc in range(8):
                MM(ps[p][:, :], hT[:, kc, j * 128:(j + 1) * 128], Win[:, kc, CV:CV + 512], kc == 0, kc == 7, ['Win', hk], [f'ps{p}'])
            V(lambda e, p=p, j=j, vs=vs: e.tensor_copy(out=vs[:, j, :, 0:64], in_=ps[p][:, :].rearrange("p (h d) -> p h d", h=8)), [f'ps{p}'], [vk])
        D(v_d[tok0:tok0 + 512, :].rearrange("(j p) c -> p j c", p=128), vs[:, :, :, :].rearrange("p j h c -> p j (h c)"), r=[vk], w=['v_d'])
        us, uk = uT_s[b], 'uTs'
        for m in range(4):
            p = nps()
            for kc in range(8):
                MM(ps[p][:, :], Win[:, kc, CU + m * 128:CU + (m + 1) * 128], hT[:, kc, :], kc == 0, kc == 7, ['Win', hk], [f'ps{p}'])
            V(lambda e, p=p, m=m, us=us: e.tensor_copy(out=us[:, m, :], in_=ps[p][:, :]), [f'ps{p}'], [uk])
        D(uT_d.rearrange("(s p) t -> p s t", p=128)[:, :, tok0:tok0 + 512], us[:, :, :], r=[uk], w=['uT_d'])
        if own:
            qts, qtk = qT_s[b], 'qTs'
            for h in range(8):
                p = nps()
                for kc in range(8):
                    MM(ps[p][0:64, :], Win[:, kc, CQ + h * 64:CQ + (h + 1) * 64], hT[:, kc, :], kc == 0, kc == 7, ['Win', hk], [f'ps{p}'])
                A(lambda e, p=p, h=h, qts=qts: e.activation(out=qts[:, h, :], in_=ps[p][0:64, :], func=AF.Copy, scale=0.125), [f'ps{p}'], [qtk])
            D(qT_d[:, 0:64, tok0 - TH:tok0 - TH + 512].rearrange("h r t -> r h t"), qts[:, :, :], r=[qtk], w=['qT_d'])
            gs, gk = gT_s[b], 'gTs'
            for m in range(16):
                p = nps()
                for kc in range(8):
                    MM(ps[p][:, :], Win[:, kc, CG + m * 128:CG + (m + 1) * 128], hT[:, kc, :], kc == 0, kc == 7, ['Win', hk], [f'ps{p}'])
                A(lambda e, p=p, m=m, gs=gs: e.activation(out=gs[:, m, :], in_=ps[p][:, :], func=AF.Sigmoid, bias=bg[:, m:m + 1]), [f'ps{p}', 'bg'], [gk])
            D(gT_d.rearrange("(m p) t -> p m t", p=128)[:, :, tok0 - TH:tok0 - TH + 512], gs[:, :, :], r=[gk], w=['gT_d'])

    c.barrier()
    c.release(base_mark)
    if 'dbg_k' in dbg_out:
        tmpk = c.sb([70, T2], BF16, "tmpk")
        tmpf = c.sb([70, T2], F32, "tmpf")
        D(tmpk[:], kT_d[0], r=['kT_d'], w=['tmpk'])
        V(lambda e, tmpf=tmpf, tmpk=tmpk: e.tensor_copy(out=tmpf[:], in_=tmpk[:]), ['tmpk'], ['tmpf'])
        D(dbg_out['dbg_k'], tmpf[:], r=['tmpf'])
        c.barrier()
        c.release(base_mark)

    if stop == 'A':
        c.emit()
        return nc
    NB2 = T2 // 128
    v_all = c.sb([128, NB2, 520], BF16, "v_all")
    for q4 in range(0, NB2, 8):
        n = min(8, NB2 - q4)
        D(v_all[:, q4:q4 + n, :], v_d[q4 * 128:(q4 + n) * 128, :].rearrange("(j p) c -> p j c", p=128), r=['v_d'], w=['v_all'])
    kT_h = [c.sb([70, T2], BF16, "kTh") for _ in range(2)]
    qT_h = [c.sb([70, TH], BF16, "qTh") for _ in range(2)]
    pT_b = [c.sb([128, 512], BF16, "pT") for _ in range(3)]
    rr = c.sb([128, 512], F32, "rr")
    rrh = c.sb([128, 512], BF16, "rrh")
    rrl = c.sb([128, 512], BF16, "rrl")
    onesb = c.sb([128, 64], BF16, "onesb")
    V(lambda e: e.memset(onesb[:], 1.0), (), ['onesb'])
    bc_sb = c.sb([64, 512], F32, "bc_sb")
    oT_s = [c.sb([64, 512], BF16, "oTs") for _ in range(2)]
    D(kT_h[0][:], kT_d[0], r=['kT_d'], w=['kTh0'])
    D(qT_h[0][:], qT_d[0], r=['qT_d'], w=['qTh0'])
    items = []
    gi = 0
    for h in range(8):
        for Gq in range(NT):
            kbs = list(range(NBH)) + [NBH + ob for ob in range(4 * Gq + 4)]
            for idx, gkb in enumerate(kbs):
                ob = gkb - NBH
                diag = ob >= 4 * Gq
                c0 = (ob - 4 * Gq) * 128 if diag else 0
                items.append(dict(h=h, Gq=Gq, gkb=gkb, c0=c0, diag=diag, first=(idx == 0), last=(idx == len(kbs) - 1), gi=gi,
                                  newhead=(Gq == 0 and idx == 0)))
            gi += 1
    DEPTH = 2

    def att_stage1(i, it):
        h, Gq, gkb, c0 = it['h'], it['Gq'], it['gkb'], it['c0']
        hb = h % 2
        if it['newhead'] and h + 1 < 8:
            D(kT_h[1 - hb][:], kT_d[h + 1], r=['kT_d'], w=[f'kTh{1 - hb}'])
            D(qT_h[1 - hb][:], qT_d[h + 1], r=['qT_d'], w=[f'qTh{1 - hb}'])
        kt, ktk = kT_h[hb], f'kTh{hb}'
        qt, qtk = qT_h[hb], f'qTh{hb}'
        p = i % 3
        pT, ptk = pT_b[i % 3], f'pT{i % 3}'
        MM(ps[p][:, c0:512], kt[:, gkb * 128:(gkb + 1) * 128], qt[:, Gq * 512 + c0:Gq * 512 + 512], True, True, [ktk, qtk], [f'ps{p}'])
        A(lambda e, p=p, pT=pT, c0=c0: e.activation(out=pT[:, c0:512], in_=ps[p][:, c0:512], func=AF.Exp), [f'ps{p}'], [ptk])
        if it['diag']:
            G(lambda e, pT=pT, c0=c0: e.tensor_tensor(out=pT[:, c0:c0 + 128], in0=pT[:, c0:c0 + 128], in1=trib[:, :], op=ALU.mult), [ptk, 'trib'], [ptk])

    def att_stage2(i, it):
        h, Gq, gkb, c0 = it['h'], it['Gq'], it['gkb'], it['c0']
        po = 3 + (it['gi'] % 2)
        pT, ptk = pT_b[i % 3], f'pT{i % 3}'
        MM(ps[po][0:65, c0:512], v_all[:, gkb, h * 65:(h + 1) * 65], pT[:, c0:512], it['first'], it['last'], ['v_all', ptk], [f'ps{po}'])
        if it['last']:
            V(lambda e, po=po: e.reciprocal(out=rr[64:65, :], in_=ps[po][64:65, :]), [f'ps{po}'], ['rr'])
            V(lambda e: e.tensor_copy(out=rrh[64:65, :], in_=rr[64:65, :]), ['rr'], ['rrh'])
            V(lambda e: e.tensor_tensor(out=rr[64:65, :], in0=rr[64:65, :], in1=rrh[64:65, :], op=ALU.subtract), ['rr', 'rrh'], ['rr'])
            V(lambda e: e.tensor_copy(out=rrl[64:65, :], in_=rr[64:65, :]), ['rr'], ['rrl'])
            MM(ps[5][0:64, :], onesb[64:65, 0:64], rrh[64:65, :], True, False, ['onesb', 'rrh'], ['ps5'])
            MM(ps[5][0:64, :], onesb[64:65, 0:64], rrl[64:65, :], False, True, ['onesb', 'rrl'], ['ps5'])
            A(lambda e: e.activation(out=bc_sb[:], in_=ps[5][0:64, :], func=AF.Copy), ['ps5'], ['bc_sb'])
            ots, otk = oT_s[it['gi'] % 2], f"oTs{it['gi'] % 2}"
            V(lambda e, po=po, ots=ots: e.tensor_tensor(out=ots[:], in0=ps[po][0:64, :], in1=bc_sb[:], op=ALU.mult), [f'ps{po}', 'bc_sb'], [otk])
            D(oT_d[h, :, Gq * 512:(Gq + 1) * 512], ots[:], r=[otk], w=['oT_d'])

    for i in range(len(items) + DEPTH):
        if i < len(items):
            att_stage1(i, items[i])
        if i - DEPTH >= 0:
            att_stage2(i - DEPTH, items[i - DEPTH])
    c.barrier()
    c.release(base_mark)
    if 'dbg_o' in dbg_out:
        tmpk = c.sb([64, 8, TH], BF16, "tmpo")
        tmpf = c.sb([64, 8, TH], F32, "tmpof")
        D(tmpk[:], oT_d.rearrange("h d t -> d h t"), r=['oT_d'], w=['tmpk'])
        V(lambda e, tmpf=tmpf, tmpk=tmpk: e.tensor_copy(out=tmpf[:], in_=tmpk[:]), ['tmpk'], ['tmpf'])
        D(dbg_out['dbg_o'].rearrange("h d t -> d h t"), tmpf[:], r=['tmpf'])
        c.barrier()
        c.release(base_mark)

    if stop == 'B':
        c.emit()
        return nc
    def ssm_prep(sfx):
        k = 'pp' + sfx
        lr = c.sb([128, 512], F32, "lr"); li = c.sb([128, 512], F32, "li"); ls = c.sb([128, 512], F32, "ls")
        D(lr[:], ssm_in["LR" + sfx], w=[k + 'lr'])
        D(li[:], ssm_in["LI" + sfx], w=[k + 'li'])
        D(ls[:], ssm_in["LS" + sfx], w=[k + 'ls'])
        dt = c.sb([128, 512], F32, "dt"); mag = c.sb([128, 512], F32, "mag"); th = c.sb([128, 512], F32, "th")
        A(lambda e: e.activation(out=dt[:], in_=ls[:], func=AF.Exp), [k + 'ls'], [k + 'dt'])
        V(lambda e: e.tensor_tensor(out=mag[:], in0=lr[:], in1=dt[:], op=ALU.mult), [k + 'lr', k + 'dt'], [k + 'mag'])
        A(lambda e: e.activation(out=mag[:], in_=mag[:], func=AF.Exp), [k + 'mag'], [k + 'mag'])
        V(lambda e: e.tensor_tensor(out=th[:], in0=li[:], in1=dt[:], op=ALU.mult), [k + 'li', k + 'dt'], [k + 'th'])

        def sin_of(shift, outt, ok):
            t = c.sb([128, 512], F32, "t"); ni = c.sb([128, 512], I32, "ni"); nf = c.sb([128, 512], F32, "nf")
            a = c.sb([128, 512], F32, "a"); mk = c.sb([128, 512], F32, "mk")
            V(lambda e: e.tensor_scalar(out=t[:], in0=th[:], scalar1=1.0 / TWO_PI, scalar2=8.5 + shift, op0=ALU.mult, op1=ALU.add), [k + 'th'], [k + 't'])
            V(lambda e: e.tensor_copy(out=ni[:], in_=t[:]), [k + 't'], [k + 'ni'])
            V(lambda e: e.tensor_copy(out=nf[:], in_=ni[:]), [k + 'ni'], [k + 'nf'])
            V(lambda e: e.scalar_tensor_tensor(out=a[:], in0=t[:], scalar=-0.5, in1=nf[:], op0=ALU.add, op1=ALU.subtract), [k + 't', k + 'nf'], [k + 'a'])
            V(lambda e: e.tensor_single_scalar(out=mk[:], in_=a[:], scalar=-0.5, op=ALU.is_lt), [k + 'a'], [k + 'mk'])
            V(lambda e: e.tensor_tensor(out=a[:], in0=a[:], in1=mk[:], op=ALU.add), [k + 'a', k + 'mk'], [k + 'a'])
            A(lambda e: e.activation(out=outt[:], in_=a[:], func=AF.Sin, scale=TWO_PI), [k + 'a'], [ok])
        ar = c.sb([128, 512], F32, "ar"); ai = c.sb([128, 512], F32, "ai")
        sin_of(0.0, ai, k + 'ai')
        sin_of(0.25, ar, k + 'ar')
        V(lambda e: e.tensor_tensor(out=ai[:], in0=ai[:], in1=mag[:], op=ALU.mult), [k + 'ai', k + 'mag'], [k + 'ai'])
        V(lambda e: e.tensor_tensor(out=ar[:], in0=ar[:], in1=mag[:], op=ALU.mult), [k + 'ar', k + 'mag'], [k + 'ar'])
        zr = c.sb([128, 512], F32, "zr"); zi = c.sb([128, 512], F32, "zi")
        nr = c.sb([128, 512], F32, "nr"); den = c.sb([128, 512], F32, "den"); t2 = c.sb([128, 512], F32, "t2")
        V(lambda e: e.tensor_scalar(out=nr[:], in0=ar[:], scalar1=-1.0, scalar2=None, op0=ALU.add), [k + 'ar'], [k + 'nr'])
        V(lambda e: e.tensor_tensor(out=den[:], in0=lr[:], in1=lr[:], op=ALU.mult), [k + 'lr'], [k + 'den'])
        V(lambda e: e.tensor_tensor(out=t2[:], in0=li[:], in1=li[:], op=ALU.mult), [k + 'li'], [k + 't2'])
        V(lambda e: e.tensor_tensor(out=den[:], in0=den[:], in1=t2[:], op=ALU.add), [k + 'den', k + 't2'], [k + 'den'])
        V(lambda e: e.reciprocal(out=den[:], in_=den[:]), [k + 'den'], [k + 'den'])
        V(lambda e: e.tensor_tensor(out=zr[:], in0=nr[:], in1=lr[:], op=ALU.mult), [k + 'nr', k + 'lr'], [k + 'zr'])
        V(lambda e: e.tensor_tensor(out=t2[:], in0=ai[:], in1=li[:], op=ALU.mult), [k + 'ai', k + 'li'], [k + 't2'])
        V(lambda e: e.tensor_tensor(out=zr[:], in0=zr[:], in1=t2[:], op=ALU.add), [k + 'zr', k + 't2'], [k + 'zr'])
        V(lambda e: e.tensor_tensor(out=zr[:], in0=zr[:], in1=den[:], op=ALU.mult), [k + 'zr', k + 'den'], [k + 'zr'])
        V(lambda e: e.tensor_tensor(out=zi[:], in0=ai[:], in1=lr[:], op=ALU.mult), [k + 'ai', k + 'lr'], [k + 'zi'])
        V(lambda e: e.tensor_tensor(out=t2[:], in0=nr[:], in1=li[:], op=ALU.mult), [k + 'nr', k + 'li'], [k + 't2'])
        V(lambda e: e.tensor_tensor(out=zi[:], in0=zi[:], in1=t2[:], op=ALU.subtract), [k + 'zi', k + 't2'], [k + 'zi'])
        V(lambda e: e.tensor_tensor(out=zi[:], in0=zi[:], in1=den[:], op=ALU.mult), [k + 'zi', k + 'den'], [k + 'zi'])
        return ar, ai, zr, zi, k

    def cmul(outr, outi, xr, xi, yr, yi, keys_in, kor, koi, negate_im=False, view=None):
        ta_t, tb_t = cm_tmp
        ta = view(ta_t[:]) if view else ta_t[:]
        tb = view(tb_t[:]) if view else tb_t[:]
        V(lambda e: e.tensor_tensor(out=ta, in0=xr, in1=yr, op=ALU.mult), keys_in, ['cm_ta'])
        V(lambda e: e.tensor_tensor(out=tb, in0=xi, in1=yi, op=ALU.mult), keys_in, ['cm_tb'])
        V(lambda e: e.tensor_tensor(out=outr, in0=ta, in1=tb, op=ALU.subtract), ['cm_ta', 'cm_tb'], [kor])
        V(lambda e: e.tensor_tensor(out=ta, in0=xr, in1=yi, op=ALU.mult), keys_in + [kor], ['cm_ta'])
        V(lambda e: e.tensor_tensor(out=tb, in0=xi, in1=yr, op=ALU.mult), keys_in + [kor], ['cm_tb'])
        if negate_im:
            V(lambda e: e.scalar_tensor_tensor(out=outi, in0=ta, scalar=-1.0, in1=tb, op0=ALU.mult, op1=ALU.subtract), ['cm_ta', 'cm_tb'], [koi])
        else:
            V(lambda e: e.tensor_tensor(out=outi, in0=ta, in1=tb, op=ALU.add), ['cm_ta', 'cm_tb'], [koi])

    cm_tmp = []
    ZBT = c.sb([128, 4, L, 2, 128], BF16, "ZBT")
    CYT = c.sb([128, 16, L + 1, 2, 32], BF16, "CYT")
    TT = c.sb([128, 4, L, 128], BF16, "TT")
    BBY = c.sb([128, 2, 512], BF16, "BBY")
    AL = c.sb([128, 2, 2, 16], F32, "AL")
    tbl_mark = c.mark()
    ar, ai, zr, zi, k = ssm_prep('X')
    br_ = c.sb([128, 512], F32, "br"); bi_ = c.sb([128, 512], F32, "bi")
    D(br_[:], ssm_in["BRX"], w=['brx'])
    D(bi_[:], ssm_in["BIX"], w=['bix'])
    bbr = c.sb([128, 512], F32, "bbr"); bbi = c.sb([128, 512], F32, "bbi")
    cm_tmp[:] = [c.sb([128, 512], F32, "ta"), c.sb([128, 512], F32, "tb")]
    cmul(bbr[:], bbi[:], zr[:], zi[:], br_[:], bi_[:], [k + 'zr', k + 'zi', 'brx', 'bix'], 'bbr', 'bbi')
    pw = [c.sb([128, 2, 512], F32, "pw") for _ in range(2)]
    V(lambda e, pw=pw: e.memset(pw[0][:, 0, :], 1.0), (), ['pw0'])
    V(lambda e, pw=pw: e.memset(pw[0][:, 1, :], 0.0), (), ['pw0'])
    for n in range(L):
        cur, ck = pw[n % 2], f'pw{n % 2}'
        nxt, nk = pw[(n + 1) % 2], f'pw{(n + 1) % 2}'
        j = L - 1 - n
        cmul(ZBT[:, :, j, 0, :], ZBT[:, :, j, 1, :], cur[:, 0, :].rearrange("p (s q) -> p s q", s=4), cur[:, 1, :].rearrange("p (s q) -> p s q", s=4),
             bbr[:].rearrange("p (s q) -> p s q", s=4), bbi[:].rearrange("p (s q) -> p s q", s=4), [ck, 'bbr', 'bbi'], 'ZBT', 'ZBT',
             view=lambda ap: ap.rearrange("p (s q) -> p s q", s=4))
        if n < L - 1:
            cmul(nxt[:, 0, :], nxt[:, 1, :], cur[:, 0, :], cur[:, 1, :], ar[:], ai[:], [ck, k + 'ar', k + 'ai'], nk, nk)
    c.barrier()
    c.release(tbl_mark)
    ar, ai, zr, zi, k = ssm_prep('Y')
    br_ = c.sb([128, 512], F32, "br"); bi_ = c.sb([128, 512], F32, "bi")
    cr_ = c.sb([128, 512], F32, "cr"); ci_ = c.sb([128, 512], F32, "ci")
    D(br_[:], ssm_in["BRY"], w=['bry'])
    D(bi_[:], ssm_in["BIY"], w=['biy'])
    D(cr_[:], ssm_in["CRY"], w=['cry'])
    D(ci_[:], ssm_in["CIY"], w=['ciy'])
    cm_tmp[:] = [c.sb([128, 512], F32, "ta"), c.sb([128, 512], F32, "tb")]
    cmul(BBY[:, 0, :], BBY[:, 1, :], zr[:], zi[:], br_[:], bi_[:], [k + 'zr', k + 'zi', 'bry', 'biy'], 'BBY', 'BBY')
    pw = [c.sb([128, 2, 512], F32, "pw") for _ in range(2)]
    V(lambda e, pw=pw: e.memset(pw[0][:, 0, :], 1.0), (), ['pw0'])
    V(lambda e, pw=pw: e.memset(pw[0][:, 1, :], 0.0), (), ['pw0'])
    for n in range(L + 1):
        cur, ck = pw[n % 2], f'pw{n % 2}'
        nxt, nk = pw[(n + 1) % 2], f'pw{(n + 1) % 2}'
        cmul(CYT[:, :, n, 0, :], CYT[:, :, n, 1, :], cr_[:].rearrange("p (k j) -> p k j", k=16), ci_[:].rearrange("p (k j) -> p k j", k=16),
             cur[:, 0, :].rearrange("p (k j) -> p k j", k=16), cur[:, 1, :].rearrange("p (k j) -> p k j", k=16), [ck, 'cry', 'ciy'], 'CYT', 'CYT', negate_im=True,
             view=lambda ap: ap.rearrange("p (k j) -> p k j", k=16))
        if n < L:
            cmul(nxt[:, 0, :], nxt[:, 1, :], cur[:, 0, :], cur[:, 1, :], ar[:], ai[:], [ck, k + 'ar', k + 'ai'], nk, nk)
    pL, pLk = pw[L % 2], f'pw{L % 2}'
    pLv_r = pL[:, 0, :].rearrange("p (k j) -> p k j", k=16)[:, :, 0]
    pLv_i = pL[:, 1, :].rearrange("p (k j) -> p k j", k=16)[:, :, 0]
    V(lambda e: e.tensor_copy(out=AL[:, 0, 0, :], in_=pLv_r), [pLk], ['AL'])
    V(lambda e: e.tensor_copy(out=AL[:, 0, 1, :], in_=pLv_r), [pLk], ['AL'])
    V(lambda e: e.tensor_scalar(out=AL[:, 1, 0, :], in0=pLv_i, scalar1=-1.0, scalar2=None, op0=ALU.mult), [pLk], ['AL'])
    V(lambda e: e.tensor_copy(out=AL[:, 1, 1, :], in_=pLv_i), [pLk], ['AL'])
    V(lambda e: e.memset(TT[:], 0.0), (), ['TT'])
    ddt = c.sb([128, 512], F32, "ddt")
    D(ddt[:], ddg, w=['ddt'])
    for kk in range(16):
        s_, k4 = kk // 4, kk % 4
        p = nps()
        MM(ps[p][32 * k4:32 * k4 + 32, :], BBY[:, 0, kk * 32:(kk + 1) * 32], CYT[:, kk, 0:L, 0, :], True, False, ['BBY', 'CYT'], [f'ps{p}'], tp=(0, 32 * k4))
        MM(ps[p][32 * k4:32 * k4 + 32, :], BBY[:, 1, kk * 32:(kk + 1) * 32], CYT[:, kk, 0:L, 1, :], False, True, ['BBY', 'CYT'], [f'ps{p}'], tp=(0, 32 * k4))
        V(lambda e, p=p, s_=s_, k4=k4: e.tensor_copy(out=TT[32 * k4:32 * k4 + 32, s_, :, 32 * k4:32 * k4 + 32], in_=ps[p][32 * k4:32 * k4 + 32, :].rearrange("p (n j) -> p n j", n=L)), [f'ps{p}'], ['TT'])
    V(lambda e: e.tensor_tensor(out=TT[:, :, 0, :], in0=TT[:, :, 0, :], in1=ddt[:].rearrange("p (s q) -> p s q", s=4), op=ALU.add), ['TT', 'ddt'], ['TT'])
    c.barrier()
    c.release(tbl_mark)

    if stop == 'C1':
        c.barrier()
        dump('dbg_ZBT', ZBT[:].rearrange("p s j r q -> p (s j r q)"), 4 * L * 2 * 128)
        dump('dbg_CYT', CYT[:].rearrange("p k n r j -> p (k n r j)"), 16 * (L + 1) * 2 * 32)
        dump('dbg_TT', TT[:].rearrange("p s n q -> p (s n q)"), 4 * L * 128)
        dump('dbg_AL', AL[:].rearrange("p a r k -> p (a r k)"), 64)
        c.emit()
        return nc
    uT_all = c.sb([128, 4, TH], BF16, "uT_all")
    Zs = c.sb([128, 2, 16, NCH], BF16, "Zs")
    H = c.sb([128, 2, 16, NCH + 1], F32, "H")
    Hb = c.sb([128, 2, 16, NCH + 1], BF16, "Hb")
    t1 = c.sb([128, 2, 16], F32, "t1")
    t2_ = c.sb([128, 2, 16], F32, "t2_")
    G(lambda e: e.memset(H[:, :, :, 0], 0.0), (), ['H'])
    NZ = min(NCH, 512)
    for half in range(2):
        D(uT_all[:], uT_d.rearrange("(s p) t -> p s t", p=128)[:, :, half * TH:(half + 1) * TH], r=['uT_d'], w=['uT_all'])
        for kk in range(16):
            s_, k4 = kk // 4, kk % 4
            for ri in range(2):
                for z0 in range(0, NCH, NZ):
                    p = nps()
                    for j in range(L):
                        uview = uT_all[32 * k4:32 * k4 + 32, s_, :].rearrange("p (n j) -> p n j", j=L)[:, z0:z0 + NZ, j]
                        MM(ps[p][:, 0:NZ], ZBT[32 * k4:32 * k4 + 32, s_, j, ri, :], uview, j == 0, j == L - 1, ['ZBT', 'uT_all'], [f'ps{p}'], tp=(32 * k4, 0))
                    if (kk + ri) % 2 == 0:
                        A(lambda e, p=p, ri=ri, kk=kk, z0=z0: e.activation(out=Zs[:, ri, kk, z0:z0 + NZ], in_=ps[p][:, 0:NZ], func=AF.Copy), [f'ps{p}'], ['Zs'])
                    else:
                        V(lambda e, p=p, ri=ri, kk=kk, z0=z0: e.tensor_copy(out=Zs[:, ri, kk, z0:z0 + NZ], in_=ps[p][:, 0:NZ]), [f'ps{p}'], ['Zs'])
        for n in range(NCH):
            V(lambda e, n=n: e.tensor_tensor(out=t1[:], in0=AL[:, 0, :, :], in1=H[:, :, :, n], op=ALU.mult), ['AL', 'H'], ['t1'])
            V(lambda e, n=n: e.tensor_tensor(out=t2_[:, 0, :], in0=AL[:, 1, 0, :], in1=H[:, 1, :, n], op=ALU.mult), ['AL', 'H'], ['t2_'])
            V(lambda e, n=n: e.tensor_tensor(out=t2_[:, 1, :], in0=AL[:, 1, 1, :], in1=H[:, 0, :, n], op=ALU.mult), ['AL', 'H'], ['t2_'])
            V(lambda e: e.tensor_tensor(out=t1[:], in0=t1[:], in1=t2_[:], op=ALU.add), ['t1', 't2_'], ['t1'])
            V(lambda e, n=n: e.tensor_tensor(out=H[:, :, :, n + 1], in0=t1[:], in1=Zs[:, :, :, n], op=ALU.add), ['t1', 'Zs'], ['H'])
        if half == 0:
            V(lambda e: e.tensor_copy(out=H[:, :, :, 0], in_=H[:, :, :, NCH]), ['H'], ['H'])
    V(lambda e: e.tensor_copy(out=Hb[:], in_=H[:]), ['H'], ['Hb'])

    if stop == 'C2':
        c.barrier()
        dump('dbg_H', H[:].rearrange("p a k n -> p (a k n)"), 2 * 16 * (NCH + 1))
        dump('dbg_Zs', Zs[:].rearrange("p a k n -> p (a k n)"), 2 * 16 * NCH)
        c.emit()
        return nc
    zT_s = [c.sb([128, 512], BF16, "zTs") for _ in range(2)]
    x2 = c.sb([128, 512], F32, "x2")
    wv = c.sb([128, 512], F32, "wv")
    zi_ = 0
    for s_ in range(4):
        for ti in range(NT):
            p = nps()
            tok0 = ti * 512
            yv = ps[p][:, :].rearrange("p (n j) -> p n j", j=L)
            uv = uT_all[:, s_, tok0:tok0 + 512].rearrange("p (n j) -> p n j", j=L)
            for n in range(L):
                MM(yv[:, :, n:L], TT[:, s_, n, :], uv[:, :, 0:L - n], n == 0, False, ['TT', 'uT_all'], [f'ps{p}'])
            cnt = 0
            for k4 in range(4):
                kk = 4 * s_ + k4
                for i in range(L):
                    for ri in range(2):
                        cnt += 1
                        MM(yv[32 * k4:32 * k4 + 32, :, i], CYT[:, kk, i + 1, ri, :], Hb[:, ri, kk, ti * 32:ti * 32 + 32], False, cnt == 4 * L * 2, ['CYT', 'Hb'], [f'ps{p}'], tp=(0, 32 * k4))
            zt, ztk = zT_s[zi_ % 2], f'zTs{zi_ % 2}'
            zi_ += 1
            A(lambda e, p=p: e.activation(out=x2[:], in_=ps[p][:, :], func=AF.Square), [f'ps{p}'], ['x2'])
            V(lambda e: e.tensor_scalar(out=wv[:], in0=x2[:], scalar1=0.044715, scalar2=1.0, op0=ALU.mult, op1=ALU.add), ['x2'], ['wv'])
            V(lambda e, p=p: e.tensor_tensor(out=wv[:], in0=wv[:], in1=ps[p][:, :], op=ALU.mult), ['wv', f'ps{p}'], ['wv'])
            A(lambda e: e.activation(out=wv[:], in_=wv[:], func=AF.Sigmoid, scale=1.5957691216057308), ['wv'], ['wv'])
            V(lambda e, p=p, zt=zt: e.tensor_tensor(out=zt[:], in0=wv[:], in1=ps[p][:, :], op=ALU.mult), ['wv', f'ps{p}'], [ztk])
            D(zT_d[s_ * 128:(s_ + 1) * 128, ti * 512:(ti + 1) * 512], zt[:], r=[ztk], w=['zT_d'])
    c.barrier()
    c.release(base_mark)

    if 'dbg_y' in dbg_out:
        tmpz = c.sb([128, 4, TH], BF16, "tmpz")
        tmpzf = c.sb([128, 4, TH], F32, "tmpzf")
        D(tmpz[:], zT_d.rearrange("(s p) t -> p s t", p=128), r=['zT_d'], w=['tmpz'])
        V(lambda e, tmpzf=tmpzf, tmpz=tmpz: e.tensor_copy(out=tmpzf[:], in_=tmpz[:]), ['tmpz'], ['tmpzf'])
        D(dbg_out['dbg_y'].rearrange("(s p) t -> p s t", p=128), tmpzf[:], r=['tmpzf'])
        c.barrier()
        c.release(base_mark)
    if stop == 'C':
        c.emit()
        return nc
    def load_w(dram_view, shape, key, scale_ap=None):
        wt = c.sb(shape, BF16, key)
        a_n, ncol = shape[1], shape[2]
        for a_ in range(a_n):
            st_ = lw_st[:, 0:ncol]
            D(st_, dram_view[:, a_, :], w=['lw_st'])
            if scale_ap is None:
                V(lambda e, a_=a_, st_=st_: e.tensor_copy(out=wt[:, a_, :], in_=st_), ['lw_st'], [key])
            else:
                V(lambda e, a_=a_, st_=st_: e.tensor_scalar(out=wt[:, a_, :], in0=st_, scalar1=scale_ap[:, a_:a_ + 1], scalar2=None, op0=ALU.mult), ['lw_st', 'gf'], [key])
        return wt

    lw_st = c.sb([128, 1024], F32, "lw_st")
    gf = c.sb([128, 8], F32, "gf")
    D(gf[:], gffnT, w=['gf'])
    bgl = c.sb([128, 4], F32, "bgl")
    D(bgl[:], bglu, w=['bgl'])
    brt = c.sb([128, 20], F32, "brt")
    D(brt[:], br, w=['brt'])
    Wglu = load_w(w_glu.rearrange("(kc p) n -> p kc n", p=128), [128, 4, 512], 'Wglu')
    Wob = load_w(w_out_b.rearrange("(kc p) n -> p kc n", p=128), [128, 4, 1024], 'Wob')
    Woa = c.sb([64, 8, 1024], BF16, "Woa")
    woa_v = w_out_a.rearrange("(h p) n -> p h n", p=64)
    for h in range(8):
        D(lw_st[0:64, :], woa_v[:, h, :], w=['lw_st'])
        V(lambda e, h=h: e.tensor_copy(out=Woa[:, h, :], in_=lw_st[0:64, :]), ['lw_st'], ['Woa'])
    Wout = load_w(w_out.rearrange("(kc p) n -> p kc n", p=128), [128, 8, 1024], 'Wout')
    Wr = c.sb([128, 8, 20], F32, "Wr")
    D(Wr[:], wr.rearrange("(kc p) n -> p kc n", p=128), w=['Wr'])
    for kc in range(8):
        V(lambda e, kc=kc: e.tensor_scalar(out=Wr[:, kc, :], in0=Wr[:, kc, :], scalar1=gf[:, kc:kc + 1], scalar2=None, op0=ALU.mult), ['Wr', 'gf'], ['Wr'])

    if stop == 'D1':
        c.emit()
        return nc
    Wrhi = c.sb([128, 8, 20], BF16, "Wrhi")
    Wrlo = c.sb([128, 8, 20], BF16, "Wrlo")
    V(lambda e: e.tensor_copy(out=Wrhi[:], in_=Wr[:]), ['Wr'], ['Wrhi'])
    V(lambda e: e.tensor_tensor(out=Wr[:], in0=Wr[:], in1=Wrhi[:], op=ALU.subtract), ['Wr', 'Wrhi'], ['Wr'])
    V(lambda e: e.tensor_copy(out=Wrlo[:], in_=Wr[:]), ['Wr'], ['Wrlo'])
    h2hi = c.sb([128, 1024], BF16, "h2hi")
    h2lo = c.sb([128, 1024], BF16, "h2lo")
    hThi = c.sb([128, 8, 128], BF16, "hThi")
    hTlo = c.sb([128, 8, 128], BF16, "hTlo")
    cwb = c.sb([128, 16], BF16, "cwb")
    zT = c.sb([128, 4, 512], BF16, "zT")
    oT = c.sb([64, 8, 512], BF16, "oT")
    gT = c.sb([128, 16, 512], BF16, "gT")
    xt = c.sb([128, 4, 1024], F32, "xtD")
    zg = c.sb([128, 4, 512], BF16, "zg")
    sg = c.sb([128, 512], F32, "sg")
    mgd = c.sb([128, 8, 512], BF16, "mgd")
    ta_ = c.sb([128, 512], F32, "taD")
    tb_ = c.sb([128, 512], F32, "tbD")
    x1 = c.sb([128, 4, 1024], F32, "x1")
    h2f = c.sb([128, 1024], F32, "h2f")
    h2Tf = c.sb([128, 8, 128], F32, "h2Tf")
    h2Tb = c.sb([128, 8, 512], BF16, "h2Tb")
    cwTb = c.sb([16, 512], BF16, "cwTb")
    ss2 = c.sb([128, 4], F32, "ss2")
    junk2 = c.sb([128, 1024], BF16, "junk2")
    junk2_b = [junk2, c.sb([128, 1024], BF16, "junk2b")]
    lg = c.sb([128, 20], F32, "lg")
    cw = c.sb([128, 16], F32, "cw")
    sm = {n_: c.sb([128, 4], F32, n_) for n_ in ["gmx", "ge", "gsum", "oh", "les", "m1", "mk1", "le2", "m2", "mk2", "w12", "tmp4"]}
    one1 = {n_: c.sb([128, 1], F32, n_) for n_ in ["psel", "d21", "w1", "w2"]}
    xin_own = xin[TH:T2, :].rearrange("(t j p) d -> t p j d", j=4, p=128)
    zT_b2 = [zT, c.sb([128, 4, 512], BF16, "zT2")]
    oT_b2 = [oT, c.sb([64, 8, 512], BF16, "oT2")]
    gT_b2 = [gT, c.sb([128, 16, 512], BF16, "gT2")]
    xt_b2 = [xt, c.sb([128, 4, 1024], F32, "xtD2")]
    lg4 = c.sb([128, 4, 20], F32, "lg4")
    R4 = {n_: c.sb([128, 4, 4], F32, n_) for n_ in ["oh", "ge", "les", "mk1", "le2", "mk2", "w12", "tq"]}
    R1 = {n_: c.sb([128, 4], F32, n_) for n_ in ["mx", "gsum", "psel", "m1", "m2", "d", "w1", "w2"]}
    prod = c.sb([128, 4, 4, 4], F32, "prod")
    cw4 = c.sb([128, 4, 16], F32, "cw4")
    cwb4 = c.sb([128, 4, 16], BF16, "cwb4")
    AXX = mybir.AxisListType.X

    def bc3(ap2):
        return ap2.unsqueeze(2).broadcast_to([128, 4, 4])

    def load_tile(ti):
        b_ = ti % 2
        t0_ = ti * 512
        D(zT_b2[b_][:], zT_d.rearrange("(s p) t -> p s t", p=128)[:, :, t0_:t0_ + 512], r=['zT_d'], w=[f'zT{b_}'])
        D(oT_b2[b_][:], oT_d.rearrange("h d t -> d h t")[:, :, t0_:t0_ + 512], r=['oT_d'], w=[f'oT{b_}'])
        for hh in range(2):
            D(gT_b2[b_][:, hh * 8:(hh + 1) * 8, :], gT_d.rearrange("(m p) t -> p m t", p=128)[:, hh * 8:(hh + 1) * 8, t0_:t0_ + 512], r=['gT_d'], w=[f'gT{b_}'])
        D(xt_b2[b_][:], xin_own[ti], w=[f'xtD{b_}'])

    load_tile(0)
    for ti in range(NT):
        t0 = ti * 512
        b_ = ti % 2
        zT, oT, gT, xt = zT_b2[b_], oT_b2[b_], gT_b2[b_], xt_b2[b_]
        zTk, oTk, gTk, xtk = f'zT{b_}', f'oT{b_}', f'gT{b_}', f'xtD{b_}'
        if ti + 1 < NT:
            load_tile(ti + 1)
        for m in range(4):
            p = nps()
            for kc in range(4):
                MM(ps[p][:, :], Wglu[:, kc, m * 128:(m + 1) * 128], zT[:, kc, :], kc == 0, kc == 3, ['Wglu', zTk], [f'ps{p}'])
            A(lambda e, p=p, m=m: e.activation(out=sg[:], in_=ps[p][:, :], func=AF.Sigmoid, bias=bgl[:, m:m + 1]), [f'ps{p}', 'bgl'], ['sg'])
            V(lambda e, m=m, zT=zT: e.tensor_tensor(out=zg[:, m, :], in0=zT[:, m, :], in1=sg[:], op=ALU.mult), [zTk, 'sg'], ['zg'])
        for m in range(8):
            p = nps()
            for kc in range(4):
                MM(ps[p][:, :], Wob[:, kc, m * 128:(m + 1) * 128], zg[:, kc, :], kc == 0, kc == 3, ['Wob', 'zg'], [f'ps{p}'])
            p2 = nps()
            for h in range(8):
                MM(ps[p2][:, :], Woa[:, h, m * 128:(m + 1) * 128], oT[:, h, :], h == 0, h == 7, ['Woa', oTk], [f'ps{p2}'])
            V(lambda e, p=p, m=m, gT=gT: e.tensor_tensor(out=ta_[:], in0=gT[:, 8 + m, :], in1=ps[p][:, :], op=ALU.mult), [gTk, f'ps{p}'], ['taD'])
            V(lambda e, p2=p2, m=m, gT=gT: e.tensor_tensor(out=tb_[:], in0=gT[:, m, :], in1=ps[p2][:, :], op=ALU.mult), [gTk, f'ps{p2}'], ['tbD'])
            G(lambda e, m=m: e.tensor_tensor(out=mgd[:, m, :], in0=ta_[:], in1=tb_[:], op=ALU.add), ['taD', 'tbD'], ['mgd'])
        for j in range(4):
            for hf in range(2):
                p = nps()
                for kc in range(8):
                    MM(ps[p][:, :], mgd[:, kc, j * 128:(j + 1) * 128], Wout[:, kc, hf * 512:(hf + 1) * 512], kc == 0, kc == 7, ['mgd', 'Wout'], [f'ps{p}'])
                V(lambda e, p=p, j=j, hf=hf, xt=xt: e.tensor_tensor(out=x1[:, j, hf * 512:(hf + 1) * 512], in0=xt[:, j, hf * 512:(hf + 1) * 512], in1=ps[p][:, :], op=ALU.add), [xtk, f'ps{p}'], ['x1'])
        D(x1_d[t0:t0 + 512, :].rearrange("(j p) d -> p j d", p=128), x1[:], r=['x1'], w=['x1_d'])
        for j in range(4):
            jk_, jkk_ = junk2_b[j % 2], f'junk2{j % 2}'
            A(lambda e, j=j, jk_=jk_: e.activation(out=jk_[:], in_=x1[:, j, :], func=AF.Square), ['x1'], [jkk_])
            V(lambda e, j=j, jk_=jk_: e.tensor_reduce(out=ss2[:, j:j + 1], in_=jk_[:], axis=mybir.AxisListType.X, op=ALU.add), [jkk_], ['Ess'])
        m0 = c.mark()
        rs2 = rstd_of(ss2, 4, 'E')
        for j in range(4):
            A(lambda e, j=j, rs2=rs2: e.activation(out=h2f[:], in_=x1[:, j, :], func=AF.Copy, scale=rs2[:, j:j + 1]), ['x1', 'Ers'], ['h2f'])
            G(lambda e: e.tensor_copy(out=h2hi[:], in_=h2f[:]), ['h2f'], ['h2hi'])
            G(lambda e: e.tensor_tensor(out=h2lo[:], in0=h2f[:], in1=h2hi[:], op=ALU.subtract), ['h2f', 'h2hi'], ['h2lo'])
            for srct, srck, dst, dstk, pbk in ((h2hi, 'h2hi', hThi, 'hThi', 0), (h2lo, 'h2lo', hTlo, 'hTlo', 1)):
                for kc in range(8):
                    TR(pb[pbk][:, kc * 128:(kc + 1) * 128], srct[:, kc * 128:(kc + 1) * 128], identb[:], [srck, 'identb'], [f'pb{pbk}'])
                A(lambda e, dst=dst, pbk=pbk: e.activation(out=dst[:], in_=pb[pbk][:, :].rearrange("p (k t) -> p k t", k=8), func=AF.Copy), [f'pb{pbk}'], [dstk])
            G(lambda e, j=j: e.tensor_copy(out=h2Tb[:, :, j * 128:(j + 1) * 128], in_=hThi[:]), ['hThi'], ['h2Tb'])
            p = nps()
            nmm = 0
            for (lt, ltk, wt_, wtk) in ((hThi, 'hThi', Wrhi, 'Wrhi'), (hTlo, 'hTlo', Wrhi, 'Wrhi'), (hThi, 'hThi', Wrlo, 'Wrlo')):
                for kc in range(8):
                    MM(ps[p][:, 0:20], lt[:, kc, :], wt_[:, kc, :], nmm == 0, nmm == 23, [ltk, wtk], [f'ps{p}'])
                    nmm += 1
            V(lambda e, p=p, j=j: e.tensor_tensor(out=lg4[:, j, :], in0=ps[p][:, 0:20], in1=brt[:], op=ALU.add), [f'ps{p}', 'brt'], ['lg4'])
        c.release(m0)
        gl = lg4[:, :, 0:4]
        le4 = lg4[:, :, 4:20].rearrange("p j (g e) -> p j g e", g=4)
        V(lambda e: e.tensor_reduce(out=R1["mx"][:], in_=gl, axis=AXX, op=ALU.max), ['lg4'], ['r_mx'])
        V(lambda e: e.tensor_tensor(out=R4["oh"][:], in0=gl, in1=bc3(R1["mx"][:, :]), op=ALU.is_ge), ['lg4', 'r_mx'], ['r_oh'])
        V(lambda e: e.tensor_tensor(out=R4["ge"][:], in0=gl, in1=bc3(R1["mx"][:, :]), op=ALU.subtract), ['lg4', 'r_mx'], ['r_ge'])
        A(lambda e: e.activation(out=R4["ge"][:], in_=R4["ge"][:], func=AF.Exp), ['r_ge'], ['r_ge'])
        V(lambda e: e.tensor_reduce(out=R1["gsum"][:], in_=R4["ge"][:], axis=AXX, op=ALU.add), ['r_ge'], ['r_gsum'])
        V(lambda e: e.reciprocal(out=R1["psel"][:], in_=R1["gsum"][:]), ['r_gsum'], ['r_psel'])
        V(lambda e: e.tensor_tensor(out=prod[:], in0=le4, in1=R4["oh"][:, :, :].unsqueeze(3).broadcast_to([128, 4, 4, 4]), op=ALU.mult), ['lg4', 'r_oh'], ['r_prod'])
        V(lambda e: e.tensor_reduce(out=R4["les"][:], in_=prod[:].rearrange("p j g e -> p j e g"), axis=AXX, op=ALU.add), ['r_prod'], ['r_les'])
        V(lambda e: e.tensor_reduce(out=R1["m1"][:], in_=R4["les"][:], axis=AXX, op=ALU.max), ['r_les'], ['r_m1'])
        V(lambda e: e.tensor_tensor(out=R4["mk1"][:], in0=R4["les"][:], in1=bc3(R1["m1"][:, :]), op=ALU.is_ge), ['r_les', 'r_m1'], ['r_mk1'])
        V(lambda e: e.scalar_tensor_tensor(out=R4["le2"][:], in0=R4["mk1"][:], scalar=-1e30, in1=R4["les"][:], op0=ALU.mult, op1=ALU.add), ['r_mk1', 'r_les'], ['r_le2'])
        V(lambda e: e.tensor_reduce(out=R1["m2"][:], in_=R4["le2"][:], axis=AXX, op=ALU.max), ['r_le2'], ['r_m2'])
        V(lambda e: e.tensor_tensor(out=R4["mk2"][:], in0=R4["le2"][:], in1=bc3(R1["m2"][:, :]), op=ALU.is_ge), ['r_le2', 'r_m2'], ['r_mk2'])
        V(lambda e: e.tensor_tensor(out=R1["d"][:], in0=R1["m2"][:], in1=R1["m1"][:], op=ALU.subtract), ['r_m1', 'r_m2'], ['r_d'])
        A(lambda e: e.activation(out=R1["d"][:], in_=R1["d"][:], func=AF.Exp), ['r_d'], ['r_d'])
        V(lambda e: e.tensor_scalar(out=R1["d"][:], in0=R1["d"][:], scalar1=1.0, scalar2=None, op0=ALU.add), ['r_d'], ['r_d'])
        V(lambda e: e.reciprocal(out=R1["w1"][:], in_=R1["d"][:]), ['r_d'], ['r_w1'])
        V(lambda e: e.tensor_scalar(out=R1["w2"][:], in0=R1["w1"][:], scalar1=-1.0, scalar2=1.0, op0=ALU.mult, op1=ALU.add), ['r_w1'], ['r_w2'])
        V(lambda e: e.tensor_tensor(out=R1["w1"][:], in0=R1["w1"][:], in1=R1["psel"][:], op=ALU.mult), ['r_w1', 'r_psel', 'r_w2'], ['r_w1'])
        V(lambda e: e.tensor_tensor(out=R1["w2"][:], in0=R1["w2"][:], in1=R1["psel"][:], op=ALU.mult), ['r_w2', 'r_psel'], ['r_w2'])
        V(lambda e: e.tensor_tensor(out=R4["w12"][:], in0=R4["mk1"][:], in1=bc3(R1["w1"][:, :]), op=ALU.mult), ['r_mk1', 'r_w1'], ['r_w12'])
        V(lambda e: e.tensor_tensor(out=R4["tq"][:], in0=R4["mk2"][:], in1=bc3(R1["w2"][:, :]), op=ALU.mult), ['r_mk2', 'r_w2'], ['r_tq'])
        V(lambda e: e.tensor_tensor(out=R4["w12"][:], in0=R4["w12"][:], in1=R4["tq"][:], op=ALU.add), ['r_w12', 'r_tq'], ['r_w12'])
        V(lambda e: e.tensor_tensor(out=cw4[:].rearrange("p j (g e) -> p j g e", g=4), in0=R4["oh"][:, :, :].unsqueeze(3).broadcast_to([128, 4, 4, 4]),
                                    in1=R4["w12"][:, :, :].unsqueeze(2).broadcast_to([128, 4, 4, 4]), op=ALU.mult), ['r_oh', 'r_w12'], ['cw4'])
        V(lambda e: e.tensor_copy(out=cwb4[:], in_=cw4[:]), ['cw4'], ['cwb4'])
        for j in range(4):
            TR(pb[0][0:16, 0:128], cwb4[:, j, :], identb[:], ['cwb4', 'identb'], ['pb0'])
            V(lambda e, j=j: e.tensor_copy(out=cwTb[:, j * 128:(j + 1) * 128], in_=pb[0][0:16, 0:128]), ['pb0'], ['cwTb'])
        D(h2T_d.rearrange("(kc p) t -> p kc t", p=128)[:, :, t0:t0 + 512], h2Tb[:], r=['h2Tb'], w=['h2T_d'])
        D(cwT_d[:, t0:t0 + 512], cwTb[:], r=['cwTb'], w=['cwT_d'])
    c.barrier()
    c.release(base_mark)
    if 'dbg_x1' in dbg_out:
        tmpx = c.sb([128, TH // 128, 1024], F32, "tmpx")
        D(tmpx[:], x1_d.rearrange("(j p) d -> p j d", p=128), r=['x1_d'], w=['tmpx'])
        D(dbg_out['dbg_x1'].rearrange("(j p) d -> p j d", p=128), tmpx[:], r=['tmpx'])
        tmpc = c.sb([16, TH], BF16, "tmpc"); tmpcf = c.sb([16, TH], F32, "tmpcf")
        D(tmpc[:], cwT_d, r=['cwT_d'], w=['tmpc'])
        V(lambda e, tmpcf=tmpcf, tmpc=tmpc: e.tensor_copy(out=tmpcf[:], in_=tmpc[:]), ['tmpc'], ['tmpcf'])
        D(dbg_out['dbg_cw'], tmpcf[:], r=['tmpcf'])
        c.barrier()
        c.release(base_mark)

    if stop == 'D':
        c.emit()
        return nc
    ST = min(TH, 1024)
    NSB = ST // 128
    gf2 = c.sb([128, 8], F32, "gf2")
    D(gf2[:], gffnT, w=['gf2'])
    selb = c.sb([16, 2048], BF16, "selb")
    m_ = c.mark()
    selst = c.sb([16, 2048], F32, "selst")
    D(selst[:], sel_d, w=['selst'])
    V(lambda e: e.tensor_copy(out=selb[:], in_=selst[:]), ['selst'], ['selb'])
    gfin_t = c.sb([128, 1024], F32, "gfin")
    D(gfin_t[:], gfin, w=['gfin'])
    h2T = c.sb([128, 8, ST], BF16, "h2T")
    cwT = c.sb([16, ST], BF16, "cwT")
    acc = c.sb([128, NSB, 1024], F32, "acc")
    Wg_b = [c.sb([128, 8, 256], BF16, "Wg") for _ in range(2)]
    Wu_b = [c.sb([128, 8, 256], BF16, "Wu") for _ in range(2)]
    Wd_b = [c.sb([128, 2, 1024], BF16, "Wd") for _ in range(2)]
    wstg = [[c.sb([128, 8, 256], F32, "wstg") for _ in range(2)] for _ in range(2)]
    wstd = [c.sb([128, 2, 1024], F32, "wstd") for _ in range(2)]
    bcs_b = [c.sb([128, 512], BF16, "bcs") for _ in range(2)]
    sgl_b = [c.sb([128, 512], F32, "sgl") for _ in range(2)]
    tmu_b = [c.sb([128, 512], F32, "tmu") for _ in range(2)]
    actT_b = [c.sb([128, 2, 512], BF16, "actT") for _ in range(2)]
    x1f = c.sb([128, 1024], F32, "x1f")
    ssf = c.sb([128, 1], F32, "ssf")
    junk3 = c.sb([128, 1024], BF16, "junk3")
    outt = [c.sb([128, 1024], F32, "outt") for _ in range(2)]
    NST = TH // ST
    NSUB = ST // 512

    def load_expert(n):
        ex, sl = n % 16, n % 2
        Wg, Wu, Wd = Wg_b[sl], Wu_b[sl], Wd_b[sl]
        wk = f'We{sl}'
        D(wstg[sl][0][:], w_eg[ex].rearrange("(kc p) f -> p kc f", p=128), w=[f'wstg{sl}0'])
        D(wstg[sl][1][:], w_eu[ex].rearrange("(kc p) f -> p kc f", p=128), w=[f'wstg{sl}1'])
        D(wstd[sl][:], w_ed[ex].rearrange("(fc p) d -> p fc d", p=128), w=[f'wstd{sl}'])
        for kc in range(8):
            V(lambda e, kc=kc, Wg=Wg, sl=sl: e.tensor_scalar(out=Wg[:, kc, :], in0=wstg[sl][0][:, kc, :], scalar1=gf2[:, kc:kc + 1], scalar2=None, op0=ALU.mult), [f'wstg{sl}0', 'gf2'], [wk])
            A(lambda e, kc=kc, Wu=Wu, sl=sl: e.activation(out=Wu[:, kc, :], in_=wstg[sl][1][:, kc, :], func=AF.Copy, scale=gf2[:, kc:kc + 1]), [f'wstg{sl}1', 'gf2'], [wk])
        A(lambda e, Wd=Wd, sl=sl: e.activation(out=Wd[:, 0, :], in_=wstd[sl][:, 0, :], func=AF.Copy), [f'wstd{sl}'], [wk])
        V(lambda e, Wd=Wd, sl=sl: e.tensor_copy(out=Wd[:, 1, :], in_=wstd[sl][:, 1, :]), [f'wstd{sl}'], [wk])

    ucount = [0]

    def moe_stage1(n, sub):
        ex, sl = n % 16, n % 2
        Wg, Wu = Wg_b[sl], Wu_b[sl]
        wk = f'We{sl}'
        ub = ucount[0] % 2
        ucount[0] += 1
        bcs, sgl, tmu, actT = bcs_b[ub], sgl_b[ub], tmu_b[ub], actT_b[ub]
        q0 = sub * 512
        p = nps()
        MM(ps[p][:, :], selb[:, ex * 128:(ex + 1) * 128], cwT[:, q0:q0 + 512], True, True, ['selb', 'cwT'], [f'ps{p}'])
        A(lambda e, p=p, bcs=bcs: e.activation(out=bcs[:], in_=ps[p][:, :], func=AF.Copy), [f'ps{p}'], [f'bcs{ub}'])
        for fc in range(2):
            pg = nps()
            for kc in range(8):
                MM(ps[pg][:, :], Wg[:, kc, fc * 128:(fc + 1) * 128], h2T[:, kc, q0:q0 + 512], kc == 0, kc == 7, [wk, 'h2T'], [f'ps{pg}'])
            pu = nps()
            for kc in range(8):
                MM(ps[pu][:, :], Wu[:, kc, fc * 128:(fc + 1) * 128], h2T[:, kc, q0:q0 + 512], kc == 0, kc == 7, [wk, 'h2T'], [f'ps{pu}'])
            A(lambda e, pg=pg, sgl=sgl: e.activation(out=sgl[:], in_=ps[pg][:, :], func=AF.Silu), [f'ps{pg}'], [f'sgl{ub}'])
            V(lambda e, pu=pu, sgl=sgl, tmu=tmu: e.tensor_tensor(out=tmu[:], in0=sgl[:], in1=ps[pu][:, :], op=ALU.mult), [f'sgl{ub}', f'ps{pu}'], [f'tmu{ub}'])
            G(lambda e, fc=fc, tmu=tmu, bcs=bcs, actT=actT: e.tensor_tensor(out=actT[:, fc, :], in0=tmu[:], in1=bcs[:], op=ALU.mult), [f'tmu{ub}', f'bcs{ub}'], [f'actT{ub}'])
        return ub

    def moe_stage2(n, sub, ub):
        sl = n % 2
        Wd = Wd_b[sl]
        wk = f'We{sl}'
        actT = actT_b[ub]
        for j in range(4):
            blk = sub * 4 + j
            for hf in range(2):
                p = nps()
                for fc in range(2):
                    MM(ps[p][:, :], actT[:, fc, j * 128:(j + 1) * 128], Wd[:, fc, hf * 512:(hf + 1) * 512], fc == 0, fc == 1, [f'actT{ub}', wk], [f'ps{p}'])
                V(lambda e, p=p, blk=blk, hf=hf: e.tensor_tensor(out=acc[:, blk, hf * 512:(hf + 1) * 512], in0=acc[:, blk, hf * 512:(hf + 1) * 512], in1=ps[p][:, :], op=ALU.add), ['acc', f'ps{p}'], ['acc'])

    load_expert(0)
    for sti in range(NST):
        s0 = sti * ST
        D(h2T[:], h2T_d.rearrange("(kc p) t -> p kc t", p=128)[:, :, s0:s0 + ST], r=['h2T_d'], w=['h2T'])
        D(cwT[:], cwT_d[:, s0:s0 + ST], r=['cwT_d'], w=['cwT'])
        G(lambda e: e.memset(acc[:], 0.0), (), ['acc'])
        units = [(sti * 16 + ex, sub) for ex in range(16) for sub in range(NSUB)]
        prev = None
        for i in range(len(units) + 1):
            cur = None
            if i < len(units):
                n, sub = units[i]
                ub = moe_stage1(n, sub)
                cur = (n, sub, ub)
            if prev is not None:
                moe_stage2(*prev)
            if cur is not None and cur[1] == 0 and cur[0] + 1 < NST * 16:
                load_expert(cur[0] + 1)
            prev = cur
        for blk in range(NSB):
            r0 = s0 + blk * 128
            ot, otk = outt[blk % 2], f'outt{blk % 2}'
            D(x1f[:], x1_d[r0:r0 + 128, :], r=['x1_d'], w=['x1f'])
            V(lambda e, blk=blk: e.tensor_tensor(out=x1f[:], in0=x1f[:], in1=acc[:, blk, :], op=ALU.add), ['x1f', 'acc'], ['x1f'])
            A(lambda e: e.activation(out=junk3[:], in_=x1f[:], func=AF.Square), ['x1f'], ['junk3'])
            V(lambda e: e.tensor_reduce(out=ssf[:, 0:1], in_=junk3[:], axis=mybir.AxisListType.X, op=ALU.add), ['junk3'], ['Fss'])
            m0 = c.mark()
            rsf = rstd_of(ssf, 1, 'F')
            V(lambda e, ot=ot, rsf=rsf: e.scalar_tensor_tensor(out=ot[:], in0=x1f[:], scalar=rsf[:, 0:1], in1=gfin_t[:], op0=ALU.mult, op1=ALU.mult), ['x1f', 'Frs', 'gfin'], [otk])
            c.release(m0)
            D(out_d[r0:r0 + 128, :], ot[:], r=[otk], w=['out_d'])
    c.emit()
    return nc


def _ssm_layouts(lambda_re, lambda_im, log_step, b_re, b_im, c_re, c_im, ssm_d):
    f = np.float32
    o = {}
    r = np.arange(128)
    k4_r, mp_r, cp_r = r // 32, (r // 16) % 2, r % 16
    s_ = np.arange(4)
    q = np.arange(128)
    m_q, p_q = q // 64, q % 64
    g = 8 * s_[None, :, None] + 2 * k4_r[:, None, None] + m_q[None, None, :]
    P = np.broadcast_to(p_q[None, None, :], g.shape)
    o["LRX"] = lambda_re[g, P].reshape(128, 512).astype(f)
    o["LIX"] = lambda_im[g, P].reshape(128, 512).astype(f)
    o["LSX"] = log_step[g].reshape(128, 512).astype(f)
    msk = (mp_r[:, None, None] == m_q[None, None, :])
    CP = np.broadcast_to(cp_r[:, None, None], g.shape)
    o["BRX"] = np.where(msk, b_re[g, P, CP], 0).reshape(128, 512).astype(f)
    o["BIX"] = np.where(msk, b_im[g, P, CP], 0).reshape(128, 512).astype(f)
    k = np.arange(16)
    j = np.arange(32)
    mp_j, c_j = j // 16, j % 16
    g2 = 2 * k[None, :, None] + m_q[:, None, None] + 0 * j[None, None, :]
    P2 = np.broadcast_to(p_q[:, None, None], g2.shape)
    C2 = np.broadcast_to(c_j[None, None, :], g2.shape)
    msk2 = (mp_j[None, None, :] == m_q[:, None, None])
    o["LRY"] = lambda_re[g2, P2].reshape(128, 512).astype(f)
    o["LIY"] = lambda_im[g2, P2].reshape(128, 512).astype(f)
    o["LSY"] = log_step[g2].reshape(128, 512).astype(f)
    o["CRY"] = np.where(msk2, c_re[g2, C2, P2], 0).reshape(128, 512).astype(f)
    o["CIY"] = np.where(msk2, c_im[g2, C2, P2], 0).reshape(128, 512).astype(f)
    o["BRY"] = np.where(msk2, b_re[g2, P2, C2], 0).reshape(128, 512).astype(f)
    o["BIY"] = np.where(msk2, b_im[g2, P2, C2], 0).reshape(128, 512).astype(f)
    dd = np.zeros((128, 4, 128), f)
    for s in range(4):
        dd[r, s, r] = ssm_d[128 * s + r]
    o["DDG"] = dd.reshape(128, 512)
    return o


_CACHE = {}


def _prep_common(inp):
    f = np.float32
    A_ = lambda a: np.ascontiguousarray(a, dtype=f)
    d = {}
    d["w_in"] = A_(inp["w_in"][0])
    d["gmixT"] = A_(inp["g_mix"][0].reshape(8, 128).T)
    d["bforget"] = A_(inp["b_forget"][0].reshape(8, 1))
    d["bgate"] = A_(inp["b_gate"][0].reshape(16, 128).T)
    d["w_out_a"] = A_(inp["w_out_a"][0])
    d["w_glu"] = A_(inp["w_glu"][0])
    d["bglu"] = A_(inp["b_glu"][0].reshape(4, 128).T)
    d["w_out_b"] = A_(inp["w_out_b"][0])
    d["w_out"] = A_(inp["w_out"][0])
    d["gffnT"] = A_(inp["g_ffn"][0].reshape(8, 128).T)
    d["wr"] = A_(np.concatenate([inp["w_router_group"][0], inp["w_router_expert"][0]], axis=1))
    d["br"] = A_(np.broadcast_to(np.concatenate([inp["b_router_group"][0], inp["b_router_expert"][0]])[None, :], (128, 20)))
    d["w_eg"] = A_(inp["w_exp_gate"][0])
    d["w_eu"] = A_(inp["w_exp_up"][0])
    d["w_ed"] = A_(inp["w_exp_down"][0])
    d["gfin"] = A_(np.broadcast_to(inp["g_final"][None, :], (128, 1024)))
    d.update(_ssm_layouts(np.asarray(inp["lambda_re"][0]), np.asarray(inp["lambda_im"][0]), np.asarray(inp["log_step"][0]),
                          np.asarray(inp["ssm_b_re"][0]), np.asarray(inp["ssm_b_im"][0]), np.asarray(inp["ssm_c_re"][0]),
                          np.asarray(inp["ssm_c_im"][0]), np.asarray(inp["ssm_d"][0])))
    d["ident"] = np.eye(128, dtype=f)
    d["tri"] = np.triu(np.ones((128, 128), f))
    sel = np.zeros((16, 16, 128), f)
    for e in range(16):
        sel[e, e, :] = 1.0
    d["sel"] = sel.reshape(16, 2048)
    return d


def run(inputs, dbg=(), stop=None):
    inp = {k: np.asarray(v) for k, v in inputs.items()}
    x = inp["x"]
    B, S, _ = x.shape
    TH = S // 2
    key = (TH, tuple(dbg), stop)
    if key not in _CACHE:
        _CACHE[key] = build_program(TH, dbg, stop)
    nc = _CACHE[key]
    common = _prep_common(inp)
    in_maps = []
    for core in range(8):
        b, par = core // 2, core % 2
        xin = np.zeros((S, 1024), np.float32)
        if par == 1:
            xin[:TH] = x[b, :TH]
        xin[TH:] = x[b, par * TH:(par + 1) * TH]
        m = dict(common)
        m["xin"] = xin
        m["flag"] = np.full((128, 1), float(par), np.float32)
        in_maps.append(m)
    res = run_bass_kernel_spmd(nc, in_maps, core_ids=list(range(8)))
    return res, TH


def kernel(**inputs):
    res, TH = run(inputs)
    x = inputs["x"]
    B, S, Dm = x.shape
    out = np.zeros((B, S, Dm), np.float32)
    for core in range(8):
        b, par = core // 2, core % 2
        out[b, par * TH:(par + 1) * TH] = res.results[core]["out"]
    return out
```

```python
import contextlib
import numpy as np
import concourse.bass as bass
import concourse.mybir as mybir
from concourse.bass_utils import run_bass_kernel_spmd

F32 = mybir.dt.float32
BF16 = mybir.dt.bfloat16
I32 = mybir.dt.int32
AF = mybir.ActivationFunctionType
ALU = mybir.AluOpType

NDSEM = 48
SAME_SYNC = {'pe': False, 'act': True, 'dve': True, 'pool': True, 'sp': False}
EPS = 1e-6
L = 16
TWO_PI = 6.283185307179586


class Ctx:
    def __init__(self, nc):
        self.nc = nc
        self.names = ['pe', 'act', 'dve', 'pool', 'sp']
        self.ops = {e: [] for e in self.names}
        self.cnt = {e: 0 for e in self.names}
        self.seen = {e: {} for e in self.names}
        self.pending = {e: {} for e in self.names}
        self.lastw = {}
        self.readers = {}
        self.dval = [0] * NDSEM
        self.dnext = 0
        self.sb_off = 16640
        self.uid = 0
        self.sb_max = 0

    def sb(self, shape, dtype, name="t"):
        esz = {F32: 4, BF16: 2, I32: 4}[dtype]
        n = 1
        for s in shape[1:]:
            n *= s
        off = (self.sb_off + 63) // 64 * 64
        self.sb_off = off + n * esz
        self.sb_max = max(self.sb_max, self.sb_off)
        assert self.sb_off <= 229376, f"SBUF overflow {self.sb_off} ({name})"
        self.uid += 1
        return self.nc.alloc_sbuf_tensor_at(f"{name}_{self.uid}", list(shape), dtype, offset=off)

    def mark(self):
        return self.sb_off

    def release(self, m):
        self.sb_off = m

    def _deps(self, reads, writes):
        deps = {}
        for k in reads:
            t = self.lastw.get(k)
            if t and deps.get(t[0], 0) < t[1]:
                deps[t[0]] = t[1]
        for k in writes:
            t = self.lastw.get(k)
            if t and deps.get(t[0], 0) < t[1]:
                deps[t[0]] = t[1]
            for s, v in self.readers.get(k, {}).items():
                if deps.get(s, 0) < v:
                    deps[s] = v
        return deps

    def _waits(self, e, deps):
        for s, v in self.pending[e].items():
            if deps.get(s, 0) < v:
                deps[s] = v
        self.pending[e] = {}
        waits = []
        for s, v in deps.items():
            if s == e and not SAME_SYNC[e]:
                continue
            if self.seen[e].get(s, 0) >= v:
                continue
            self.seen[e][s] = v
            waits.append((s, v))
        return waits

    def _commit(self, tok, reads, writes):
        for k in reads:
            r = self.readers.setdefault(k, {})
            if r.get(tok[0], 0) < tok[1]:
                r[tok[0]] = tok[1]
        for k in writes:
            self.lastw[k] = tok
            self.readers[k] = {}

    def op(self, e, fn, reads=(), writes=()):
        deps = self._deps(reads, writes)
        waits = self._waits(e, deps)
        self.cnt[e] += 1
        tok = (e, self.cnt[e])
        self.ops[e].append((waits, fn, e))
        self._commit(tok, reads, writes)
        return tok

    def dma(self, e, out, in_, reads=(), writes=()):
        i = self.dnext
        self.dnext = (self.dnext + 1) % NDSEM
        deps = self._deps(reads, writes)
        if self.dval[i] > 0:
            s = ('d', i)
            if deps.get(s, 0) < self.dval[i]:
                deps[s] = self.dval[i]
        waits = self._waits(e, deps)
        self.dval[i] += 16
        tok = (('d', i), self.dval[i])
        self.ops[e].append((waits, lambda eng: eng.dma_start(out=out, in_=in_), ('d', i)))
        self._commit(tok, reads, writes)
        return tok

    def barrier(self):
        allt = {e: self.cnt[e] for e in self.names if self.cnt[e] > 0}
        for i in range(NDSEM):
            if self.dval[i] > 0:
                allt[('d', i)] = self.dval[i]
        for e in self.names:
            for s, v in allt.items():
                if self.pending[e].get(s, 0) < v:
                    self.pending[e][s] = v

    def emit(self):
        nc = self.nc
        self.barrier()
        self.op('sp', lambda eng: eng.nop(), (), ())
        with contextlib.ExitStack() as st:
            sems = {e: st.enter_context(nc.semaphore(f"s_{e}")) for e in self.names}
            for i in range(NDSEM):
                sems[('d', i)] = st.enter_context(nc.semaphore(f"d_{i}"))
            block = st.enter_context(nc.Block())

            def run(e, eng):
                for waits, fn, inc in self.ops[e]:
                    for s, v in waits:
                        eng.wait_ge(sems[s], v)
                    ins = fn(eng)
                    if isinstance(inc, tuple):
                        ins.then_inc(sems[inc], 16)
                    else:
                        ins.then_inc(sems[inc], 1)

            @block.sync
            def _(eng):
                run('sp', eng)

            @block.tensor
            def _(eng):
                run('pe', eng)

            @block.scalar
            def _(eng):
                run('act', eng)

            @block.vector
            def _(eng):
                run('dve', eng)

            @block.gpsimd
            def _(eng):
                run('pool', eng)


def build_program(TH, dbg=(), stop=None):
    nc = bass.Bass("TRN2", target_bir_lowering=False)
    T2 = 2 * TH
    NT = TH // 512
    NBH = TH // 128
    NCH = TH // L
    c = Ctx(nc)

    def din(name, shape, dt=F32):
        return nc.dram_tensor(name, list(shape), dt, kind="ExternalInput").ap()

    def dscr(name, shape, dt):
        return nc.dram_tensor(name, list(shape), dt).ap()

    xin = din("xin", [T2, 1024])
    flag_d = din("flag", [128, 1])
    w_in = din("w_in", [1024, 4104])
    gmixT = din("gmixT", [128, 8])
    bforget = din("bforget", [8, 1])
    bgate = din("bgate", [128, 16])
    w_out_a = din("w_out_a", [512, 1024])
    w_glu = din("w_glu", [512, 512])
    bglu = din("bglu", [128, 4])
    w_out_b = din("w_out_b", [512, 1024])
    w_out = din("w_out", [1024, 1024])
    gffnT = din("gffnT", [128, 8])
    wr = din("wr", [1024, 20])
    br = din("br", [128, 20])
    w_eg = din("w_eg", [16, 1024, 256])
    w_eu = din("w_eu", [16, 1024, 256])
    w_ed = din("w_ed", [16, 256, 1024])
    gfin = din("gfin", [128, 1024])
    ssm_names = ["LRX", "LIX", "LSX", "BRX", "BIX", "LRY", "LIY", "LSY", "CRY", "CIY", "BRY", "BIY"]
    ssm_in = {n: din(n, [128, 512]) for n in ssm_names}
    ddg = din("DDG", [128, 512])
    ident_d = din("ident", [128, 128])
    tri_d = din("tri", [128, 128])
    sel_d = din("sel", [16, 2048])
    out_d = nc.dram_tensor("out", [TH, 1024], F32, kind="ExternalOutput").ap()
    dbg_out = {}
    for name, shape in dbg:
        dbg_out[name] = nc.dram_tensor(name, list(shape), F32, kind="ExternalOutput").ap()

    kT_d = dscr("kT_d", [8, 70, T2], BF16)
    qT_d = dscr("qT_d", [8, 70, TH], BF16)
    v_d = dscr("v_d", [T2, 520], BF16)
    uT_d = dscr("uT_d", [512, T2], BF16)
    gT_d = dscr("gT_d", [2048, TH], BF16)
    oT_d = dscr("oT_d", [8, 64, TH], BF16)
    zT_d = dscr("zT_d", [512, TH], BF16)
    x1_d = dscr("x1_d", [TH, 1024], F32)
    h2T_d = dscr("h2T_d", [1024, TH], BF16)
    cwT_d = dscr("cwT_d", [16, TH], BF16)

    ps = [nc.alloc_psum_tensor(f"ps{i}", [128, 512], F32) for i in range(6)]
    pb = [nc.alloc_psum_tensor(f"pb{i}", [128, 1024], BF16) for i in range(2)]

    def V(fn, r=(), w=()):
        return c.op('dve', fn, r, w)

    def A(fn, r=(), w=()):
        return c.op('act', fn, r, w)

    def G(fn, r=(), w=()):
        return c.op('pool', fn, r, w)

    def MM(out, lhsT, rhs, st, sp_, r, w, tp=None):
        kw = dict(start=st, stop=sp_)
        if tp is not None:
            kw['tile_position'] = tp
        return c.op('pe', lambda e: e.matmul(out, lhsT=lhsT, rhs=rhs, **kw), r, w)

    def TR(out, in_, ident, r, w):
        return c.op('pe', lambda e: e.transpose(out, in_, ident), r, w)

    def D(out, in_, r=(), w=(), q='sp'):
        return c.dma(q, out, in_, r, w)


    dump_i = [0]
    dump_st = []

    def dump(name, ap2d, ncols):
        if name not in dbg_out:
            return
        for c0 in range(0, ncols, 2048):
            n = min(2048, ncols - c0)
            dump_i[0] += 1
            kx = 'dump0'
            if not dump_st:
                dump_st.append(c.sb([128, 2048], F32, "dumpst"))
            stt = dump_st[0]
            V(lambda e, stt=stt, c0=c0, n=n: e.tensor_copy(out=stt[:, 0:n], in_=ap2d[:, c0:c0 + n]), [], [kx])
            D(dbg_out[name][:, c0:c0 + n], stt[:, 0:n], r=[kx])
    identf = c.sb([128, 128], F32, "identf")
    identb = c.sb([128, 128], BF16, "identb")
    trib = c.sb([128, 128], BF16, "trib")
    onesf = c.sb([128, 512], F32, "onesf")
    flag = c.sb([128, 1], F32, "flag")
    stg = c.sb([128, 128], F32, "stg")
    D(identf[:], ident_d, w=['identf'])
    D(stg[:], tri_d, w=['stg'])
    D(flag[:], flag_d, w=['flag'])
    V(lambda e: e.tensor_copy(out=identb[:], in_=identf[:]), ['identf'], ['identb'])
    V(lambda e: e.tensor_copy(out=trib[:], in_=stg[:]), ['stg'], ['trib'])
    V(lambda e: e.memset(onesf[:], 1.0), (), ['onesf'])
    base_mark = c.mark()

    def rstd_of(ss, n, tag):
        ms = c.sb([128, n], F32, "ms")
        rs = c.sb([128, n], F32, "rs")
        V(lambda e: e.tensor_scalar(out=ms[:], in0=ss[:], scalar1=1.0 / 1024, scalar2=EPS, op0=ALU.mult, op1=ALU.add), [tag + 'ss'], [tag + 'ms'])
        A(lambda e: e.activation(out=ms[:], in_=ms[:], func=AF.Sqrt), [tag + 'ms'], [tag + 'ms'])
        V(lambda e: e.reciprocal(out=rs[:], in_=ms[:]), [tag + 'ms'], [tag + 'rs'])
        return rs

    Win = c.sb([128, 8, 4104], BF16, "Win")
    gm = c.sb([128, 8], F32, "gm")
    negb = c.sb([8, 1], F32, "negb")
    bg = c.sb([128, 16], F32, "bg")
    D(gm[:], gmixT, w=['gm'])
    D(negb[:], bforget, w=['negb'])
    D(bg[:], bgate, w=['bg'])
    V(lambda e: e.tensor_scalar(out=negb[:], in0=negb[:], scalar1=-1.0, scalar2=None, op0=ALU.mult), ['negb'], ['negb'])
    wst = [c.sb([128, 8, 256], F32, "wst")] * 2
    w_in_v = w_in.rearrange("(kc p) n -> p kc n", p=128)
    ei = 0
    for cc in range(17):
        c0 = cc * 256
        ncol = min(256, 4104 - c0)
        st = wst[cc % 2]
        sk = 'wst'
        D(st[:, :, 0:ncol], w_in_v[:, :, c0:c0 + ncol], w=[sk])
        for kc in range(8):
            if ei % 2 == 0:
                A(lambda e, st=st, kc=kc, c0=c0, ncol=ncol: e.activation(out=Win[:, kc, c0:c0 + ncol], in_=st[:, kc, 0:ncol], func=AF.Copy, scale=gm[:, kc:kc + 1]), [sk, 'gm'], ['Win'])
            else:
                V(lambda e, st=st, kc=kc, c0=c0, ncol=ncol: e.tensor_scalar(out=Win[:, kc, c0:c0 + ncol], in0=st[:, kc, 0:ncol], scalar1=gm[:, kc:kc + 1], scalar2=None, op0=ALU.mult), [sk, 'gm'], ['Win'])
            ei += 1

    CQ, CK, CV, CF, CU, CG = 0, 512, 1024, 1536, 1544, 2056
    xt_b = [c.sb([128, 4, 1024], F32, "xt") for _ in range(2)]
    xs = c.sb([128, 4, 1024], BF16, "xs")
    hT_b = [c.sb([128, 8, 512], BF16, "hT") for _ in range(2)]
    junk = c.sb([128, 1024], BF16, "junk")
    junk_b = [junk, c.sb([128, 1024], BF16, "junkb")]
    ss = c.sb([128, 4], F32, "ss")
    kT_s = [c.sb([64, 8, 512], BF16, "kTs")] * 2
    qT_s = [c.sb([64, 8, 512], BF16, "qTs")] * 2
    v_s = [c.sb([128, 4, 8, 65], BF16, "vs")] * 2
    uT_s = [c.sb([128, 4, 512], BF16, "uTs")] * 2
    gT_s = [c.sb([128, 16, 512], BF16, "gTs")] * 2
    CPK = c.sb([8, 6, 512], BF16, "CPK")
    CPQ = c.sb([8, 6, 512], BF16, "CPQ")
    e1 = c.sb([8, 512], F32, "e1")
    negc = c.sb([8, 512], F32, "negc")
    r1 = c.sb([8, 512], F32, "r1")
    carry = c.sb([8, 1], F32, "carry")
    ones_own = c.sb([128, 32], BF16, "ones_own")
    ones_ctx = c.sb([128, 32], BF16, "ones_ctx")
    V(lambda e: e.memset(ones_own[:], 1.0), (), ['ones_own'])
    V(lambda e: e.tensor_scalar(out=ones_ctx[:], in0=onesf[:, 0:32], scalar1=flag[:, 0:1], scalar2=None, op0=ALU.mult), ['onesf', 'flag'], ['ones_ctx'])
    V(lambda e: e.memset(CPK[:, 0:3, :], 1.0), (), ['CPK'])
    V(lambda e: e.memset(CPQ[:, 3:6, :], 1.0), (), ['CPQ'])
    V(lambda e: e.memset(carry[:], 0.0), (), ['carry'])

    xin_v = xin.rearrange("(t j p) d -> t p j d", j=4, p=128)
    D(xt_b[0][:], xin_v[0], w=['xt0'])
    pi = [0]

    def nps():
        pi[0] = (pi[0] + 1) % 6
        return pi[0]

    for i in range(2 * NT):
        own = i >= NT
        b = i % 2
        xt, xk = xt_b[b], f'xt{b}'
        hT, hk = hT_b[b], f'hT{b}'
        if i + 1 < 2 * NT:
            D(xt_b[1 - b][:], xin_v[i + 1], w=[f'xt{1 - b}'])
        for j in range(4):
            jk_, jkk_ = junk_b[j % 2], f'junk{j % 2}'
            A(lambda e, j=j, xt=xt, jk_=jk_: e.activation(out=jk_[:], in_=xt[:, j, :], func=AF.Square), [xk], [jkk_])
            V(lambda e, j=j, jk_=jk_: e.tensor_reduce(out=ss[:, j:j + 1], in_=jk_[:], axis=mybir.AxisListType.X, op=ALU.add), [jkk_], ['Ass'])
        m0 = c.mark()
        rs = rstd_of(ss, 4, 'A')
        for j in range(4):
            V(lambda e, j=j, xt=xt, rs=rs: e.tensor_scalar(out=xs[:, j, :], in0=xt[:, j, :], scalar1=rs[:, j:j + 1], scalar2=None, op0=ALU.mult), [xk, 'Ars'], [f'xs{j}'])
        c.release(m0)
        for j in range(4):
            pbk = j % 2
            for kc in range(8):
                TR(pb[pbk][:, kc * 128:(kc + 1) * 128], xs[:, j, kc * 128:(kc + 1) * 128], identb[:], [f'xs{j}', 'identb'], [f'pb{pbk}'])
            src = pb[pbk][:, :].rearrange("p (k t) -> p k t", k=8)
            if j % 2 == 0:
                A(lambda e, j=j, hT=hT, src=src: e.activation(out=hT[:, :, j * 128:(j + 1) * 128], in_=src, func=AF.Copy), [f'pb{pbk}'], [hk])
            else:
                V(lambda e, j=j, hT=hT, src=src: e.tensor_copy(out=hT[:, :, j * 128:(j + 1) * 128], in_=src), [f'pb{pbk}'], [hk])
        tok0 = i * 512
        p = nps()
        for kc in range(8):
            MM(ps[p][0:8, :], Win[:, kc, CF:CF + 8], hT[:, kc, :], kc == 0, kc == 7, ['Win', hk], [f'ps{p}'])
        A(lambda e, p=p: e.activation(out=e1[:], in_=ps[p][0:8, :], func=AF.Exp, scale=-1.0, bias=negb[:, 0:1]), [f'ps{p}', 'negb'], ['e1'])
        A(lambda e: e.activation(out=e1[:], in_=e1[:], func=AF.Ln, bias=1.0), ['e1'], ['e1'])
        V(lambda e: e.tensor_tensor_scan(out=negc[:], data0=onesf[0:8, 0:512], data1=e1[:], initial=carry[:, 0:1], op0=ALU.mult, op1=ALU.add), ['e1', 'carry', 'onesf'], ['negc'])
        V(lambda e: e.tensor_copy(out=carry[:], in_=negc[:, 511:512]), ['negc'], ['carry'])
        V(lambda e: e.tensor_copy(out=CPK[:, 3, :], in_=negc[:]), ['negc'], ['CPK'])
        V(lambda e: e.tensor_tensor(out=r1[:], in0=negc[:], in1=CPK[:, 3, :], op=ALU.subtract), ['negc', 'CPK'], ['r1'])
        V(lambda e: e.tensor_copy(out=CPK[:, 4, :], in_=r1[:]), ['r1'], ['CPK'])
        V(lambda e: e.tensor_tensor(out=r1[:], in0=r1[:], in1=CPK[:, 4, :], op=ALU.subtract), ['r1', 'CPK'], ['r1'])
        V(lambda e: e.tensor_copy(out=CPK[:, 5, :], in_=r1[:]), ['r1'], ['CPK'])
        D(kT_d[:, 64:70, tok0:tok0 + 512], CPK[:, :, :], r=['CPK'], w=['kT_d'])
        if own:
            V(lambda e: e.tensor_scalar(out=CPQ[:, 0:3, :], in0=CPK[:, 3:6, :], scalar1=-1.0, scalar2=None, op0=ALU.mult), ['CPK'], ['CPQ'])
            D(qT_d[:, 64:70, tok0 - TH:tok0 - TH + 512], CPQ[:, :, :], r=['CPQ'], w=['qT_d'])
        kts, ktk = kT_s[b], 'kTs'
        for h in range(8):
            p = nps()
            for kc in range(8):
                MM(ps[p][0:64, :], Win[:, kc, CK + h * 64:CK + (h + 1) * 64], hT[:, kc, :], kc == 0, kc == 7, ['Win', hk], [f'ps{p}'])
            if h % 2 == 0:
                A(lambda e, p=p, h=h, kts=kts: e.activation(out=kts[:, h, :], in_=ps[p][0:64, :], func=AF.Copy), [f'ps{p}'], [ktk])
            else:
                V(lambda e, p=p, h=h, kts=kts: e.tensor_copy(out=kts[:, h, :], in_=ps[p][0:64, :]), [f'ps{p}'], [ktk])
        D(kT_d[:, 0:64, tok0:tok0 + 512].rearrange("h r t -> r h t"), kts[:, :, :], r=[ktk], w=['kT_d'])
        vs, vk = v_s[b], 'vs'
        V(lambda e, vs=vs, own=own: e.tensor_copy(out=vs[:, :, :, 64:65], in_=(ones_own if own else ones_ctx)[:, :].rearrange("p (j h o) -> p j h o", j=4, o=1)), ['ones_own', 'ones_ctx'], [vk])
        for j in range(4):
            p = nps()
            for kc in range(8):
                MM(ps[p][:, :], hT[:, kc, j * 128:(j + 1) * 128], Win[:, kc, CV:CV + 512], kc == 0, kc == 7, ['Win', hk], [f'ps{p}'])
            V(lambda e, p=p, j=j, vs=vs: e.tensor_copy(out=vs[:, j, :, 0:64], in_=ps[p][:, :].rearrange("p (h d) -> p h d", h=8)), [f'ps{p}'], [vk])
        D(v_d[tok0:tok0 + 512, :].rearrange("(j p) c -> p j c", p=128), vs[:, :, :, :].rearrange("p j h c -> p j (h c)"), r=[vk], w=['v_d'])
        us, uk = uT_s[b], 'uTs'
        for m in range(4):
            p = nps()
            for kc in range(8):
                MM(ps[p][:, :], Win[:, kc, CU + m * 128:CU + (m + 1) * 128], hT[:, kc, :], kc == 0, kc == 7, ['Win', hk], [f'ps{p}'])
            V(lambda e, p=p, m=m, us=us: e.tensor_copy(out=us[:, m, :], in_=ps[p][:, :]), [f'ps{p}'], [uk])
        D(uT_d.rearrange("(s p) t -> p s t", p=128)[:, :, tok0:tok0 + 512], us[:, :, :], r=[uk], w=['uT_d'])
        if own:
            qts, qtk = qT_s[b], 'qTs'
            for h in range(8):
                p = nps()
                for kc in range(8):
                    MM(ps[p][0:64, :], Win[:, kc, CQ + h * 64:CQ + (h + 1) * 64], hT[:, kc, :], kc == 0, kc == 7, ['Win', hk], [f'ps{p}'])
                A(lambda e, p=p, h=h, qts=qts: e.activation(out=qts[:, h, :], in_=ps[p][0:64, :], func=AF.Copy, scale=0.125), [f'ps{p}'], [qtk])
            D(qT_d[:, 0:64, tok0 - TH:tok0 - TH + 512].rearrange("h r t -> r h t"), qts[:, :, :], r=[qtk], w=['qT_d'])
            gs, gk = gT_s[b], 'gTs'
            for m in range(16):
                p = nps()
                for kc in range(8):
                    MM(ps[p][:, :], Win[:, kc, CG + m * 128:CG + (m + 1) * 128], hT[:, kc, :], kc == 0, kc == 7, ['Win', hk], [f'ps{p}'])
                A(lambda e, p=p, m=m, gs=gs: e.activation(out=gs[:, m, :], in_=ps[p][:, :], func=AF.Sigmoid, bias=bg[:, m:m + 1]), [f'ps{p}', 'bg'], [gk])
            D(gT_d.rearrange("(m p) t -> p m t", p=128)[:, :, tok0 - TH:tok0 - TH + 512], gs[:, :, :], r=[gk], w=['gT_d'])

    c.barrier()
    c.release(base_mark)
    if 'dbg_k' in dbg_out:
        tmpk = c.sb([70, T2], BF16, "tmpk")
        tmpf = c.sb([70, T2], F32, "tmpf")
        D(tmpk[:], kT_d[0], r=['kT_d'], w=['tmpk'])
        V(lambda e, tmpf=tmpf, tmpk=tmpk: e.tensor_copy(out=tmpf[:], in_=tmpk[:]), ['tmpk'], ['tmpf'])
        D(dbg_out['dbg_k'], tmpf[:], r=['tmpf'])
        c.barrier()
        c.release(base_mark)

    if stop == 'A':
        c.emit()
        return nc
    NB2 = T2 // 128
    v_all = c.sb([128, NB2, 520], BF16, "v_all")
    for q4 in range(0, NB2, 8):
        n = min(8, NB2 - q4)
        D(v_all[:, q4:q4 + n, :], v_d[q4 * 128:(q4 + n) * 128, :].rearrange("(j p) c -> p j c", p=128), r=['v_d'], w=['v_all'])
    kT_h = [c.sb([70, T2], BF16, "kTh") for _ in range(2)]
    qT_h = [c.sb([70, TH], BF16, "qTh") for _ in range(2)]
    pT_b = [c.sb([128, 512], BF16, "pT") for _ in range(3)]
    rr = c.sb([128, 512], F32, "rr")
    rrh = c.sb([128, 512], BF16, "rrh")
    rrl = c.sb([128, 512], BF16, "rrl")
    onesb = c.sb([128, 64], BF16, "onesb")
    V(lambda e: e.memset(onesb[:], 1.0), (), ['onesb'])
    bc_sb = c.sb([64, 512], F32, "bc_sb")
    oT_s = [c.sb([64, 512], BF16, "oTs") for _ in range(2)]
    D(kT_h[0][:], kT_d[0], r=['kT_d'], w=['kTh0'])
    D(qT_h[0][:], qT_d[0], r=['qT_d'], w=['qTh0'])
    items = []
    gi = 0
    for h in range(8):
        for Gq in range(NT):
            kbs = list(range(NBH)) + [NBH + ob for ob in range(4 * Gq + 4)]
            for idx, gkb in enumerate(kbs):
                ob = gkb - NBH
                diag = ob >= 4 * Gq
                c0 = (ob - 4 * Gq) * 128 if diag else 0
                items.append(dict(h=h, Gq=Gq, gkb=gkb, c0=c0, diag=diag, first=(idx == 0), last=(idx == len(kbs) - 1), gi=gi,
                                  newhead=(Gq == 0 and idx == 0)))
            gi += 1
    DEPTH = 2

    def att_stage1(i, it):
        h, Gq, gkb, c0 = it['h'], it['Gq'], it['gkb'], it['c0']
        hb = h % 2
        if it['newhead'] and h + 1 < 8:
            D(kT_h[1 - hb][:], kT_d[h + 1], r=['kT_d'], w=[f'kTh{1 - hb}'])
            D(qT_h[1 - hb][:], qT_d[h + 1], r=['qT_d'], w=[f'qTh{1 - hb}'])
        kt, ktk = kT_h[hb], f'kTh{hb}'
        qt, qtk = qT_h[hb], f'qTh{hb}'
        p = i % 3
        pT, ptk = pT_b[i % 3], f'pT{i % 3}'
        MM(ps[p][:, c0:512], kt[:, gkb * 128:(gkb + 1) * 128], qt[:, Gq * 512 + c0:Gq * 512 + 512], True, True, [ktk, qtk], [f'ps{p}'])
        A(lambda e, p=p, pT=pT, c0=c0: e.activation(out=pT[:, c0:512], in_=ps[p][:, c0:512], func=AF.Exp), [f'ps{p}'], [ptk])
        if it['diag']:
            G(lambda e, pT=pT, c0=c0: e.tensor_tensor(out=pT[:, c0:c0 + 128], in0=pT[:, c0:c0 + 128], in1=trib[:, :], op=ALU.mult), [ptk, 'trib'], [ptk])

    def att_stage2(i, it):
        h, Gq, gkb, c0 = it['h'], it['Gq'], it['gkb'], it['c0']
        po = 3 + (it['gi'] % 2)
        pT, ptk = pT_b[i % 3], f'pT{i % 3}'
        MM(ps[po][0:65, c0:512], v_all[:, gkb, h * 65:(h + 1) * 65], pT[:, c0:512], it['first'], it['last'], ['v_all', ptk], [f'ps{po}'])
        if it['last']:
            V(lambda e, po=po: e.reciprocal(out=rr[64:65, :], in_=ps[po][64:65, :]), [f'ps{po}'], ['rr'])
            V(lambda e: e.tensor_copy(out=rrh[64:65, :], in_=rr[64:65, :]), ['rr'], ['rrh'])
            V(lambda e: e.tensor_tensor(out=rr[64:65, :], in0=rr[64:65, :], in1=rrh[64:65, :], op=ALU.subtract), ['rr', 'rrh'], ['rr'])
            V(lambda e: e.tensor_copy(out=rrl[64:65, :], in_=rr[64:65, :]), ['rr'], ['rrl'])
            MM(ps[5][0:64, :], onesb[64:65, 0:64], rrh[64:65, :], True, False, ['onesb', 'rrh'], ['ps5'])
            MM(ps[5][0:64, :], onesb[64:65, 0:64], rrl[64:65, :], False, True, ['onesb', 'rrl'], ['ps5'])
            A(lambda e: e.activation(out=bc_sb[:], in_=ps[5][0:64, :], func=AF.Copy), ['ps5'], ['bc_sb'])
            ots, otk = oT_s[it['gi'] % 2], f"oTs{it['gi'] % 2}"
            V(lambda e, po=po, ots=ots: e.tensor_tensor(out=ots[:], in0=ps[po][0:64, :], in1=bc_sb[:], op=ALU.mult), [f'ps{po}', 'bc_sb'], [otk])
            D(oT_d[h, :, Gq * 512:(Gq + 1) * 512], ots[:], r=[otk], w=['oT_d'])

    for i in range(len(items) + DEPTH):
        if i < len(items):
            att_stage1(i, items[i])
        if i - DEPTH >= 0:
            att_stage2(i - DEPTH, items[i - DEPTH])
    c.barrier()
    c.release(base_mark)
    if 'dbg_o' in dbg_out:
        tmpk = c.sb([64, 8, TH], BF16, "tmpo")
        tmpf = c.sb([64, 8, TH], F32, "tmpof")
        D(tmpk[:], oT_d.rearrange("h d t -> d h t"), r=['oT_d'], w=['tmpk'])
        V(lambda e, tmpf=tmpf, tmpk=tmpk: e.tensor_copy(out=tmpf[:], in_=tmpk[:]), ['tmpk'], ['tmpf'])
        D(dbg_out['dbg_o'].rearrange("h d t -> d h t"), tmpf[:], r=['tmpf'])
        c.barrier()
        c.release(base_mark)

    if stop == 'B':
        c.emit()
        return nc
    def ssm_prep(sfx):
        k = 'pp' + sfx
        lr = c.sb([128, 512], F32, "lr"); li = c.sb([128, 512], F32, "li"); ls = c.sb([128, 512], F32, "ls")
        D(lr[:], ssm_in["LR" + sfx], w=[k + 'lr'])
        D(li[:], ssm_in["LI" + sfx], w=[k + 'li'])
        D(ls[:], ssm_in["LS" + sfx], w=[k + 'ls'])
        dt = c.sb([128, 512], F32, "dt"); mag = c.sb([128, 512], F32, "mag"); th = c.sb([128, 512], F32, "th")
        A(lambda e: e.activation(out=dt[:], in_=ls[:], func=AF.Exp), [k + 'ls'], [k + 'dt'])
        V(lambda e: e.tensor_tensor(out=mag[:], in0=lr[:], in1=dt[:], op=ALU.mult), [k + 'lr', k + 'dt'], [k + 'mag'])
        A(lambda e: e.activation(out=mag[:], in_=mag[:], func=AF.Exp), [k + 'mag'], [k + 'mag'])
        V(lambda e: e.tensor_tensor(out=th[:], in0=li[:], in1=dt[:], op=ALU.mult), [k + 'li', k + 'dt'], [k + 'th'])

        def sin_of(shift, outt, ok):
            t = c.sb([128, 512], F32, "t"); ni = c.sb([128, 512], I32, "ni"); nf = c.sb([128, 512], F32, "nf")
            a = c.sb([128, 512], F32, "a"); mk = c.sb([128, 512], F32, "mk")
            V(lambda e: e.tensor_scalar(out=t[:], in0=th[:], scalar1=1.0 / TWO_PI, scalar2=8.5 + shift, op0=ALU.mult, op1=ALU.add), [k + 'th'], [k + 't'])
            V(lambda e: e.tensor_copy(out=ni[:], in_=t[:]), [k + 't'], [k + 'ni'])
            V(lambda e: e.tensor_copy(out=nf[:], in_=ni[:]), [k + 'ni'], [k + 'nf'])
            V(lambda e: e.scalar_tensor_tensor(out=a[:], in0=t[:], scalar=-0.5, in1=nf[:], op0=ALU.add, op1=ALU.subtract), [k + 't', k + 'nf'], [k + 'a'])
            V(lambda e: e.tensor_single_scalar(out=mk[:], in_=a[:], scalar=-0.5, op=ALU.is_lt), [k + 'a'], [k + 'mk'])
            V(lambda e: e.tensor_tensor(out=a[:], in0=a[:], in1=mk[:], op=ALU.add), [k + 'a', k + 'mk'], [k + 'a'])
            A(lambda e: e.activation(out=outt[:], in_=a[:], func=AF.Sin, scale=TWO_PI), [k + 'a'], [ok])
        ar = c.sb([128, 512], F32, "ar"); ai = c.sb([128, 512], F32, "ai")
        sin_of(0.0, ai, k + 'ai')
        sin_of(0.25, ar, k + 'ar')
        V(lambda e: e.tensor_tensor(out=ai[:], in0=ai[:], in1=mag[:], op=ALU.mult), [k + 'ai', k + 'mag'], [k + 'ai'])
        V(lambda e: e.tensor_tensor(out=ar[:], in0=ar[:], in1=mag[:], op=ALU.mult), [k + 'ar', k + 'mag'], [k + 'ar'])
        zr = c.sb([128, 512], F32, "zr"); zi = c.sb([128, 512], F32, "zi")
        nr = c.sb([128, 512], F32, "nr"); den = c.sb([128, 512], F32, "den"); t2 = c.sb([128, 512], F32, "t2")
        V(lambda e: e.tensor_scalar(out=nr[:], in0=ar[:], scalar1=-1.0, scalar2=None, op0=ALU.add), [k + 'ar'], [k + 'nr'])
        V(lambda e: e.tensor_tensor(out=den[:], in0=lr[:], in1=lr[:], op=ALU.mult), [k + 'lr'], [k + 'den'])
        V(lambda e: e.tensor_tensor(out=t2[:], in0=li[:], in1=li[:], op=ALU.mult), [k + 'li'], [k + 't2'])
        V(lambda e: e.tensor_tensor(out=den[:], in0=den[:], in1=t2[:], op=ALU.add), [k + 'den', k + 't2'], [k + 'den'])
        V(lambda e: e.reciprocal(out=den[:], in_=den[:]), [k + 'den'], [k + 'den'])
        V(lambda e: e.tensor_tensor(out=zr[:], in0=nr[:], in1=lr[:], op=ALU.mult), [k + 'nr', k + 'lr'], [k + 'zr'])
        V(lambda e: e.tensor_tensor(out=t2[:], in0=ai[:], in1=li[:], op=ALU.mult), [k + 'ai', k + 'li'], [k + 't2'])
        V(lambda e: e.tensor_tensor(out=zr[:], in0=zr[:], in1=t2[:], op=ALU.add), [k + 'zr', k + 't2'], [k + 'zr'])
        V(lambda e: e.tensor_tensor(out=zr[:], in0=zr[:], in1=den[:], op=ALU.mult), [k + 'zr', k + 'den'], [k + 'zr'])
        V(lambda e: e.tensor_tensor(out=zi[:], in0=ai[:], in1=lr[:], op=ALU.mult), [k + 'ai', k + 'lr'], [k + 'zi'])
        V(lambda e: e.tensor_tensor(out=t2[:], in0=nr[:], in1=li[:], op=ALU.mult), [k + 'nr', k + 'li'], [k + 't2'])
        V(lambda e: e.tensor_tensor(out=zi[:], in0=zi[:], in1=t2[:], op=ALU.subtract), [k + 'zi', k + 't2'], [k + 'zi'])
        V(lambda e: e.tensor_tensor(out=zi[:], in0=zi[:], in1=den[:], op=ALU.mult), [k + 'zi', k + 'den'], [k + 'zi'])
        return ar, ai, zr, zi, k

    def cmul(outr, outi, xr, xi, yr, yi, keys_in, kor, koi, negate_im=False, view=None):
        ta_t, tb_t = cm_tmp
        ta = view(ta_t[:]) if view else ta_t[:]
        tb = view(tb_t[:]) if view else tb_t[:]
        V(lambda e: e.tensor_tensor(out=ta, in0=xr, in1=yr, op=ALU.mult), keys_in, ['cm_ta'])
        V(lambda e: e.tensor_tensor(out=tb, in0=xi, in1=yi, op=ALU.mult), keys_in, ['cm_tb'])
        V(lambda e: e.tensor_tensor(out=outr, in0=ta, in1=tb, op=ALU.subtract), ['cm_ta', 'cm_tb'], [kor])
        V(lambda e: e.tensor_tensor(out=ta, in0=xr, in1=yi, op=ALU.mult), keys_in + [kor], ['cm_ta'])
        V(lambda e: e.tensor_tensor(out=tb, in0=xi, in1=yr, op=ALU.mult), keys_in + [kor], ['cm_tb'])
        if negate_im:
            V(lambda e: e.scalar_tensor_tensor(out=outi, in0=ta, scalar=-1.0, in1=tb, op0=ALU.mult, op1=ALU.subtract), ['cm_ta', 'cm_tb'], [koi])
        else:
            V(lambda e: e.tensor_tensor(out=outi, in0=ta, in1=tb, op=ALU.add), ['cm_ta', 'cm_tb'], [koi])

    cm_tmp = []
    ZBT = c.sb([128, 4, L, 2, 128], BF16, "ZBT")
    CYT = c.sb([128, 16, L + 1, 2, 32], BF16, "CYT")
    TT = c.sb([128, 4, L, 128], BF16, "TT")
    BBY = c.sb([128, 2, 512], BF16, "BBY")
    AL = c.sb([128, 2, 2, 16], F32, "AL")
    tbl_mark = c.mark()
    ar, ai, zr, zi, k = ssm_prep('X')
    br_ = c.sb([128, 512], F32, "br"); bi_ = c.sb([128, 512], F32, "bi")
    D(br_[:], ssm_in["BRX"], w=['brx'])
    D(bi_[:], ssm_in["BIX"], w=['bix'])
    bbr = c.sb([128, 512], F32, "bbr"); bbi = c.sb([128, 512], F32, "bbi")
    cm_tmp[:] = [c.sb([128, 512], F32, "ta"), c.sb([128, 512], F32, "tb")]
    cmul(bbr[:], bbi[:], zr[:], zi[:], br_[:], bi_[:], [k + 'zr', k + 'zi', 'brx', 'bix'], 'bbr', 'bbi')
    pw = [c.sb([128, 2, 512], F32, "pw") for _ in range(2)]
    V(lambda e, pw=pw: e.memset(pw[0][:, 0, :], 1.0), (), ['pw0'])
    V(lambda e, pw=pw: e.memset(pw[0][:, 1, :], 0.0), (), ['pw0'])
    for n in range(L):
        cur, ck = pw[n % 2], f'pw{n % 2}'
        nxt, nk = pw[(n + 1) % 2], f'pw{(n + 1) % 2}'
        j = L - 1 - n
        cmul(ZBT[:, :, j, 0, :], ZBT[:, :, j, 1, :], cur[:, 0, :].rearrange("p (s q) -> p s q", s=4), cur[:, 1, :].rearrange("p (s q) -> p s q", s=4),
             bbr[:].rearrange("p (s q) -> p s q", s=4), bbi[:].rearrange("p (s q) -> p s q", s=4), [ck, 'bbr', 'bbi'], 'ZBT', 'ZBT',
             view=lambda ap: ap.rearrange("p (s q) -> p s q", s=4))
        if n < L - 1:
            cmul(nxt[:, 0, :], nxt[:, 1, :], cur[:, 0, :], cur[:, 1, :], ar[:], ai[:], [ck, k + 'ar', k + 'ai'], nk, nk)
    c.barrier()
    c.release(tbl_mark)
    ar, ai, zr, zi, k = ssm_prep('Y')
    br_ = c.sb([128, 512], F32, "br"); bi_ = c.sb([128, 512], F32, "bi")
    cr_ = c.sb([128, 512], F32, "cr"); ci_ = c.sb([128, 512], F32, "ci")
    D(br_[:], ssm_in["BRY"], w=['bry'])
    D(bi_[:], ssm_in["BIY"], w=['biy'])
    D(cr_[:], ssm_in["CRY"], w=['cry'])
    D(ci_[:], ssm_in["CIY"], w=['ciy'])
    cm_tmp[:] = [c.sb([128, 512], F32, "ta"), c.sb([128, 512], F32, "tb")]
    cmul(BBY[:, 0, :], BBY[:, 1, :], zr[:], zi[:], br_[:], bi_[:], [k + 'zr', k + 'zi', 'bry', 'biy'], 'BBY', 'BBY')
    pw = [c.sb([128, 2, 512], F32, "pw") for _ in range(2)]
    V(lambda e, pw=pw: e.memset(pw[0][:, 0, :], 1.0), (), ['pw0'])
    V(lambda e, pw=pw: e.memset(pw[0][:, 1, :], 0.0), (), ['pw0'])
    for n in range(L + 1):
        cur, ck = pw[n % 2], f'pw{n % 2}'
        nxt, nk = pw[(n + 1) % 2], f'pw{(n + 1) % 2}'
        cmul(CYT[:, :, n, 0, :], CYT[:, :, n, 1, :], cr_[:].rearrange("p (k j) -> p k j", k=16), ci_[:].rearrange("p (k j) -> p k j", k=16),
             cur[:, 0, :].rearrange("p (k j) -> p k j", k=16), cur[:, 1, :].rearrange("p (k j) -> p k j", k=16), [ck, 'cry', 'ciy'], 'CYT', 'CYT', negate_im=True,
             view=lambda ap: ap.rearrange("p (k j) -> p k j", k=16))
        if n < L:
            cmul(nxt[:, 0, :], nxt[:, 1, :], cur[:, 0, :], cur[:, 1, :], ar[:], ai[:], [ck, k + 'ar', k + 'ai'], nk, nk)
    pL, pLk = pw[L % 2], f'pw{L % 2}'
    pLv_r = pL[:, 0, :].rearrange("p (k j) -> p k j", k=16)[:, :, 0]
    pLv_i = pL[:, 1, :].rearrange("p (k j) -> p k j", k=16)[:, :, 0]
    V(lambda e: e.tensor_copy(out=AL[:, 0, 0, :], in_=pLv_r), [pLk], ['AL'])
    V(lambda e: e.tensor_copy(out=AL[:, 0, 1, :], in_=pLv_r), [pLk], ['AL'])
    V(lambda e: e.tensor_scalar(out=AL[:, 1, 0, :], in0=pLv_i, scalar1=-1.0, scalar2=None, op0=ALU.mult), [pLk], ['AL'])
    V(lambda e: e.tensor_copy(out=AL[:, 1, 1, :], in_=pLv_i), [pLk], ['AL'])
    V(lambda e: e.memset(TT[:], 0.0), (), ['TT'])
    ddt = c.sb([128, 512], F32, "ddt")
    D(ddt[:], ddg, w=['ddt'])
    for kk in range(16):
        s_, k4 = kk // 4, kk % 4
        p = nps()
        MM(ps[p][32 * k4:32 * k4 + 32, :], BBY[:, 0, kk * 32:(kk + 1) * 32], CYT[:, kk, 0:L, 0, :], True, False, ['BBY', 'CYT'], [f'ps{p}'], tp=(0, 32 * k4))
        MM(ps[p][32 * k4:32 * k4 + 32, :], BBY[:, 1, kk * 32:(kk + 1) * 32], CYT[:, kk, 0:L, 1, :], False, True, ['BBY', 'CYT'], [f'ps{p}'], tp=(0, 32 * k4))
        V(lambda e, p=p, s_=s_, k4=k4: e.tensor_copy(out=TT[32 * k4:32 * k4 + 32, s_, :, 32 * k4:32 * k4 + 32], in_=ps[p][32 * k4:32 * k4 + 32, :].rearrange("p (n j) -> p n j", n=L)), [f'ps{p}'], ['TT'])
    V(lambda e: e.tensor_tensor(out=TT[:, :, 0, :], in0=TT[:, :, 0, :], in1=ddt[:].rearrange("p (s q) -> p s q", s=4), op=ALU.add), ['TT', 'ddt'], ['TT'])
    c.barrier()
    c.release(tbl_mark)

    if stop == 'C1':
        c.barrier()
        dump('dbg_ZBT', ZBT[:].rearrange("p s j r q -> p (s j r q)"), 4 * L * 2 * 128)
        dump('dbg_CYT', CYT[:].rearrange("p k n r j -> p (k n r j)"), 16 * (L + 1) * 2 * 32)
        dump('dbg_TT', TT[:].rearrange("p s n q -> p (s n q)"), 4 * L * 128)
        dump('dbg_AL', AL[:].rearrange("p a r k -> p (a r k)"), 64)
        c.emit()
        return nc
    uT_all = c.sb([128, 4, TH], BF16, "uT_all")
    Zs = c.sb([128, 2, 16, NCH], BF16, "Zs")
    H = c.sb([128, 2, 16, NCH + 1], F32, "H")
    Hb = c.sb([128, 2, 16, NCH + 1], BF16, "Hb")
    t1 = c.sb([128, 2, 16], F32, "t1")
    t4 = c.sb([128, 2, 2, 16], F32, "t4")
    AL4 = c.sb([128, 2, 2, 16], F32, "AL4")
    V(lambda e: e.tensor_copy(out=AL4[:, 0, 0, :], in_=AL[:, 0, 0, :]), ['AL'], ['AL4'])
    V(lambda e: e.tensor_copy(out=AL4[:, 0, 1, :], in_=AL[:, 1, 0, :]), ['AL'], ['AL4'])
    V(lambda e: e.tensor_copy(out=AL4[:, 1, 0, :], in_=AL[:, 1, 1, :]), ['AL'], ['AL4'])
    V(lambda e: e.tensor_copy(out=AL4[:, 1, 1, :], in_=AL[:, 0, 1, :]), ['AL'], ['AL4'])
    t2_ = c.sb([128, 2, 16], F32, "t2_")
    G(lambda e: e.memset(H[:, :, :, 0], 0.0), (), ['H'])
    NZ = min(NCH, 512)
    for half in range(2):
        D(uT_all[:], uT_d.rearrange("(s p) t -> p s t", p=128)[:, :, half * TH:(half + 1) * TH], r=['uT_d'], w=['uT_all'])
        for kk in range(16):
            s_, k4 = kk // 4, kk % 4
            for ri in range(2):
                for z0 in range(0, NCH, NZ):
                    p = nps()
                    for j in range(L):
                        uview = uT_all[32 * k4:32 * k4 + 32, s_, :].rearrange("p (n j) -> p n j", j=L)[:, z0:z0 + NZ, j]
                        MM(ps[p][:, 0:NZ], ZBT[32 * k4:32 * k4 + 32, s_, j, ri, :], uview, j == 0, j == L - 1, ['ZBT', 'uT_all'], [f'ps{p}'], tp=(32 * k4, 0))
                    if (kk + ri) % 2 == 0:
                        A(lambda e, p=p, ri=ri, kk=kk, z0=z0: e.activation(out=Zs[:, ri, kk, z0:z0 + NZ], in_=ps[p][:, 0:NZ], func=AF.Copy), [f'ps{p}'], ['Zs'])
                    else:
                        V(lambda e, p=p, ri=ri, kk=kk, z0=z0: e.tensor_copy(out=Zs[:, ri, kk, z0:z0 + NZ], in_=ps[p][:, 0:NZ]), [f'ps{p}'], ['Zs'])
        for n in range(NCH):
            V(lambda e, n=n: e.tensor_tensor(out=t4[:], in0=AL4[:], in1=H[:, :, :, n].unsqueeze(1).broadcast_to([128, 2, 2, 16]), op=ALU.mult), ['AL4', 'H'], ['t4'])
            V(lambda e, n=n: e.tensor_tensor(out=t1[:], in0=t4[:, :, 0, :], in1=Zs[:, :, :, n], op=ALU.add), ['t4', 'Zs'], ['t1'])
            V(lambda e, n=n: e.tensor_tensor(out=H[:, :, :, n + 1], in0=t1[:], in1=t4[:, :, 1, :], op=ALU.add), ['t1', 't4'], ['H'])
        if half == 0:
            V(lambda e: e.tensor_copy(out=H[:, :, :, 0], in_=H[:, :, :, NCH]), ['H'], ['H'])
    V(lambda e: e.tensor_copy(out=Hb[:], in_=H[:]), ['H'], ['Hb'])

    if stop == 'C2':
        c.barrier()
        dump('dbg_H', H[:].rearrange("p a k n -> p (a k n)"), 2 * 16 * (NCH + 1))
        dump('dbg_Zs', Zs[:].rearrange("p a k n -> p (a k n)"), 2 * 16 * NCH)
        c.emit()
        return nc
    zT_s = [c.sb([128, 512], BF16, "zTs") for _ in range(2)]
    x2 = c.sb([128, 512], F32, "x2")
    wv = c.sb([128, 512], F32, "wv")
    zi_ = 0
    for s_ in range(4):
        for ti in range(NT):
            p = nps()
            tok0 = ti * 512
            yv = ps[p][:, :].rearrange("p (n j) -> p n j", j=L)
            uv = uT_all[:, s_, tok0:tok0 + 512].rearrange("p (n j) -> p n j", j=L)
            for n in range(L):
                MM(yv[:, :, n:L], TT[:, s_, n, :], uv[:, :, 0:L - n], n == 0, False, ['TT', 'uT_all'], [f'ps{p}'])
            cnt = 0
            for k4 in range(4):
                kk = 4 * s_ + k4
                for i in range(L):
                    for ri in range(2):
                        cnt += 1
                        MM(yv[32 * k4:32 * k4 + 32, :, i], CYT[:, kk, i + 1, ri, :], Hb[:, ri, kk, ti * 32:ti * 32 + 32], False, cnt == 4 * L * 2, ['CYT', 'Hb'], [f'ps{p}'], tp=(0, 32 * k4))
            zt, ztk = zT_s[zi_ % 2], f'zTs{zi_ % 2}'
            zi_ += 1
            A(lambda e, p=p: e.activation(out=x2[:], in_=ps[p][:, :], func=AF.Square), [f'ps{p}'], ['x2'])
            V(lambda e: e.tensor_scalar(out=wv[:], in0=x2[:], scalar1=0.044715, scalar2=1.0, op0=ALU.mult, op1=ALU.add), ['x2'], ['wv'])
            V(lambda e, p=p: e.tensor_tensor(out=wv[:], in0=wv[:], in1=ps[p][:, :], op=ALU.mult), ['wv', f'ps{p}'], ['wv'])
            A(lambda e: e.activation(out=wv[:], in_=wv[:], func=AF.Sigmoid, scale=1.5957691216057308), ['wv'], ['wv'])
            V(lambda e, p=p, zt=zt: e.tensor_tensor(out=zt[:], in0=wv[:], in1=ps[p][:, :], op=ALU.mult), ['wv', f'ps{p}'], [ztk])
            D(zT_d[s_ * 128:(s_ + 1) * 128, ti * 512:(ti + 1) * 512], zt[:], r=[ztk], w=['zT_d'])
    c.barrier()
    c.release(base_mark)

    if 'dbg_y' in dbg_out:
        tmpz = c.sb([128, 4, TH], BF16, "tmpz")
        tmpzf = c.sb([128, 4, TH], F32, "tmpzf")
        D(tmpz[:], zT_d.rearrange("(s p) t -> p s t", p=128), r=['zT_d'], w=['tmpz'])
        V(lambda e, tmpzf=tmpzf, tmpz=tmpz: e.tensor_copy(out=tmpzf[:], in_=tmpz[:]), ['tmpz'], ['tmpzf'])
        D(dbg_out['dbg_y'].rearrange("(s p) t -> p s t", p=128), tmpzf[:], r=['tmpzf'])
        c.barrier()
        c.release(base_mark)
    if stop == 'C':
        c.emit()
        return nc
    def load_w(dram_view, shape, key, scale_ap=None):
        wt = c.sb(shape, BF16, key)
        a_n, ncol = shape[1], shape[2]
        for a_ in range(a_n):
            st_ = lw_st[:, 0:ncol]
            D(st_, dram_view[:, a_, :], w=['lw_st'])
            if scale_ap is None:
                V(lambda e, a_=a_, st_=st_: e.tensor_copy(out=wt[:, a_, :], in_=st_), ['lw_st'], [key])
            else:
                V(lambda e, a_=a_, st_=st_: e.tensor_scalar(out=wt[:, a_, :], in0=st_, scalar1=scale_ap[:, a_:a_ + 1], scalar2=None, op0=ALU.mult), ['lw_st', 'gf'], [key])
        return wt

    lw_st = c.sb([128, 1024], F32, "lw_st")
    gf = c.sb([128, 8], F32, "gf")
    D(gf[:], gffnT, w=['gf'])
    bgl = c.sb([128, 4], F32, "bgl")
    D(bgl[:], bglu, w=['bgl'])
    brt = c.sb([128, 20], F32, "brt")
    D(brt[:], br, w=['brt'])
    Wglu = load_w(w_glu.rearrange("(kc p) n -> p kc n", p=128), [128, 4, 512], 'Wglu')
    Wob = load_w(w_out_b.rearrange("(kc p) n -> p kc n", p=128), [128, 4, 1024], 'Wob')
    Woa = c.sb([64, 8, 1024], BF16, "Woa")
    woa_v = w_out_a.rearrange("(h p) n -> p h n", p=64)
    for h in range(8):
        D(lw_st[0:64, :], woa_v[:, h, :], w=['lw_st'])
        V(lambda e, h=h: e.tensor_copy(out=Woa[:, h, :], in_=lw_st[0:64, :]), ['lw_st'], ['Woa'])
    Wout = load_w(w_out.rearrange("(kc p) n -> p kc n", p=128), [128, 8, 1024], 'Wout')
    Wr = c.sb([128, 8, 20], F32, "Wr")
    D(Wr[:], wr.rearrange("(kc p) n -> p kc n", p=128), w=['Wr'])
    for kc in range(8):
        V(lambda e, kc=kc: e.tensor_scalar(out=Wr[:, kc, :], in0=Wr[:, kc, :], scalar1=gf[:, kc:kc + 1], scalar2=None, op0=ALU.mult), ['Wr', 'gf'], ['Wr'])

    if stop == 'D1':
        c.emit()
        return nc
    Wrhi = c.sb([128, 8, 20], BF16, "Wrhi")
    Wrlo = c.sb([128, 8, 20], BF16, "Wrlo")
    V(lambda e: e.tensor_copy(out=Wrhi[:], in_=Wr[:]), ['Wr'], ['Wrhi'])
    V(lambda e: e.tensor_tensor(out=Wr[:], in0=Wr[:], in1=Wrhi[:], op=ALU.subtract), ['Wr', 'Wrhi'], ['Wr'])
    V(lambda e: e.tensor_copy(out=Wrlo[:], in_=Wr[:]), ['Wr'], ['Wrlo'])
    h2hi = c.sb([128, 1024], BF16, "h2hi")
    h2lo = c.sb([128, 1024], BF16, "h2lo")
    hThi = c.sb([128, 8, 128], BF16, "hThi")
    hTlo = c.sb([128, 8, 128], BF16, "hTlo")
    cwb = c.sb([128, 16], BF16, "cwb")
    zT = c.sb([128, 4, 512], BF16, "zT")
    oT = c.sb([64, 8, 512], BF16, "oT")
    gT = c.sb([128, 16, 512], BF16, "gT")
    xt = c.sb([128, 4, 1024], F32, "xtD")
    zg = c.sb([128, 4, 512], BF16, "zg")
    sg = c.sb([128, 512], F32, "sg")
    mgd = c.sb([128, 8, 512], BF16, "mgd")
    ta_ = c.sb([128, 512], F32, "taD")
    tb_ = c.sb([128, 512], F32, "tbD")
    x1 = c.sb([128, 4, 1024], F32, "x1")
    h2f = c.sb([128, 1024], F32, "h2f")
    h2Tf = c.sb([128, 8, 128], F32, "h2Tf")
    h2Tb = c.sb([128, 8, 512], BF16, "h2Tb")
    cwTb = c.sb([16, 512], BF16, "cwTb")
    ss2 = c.sb([128, 4], F32, "ss2")
    junk2 = c.sb([128, 1024], BF16, "junk2")
    junk2_b = [junk2, c.sb([128, 1024], BF16, "junk2b")]
    lg = c.sb([128, 20], F32, "lg")
    cw = c.sb([128, 16], F32, "cw")
    sm = {n_: c.sb([128, 4], F32, n_) for n_ in ["gmx", "ge", "gsum", "oh", "les", "m1", "mk1", "le2", "m2", "mk2", "w12", "tmp4"]}
    one1 = {n_: c.sb([128, 1], F32, n_) for n_ in ["psel", "d21", "w1", "w2"]}
    xin_own = xin[TH:T2, :].rearrange("(t j p) d -> t p j d", j=4, p=128)
    zT_b2 = [zT, c.sb([128, 4, 512], BF16, "zT2")]
    oT_b2 = [oT, c.sb([64, 8, 512], BF16, "oT2")]
    gT_b2 = [gT, c.sb([128, 16, 512], BF16, "gT2")]
    xt_b2 = [xt, c.sb([128, 4, 1024], F32, "xtD2")]
    lg4 = c.sb([128, 4, 20], F32, "lg4")
    R4 = {n_: c.sb([128, 4, 4], F32, n_) for n_ in ["oh", "ge", "les", "mk1", "le2", "mk2", "w12", "tq"]}
    R1 = {n_: c.sb([128, 4], F32, n_) for n_ in ["mx", "gsum", "psel", "m1", "m2", "d", "w1", "w2"]}
    prod = c.sb([128, 4, 4, 4], F32, "prod")
    cw4 = c.sb([128, 4, 16], F32, "cw4")
    cwb4 = c.sb([128, 4, 16], BF16, "cwb4")
    AXX = mybir.AxisListType.X

    def bc3(ap2):
        return ap2.unsqueeze(2).broadcast_to([128, 4, 4])

    def load_tile(ti):
        b_ = ti % 2
        t0_ = ti * 512
        D(zT_b2[b_][:], zT_d.rearrange("(s p) t -> p s t", p=128)[:, :, t0_:t0_ + 512], r=['zT_d'], w=[f'zT{b_}'])
        D(oT_b2[b_][:], oT_d.rearrange("h d t -> d h t")[:, :, t0_:t0_ + 512], r=['oT_d'], w=[f'oT{b_}'])
        for hh in range(2):
            D(gT_b2[b_][:, hh * 8:(hh + 1) * 8, :], gT_d.rearrange("(m p) t -> p m t", p=128)[:, hh * 8:(hh + 1) * 8, t0_:t0_ + 512], r=['gT_d'], w=[f'gT{b_}'])
        D(xt_b2[b_][:], xin_own[ti], w=[f'xtD{b_}'])

    load_tile(0)
    for ti in range(NT):
        t0 = ti * 512
        b_ = ti % 2
        zT, oT, gT, xt = zT_b2[b_], oT_b2[b_], gT_b2[b_], xt_b2[b_]
        zTk, oTk, gTk, xtk = f'zT{b_}', f'oT{b_}', f'gT{b_}', f'xtD{b_}'
        if ti + 1 < NT:
            load_tile(ti + 1)
        for m in range(4):
            p = nps()
            for kc in range(4):
                MM(ps[p][:, :], Wglu[:, kc, m * 128:(m + 1) * 128], zT[:, kc, :], kc == 0, kc == 3, ['Wglu', zTk], [f'ps{p}'])
            A(lambda e, p=p, m=m: e.activation(out=sg[:], in_=ps[p][:, :], func=AF.Sigmoid, bias=bgl[:, m:m + 1]), [f'ps{p}', 'bgl'], ['sg'])
            V(lambda e, m=m, zT=zT: e.tensor_tensor(out=zg[:, m, :], in0=zT[:, m, :], in1=sg[:], op=ALU.mult), [zTk, 'sg'], ['zg'])
        for m in range(8):
            p = nps()
            for kc in range(4):
                MM(ps[p][:, :], Wob[:, kc, m * 128:(m + 1) * 128], zg[:, kc, :], kc == 0, kc == 3, ['Wob', 'zg'], [f'ps{p}'])
            p2 = nps()
            for h in range(8):
                MM(ps[p2][:, :], Woa[:, h, m * 128:(m + 1) * 128], oT[:, h, :], h == 0, h == 7, ['Woa', oTk], [f'ps{p2}'])
            V(lambda e, p=p, m=m, gT=gT: e.tensor_tensor(out=ta_[:], in0=gT[:, 8 + m, :], in1=ps[p][:, :], op=ALU.mult), [gTk, f'ps{p}'], ['taD'])
            V(lambda e, p2=p2, m=m, gT=gT: e.tensor_tensor(out=tb_[:], in0=gT[:, m, :], in1=ps[p2][:, :], op=ALU.mult), [gTk, f'ps{p2}'], ['tbD'])
            G(lambda e, m=m: e.tensor_tensor(out=mgd[:, m, :], in0=ta_[:], in1=tb_[:], op=ALU.add), ['taD', 'tbD'], ['mgd'])
        for j in range(4):
            for hf in range(2):
                p = nps()
                for kc in range(8):
                    MM(ps[p][:, :], mgd[:, kc, j * 128:(j + 1) * 128], Wout[:, kc, hf * 512:(hf + 1) * 512], kc == 0, kc == 7, ['mgd', 'Wout'], [f'ps{p}'])
                V(lambda e, p=p, j=j, hf=hf, xt=xt: e.tensor_tensor(out=x1[:, j, hf * 512:(hf + 1) * 512], in0=xt[:, j, hf * 512:(hf + 1) * 512], in1=ps[p][:, :], op=ALU.add), [xtk, f'ps{p}'], ['x1'])
        D(x1_d[t0:t0 + 512, :].rearrange("(j p) d -> p j d", p=128), x1[:], r=['x1'], w=['x1_d'])
        for j in range(4):
            jk_, jkk_ = junk2_b[j % 2], f'junk2{j % 2}'
            A(lambda e, j=j, jk_=jk_: e.activation(out=jk_[:], in_=x1[:, j, :], func=AF.Square), ['x1'], [jkk_])
            V(lambda e, j=j, jk_=jk_: e.tensor_reduce(out=ss2[:, j:j + 1], in_=jk_[:], axis=mybir.AxisListType.X, op=ALU.add), [jkk_], ['Ess'])
        m0 = c.mark()
        rs2 = rstd_of(ss2, 4, 'E')
        for j in range(4):
            A(lambda e, j=j, rs2=rs2: e.activation(out=h2f[:], in_=x1[:, j, :], func=AF.Copy, scale=rs2[:, j:j + 1]), ['x1', 'Ers'], ['h2f'])
            G(lambda e: e.tensor_copy(out=h2hi[:], in_=h2f[:]), ['h2f'], ['h2hi'])
            G(lambda e: e.tensor_tensor(out=h2lo[:], in0=h2f[:], in1=h2hi[:], op=ALU.subtract), ['h2f', 'h2hi'], ['h2lo'])
            for srct, srck, dst, dstk, pbk in ((h2hi, 'h2hi', hThi, 'hThi', 0), (h2lo, 'h2lo', hTlo, 'hTlo', 1)):
                for kc in range(8):
                    TR(pb[pbk][:, kc * 128:(kc + 1) * 128], srct[:, kc * 128:(kc + 1) * 128], identb[:], [srck, 'identb'], [f'pb{pbk}'])
                A(lambda e, dst=dst, pbk=pbk: e.activation(out=dst[:], in_=pb[pbk][:, :].rearrange("p (k t) -> p k t", k=8), func=AF.Copy), [f'pb{pbk}'], [dstk])
            G(lambda e, j=j: e.tensor_copy(out=h2Tb[:, :, j * 128:(j + 1) * 128], in_=hThi[:]), ['hThi'], ['h2Tb'])
            p = nps()
            nmm = 0
            for (lt, ltk, wt_, wtk) in ((hThi, 'hThi', Wrhi, 'Wrhi'), (hTlo, 'hTlo', Wrhi, 'Wrhi'), (hThi, 'hThi', Wrlo, 'Wrlo')):
                for kc in range(8):
                    MM(ps[p][:, 0:20], lt[:, kc, :], wt_[:, kc, :], nmm == 0, nmm == 23, [ltk, wtk], [f'ps{p}'])
                    nmm += 1
            V(lambda e, p=p, j=j: e.tensor_tensor(out=lg4[:, j, :], in0=ps[p][:, 0:20], in1=brt[:], op=ALU.add), [f'ps{p}', 'brt'], ['lg4'])
        c.release(m0)
        gl = lg4[:, :, 0:4]
        le4 = lg4[:, :, 4:20].rearrange("p j (g e) -> p j g e", g=4)
        V(lambda e: e.tensor_reduce(out=R1["mx"][:], in_=gl, axis=AXX, op=ALU.max), ['lg4'], ['r_mx'])
        V(lambda e: e.tensor_tensor(out=R4["oh"][:], in0=gl, in1=bc3(R1["mx"][:, :]), op=ALU.is_ge), ['lg4', 'r_mx'], ['r_oh'])
        V(lambda e: e.tensor_tensor(out=R4["ge"][:], in0=gl, in1=bc3(R1["mx"][:, :]), op=ALU.subtract), ['lg4', 'r_mx'], ['r_ge'])
        A(lambda e: e.activation(out=R4["ge"][:], in_=R4["ge"][:], func=AF.Exp), ['r_ge'], ['r_ge'])
        V(lambda e: e.tensor_reduce(out=R1["gsum"][:], in_=R4["ge"][:], axis=AXX, op=ALU.add), ['r_ge'], ['r_gsum'])
        V(lambda e: e.reciprocal(out=R1["psel"][:], in_=R1["gsum"][:]), ['r_gsum'], ['r_psel'])
        V(lambda e: e.tensor_tensor(out=prod[:], in0=le4, in1=R4["oh"][:, :, :].unsqueeze(3).broadcast_to([128, 4, 4, 4]), op=ALU.mult), ['lg4', 'r_oh'], ['r_prod'])
        V(lambda e: e.tensor_reduce(out=R4["les"][:], in_=prod[:].rearrange("p j g e -> p j e g"), axis=AXX, op=ALU.add), ['r_prod'], ['r_les'])
        V(lambda e: e.tensor_reduce(out=R1["m1"][:], in_=R4["les"][:], axis=AXX, op=ALU.max), ['r_les'], ['r_m1'])
        V(lambda e: e.tensor_tensor(out=R4["mk1"][:], in0=R4["les"][:], in1=bc3(R1["m1"][:, :]), op=ALU.is_ge), ['r_les', 'r_m1'], ['r_mk1'])
        V(lambda e: e.scalar_tensor_tensor(out=R4["le2"][:], in0=R4["mk1"][:], scalar=-1e30, in1=R4["les"][:], op0=ALU.mult, op1=ALU.add), ['r_mk1', 'r_les'], ['r_le2'])
        V(lambda e: e.tensor_reduce(out=R1["m2"][:], in_=R4["le2"][:], axis=AXX, op=ALU.max), ['r_le2'], ['r_m2'])
        V(lambda e: e.tensor_tensor(out=R4["mk2"][:], in0=R4["le2"][:], in1=bc3(R1["m2"][:, :]), op=ALU.is_ge), ['r_le2', 'r_m2'], ['r_mk2'])
        V(lambda e: e.tensor_tensor(out=R1["d"][:], in0=R1["m2"][:], in1=R1["m1"][:], op=ALU.subtract), ['r_m1', 'r_m2'], ['r_d'])
        A(lambda e: e.activation(out=R1["d"][:], in_=R1["d"][:], func=AF.Exp), ['r_d'], ['r_d'])
        V(lambda e: e.tensor_scalar(out=R1["d"][:], in0=R1["d"][:], scalar1=1.0, scalar2=None, op0=ALU.add), ['r_d'], ['r_d'])
        V(lambda e: e.reciprocal(out=R1["w1"][:], in_=R1["d"][:]), ['r_d'], ['r_w1'])
        V(lambda e: e.tensor_scalar(out=R1["w2"][:], in0=R1["w1"][:], scalar1=-1.0, scalar2=1.0, op0=ALU.mult, op1=ALU.add), ['r_w1'], ['r_w2'])
        V(lambda e: e.tensor_tensor(out=R1["w1"][:], in0=R1["w1"][:], in1=R1["psel"][:], op=ALU.mult), ['r_w1', 'r_psel', 'r_w2'], ['r_w1'])
        V(lambda e: e.tensor_tensor(out=R1["w2"][:], in0=R1["w2"][:], in1=R1["psel"][:], op=ALU.mult), ['r_w2', 'r_psel'], ['r_w2'])
        V(lambda e: e.tensor_tensor(out=R4["w12"][:], in0=R4["mk1"][:], in1=bc3(R1["w1"][:, :]), op=ALU.mult), ['r_mk1', 'r_w1'], ['r_w12'])
        V(lambda e: e.tensor_tensor(out=R4["tq"][:], in0=R4["mk2"][:], in1=bc3(R1["w2"][:, :]), op=ALU.mult), ['r_mk2', 'r_w2'], ['r_tq'])
        V(lambda e: e.tensor_tensor(out=R4["w12"][:], in0=R4["w12"][:], in1=R4["tq"][:], op=ALU.add), ['r_w12', 'r_tq'], ['r_w12'])
        V(lambda e: e.tensor_tensor(out=cw4[:].rearrange("p j (g e) -> p j g e", g=4), in0=R4["oh"][:, :, :].unsqueeze(3).broadcast_to([128, 4, 4, 4]),
                                    in1=R4["w12"][:, :, :].unsqueeze(2).broadcast_to([128, 4, 4, 4]), op=ALU.mult), ['r_oh', 'r_w12'], ['cw4'])
        V(lambda e: e.tensor_copy(out=cwb4[:], in_=cw4[:]), ['cw4'], ['cwb4'])
        for j in range(4):
            TR(pb[0][0:16, 0:128], cwb4[:, j, :], identb[:], ['cwb4', 'identb'], ['pb0'])
            V(lambda e, j=j: e.tensor_copy(out=cwTb[:, j * 128:(j + 1) * 128], in_=pb[0][0:16, 0:128]), ['pb0'], ['cwTb'])
        D(h2T_d.rearrange("(kc p) t -> p kc t", p=128)[:, :, t0:t0 + 512], h2Tb[:], r=['h2Tb'], w=['h2T_d'])
        D(cwT_d[:, t0:t0 + 512], cwTb[:], r=['cwTb'], w=['cwT_d'])
    c.barrier()
    c.release(base_mark)
    if 'dbg_x1' in dbg_out:
        tmpx = c.sb([128, TH // 128, 1024], F32, "tmpx")
        D(tmpx[:], x1_d.rearrange("(j p) d -> p j d", p=128), r=['x1_d'], w=['tmpx'])
        D(dbg_out['dbg_x1'].rearrange("(j p) d -> p j d", p=128), tmpx[:], r=['tmpx'])
        tmpc = c.sb([16, TH], BF16, "tmpc"); tmpcf = c.sb([16, TH], F32, "tmpcf")
        D(tmpc[:], cwT_d, r=['cwT_d'], w=['tmpc'])
        V(lambda e, tmpcf=tmpcf, tmpc=tmpc: e.tensor_copy(out=tmpcf[:], in_=tmpc[:]), ['tmpc'], ['tmpcf'])
        D(dbg_out['dbg_cw'], tmpcf[:], r=['tmpcf'])
        c.barrier()
        c.release(base_mark)

    if stop == 'D':
        c.emit()
        return nc
    ST = min(TH, 1024)
    NSB = ST // 128
    gf2 = c.sb([128, 8], F32, "gf2")
    D(gf2[:], gffnT, w=['gf2'])
    selb = c.sb([16, 2048], BF16, "selb")
    m_ = c.mark()
    selst = c.sb([16, 2048], F32, "selst")
    D(selst[:], sel_d, w=['selst'])
    V(lambda e: e.tensor_copy(out=selb[:], in_=selst[:]), ['selst'], ['selb'])
    gfin_t = c.sb([128, 1024], F32, "gfin")
    D(gfin_t[:], gfin, w=['gfin'])
    h2T = c.sb([128, 8, ST], BF16, "h2T")
    cwT = c.sb([16, ST], BF16, "cwT")
    acc = c.sb([128, NSB, 1024], F32, "acc")
    Wg_b = [c.sb([128, 8, 256], BF16, "Wg") for _ in range(2)]
    Wu_b = [c.sb([128, 8, 256], BF16, "Wu") for _ in range(2)]
    Wd_b = [c.sb([128, 2, 1024], BF16, "Wd") for _ in range(2)]
    wstg = [[c.sb([128, 8, 256], F32, "wstg") for _ in range(2)] for _ in range(2)]
    wstd = [c.sb([128, 2, 1024], F32, "wstd") for _ in range(2)]
    bcs_b = [c.sb([128, 512], BF16, "bcs") for _ in range(2)]
    sgl_b = [c.sb([128, 512], F32, "sgl") for _ in range(2)]
    tmu_b = [c.sb([128, 512], F32, "tmu") for _ in range(2)]
    actT_b = [c.sb([128, 2, 512], BF16, "actT") for _ in range(2)]
    x1f = c.sb([128, 1024], F32, "x1f")
    ssf = c.sb([128, 1], F32, "ssf")
    junk3 = c.sb([128, 1024], BF16, "junk3")
    outt = [c.sb([128, 1024], F32, "outt") for _ in range(2)]
    NST = TH // ST
    NSUB = ST // 512

    def load_expert(n):
        ex, sl = n % 16, n % 2
        Wg, Wu, Wd = Wg_b[sl], Wu_b[sl], Wd_b[sl]
        wk = f'We{sl}'
        D(wstg[sl][0][:], w_eg[ex].rearrange("(kc p) f -> p kc f", p=128), w=[f'wstg{sl}0'])
        D(wstg[sl][1][:], w_eu[ex].rearrange("(kc p) f -> p kc f", p=128), w=[f'wstg{sl}1'])
        D(wstd[sl][:], w_ed[ex].rearrange("(fc p) d -> p fc d", p=128), w=[f'wstd{sl}'])
        for kc in range(8):
            A(lambda e, kc=kc, Wg=Wg, sl=sl: e.activation(out=Wg[:, kc, :], in_=wstg[sl][0][:, kc, :], func=AF.Copy, scale=gf2[:, kc:kc + 1]), [f'wstg{sl}0', 'gf2'], [wk])
            A(lambda e, kc=kc, Wu=Wu, sl=sl: e.activation(out=Wu[:, kc, :], in_=wstg[sl][1][:, kc, :], func=AF.Copy, scale=gf2[:, kc:kc + 1]), [f'wstg{sl}1', 'gf2'], [wk])
        A(lambda e, Wd=Wd, sl=sl: e.activation(out=Wd[:, 0, :], in_=wstd[sl][:, 0, :], func=AF.Copy), [f'wstd{sl}'], [wk])
        V(lambda e, Wd=Wd, sl=sl: e.tensor_copy(out=Wd[:, 1, :], in_=wstd[sl][:, 1, :]), [f'wstd{sl}'], [wk])

    ucount = [0]

    def moe_stage1(n, sub):
        ex, sl = n % 16, n % 2
        Wg, Wu = Wg_b[sl], Wu_b[sl]
        wk = f'We{sl}'
        ub = ucount[0] % 2
        ucount[0] += 1
        bcs, sgl, tmu, actT = bcs_b[ub], sgl_b[ub], tmu_b[ub], actT_b[ub]
        q0 = sub * 512
        p = nps()
        MM(ps[p][:, :], selb[:, ex * 128:(ex + 1) * 128], cwT[:, q0:q0 + 512], True, True, ['selb', 'cwT'], [f'ps{p}'])
        A(lambda e, p=p, bcs=bcs: e.activation(out=bcs[:], in_=ps[p][:, :], func=AF.Copy), [f'ps{p}'], [f'bcs{ub}'])
        for fc in range(2):
            pg = nps()
            for kc in range(8):
                MM(ps[pg][:, :], Wg[:, kc, fc * 128:(fc + 1) * 128], h2T[:, kc, q0:q0 + 512], kc == 0, kc == 7, [wk, 'h2T'], [f'ps{pg}'])
            pu = nps()
            for kc in range(8):
                MM(ps[pu][:, :], Wu[:, kc, fc * 128:(fc + 1) * 128], h2T[:, kc, q0:q0 + 512], kc == 0, kc == 7, [wk, 'h2T'], [f'ps{pu}'])
            A(lambda e, pg=pg, sgl=sgl: e.activation(out=sgl[:], in_=ps[pg][:, :], func=AF.Silu), [f'ps{pg}'], [f'sgl{ub}'])
            V(lambda e, pu=pu, sgl=sgl, tmu=tmu: e.tensor_tensor(out=tmu[:], in0=sgl[:], in1=ps[pu][:, :], op=ALU.mult), [f'sgl{ub}', f'ps{pu}'], [f'tmu{ub}'])
            G(lambda e, fc=fc, tmu=tmu, bcs=bcs, actT=actT: e.tensor_tensor(out=actT[:, fc, :], in0=tmu[:], in1=bcs[:], op=ALU.mult), [f'tmu{ub}', f'bcs{ub}'], [f'actT{ub}'])
        return ub

    def moe_stage2(n, sub, ub):
        sl = n % 2
        Wd = Wd_b[sl]
        wk = f'We{sl}'
        actT = actT_b[ub]
        for j in range(4):
            blk = sub * 4 + j
            for hf in range(2):
                p = nps()
                for fc in range(2):
                    MM(ps[p][:, :], actT[:, fc, j * 128:(j + 1) * 128], Wd[:, fc, hf * 512:(hf + 1) * 512], fc == 0, fc == 1, [f'actT{ub}', wk], [f'ps{p}'])
                V(lambda e, p=p, blk=blk, hf=hf: e.tensor_tensor(out=acc[:, blk, hf * 512:(hf + 1) * 512], in0=acc[:, blk, hf * 512:(hf + 1) * 512], in1=ps[p][:, :], op=ALU.add), ['acc', f'ps{p}'], ['acc'])

    load_expert(0)
    for sti in range(NST):
        s0 = sti * ST
        D(h2T[:], h2T_d.rearrange("(kc p) t -> p kc t", p=128)[:, :, s0:s0 + ST], r=['h2T_d'], w=['h2T'])
        D(cwT[:], cwT_d[:, s0:s0 + ST], r=['cwT_d'], w=['cwT'])
        G(lambda e: e.memset(acc[:], 0.0), (), ['acc'])
        units = [(sti * 16 + ex, sub) for ex in range(16) for sub in range(NSUB)]
        prev = None
        for i in range(len(units) + 1):
            cur = None
            if i < len(units):
                n, sub = units[i]
                ub = moe_stage1(n, sub)
                cur = (n, sub, ub)
            if prev is not None:
                moe_stage2(*prev)
            if cur is not None and cur[1] == 0 and cur[0] + 1 < NST * 16:
                load_expert(cur[0] + 1)
            prev = cur
        for blk in range(NSB):
            r0 = s0 + blk * 128
            ot, otk = outt[blk % 2], f'outt{blk % 2}'
            D(x1f[:], x1_d[r0:r0 + 128, :], r=['x1_d'], w=['x1f'])
            V(lambda e, blk=blk: e.tensor_tensor(out=x1f[:], in0=x1f[:], in1=acc[:, blk, :], op=ALU.add), ['x1f', 'acc'], ['x1f'])
            A(lambda e: e.activation(out=junk3[:], in_=x1f[:], func=AF.Square), ['x1f'], ['junk3'])
            V(lambda e: e.tensor_reduce(out=ssf[:, 0:1], in_=junk3[:], axis=mybir.AxisListType.X, op=ALU.add), ['junk3'], ['Fss'])
            m0 = c.mark()
            rsf = rstd_of(ssf, 1, 'F')
            V(lambda e, ot=ot, rsf=rsf: e.scalar_tensor_tensor(out=ot[:], in0=x1f[:], scalar=rsf[:, 0:1], in1=gfin_t[:], op0=ALU.mult, op1=ALU.mult), ['x1f', 'Frs', 'gfin'], [otk])
            c.release(m0)
            D(out_d[r0:r0 + 128, :], ot[:], r=[otk], w=['out_d'])
    c.emit()
    return nc


def _ssm_layouts(lambda_re, lambda_im, log_step, b_re, b_im, c_re, c_im, ssm_d):
    f = np.float32
    o = {}
    r = np.arange(128)
    k4_r, mp_r, cp_r = r // 32, (r // 16) % 2, r % 16
    s_ = np.arange(4)
    q = np.arange(128)
    m_q, p_q = q // 64, q % 64
    g = 8 * s_[None, :, None] + 2 * k4_r[:, None, None] + m_q[None, None, :]
    P = np.broadcast_to(p_q[None, None, :], g.shape)
    o["LRX"] = lambda_re[g, P].reshape(128, 512).astype(f)
    o["LIX"] = lambda_im[g, P].reshape(128, 512).astype(f)
    o["LSX"] = log_step[g].reshape(128, 512).astype(f)
    msk = (mp_r[:, None, None] == m_q[None, None, :])
    CP = np.broadcast_to(cp_r[:, None, None], g.shape)
    o["BRX"] = np.where(msk, b_re[g, P, CP], 0).reshape(128, 512).astype(f)
    o["BIX"] = np.where(msk, b_im[g, P, CP], 0).reshape(128, 512).astype(f)
    k = np.arange(16)
    j = np.arange(32)
    mp_j, c_j = j // 16, j % 16
    g2 = 2 * k[None, :, None] + m_q[:, None, None] + 0 * j[None, None, :]
    P2 = np.broadcast_to(p_q[:, None, None], g2.shape)
    C2 = np.broadcast_to(c_j[None, None, :], g2.shape)
    msk2 = (mp_j[None, None, :] == m_q[:, None, None])
    o["LRY"] = lambda_re[g2, P2].reshape(128, 512).astype(f)
    o["LIY"] = lambda_im[g2, P2].reshape(128, 512).astype(f)
    o["LSY"] = log_step[g2].reshape(128, 512).astype(f)
    o["CRY"] = np.where(msk2, c_re[g2, C2, P2], 0).reshape(128, 512).astype(f)
    o["CIY"] = np.where(msk2, c_im[g2, C2, P2], 0).reshape(128, 512).astype(f)
    o["BRY"] = np.where(msk2, b_re[g2, P2, C2], 0).reshape(128, 512).astype(f)
    o["BIY"] = np.where(msk2, b_im[g2, P2, C2], 0).reshape(128, 512).astype(f)
    dd = np.zeros((128, 4, 128), f)
    for s in range(4):
        dd[r, s, r] = ssm_d[128 * s + r]
    o["DDG"] = dd.reshape(128, 512)
    return o


_CACHE = {}


def _prep_common(inp):
    f = np.float32
    A_ = lambda a: np.ascontiguousarray(a, dtype=f)
    d = {}
    d["w_in"] = A_(inp["w_in"][0])
    d["gmixT"] = A_(inp["g_mix"][0].reshape(8, 128).T)
    d["bforget"] = A_(inp["b_forget"][0].reshape(8, 1))
    d["bgate"] = A_(inp["b_gate"][0].reshape(16, 128).T)
    d["w_out_a"] = A_(inp["w_out_a"][0])
    d["w_glu"] = A_(inp["w_glu"][0])
    d["bglu"] = A_(inp["b_glu"][0].reshape(4, 128).T)
    d["w_out_b"] = A_(inp["w_out_b"][0])
    d["w_out"] = A_(inp["w_out"][0])
    d["gffnT"] = A_(inp["g_ffn"][0].reshape(8, 128).T)
    d["wr"] = A_(np.concatenate([inp["w_router_group"][0], inp["w_router_expert"][0]], axis=1))
    d["br"] = A_(np.broadcast_to(np.concatenate([inp["b_router_group"][0], inp["b_router_expert"][0]])[None, :], (128, 20)))
    d["w_eg"] = A_(inp["w_exp_gate"][0])
    d["w_eu"] = A_(inp["w_exp_up"][0])
    d["w_ed"] = A_(inp["w_exp_down"][0])
    d["gfin"] = A_(np.broadcast_to(inp["g_final"][None, :], (128, 1024)))
    d.update(_ssm_layouts(np.asarray(inp["lambda_re"][0]), np.asarray(inp["lambda_im"][0]), np.asarray(inp["log_step"][0]),
                          np.asarray(inp["ssm_b_re"][0]), np.asarray(inp["ssm_b_im"][0]), np.asarray(inp["ssm_c_re"][0]),
                          np.asarray(inp["ssm_c_im"][0]), np.asarray(inp["ssm_d"][0])))
    d["ident"] = np.eye(128, dtype=f)
    d["tri"] = np.triu(np.ones((128, 128), f))
    sel = np.zeros((16, 16, 128), f)
    for e in range(16):
        sel[e, e, :] = 1.0
    d["sel"] = sel.reshape(16, 2048)
    return d


def run(inputs, dbg=(), stop=None):
    inp = {k: np.asarray(v) for k, v in inputs.items()}
    x = inp["x"]
    B, S, _ = x.shape
    TH = S // 2
    key = (TH, tuple(dbg), stop)
    if key not in _CACHE:
        _CACHE[key] = build_program(TH, dbg, stop)
    nc = _CACHE[key]
    common = _prep_common(inp)
    in_maps = []
    for core in range(8):
        b, par = core // 2, core % 2
        xin = np.zeros((S, 1024), np.float32)
        if par == 1:
            xin[:TH] = x[b, :TH]
        xin[TH:] = x[b, par * TH:(par + 1) * TH]
        m = dict(common)
        m["xin"] = xin
        m["flag"] = np.full((128, 1), float(par), np.float32)
        in_maps.append(m)
    res = run_bass_kernel_spmd(nc, in_maps, core_ids=list(range(8)))
    return res, TH


def kernel(**inputs):
    res, TH = run(inputs)
    x = inputs["x"]
    B, S, Dm = x.shape
    out = np.zeros((B, S, Dm), np.float32)
    for core in range(8):
        b, par = core // 2, core % 2
        out[b, par * TH:(par + 1) * TH] = res.results[core]["out"]
    return out
```

```python
import contextlib
import numpy as np
import concourse.bass as bass
import concourse.mybir as mybir
from concourse.bass_utils import run_bass_kernel_spmd

F32 = mybir.dt.float32
BF16 = mybir.dt.bfloat16
I32 = mybir.dt.int32
AF = mybir.ActivationFunctionType
ALU = mybir.AluOpType

NDSEM = 48
SAME_SYNC = {'pe': False, 'act': True, 'dve': True, 'pool': True, 'sp': False}
EPS = 1e-6
L = 16
TWO_PI = 6.283185307179586


class Ctx:
    def __init__(self, nc):
        self.nc = nc
        self.names = ['pe', 'act', 'dve', 'pool', 'sp']
        self.ops = {e: [] for e in self.names}
        self.cnt = {e: 0 for e in self.names}
        self.seen = {e: {} for e in self.names}
        self.pending = {e: {} for e in self.names}
        self.lastw = {}
        self.readers = {}
        self.dval = [0] * NDSEM
        self.dnext = 0
        self.sb_off = 16640
        self.uid = 0
        self.sb_max = 0

    def sb(self, shape, dtype, name="t"):
        esz = {F32: 4, BF16: 2, I32: 4}[dtype]
        n = 1
        for s in shape[1:]:
            n *= s
        off = (self.sb_off + 63) // 64 * 64
        self.sb_off = off + n * esz
        self.sb_max = max(self.sb_max, self.sb_off)
        assert self.sb_off <= 229376, f"SBUF overflow {self.sb_off} ({name})"
        self.uid += 1
        return self.nc.alloc_sbuf_tensor_at(f"{name}_{self.uid}", list(shape), dtype, offset=off)

    def mark(self):
        return self.sb_off

    def release(self, m):
        self.sb_off = m

    def _deps(self, reads, writes):
        deps = {}
        for k in reads:
            t = self.lastw.get(k)
            if t and deps.get(t[0], 0) < t[1]:
                deps[t[0]] = t[1]
        for k in writes:
            t = self.lastw.get(k)
            if t and deps.get(t[0], 0) < t[1]:
                deps[t[0]] = t[1]
            for s, v in self.readers.get(k, {}).items():
                if deps.get(s, 0) < v:
                    deps[s] = v
        return deps

    def _waits(self, e, deps):
        for s, v in self.pending[e].items():
            if deps.get(s, 0) < v:
                deps[s] = v
        self.pending[e] = {}
        waits = []
        for s, v in deps.items():
            if s == e and not SAME_SYNC[e]:
                continue
            if self.seen[e].get(s, 0) >= v:
                continue
            self.seen[e][s] = v
            waits.append((s, v))
        return waits

    def _commit(self, tok, reads, writes):
        for k in reads:
            r = self.readers.setdefault(k, {})
            if r.get(tok[0], 0) < tok[1]:
                r[tok[0]] = tok[1]
        for k in writes:
            self.lastw[k] = tok
            self.readers[k] = {}

    def op(self, e, fn, reads=(), writes=()):
        deps = self._deps(reads, writes)
        waits = self._waits(e, deps)
        self.cnt[e] += 1
        tok = (e, self.cnt[e])
        self.ops[e].append((waits, fn, e))
        self._commit(tok, reads, writes)
        return tok

    def dma(self, e, out, in_, reads=(), writes=()):
        i = self.dnext
        self.dnext = (self.dnext + 1) % NDSEM
        deps = self._deps(reads, writes)
        if self.dval[i] > 0:
            s = ('d', i)
            if deps.get(s, 0) < self.dval[i]:
                deps[s] = self.dval[i]
        waits = self._waits(e, deps)
        self.dval[i] += 16
        tok = (('d', i), self.dval[i])
        self.ops[e].append((waits, lambda eng: eng.dma_start(out=out, in_=in_), ('d', i)))
        self._commit(tok, reads, writes)
        return tok

    def barrier(self):
        allt = {e: self.cnt[e] for e in self.names if self.cnt[e] > 0}
        for i in range(NDSEM):
            if self.dval[i] > 0:
                allt[('d', i)] = self.dval[i]
        for e in self.names:
            for s, v in allt.items():
                if self.pending[e].get(s, 0) < v:
                    self.pending[e][s] = v

    def emit(self):
        nc = self.nc
        self.barrier()
        self.op('sp', lambda eng: eng.nop(), (), ())
        with contextlib.ExitStack() as st:
            sems = {e: st.enter_context(nc.semaphore(f"s_{e}")) for e in self.names}
            for i in range(NDSEM):
                sems[('d', i)] = st.enter_context(nc.semaphore(f"d_{i}"))
            block = st.enter_context(nc.Block())

            def run(e, eng):
                for waits, fn, inc in self.ops[e]:
                    for s, v in waits:
                        eng.wait_ge(sems[s], v)
                    ins = fn(eng)
                    if isinstance(inc, tuple):
                        ins.then_inc(sems[inc], 16)
                    else:
                        ins.then_inc(sems[inc], 1)

            @block.sync
            def _(eng):
                run('sp', eng)

            @block.tensor
            def _(eng):
                run('pe', eng)

            @block.scalar
            def _(eng):
                run('act', eng)

            @block.vector
            def _(eng):
                run('dve', eng)

            @block.gpsimd
            def _(eng):
                run('pool', eng)


def build_program(TH, dbg=(), stop=None):
    nc = bass.Bass("TRN2", target_bir_lowering=False)
    T2 = 2 * TH
    NT = TH // 512
    NBH = TH // 128
    NCH = TH // L
    c = Ctx(nc)

    def din(name, shape, dt=F32):
        return nc.dram_tensor(name, list(shape), dt, kind="ExternalInput").ap()

    def dscr(name, shape, dt):
        return nc.dram_tensor(name, list(shape), dt).ap()

    xin = din("xin", [T2, 1024])
    flag_d = din("flag", [128, 1])
    w_in = din("w_in", [1024, 4104])
    gmixT = din("gmixT", [128, 8])
    bforget = din("bforget", [8, 1])
    bgate = din("bgate", [128, 16])
    w_out_a = din("w_out_a", [512, 1024])
    w_glu = din("w_glu", [512, 512])
    bglu = din("bglu", [128, 4])
    w_out_b = din("w_out_b", [512, 1024])
    w_out = din("w_out", [1024, 1024])
    gffnT = din("gffnT", [128, 8])
    wr = din("wr", [1024, 20])
    br = din("br", [128, 20])
    w_eg = din("w_eg", [16, 1024, 256])
    w_eu = din("w_eu", [16, 1024, 256])
    w_ed = din("w_ed", [16, 256, 1024])
    gfin = din("gfin", [128, 1024])
    ssm_names = ["LRX", "LIX", "LSX", "BRX", "BIX", "LRY", "LIY", "LSY", "CRY", "CIY", "BRY", "BIY"]
    ssm_in = {n: din(n, [128, 512]) for n in ssm_names}
    ddg = din("DDG", [128, 512])
    ident_d = din("ident", [128, 128])
    tri_d = din("tri", [128, 128])
    sel_d = din("sel", [16, 2048])
    out_d = nc.dram_tensor("out", [TH, 1024], F32, kind="ExternalOutput").ap()
    dbg_out = {}
    for name, shape in dbg:
        dbg_out[name] = nc.dram_tensor(name, list(shape), F32, kind="ExternalOutput").ap()

    kT_d = dscr("kT_d", [8, 70, T2], BF16)
    qT_d = dscr("qT_d", [8, 70, TH], BF16)
    v_d = dscr("v_d", [T2, 520], BF16)
    uT_d = dscr("uT_d", [512, T2], BF16)
    gT_d = dscr("gT_d", [2048, TH], BF16)
    oT_d = dscr("oT_d", [8, 64, TH], BF16)
    zT_d = dscr("zT_d", [512, TH], BF16)
    x1_d = dscr("x1_d", [TH, 1024], F32)
    h2T_d = dscr("h2T_d", [1024, TH], BF16)
    cwT_d = dscr("cwT_d", [16, TH], BF16)

    ps = [nc.alloc_psum_tensor(f"ps{i}", [128, 512], F32) for i in range(6)]
    pb = [nc.alloc_psum_tensor(f"pb{i}", [128, 1024], BF16) for i in range(2)]

    def V(fn, r=(), w=()):
        return c.op('dve', fn, r, w)

    def A(fn, r=(), w=()):
        return c.op('act', fn, r, w)

    def G(fn, r=(), w=()):
        return c.op('pool', fn, r, w)

    def MM(out, lhsT, rhs, st, sp_, r, w, tp=None):
        kw = dict(start=st, stop=sp_)
        if tp is not None:
            kw['tile_position'] = tp
        return c.op('pe', lambda e: e.matmul(out, lhsT=lhsT, rhs=rhs, **kw), r, w)

    def TR(out, in_, ident, r, w):
        return c.op('pe', lambda e: e.transpose(out, in_, ident), r, w)

    def D(out, in_, r=(), w=(), q='sp'):
        return c.dma(q, out, in_, r, w)


    dump_i = [0]
    dump_st = []

    def dump(name, ap2d, ncols):
        if name not in dbg_out:
            return
        for c0 in range(0, ncols, 2048):
            n = min(2048, ncols - c0)
            dump_i[0] += 1
            kx = 'dump0'
            if not dump_st:
                dump_st.append(c.sb([128, 2048], F32, "dumpst"))
            stt = dump_st[0]
            V(lambda e, stt=stt, c0=c0, n=n: e.tensor_copy(out=stt[:, 0:n], in_=ap2d[:, c0:c0 + n]), [], [kx])
            D(dbg_out[name][:, c0:c0 + n], stt[:, 0:n], r=[kx])
    identf = c.sb([128, 128], F32, "identf")
    identb = c.sb([128, 128], BF16, "identb")
    trib = c.sb([128, 128], BF16, "trib")
    onesf = c.sb([128, 512], F32, "onesf")
    flag = c.sb([128, 1], F32, "flag")
    stg = c.sb([128, 128], F32, "stg")
    D(identf[:], ident_d, w=['identf'])
    D(stg[:], tri_d, w=['stg'])
    D(flag[:], flag_d, w=['flag'])
    V(lambda e: e.tensor_copy(out=identb[:], in_=identf[:]), ['identf'], ['identb'])
    V(lambda e: e.tensor_copy(out=trib[:], in_=stg[:]), ['stg'], ['trib'])
    V(lambda e: e.memset(onesf[:], 1.0), (), ['onesf'])
    base_mark = c.mark()

    def rstd_of(ss, n, tag):
        ms = c.sb([128, n], F32, "ms")
        rs = c.sb([128, n], F32, "rs")
        V(lambda e: e.tensor_scalar(out=ms[:], in0=ss[:], scalar1=1.0 / 1024, scalar2=EPS, op0=ALU.mult, op1=ALU.add), [tag + 'ss'], [tag + 'ms'])
        A(lambda e: e.activation(out=ms[:], in_=ms[:], func=AF.Sqrt), [tag + 'ms'], [tag + 'ms'])
        V(lambda e: e.reciprocal(out=rs[:], in_=ms[:]), [tag + 'ms'], [tag + 'rs'])
        return rs

    Win = c.sb([128, 8, 4104], BF16, "Win")
    gm = c.sb([128, 8], F32, "gm")
    negb = c.sb([8, 1], F32, "negb")
    bg = c.sb([128, 16], F32, "bg")
    D(gm[:], gmixT, w=['gm'])
    D(negb[:], bforget, w=['negb'])
    D(bg[:], bgate, w=['bg'])
    V(lambda e: e.tensor_scalar(out=negb[:], in0=negb[:], scalar1=-1.0, scalar2=None, op0=ALU.mult), ['negb'], ['negb'])
    wst = [c.sb([128, 8, 256], F32, "wst")] * 2
    w_in_v = w_in.rearrange("(kc p) n -> p kc n", p=128)
    ei = 0
    for cc in range(17):
        c0 = cc * 256
        ncol = min(256, 4104 - c0)
        st = wst[cc % 2]
        sk = 'wst'
        D(st[:, :, 0:ncol], w_in_v[:, :, c0:c0 + ncol], w=[sk])
        for kc in range(8):
            if cc % 2 == 0:
                A(lambda e, st=st, kc=kc, c0=c0, ncol=ncol: e.activation(out=Win[:, kc, c0:c0 + ncol], in_=st[:, kc, 0:ncol], func=AF.Copy, scale=gm[:, kc:kc + 1]), [sk, 'gm'], ['Win0'])
            else:
                V(lambda e, st=st, kc=kc, c0=c0, ncol=ncol: e.tensor_scalar(out=Win[:, kc, c0:c0 + ncol], in0=st[:, kc, 0:ncol], scalar1=gm[:, kc:kc + 1], scalar2=None, op0=ALU.mult), [sk, 'gm'], ['Win1'])
            ei += 1

    CQ, CK, CV, CF, CU, CG = 0, 512, 1024, 1536, 1544, 2056
    xt_b = [c.sb([128, 4, 1024], F32, "xt") for _ in range(2)]
    xs = c.sb([128, 4, 1024], BF16, "xs")
    hT_b = [c.sb([128, 8, 512], BF16, "hT") for _ in range(2)]
    junk = c.sb([128, 1024], BF16, "junk")
    junk_b = [junk, c.sb([128, 1024], BF16, "junkb")]
    ss = c.sb([128, 4], F32, "ss")
    kT_s = [c.sb([64, 8, 512], BF16, "kTs")] * 2
    qT_s = [c.sb([64, 8, 512], BF16, "qTs")] * 2
    v_s = [c.sb([128, 4, 8, 65], BF16, "vs")] * 2
    uT_s = [c.sb([128, 4, 512], BF16, "uTs")] * 2
    gT_s = [c.sb([128, 16, 512], BF16, "gTs")] * 2
    CPK = c.sb([8, 6, 512], BF16, "CPK")
    CPQ = c.sb([8, 6, 512], BF16, "CPQ")
    e1 = c.sb([8, 512], F32, "e1")
    negc = c.sb([8, 512], F32, "negc")
    r1 = c.sb([8, 512], F32, "r1")
    carry = c.sb([8, 1], F32, "carry")
    ones_own = c.sb([128, 32], BF16, "ones_own")
    ones_ctx = c.sb([128, 32], BF16, "ones_ctx")
    V(lambda e: e.memset(ones_own[:], 1.0), (), ['ones_own'])
    V(lambda e: e.tensor_scalar(out=ones_ctx[:], in0=onesf[:, 0:32], scalar1=flag[:, 0:1], scalar2=None, op0=ALU.mult), ['onesf', 'flag'], ['ones_ctx'])
    V(lambda e: e.memset(CPK[:, 0:3, :], 1.0), (), ['CPK'])
    V(lambda e: e.memset(CPQ[:, 3:6, :], 1.0), (), ['CPQ'])
    V(lambda e: e.memset(carry[:], 0.0), (), ['carry'])

    xin_v = xin.rearrange("(t j p) d -> t p j d", j=4, p=128)
    D(xt_b[0][:], xin_v[0], w=['xt0'])
    pi = [0]

    def nps():
        pi[0] = (pi[0] + 1) % 6
        return pi[0]

    for i in range(2 * NT):
        own = i >= NT
        b = i % 2
        xt, xk = xt_b[b], f'xt{b}'
        hT, hk = hT_b[b], f'hT{b}'
        if i + 1 < 2 * NT:
            D(xt_b[1 - b][:], xin_v[i + 1], w=[f'xt{1 - b}'])
        for j in range(4):
            jk_, jkk_ = junk_b[j % 2], f'junk{j % 2}'
            A(lambda e, j=j, xt=xt, jk_=jk_: e.activation(out=jk_[:], in_=xt[:, j, :], func=AF.Square), [xk], [jkk_])
            V(lambda e, j=j, jk_=jk_: e.tensor_reduce(out=ss[:, j:j + 1], in_=jk_[:], axis=mybir.AxisListType.X, op=ALU.add), [jkk_], ['Ass'])
        m0 = c.mark()
        rs = rstd_of(ss, 4, 'A')
        for j in range(4):
            V(lambda e, j=j, xt=xt, rs=rs: e.tensor_scalar(out=xs[:, j, :], in0=xt[:, j, :], scalar1=rs[:, j:j + 1], scalar2=None, op0=ALU.mult), [xk, 'Ars'], [f'xs{j}'])
        c.release(m0)
        for j in range(4):
            pbk = j % 2
            for kc in range(8):
                TR(pb[pbk][:, kc * 128:(kc + 1) * 128], xs[:, j, kc * 128:(kc + 1) * 128], identb[:], [f'xs{j}', 'identb'], [f'pb{pbk}'])
            src = pb[pbk][:, :].rearrange("p (k t) -> p k t", k=8)
            if j % 2 == 0:
                A(lambda e, j=j, hT=hT, src=src: e.activation(out=hT[:, :, j * 128:(j + 1) * 128], in_=src, func=AF.Copy), [f'pb{pbk}'], [hk])
            else:
                V(lambda e, j=j, hT=hT, src=src: e.tensor_copy(out=hT[:, :, j * 128:(j + 1) * 128], in_=src), [f'pb{pbk}'], [hk])
        tok0 = i * 512
        p = nps()
        for kc in range(8):
            MM(ps[p][0:8, :], Win[:, kc, CF:CF + 8], hT[:, kc, :], kc == 0, kc == 7, ['Win0', 'Win1', hk], [f'ps{p}'])
        A(lambda e, p=p: e.activation(out=e1[:], in_=ps[p][0:8, :], func=AF.Exp, scale=-1.0, bias=negb[:, 0:1]), [f'ps{p}', 'negb'], ['e1'])
        A(lambda e: e.activation(out=e1[:], in_=e1[:], func=AF.Ln, bias=1.0), ['e1'], ['e1'])
        V(lambda e: e.tensor_tensor_scan(out=negc[:], data0=onesf[0:8, 0:512], data1=e1[:], initial=carry[:, 0:1], op0=ALU.mult, op1=ALU.add), ['e1', 'carry', 'onesf'], ['negc'])
        V(lambda e: e.tensor_copy(out=carry[:], in_=negc[:, 511:512]), ['negc'], ['carry'])
        V(lambda e: e.tensor_copy(out=CPK[:, 3, :], in_=negc[:]), ['negc'], ['CPK'])
        V(lambda e: e.tensor_tensor(out=r1[:], in0=negc[:], in1=CPK[:, 3, :], op=ALU.subtract), ['negc', 'CPK'], ['r1'])
        V(lambda e: e.tensor_copy(out=CPK[:, 4, :], in_=r1[:]), ['r1'], ['CPK'])
        V(lambda e: e.tensor_tensor(out=r1[:], in0=r1[:], in1=CPK[:, 4, :], op=ALU.subtract), ['r1', 'CPK'], ['r1'])
        V(lambda e: e.tensor_copy(out=CPK[:, 5, :], in_=r1[:]), ['r1'], ['CPK'])
        D(kT_d[:, 64:70, tok0:tok0 + 512], CPK[:, :, :], r=['CPK'], w=['kT_d'])
        if own:
            V(lambda e: e.tensor_scalar(out=CPQ[:, 0:3, :], in0=CPK[:, 3:6, :], scalar1=-1.0, scalar2=None, op0=ALU.mult), ['CPK'], ['CPQ'])
            D(qT_d[:, 64:70, tok0 - TH:tok0 - TH + 512], CPQ[:, :, :], r=['CPQ'], w=['qT_d'])
        kts, ktk = kT_s[b], 'kTs'
        for h in range(8):
            p = nps()
            for kc in range(8):
                MM(ps[p][0:64, :], Win[:, kc, CK + h * 64:CK + (h + 1) * 64], hT[:, kc, :], kc == 0, kc == 7, ['Win0', 'Win1', hk], [f'ps{p}'])
            if h % 2 == 0:
                A(lambda e, p=p, h=h, kts=kts: e.activation(out=kts[:, h, :], in_=ps[p][0:64, :], func=AF.Copy), [f'ps{p}'], [ktk])
            else:
                V(lambda e, p=p, h=h, kts=kts: e.tensor_copy(out=kts[:, h, :], in_=ps[p][0:64, :]), [f'ps{p}'], [ktk])
        D(kT_d[:, 0:64, tok0:tok0 + 512].rearrange("h r t -> r h t"), kts[:, :, :], r=[ktk], w=['kT_d'])
        vs, vk = v_s[b], 'vs'
        V(lambda e, vs=vs, own=own: e.tensor_copy(out=vs[:, :, :, 64:65], in_=(ones_own if own else ones_ctx)[:, :].rearrange("p (j h o) -> p j h o", j=4, o=1)), ['ones_own', 'ones_ctx'], [vk])
        for j in range(4):
            p = nps()
            for kc in range(8):
                MM(ps[p][:, :], hT[:, kc, j * 128:(j + 1) * 128], Win[:, kc, CV:CV + 512], kc == 0, kc == 7, ['Win0', 'Win1', hk], [f'ps{p}'])
            V(lambda e, p=p, j=j, vs=vs: e.tensor_copy(out=vs[:, j, :, 0:64], in_=ps[p][:, :].rearrange("p (h d) -> p h d", h=8)), [f'ps{p}'], [vk])
        D(v_d[tok0:tok0 + 512, :].rearrange("(j p) c -> p j c", p=128), vs[:, :, :, :].rearrange("p j h c -> p j (h c)"), r=[vk], w=['v_d'])
        us, uk = uT_s[b], 'uTs'
        for m in range(4):
            p = nps()
            for kc in range(8):
                MM(ps[p][:, :], Win[:, kc, CU + m * 128:CU + (m + 1) * 128], hT[:, kc, :], kc == 0, kc == 7, ['Win0', 'Win1', hk], [f'ps{p}'])
            V(lambda e, p=p, m=m, us=us: e.tensor_copy(out=us[:, m, :], in_=ps[p][:, :]), [f'ps{p}'], [uk])
        D(uT_d.rearrange("(s p) t -> p s t", p=128)[:, :, tok0:tok0 + 512], us[:, :, :], r=[uk], w=['uT_d'])
        if own:
            qts, qtk = qT_s[b], 'qTs'
            for h in range(8):
                p = nps()
                for kc in range(8):
                    MM(ps[p][0:64, :], Win[:, kc, CQ + h * 64:CQ + (h + 1) * 64], hT[:, kc, :], kc == 0, kc == 7, ['Win0', 'Win1', hk], [f'ps{p}'])
                A(lambda e, p=p, h=h, qts=qts: e.activation(out=qts[:, h, :], in_=ps[p][0:64, :], func=AF.Copy, scale=0.125), [f'ps{p}'], [qtk])
            D(qT_d[:, 0:64, tok0 - TH:tok0 - TH + 512].rearrange("h r t -> r h t"), qts[:, :, :], r=[qtk], w=['qT_d'])
            gs, gk = gT_s[b], 'gTs'
            for m in range(16):
                p = nps()
                for kc in range(8):
                    MM(ps[p][:, :], Win[:, kc, CG + m * 128:CG + (m + 1) * 128], hT[:, kc, :], kc == 0, kc == 7, ['Win0', 'Win1', hk], [f'ps{p}'])
                A(lambda e, p=p, m=m, gs=gs: e.activation(out=gs[:, m, :], in_=ps[p][:, :], func=AF.Sigmoid, bias=bg[:, m:m + 1]), [f'ps{p}', 'bg'], [gk])
            D(gT_d.rearrange("(m p) t -> p m t", p=128)[:, :, tok0 - TH:tok0 - TH + 512], gs[:, :, :], r=[gk], w=['gT_d'])

    c.barrier()
    c.release(base_mark)
    if 'dbg_k' in dbg_out:
        tmpk = c.sb([70, T2], BF16, "tmpk")
        tmpf = c.sb([70, T2], F32, "tmpf")
        D(tmpk[:], kT_d[0], r=['kT_d'], w=['tmpk'])
        V(lambda e, tmpf=tmpf, tmpk=tmpk: e.tensor_copy(out=tmpf[:], in_=tmpk[:]), ['tmpk'], ['tmpf'])
        D(dbg_out['dbg_k'], tmpf[:], r=['tmpf'])
        c.barrier()
        c.release(base_mark)

    if stop == 'A':
        c.emit()
        return nc
    NB2 = T2 // 128
    v_all = c.sb([128, NB2, 520], BF16, "v_all")
    for q4 in range(0, NB2, 8):
        n = min(8, NB2 - q4)
        D(v_all[:, q4:q4 + n, :], v_d[q4 * 128:(q4 + n) * 128, :].rearrange("(j p) c -> p j c", p=128), r=['v_d'], w=['v_all'])
    kT_h = [c.sb([70, T2], BF16, "kTh") for _ in range(2)]
    qT_h = [c.sb([70, TH], BF16, "qTh") for _ in range(2)]
    pT_b = [c.sb([128, 512], BF16, "pT") for _ in range(3)]
    rr = c.sb([128, 512], F32, "rr")
    rrh = c.sb([128, 512], BF16, "rrh")
    rrl = c.sb([128, 512], BF16, "rrl")
    onesb = c.sb([128, 64], BF16, "onesb")
    V(lambda e: e.memset(onesb[:], 1.0), (), ['onesb'])
    bc_sb = c.sb([64, 512], F32, "bc_sb")
    oT_s = [c.sb([64, 512], BF16, "oTs") for _ in range(2)]
    D(kT_h[0][:], kT_d[0], r=['kT_d'], w=['kTh0'])
    D(qT_h[0][:], qT_d[0], r=['qT_d'], w=['qTh0'])
    items = []
    gi = 0
    for h in range(8):
        for Gq in range(NT):
            kbs = list(range(NBH)) + [NBH + ob for ob in range(4 * Gq + 4)]
            for idx, gkb in enumerate(kbs):
                ob = gkb - NBH
                diag = ob >= 4 * Gq
                c0 = (ob - 4 * Gq) * 128 if diag else 0
                items.append(dict(h=h, Gq=Gq, gkb=gkb, c0=c0, diag=diag, first=(idx == 0), last=(idx == len(kbs) - 1), gi=gi,
                                  newhead=(Gq == 0 and idx == 0)))
            gi += 1
    DEPTH = 2

    def att_stage1(i, it):
        h, Gq, gkb, c0 = it['h'], it['Gq'], it['gkb'], it['c0']
        hb = h % 2
        if it['newhead'] and h + 1 < 8:
            D(kT_h[1 - hb][:], kT_d[h + 1], r=['kT_d'], w=[f'kTh{1 - hb}'])
            D(qT_h[1 - hb][:], qT_d[h + 1], r=['qT_d'], w=[f'qTh{1 - hb}'])
        kt, ktk = kT_h[hb], f'kTh{hb}'
        qt, qtk = qT_h[hb], f'qTh{hb}'
        p = i % 3
        pT, ptk = pT_b[i % 3], f'pT{i % 3}'
        MM(ps[p][:, c0:512], kt[:, gkb * 128:(gkb + 1) * 128], qt[:, Gq * 512 + c0:Gq * 512 + 512], True, True, [ktk, qtk], [f'ps{p}'])
        A(lambda e, p=p, pT=pT, c0=c0: e.activation(out=pT[:, c0:512], in_=ps[p][:, c0:512], func=AF.Exp), [f'ps{p}'], [ptk])
        if it['diag']:
            G(lambda e, pT=pT, c0=c0: e.tensor_tensor(out=pT[:, c0:c0 + 128], in0=pT[:, c0:c0 + 128], in1=trib[:, :], op=ALU.mult), [ptk, 'trib'], [ptk])

    def att_stage2(i, it):
        h, Gq, gkb, c0 = it['h'], it['Gq'], it['gkb'], it['c0']
        po = 3 + (it['gi'] % 2)
        pT, ptk = pT_b[i % 3], f'pT{i % 3}'
        MM(ps[po][0:65, c0:512], v_all[:, gkb, h * 65:(h + 1) * 65], pT[:, c0:512], it['first'], it['last'], ['v_all', ptk], [f'ps{po}'])
        if it['last']:
            V(lambda e, po=po: e.reciprocal(out=rr[64:65, :], in_=ps[po][64:65, :]), [f'ps{po}'], ['rr'])
            V(lambda e: e.tensor_copy(out=rrh[64:65, :], in_=rr[64:65, :]), ['rr'], ['rrh'])
            V(lambda e: e.tensor_tensor(out=rr[64:65, :], in0=rr[64:65, :], in1=rrh[64:65, :], op=ALU.subtract), ['rr', 'rrh'], ['rr'])
            V(lambda e: e.tensor_copy(out=rrl[64:65, :], in_=rr[64:65, :]), ['rr'], ['rrl'])
            MM(ps[5][0:64, :], onesb[64:65, 0:64], rrh[64:65, :], True, False, ['onesb', 'rrh'], ['ps5'])
            MM(ps[5][0:64, :], onesb[64:65, 0:64], rrl[64:65, :], False, True, ['onesb', 'rrl'], ['ps5'])
            A(lambda e: e.activation(out=bc_sb[:], in_=ps[5][0:64, :], func=AF.Copy), ['ps5'], ['bc_sb'])
            ots, otk = oT_s[it['gi'] % 2], f"oTs{it['gi'] % 2}"
            V(lambda e, po=po, ots=ots: e.tensor_tensor(out=ots[:], in0=ps[po][0:64, :], in1=bc_sb[:], op=ALU.mult), [f'ps{po}', 'bc_sb'], [otk])
            D(oT_d[h, :, Gq * 512:(Gq + 1) * 512], ots[:], r=[otk], w=['oT_d'])

    for i in range(len(items) + DEPTH):
        if i < len(items):
            att_stage1(i, items[i])
        if i - DEPTH >= 0:
            att_stage2(i - DEPTH, items[i - DEPTH])
    c.barrier()
    c.release(base_mark)
    if 'dbg_o' in dbg_out:
        tmpk = c.sb([64, 8, TH], BF16, "tmpo")
        tmpf = c.sb([64, 8, TH], F32, "tmpof")
        D(tmpk[:], oT_d.rearrange("h d t -> d h t"), r=['oT_d'], w=['tmpk'])
        V(lambda e, tmpf=tmpf, tmpk=tmpk: e.tensor_copy(out=tmpf[:], in_=tmpk[:]), ['tmpk'], ['tmpf'])
        D(dbg_out['dbg_o'].rearrange("h d t -> d h t"), tmpf[:], r=['tmpf'])
        c.barrier()
        c.release(base_mark)

    if stop == 'B':
        c.emit()
        return nc
    def ssm_prep(sfx):
        k = 'pp' + sfx
        lr = c.sb([128, 512], F32, "lr"); li = c.sb([128, 512], F32, "li"); ls = c.sb([128, 512], F32, "ls")
        D(lr[:], ssm_in["LR" + sfx], w=[k + 'lr'])
        D(li[:], ssm_in["LI" + sfx], w=[k + 'li'])
        D(ls[:], ssm_in["LS" + sfx], w=[k + 'ls'])
        dt = c.sb([128, 512], F32, "dt"); mag = c.sb([128, 512], F32, "mag"); th = c.sb([128, 512], F32, "th")
        A(lambda e: e.activation(out=dt[:], in_=ls[:], func=AF.Exp), [k + 'ls'], [k + 'dt'])
        V(lambda e: e.tensor_tensor(out=mag[:], in0=lr[:], in1=dt[:], op=ALU.mult), [k + 'lr', k + 'dt'], [k + 'mag'])
        A(lambda e: e.activation(out=mag[:], in_=mag[:], func=AF.Exp), [k + 'mag'], [k + 'mag'])
        V(lambda e: e.tensor_tensor(out=th[:], in0=li[:], in1=dt[:], op=ALU.mult), [k + 'li', k + 'dt'], [k + 'th'])

        def sin_of(shift, outt, ok):
            t = c.sb([128, 512], F32, "t"); ni = c.sb([128, 512], I32, "ni"); nf = c.sb([128, 512], F32, "nf")
            a = c.sb([128, 512], F32, "a"); mk = c.sb([128, 512], F32, "mk")
            V(lambda e: e.tensor_scalar(out=t[:], in0=th[:], scalar1=1.0 / TWO_PI, scalar2=8.5 + shift, op0=ALU.mult, op1=ALU.add), [k + 'th'], [k + 't'])
            V(lambda e: e.tensor_copy(out=ni[:], in_=t[:]), [k + 't'], [k + 'ni'])
            V(lambda e: e.tensor_copy(out=nf[:], in_=ni[:]), [k + 'ni'], [k + 'nf'])
            V(lambda e: e.scalar_tensor_tensor(out=a[:], in0=t[:], scalar=-0.5, in1=nf[:], op0=ALU.add, op1=ALU.subtract), [k + 't', k + 'nf'], [k + 'a'])
            V(lambda e: e.tensor_single_scalar(out=mk[:], in_=a[:], scalar=-0.5, op=ALU.is_lt), [k + 'a'], [k + 'mk'])
            V(lambda e: e.tensor_tensor(out=a[:], in0=a[:], in1=mk[:], op=ALU.add), [k + 'a', k + 'mk'], [k + 'a'])
            A(lambda e: e.activation(out=outt[:], in_=a[:], func=AF.Sin, scale=TWO_PI), [k + 'a'], [ok])
        ar = c.sb([128, 512], F32, "ar"); ai = c.sb([128, 512], F32, "ai")
        sin_of(0.0, ai, k + 'ai')
        sin_of(0.25, ar, k + 'ar')
        V(lambda e: e.tensor_tensor(out=ai[:], in0=ai[:], in1=mag[:], op=ALU.mult), [k + 'ai', k + 'mag'], [k + 'ai'])
        V(lambda e: e.tensor_tensor(out=ar[:], in0=ar[:], in1=mag[:], op=ALU.mult), [k + 'ar', k + 'mag'], [k + 'ar'])
        zr = c.sb([128, 512], F32, "zr"); zi = c.sb([128, 512], F32, "zi")
        nr = c.sb([128, 512], F32, "nr"); den = c.sb([128, 512], F32, "den"); t2 = c.sb([128, 512], F32, "t2")
        V(lambda e: e.tensor_scalar(out=nr[:], in0=ar[:], scalar1=-1.0, scalar2=None, op0=ALU.add), [k + 'ar'], [k + 'nr'])
        V(lambda e: e.tensor_tensor(out=den[:], in0=lr[:], in1=lr[:], op=ALU.mult), [k + 'lr'], [k + 'den'])
        V(lambda e: e.tensor_tensor(out=t2[:], in0=li[:], in1=li[:], op=ALU.mult), [k + 'li'], [k + 't2'])
        V(lambda e: e.tensor_tensor(out=den[:], in0=den[:], in1=t2[:], op=ALU.add), [k + 'den', k + 't2'], [k + 'den'])
        V(lambda e: e.reciprocal(out=den[:], in_=den[:]), [k + 'den'], [k + 'den'])
        V(lambda e: e.tensor_tensor(out=zr[:], in0=nr[:], in1=lr[:], op=ALU.mult), [k + 'nr', k + 'lr'], [k + 'zr'])
        V(lambda e: e.tensor_tensor(out=t2[:], in0=ai[:], in1=li[:], op=ALU.mult), [k + 'ai', k + 'li'], [k + 't2'])
        V(lambda e: e.tensor_tensor(out=zr[:], in0=zr[:], in1=t2[:], op=ALU.add), [k + 'zr', k + 't2'], [k + 'zr'])
        V(lambda e: e.tensor_tensor(out=zr[:], in0=zr[:], in1=den[:], op=ALU.mult), [k + 'zr', k + 'den'], [k + 'zr'])
        V(lambda e: e.tensor_tensor(out=zi[:], in0=ai[:], in1=lr[:], op=ALU.mult), [k + 'ai', k + 'lr'], [k + 'zi'])
        V(lambda e: e.tensor_tensor(out=t2[:], in0=nr[:], in1=li[:], op=ALU.mult), [k + 'nr', k + 'li'], [k + 't2'])
        V(lambda e: e.tensor_tensor(out=zi[:], in0=zi[:], in1=t2[:], op=ALU.subtract), [k + 'zi', k + 't2'], [k + 'zi'])
        V(lambda e: e.tensor_tensor(out=zi[:], in0=zi[:], in1=den[:], op=ALU.mult), [k + 'zi', k + 'den'], [k + 'zi'])
        return ar, ai, zr, zi, k

    def cmul(outr, outi, xr, xi, yr, yi, keys_in, kor, koi, negate_im=False, view=None):
        ta_t, tb_t = cm_tmp
        ta = view(ta_t[:]) if view else ta_t[:]
        tb = view(tb_t[:]) if view else tb_t[:]
        V(lambda e: e.tensor_tensor(out=ta, in0=xr, in1=yr, op=ALU.mult), keys_in, ['cm_ta'])
        V(lambda e: e.tensor_tensor(out=tb, in0=xi, in1=yi, op=ALU.mult), keys_in, ['cm_tb'])
        V(lambda e: e.tensor_tensor(out=outr, in0=ta, in1=tb, op=ALU.subtract), ['cm_ta', 'cm_tb'], [kor])
        V(lambda e: e.tensor_tensor(out=ta, in0=xr, in1=yi, op=ALU.mult), keys_in + [kor], ['cm_ta'])
        V(lambda e: e.tensor_tensor(out=tb, in0=xi, in1=yr, op=ALU.mult), keys_in + [kor], ['cm_tb'])
        if negate_im:
            V(lambda e: e.scalar_tensor_tensor(out=outi, in0=ta, scalar=-1.0, in1=tb, op0=ALU.mult, op1=ALU.subtract), ['cm_ta', 'cm_tb'], [koi])
        else:
            V(lambda e: e.tensor_tensor(out=outi, in0=ta, in1=tb, op=ALU.add), ['cm_ta', 'cm_tb'], [koi])

    cm_tmp = []
    ZBT = c.sb([128, 4, L, 2, 128], BF16, "ZBT")
    CYT = c.sb([128, 16, L + 1, 2, 32], BF16, "CYT")
    TT = c.sb([128, 4, L, 128], BF16, "TT")
    BBY = c.sb([128, 2, 512], BF16, "BBY")
    AL = c.sb([128, 2, 2, 16], F32, "AL")
    tbl_mark = c.mark()
    ar, ai, zr, zi, k = ssm_prep('X')
    br_ = c.sb([128, 512], F32, "br"); bi_ = c.sb([128, 512], F32, "bi")
    D(br_[:], ssm_in["BRX"], w=['brx'])
    D(bi_[:], ssm_in["BIX"], w=['bix'])
    bbr = c.sb([128, 512], F32, "bbr"); bbi = c.sb([128, 512], F32, "bbi")
    cm_tmp[:] = [c.sb([128, 512], F32, "ta"), c.sb([128, 512], F32, "tb")]
    cmul(bbr[:], bbi[:], zr[:], zi[:], br_[:], bi_[:], [k + 'zr', k + 'zi', 'brx', 'bix'], 'bbr', 'bbi')
    pw = [c.sb([128, 2, 512], F32, "pw") for _ in range(2)]
    V(lambda e, pw=pw: e.memset(pw[0][:, 0, :], 1.0), (), ['pw0'])
    V(lambda e, pw=pw: e.memset(pw[0][:, 1, :], 0.0), (), ['pw0'])
    for n in range(L):
        cur, ck = pw[n % 2], f'pw{n % 2}'
        nxt, nk = pw[(n + 1) % 2], f'pw{(n + 1) % 2}'
        j = L - 1 - n
        cmul(ZBT[:, :, j, 0, :], ZBT[:, :, j, 1, :], cur[:, 0, :].rearrange("p (s q) -> p s q", s=4), cur[:, 1, :].rearrange("p (s q) -> p s q", s=4),
             bbr[:].rearrange("p (s q) -> p s q", s=4), bbi[:].rearrange("p (s q) -> p s q", s=4), [ck, 'bbr', 'bbi'], 'ZBT', 'ZBT',
             view=lambda ap: ap.rearrange("p (s q) -> p s q", s=4))
        if n < L - 1:
            cmul(nxt[:, 0, :], nxt[:, 1, :], cur[:, 0, :], cur[:, 1, :], ar[:], ai[:], [ck, k + 'ar', k + 'ai'], nk, nk)
    c.barrier()
    c.release(tbl_mark)
    ar, ai, zr, zi, k = ssm_prep('Y')
    br_ = c.sb([128, 512], F32, "br"); bi_ = c.sb([128, 512], F32, "bi")
    cr_ = c.sb([128, 512], F32, "cr"); ci_ = c.sb([128, 512], F32, "ci")
    D(br_[:], ssm_in["BRY"], w=['bry'])
    D(bi_[:], ssm_in["BIY"], w=['biy'])
    D(cr_[:], ssm_in["CRY"], w=['cry'])
    D(ci_[:], ssm_in["CIY"], w=['ciy'])
    cm_tmp[:] = [c.sb([128, 512], F32, "ta"), c.sb([128, 512], F32, "tb")]
    cmul(BBY[:, 0, :], BBY[:, 1, :], zr[:], zi[:], br_[:], bi_[:], [k + 'zr', k + 'zi', 'bry', 'biy'], 'BBY', 'BBY')
    pw = [c.sb([128, 2, 512], F32, "pw") for _ in range(2)]
    V(lambda e, pw=pw: e.memset(pw[0][:, 0, :], 1.0), (), ['pw0'])
    V(lambda e, pw=pw: e.memset(pw[0][:, 1, :], 0.0), (), ['pw0'])
    for n in range(L + 1):
        cur, ck = pw[n % 2], f'pw{n % 2}'
        nxt, nk = pw[(n + 1) % 2], f'pw{(n + 1) % 2}'
        cmul(CYT[:, :, n, 0, :], CYT[:, :, n, 1, :], cr_[:].rearrange("p (k j) -> p k j", k=16), ci_[:].rearrange("p (k j) -> p k j", k=16),
             cur[:, 0, :].rearrange("p (k j) -> p k j", k=16), cur[:, 1, :].rearrange("p (k j) -> p k j", k=16), [ck, 'cry', 'ciy'], 'CYT', 'CYT', negate_im=True,
             view=lambda ap: ap.rearrange("p (k j) -> p k j", k=16))
        if n < L:
            cmul(nxt[:, 0, :], nxt[:, 1, :], cur[:, 0, :], cur[:, 1, :], ar[:], ai[:], [ck, k + 'ar', k + 'ai'], nk, nk)
    pL, pLk = pw[L % 2], f'pw{L % 2}'
    pLv_r = pL[:, 0, :].rearrange("p (k j) -> p k j", k=16)[:, :, 0]
    pLv_i = pL[:, 1, :].rearrange("p (k j) -> p k j", k=16)[:, :, 0]
    V(lambda e: e.tensor_copy(out=AL[:, 0, 0, :], in_=pLv_r), [pLk], ['AL'])
    V(lambda e: e.tensor_copy(out=AL[:, 0, 1, :], in_=pLv_r), [pLk], ['AL'])
    V(lambda e: e.tensor_scalar(out=AL[:, 1, 0, :], in0=pLv_i, scalar1=-1.0, scalar2=None, op0=ALU.mult), [pLk], ['AL'])
    V(lambda e: e.tensor_copy(out=AL[:, 1, 1, :], in_=pLv_i), [pLk], ['AL'])
    V(lambda e: e.memset(TT[:], 0.0), (), ['TT'])
    ddt = c.sb([128, 512], F32, "ddt")
    D(ddt[:], ddg, w=['ddt'])
    for kk in range(16):
        s_, k4 = kk // 4, kk % 4
        p = nps()
        MM(ps[p][32 * k4:32 * k4 + 32, :], BBY[:, 0, kk * 32:(kk + 1) * 32], CYT[:, kk, 0:L, 0, :], True, False, ['BBY', 'CYT'], [f'ps{p}'], tp=(0, 32 * k4))
        MM(ps[p][32 * k4:32 * k4 + 32, :], BBY[:, 1, kk * 32:(kk + 1) * 32], CYT[:, kk, 0:L, 1, :], False, True, ['BBY', 'CYT'], [f'ps{p}'], tp=(0, 32 * k4))
        V(lambda e, p=p, s_=s_, k4=k4: e.tensor_copy(out=TT[32 * k4:32 * k4 + 32, s_, :, 32 * k4:32 * k4 + 32], in_=ps[p][32 * k4:32 * k4 + 32, :].rearrange("p (n j) -> p n j", n=L)), [f'ps{p}'], ['TT'])
    V(lambda e: e.tensor_tensor(out=TT[:, :, 0, :], in0=TT[:, :, 0, :], in1=ddt[:].rearrange("p (s q) -> p s q", s=4), op=ALU.add), ['TT', 'ddt'], ['TT'])
    c.barrier()
    c.release(tbl_mark)

    if stop == 'C1':
        c.barrier()
        dump('dbg_ZBT', ZBT[:].rearrange("p s j r q -> p (s j r q)"), 4 * L * 2 * 128)
        dump('dbg_CYT', CYT[:].rearrange("p k n r j -> p (k n r j)"), 16 * (L + 1) * 2 * 32)
        dump('dbg_TT', TT[:].rearrange("p s n q -> p (s n q)"), 4 * L * 128)
        dump('dbg_AL', AL[:].rearrange("p a r k -> p (a r k)"), 64)
        c.emit()
        return nc
    uT_all = c.sb([128, 4, TH], BF16, "uT_all")
    Zs = c.sb([128, 2, 16, NCH], BF16, "Zs")
    H = c.sb([128, 2, 16, NCH + 1], F32, "H")
    Hb = c.sb([128, 2, 16, NCH + 1], BF16, "Hb")
    t1 = c.sb([128, 2, 16], F32, "t1")
    t4 = c.sb([128, 2, 2, 16], F32, "t4")
    AL4 = c.sb([128, 2, 2, 16], F32, "AL4")
    V(lambda e: e.tensor_copy(out=AL4[:, 0, 0, :], in_=AL[:, 0, 0, :]), ['AL'], ['AL4'])
    V(lambda e: e.tensor_copy(out=AL4[:, 0, 1, :], in_=AL[:, 1, 0, :]), ['AL'], ['AL4'])
    V(lambda e: e.tensor_copy(out=AL4[:, 1, 0, :], in_=AL[:, 1, 1, :]), ['AL'], ['AL4'])
    V(lambda e: e.tensor_copy(out=AL4[:, 1, 1, :], in_=AL[:, 0, 1, :]), ['AL'], ['AL4'])
    t2_ = c.sb([128, 2, 16], F32, "t2_")
    G(lambda e: e.memset(H[:, :, :, 0], 0.0), (), ['H'])
    NZ = min(NCH, 512)
    for half in range(2):
        D(uT_all[:], uT_d.rearrange("(s p) t -> p s t", p=128)[:, :, half * TH:(half + 1) * TH], r=['uT_d'], w=['uT_all'])
        for kk in range(16):
            s_, k4 = kk // 4, kk % 4
            for ri in range(2):
                for z0 in range(0, NCH, NZ):
                    p = nps()
                    for j in range(L):
                        uview = uT_all[32 * k4:32 * k4 + 32, s_, :].rearrange("p (n j) -> p n j", j=L)[:, z0:z0 + NZ, j]
                        MM(ps[p][:, 0:NZ], ZBT[32 * k4:32 * k4 + 32, s_, j, ri, :], uview, j == 0, j == L - 1, ['ZBT', 'uT_all'], [f'ps{p}'], tp=(32 * k4, 0))
                    if (kk + ri) % 2 == 0:
                        A(lambda e, p=p, ri=ri, kk=kk, z0=z0: e.activation(out=Zs[:, ri, kk, z0:z0 + NZ], in_=ps[p][:, 0:NZ], func=AF.Copy), [f'ps{p}'], ['Zs'])
                    else:
                        V(lambda e, p=p, ri=ri, kk=kk, z0=z0: e.tensor_copy(out=Zs[:, ri, kk, z0:z0 + NZ], in_=ps[p][:, 0:NZ]), [f'ps{p}'], ['Zs'])
        for n in range(NCH):
            V(lambda e, n=n: e.tensor_tensor(out=t4[:], in0=AL4[:], in1=H[:, :, :, n].unsqueeze(1).broadcast_to([128, 2, 2, 16]), op=ALU.mult), ['AL4', 'H'], ['t4'])
            V(lambda e, n=n: e.tensor_tensor(out=t1[:], in0=t4[:, :, 0, :], in1=Zs[:, :, :, n], op=ALU.add), ['t4', 'Zs'], ['t1'])
            V(lambda e, n=n: e.tensor_tensor(out=H[:, :, :, n + 1], in0=t1[:], in1=t4[:, :, 1, :], op=ALU.add), ['t1', 't4'], ['H'])
        if half == 0:
            V(lambda e: e.tensor_copy(out=H[:, :, :, 0], in_=H[:, :, :, NCH]), ['H'], ['H'])
    V(lambda e: e.tensor_copy(out=Hb[:], in_=H[:]), ['H'], ['Hb'])

    if stop == 'C2':
        c.barrier()
        dump('dbg_H', H[:].rearrange("p a k n -> p (a k n)"), 2 * 16 * (NCH + 1))
        dump('dbg_Zs', Zs[:].rearrange("p a k n -> p (a k n)"), 2 * 16 * NCH)
        c.emit()
        return nc
    zT_s = [c.sb([128, 512], BF16, "zTs") for _ in range(2)]
    x2 = c.sb([128, 512], F32, "x2")
    wv = c.sb([128, 512], F32, "wv")
    zi_ = 0
    for s_ in range(4):
        for ti in range(NT):
            p = nps()
            tok0 = ti * 512
            yv = ps[p][:, :].rearrange("p (n j) -> p n j", j=L)
            uv = uT_all[:, s_, tok0:tok0 + 512].rearrange("p (n j) -> p n j", j=L)
            for n in range(L):
                MM(yv[:, :, n:L], TT[:, s_, n, :], uv[:, :, 0:L - n], n == 0, False, ['TT', 'uT_all'], [f'ps{p}'])
            cnt = 0
            for k4 in range(4):
                kk = 4 * s_ + k4
                for i in range(L):
                    for ri in range(2):
                        cnt += 1
                        MM(yv[32 * k4:32 * k4 + 32, :, i], CYT[:, kk, i + 1, ri, :], Hb[:, ri, kk, ti * 32:ti * 32 + 32], False, cnt == 4 * L * 2, ['CYT', 'Hb'], [f'ps{p}'], tp=(0, 32 * k4))
            zt, ztk = zT_s[zi_ % 2], f'zTs{zi_ % 2}'
            zi_ += 1
            A(lambda e, p=p: e.activation(out=x2[:], in_=ps[p][:, :], func=AF.Square), [f'ps{p}'], ['x2'])
            V(lambda e: e.tensor_scalar(out=wv[:], in0=x2[:], scalar1=0.044715, scalar2=1.0, op0=ALU.mult, op1=ALU.add), ['x2'], ['wv'])
            V(lambda e, p=p: e.tensor_tensor(out=wv[:], in0=wv[:], in1=ps[p][:, :], op=ALU.mult), ['wv', f'ps{p}'], ['wv'])
            A(lambda e: e.activation(out=wv[:], in_=wv[:], func=AF.Sigmoid, scale=1.5957691216057308), ['wv'], ['wv'])
            V(lambda e, p=p, zt=zt: e.tensor_tensor(out=zt[:], in0=wv[:], in1=ps[p][:, :], op=ALU.mult), ['wv', f'ps{p}'], [ztk])
            D(zT_d[s_ * 128:(s_ + 1) * 128, ti * 512:(ti + 1) * 512], zt[:], r=[ztk], w=['zT_d'])
    c.barrier()
    c.release(base_mark)

    if 'dbg_y' in dbg_out:
        tmpz = c.sb([128, 4, TH], BF16, "tmpz")
        tmpzf = c.sb([128, 4, TH], F32, "tmpzf")
        D(tmpz[:], zT_d.rearrange("(s p) t -> p s t", p=128), r=['zT_d'], w=['tmpz'])
        V(lambda e, tmpzf=tmpzf, tmpz=tmpz: e.tensor_copy(out=tmpzf[:], in_=tmpz[:]), ['tmpz'], ['tmpzf'])
        D(dbg_out['dbg_y'].rearrange("(s p) t -> p s t", p=128), tmpzf[:], r=['tmpzf'])
        c.barrier()
        c.release(base_mark)
    if stop == 'C':
        c.emit()
        return nc
    def load_w(dram_view, shape, key, scale_ap=None):
        wt = c.sb(shape, BF16, key)
        a_n, ncol = shape[1], shape[2]
        for a_ in range(a_n):
            lw_i[0] += 1
            lwk = 'lw_st0'
            st_ = lw_sts[lw_i[0] % 2][:, 0:ncol]
            D(st_, dram_view[:, a_, :], w=[lwk])
            if scale_ap is None:
                V(lambda e, a_=a_, st_=st_: e.tensor_copy(out=wt[:, a_, :], in_=st_), [lwk], [key])
            else:
                V(lambda e, a_=a_, st_=st_: e.tensor_scalar(out=wt[:, a_, :], in0=st_, scalar1=scale_ap[:, a_:a_ + 1], scalar2=None, op0=ALU.mult), [lwk, 'gf'], [key])
        return wt

    lw_sts = [c.sb([128, 1024], F32, "lw_st")] * 2
    lw_i = [0]
    gf = c.sb([128, 8], F32, "gf")
    D(gf[:], gffnT, w=['gf'])
    bgl = c.sb([128, 4], F32, "bgl")
    D(bgl[:], bglu, w=['bgl'])
    brt = c.sb([128, 20], F32, "brt")
    D(brt[:], br, w=['brt'])
    Wglu = load_w(w_glu.rearrange("(kc p) n -> p kc n", p=128), [128, 4, 512], 'Wglu')
    Wob = load_w(w_out_b.rearrange("(kc p) n -> p kc n", p=128), [128, 4, 1024], 'Wob')
    Woa = c.sb([64, 8, 1024], BF16, "Woa")
    woa_v = w_out_a.rearrange("(h p) n -> p h n", p=64)
    for h in range(8):
        lw_i[0] += 1
        lwk = 'lw_st0'
        lwt = lw_sts[lw_i[0] % 2]
        D(lwt[0:64, :], woa_v[:, h, :], w=[lwk])
        V(lambda e, h=h, lwt=lwt: e.tensor_copy(out=Woa[:, h, :], in_=lwt[0:64, :]), [lwk], ['Woa'])
    Wout = load_w(w_out.rearrange("(kc p) n -> p kc n", p=128), [128, 8, 1024], 'Wout')
    Wr = c.sb([128, 8, 20], F32, "Wr")
    D(Wr[:], wr.rearrange("(kc p) n -> p kc n", p=128), w=['Wr'])
    for kc in range(8):
        V(lambda e, kc=kc: e.tensor_scalar(out=Wr[:, kc, :], in0=Wr[:, kc, :], scalar1=gf[:, kc:kc + 1], scalar2=None, op0=ALU.mult), ['Wr', 'gf'], ['Wr'])

    if stop == 'D1':
        c.emit()
        return nc
    Wrhi = c.sb([128, 8, 20], BF16, "Wrhi")
    Wrlo = c.sb([128, 8, 20], BF16, "Wrlo")
    V(lambda e: e.tensor_copy(out=Wrhi[:], in_=Wr[:]), ['Wr'], ['Wrhi'])
    V(lambda e: e.tensor_tensor(out=Wr[:], in0=Wr[:], in1=Wrhi[:], op=ALU.subtract), ['Wr', 'Wrhi'], ['Wr'])
    V(lambda e: e.tensor_copy(out=Wrlo[:], in_=Wr[:]), ['Wr'], ['Wrlo'])
    h2hi = c.sb([128, 1024], BF16, "h2hi")
    h2lo = c.sb([128, 1024], BF16, "h2lo")
    hThi = c.sb([128, 8, 128], BF16, "hThi")
    hTlo = c.sb([128, 8, 128], BF16, "hTlo")
    cwb = c.sb([128, 16], BF16, "cwb")
    zT = c.sb([128, 4, 512], BF16, "zT")
    oT = c.sb([64, 8, 512], BF16, "oT")
    gT = c.sb([128, 16, 512], BF16, "gT")
    xt = c.sb([128, 4, 1024], F32, "xtD")
    zg = c.sb([128, 4, 512], BF16, "zg")
    sg = c.sb([128, 512], F32, "sg")
    mgd = c.sb([128, 8, 512], BF16, "mgd")
    ta_ = c.sb([128, 512], F32, "taD")
    tb_ = c.sb([128, 512], F32, "tbD")
    x1 = c.sb([128, 4, 1024], F32, "x1")
    h2f = c.sb([128, 1024], F32, "h2f")
    h2Tf = c.sb([128, 8, 128], F32, "h2Tf")
    h2Tb = c.sb([128, 8, 512], BF16, "h2Tb")
    cwTb = c.sb([16, 512], BF16, "cwTb")
    ss2 = c.sb([128, 4], F32, "ss2")
    junk2 = c.sb([128, 1024], BF16, "junk2")
    junk2_b = [junk2, c.sb([128, 1024], BF16, "junk2b")]
    lg = c.sb([128, 20], F32, "lg")
    cw = c.sb([128, 16], F32, "cw")
    sm = {n_: c.sb([128, 4], F32, n_) for n_ in ["gmx", "ge", "gsum", "oh", "les", "m1", "mk1", "le2", "m2", "mk2", "w12", "tmp4"]}
    one1 = {n_: c.sb([128, 1], F32, n_) for n_ in ["psel", "d21", "w1", "w2"]}
    xin_own = xin[TH:T2, :].rearrange("(t j p) d -> t p j d", j=4, p=128)
    zT_b2 = [zT, c.sb([128, 4, 512], BF16, "zT2")]
    oT_b2 = [oT, c.sb([64, 8, 512], BF16, "oT2")]
    gT_b2 = [gT, c.sb([128, 16, 512], BF16, "gT2")]
    xt_b2 = [xt, c.sb([128, 4, 1024], F32, "xtD2")]
    lg4 = c.sb([128, 4, 20], F32, "lg4")
    R4 = {n_: c.sb([128, 4, 4], F32, n_) for n_ in ["oh", "ge", "les", "mk1", "le2", "mk2", "w12", "tq"]}
    R1 = {n_: c.sb([128, 4], F32, n_) for n_ in ["mx", "gsum", "psel", "m1", "m2", "d", "w1", "w2"]}
    prod = c.sb([128, 4, 4, 4], F32, "prod")
    cw4 = c.sb([128, 4, 16], F32, "cw4")
    cwb4 = c.sb([128, 4, 16], BF16, "cwb4")
    AXX = mybir.AxisListType.X

    def bc3(ap2):
        return ap2.unsqueeze(2).broadcast_to([128, 4, 4])

    def load_tile(ti):
        b_ = ti % 2
        t0_ = ti * 512
        D(zT_b2[b_][:], zT_d.rearrange("(s p) t -> p s t", p=128)[:, :, t0_:t0_ + 512], r=['zT_d'], w=[f'zT{b_}'])
        D(oT_b2[b_][:], oT_d.rearrange("h d t -> d h t")[:, :, t0_:t0_ + 512], r=['oT_d'], w=[f'oT{b_}'])
        for hh in range(2):
            D(gT_b2[b_][:, hh * 8:(hh + 1) * 8, :], gT_d.rearrange("(m p) t -> p m t", p=128)[:, hh * 8:(hh + 1) * 8, t0_:t0_ + 512], r=['gT_d'], w=[f'gT{b_}'])
        D(xt_b2[b_][:], xin_own[ti], w=[f'xtD{b_}'])

    load_tile(0)
    for ti in range(NT):
        t0 = ti * 512
        b_ = ti % 2
        zT, oT, gT, xt = zT_b2[b_], oT_b2[b_], gT_b2[b_], xt_b2[b_]
        zTk, oTk, gTk, xtk = f'zT{b_}', f'oT{b_}', f'gT{b_}', f'xtD{b_}'
        if ti + 1 < NT:
            load_tile(ti + 1)
        for m in range(4):
            p = nps()
            for kc in range(4):
                MM(ps[p][:, :], Wglu[:, kc, m * 128:(m + 1) * 128], zT[:, kc, :], kc == 0, kc == 3, ['Wglu', zTk], [f'ps{p}'])
            A(lambda e, p=p, m=m: e.activation(out=sg[:], in_=ps[p][:, :], func=AF.Sigmoid, bias=bgl[:, m:m + 1]), [f'ps{p}', 'bgl'], ['sg'])
            V(lambda e, m=m, zT=zT: e.tensor_tensor(out=zg[:, m, :], in0=zT[:, m, :], in1=sg[:], op=ALU.mult), [zTk, 'sg'], ['zg'])
        for m in range(8):
            p = nps()
            for kc in range(4):
                MM(ps[p][:, :], Wob[:, kc, m * 128:(m + 1) * 128], zg[:, kc, :], kc == 0, kc == 3, ['Wob', 'zg'], [f'ps{p}'])
            p2 = nps()
            for h in range(8):
                MM(ps[p2][:, :], Woa[:, h, m * 128:(m + 1) * 128], oT[:, h, :], h == 0, h == 7, ['Woa', oTk], [f'ps{p2}'])
            V(lambda e, p=p, m=m, gT=gT: e.tensor_tensor(out=ta_[:], in0=gT[:, 8 + m, :], in1=ps[p][:, :], op=ALU.mult), [gTk, f'ps{p}'], ['taD'])
            V(lambda e, p2=p2, m=m, gT=gT: e.tensor_tensor(out=tb_[:], in0=gT[:, m, :], in1=ps[p2][:, :], op=ALU.mult), [gTk, f'ps{p2}'], ['tbD'])
            G(lambda e, m=m: e.tensor_tensor(out=mgd[:, m, :], in0=ta_[:], in1=tb_[:], op=ALU.add), ['taD', 'tbD'], ['mgd'])
        for j in range(4):
            for hf in range(2):
                p = nps()
                for kc in range(8):
                    MM(ps[p][:, :], mgd[:, kc, j * 128:(j + 1) * 128], Wout[:, kc, hf * 512:(hf + 1) * 512], kc == 0, kc == 7, ['mgd', 'Wout'], [f'ps{p}'])
                V(lambda e, p=p, j=j, hf=hf, xt=xt: e.tensor_tensor(out=x1[:, j, hf * 512:(hf + 1) * 512], in0=xt[:, j, hf * 512:(hf + 1) * 512], in1=ps[p][:, :], op=ALU.add), [xtk, f'ps{p}'], ['x1'])
        D(x1_d[t0:t0 + 512, :].rearrange("(j p) d -> p j d", p=128), x1[:], r=['x1'], w=['x1_d'])
        for j in range(4):
            jk_, jkk_ = junk2_b[j % 2], f'junk2{j % 2}'
            A(lambda e, j=j, jk_=jk_: e.activation(out=jk_[:], in_=x1[:, j, :], func=AF.Square), ['x1'], [jkk_])
            V(lambda e, j=j, jk_=jk_: e.tensor_reduce(out=ss2[:, j:j + 1], in_=jk_[:], axis=mybir.AxisListType.X, op=ALU.add), [jkk_], ['Ess'])
        m0 = c.mark()
        rs2 = rstd_of(ss2, 4, 'E')
        for j in range(4):
            A(lambda e, j=j, rs2=rs2: e.activation(out=h2f[:], in_=x1[:, j, :], func=AF.Copy, scale=rs2[:, j:j + 1]), ['x1', 'Ers'], ['h2f'])
            G(lambda e: e.tensor_copy(out=h2hi[:], in_=h2f[:]), ['h2f'], ['h2hi'])
            G(lambda e: e.tensor_tensor(out=h2lo[:], in0=h2f[:], in1=h2hi[:], op=ALU.subtract), ['h2f', 'h2hi'], ['h2lo'])
            for srct, srck, dst, dstk, pbk in ((h2hi, 'h2hi', hThi, 'hThi', 0), (h2lo, 'h2lo', hTlo, 'hTlo', 1)):
                for kc in range(8):
                    TR(pb[pbk][:, kc * 128:(kc + 1) * 128], srct[:, kc * 128:(kc + 1) * 128], identb[:], [srck, 'identb'], [f'pb{pbk}'])
                A(lambda e, dst=dst, pbk=pbk: e.activation(out=dst[:], in_=pb[pbk][:, :].rearrange("p (k t) -> p k t", k=8), func=AF.Copy), [f'pb{pbk}'], [dstk])
            G(lambda e, j=j: e.tensor_copy(out=h2Tb[:, :, j * 128:(j + 1) * 128], in_=hThi[:]), ['hThi'], ['h2Tb'])
            p = nps()
            nmm = 0
            for (lt, ltk, wt_, wtk) in ((hThi, 'hThi', Wrhi, 'Wrhi'), (hTlo, 'hTlo', Wrhi, 'Wrhi'), (hThi, 'hThi', Wrlo, 'Wrlo')):
                for kc in range(8):
                    MM(ps[p][:, 0:20], lt[:, kc, :], wt_[:, kc, :], nmm == 0, nmm == 23, [ltk, wtk], [f'ps{p}'])
                    nmm += 1
            V(lambda e, p=p, j=j: e.tensor_tensor(out=lg4[:, j, :], in0=ps[p][:, 0:20], in1=brt[:], op=ALU.add), [f'ps{p}', 'brt'], ['lg4'])
        c.release(m0)
        gl = lg4[:, :, 0:4]
        le4 = lg4[:, :, 4:20].rearrange("p j (g e) -> p j g e", g=4)
        V(lambda e: e.tensor_reduce(out=R1["mx"][:], in_=gl, axis=AXX, op=ALU.max), ['lg4'], ['r_mx'])
        V(lambda e: e.tensor_tensor(out=R4["oh"][:], in0=gl, in1=bc3(R1["mx"][:, :]), op=ALU.is_ge), ['lg4', 'r_mx'], ['r_oh'])
        V(lambda e: e.tensor_tensor(out=R4["ge"][:], in0=gl, in1=bc3(R1["mx"][:, :]), op=ALU.subtract), ['lg4', 'r_mx'], ['r_ge'])
        A(lambda e: e.activation(out=R4["ge"][:], in_=R4["ge"][:], func=AF.Exp), ['r_ge'], ['r_ge'])
        V(lambda e: e.tensor_reduce(out=R1["gsum"][:], in_=R4["ge"][:], axis=AXX, op=ALU.add), ['r_ge'], ['r_gsum'])
        V(lambda e: e.reciprocal(out=R1["psel"][:], in_=R1["gsum"][:]), ['r_gsum'], ['r_psel'])
        V(lambda e: e.tensor_tensor(out=prod[:], in0=le4, in1=R4["oh"][:, :, :].unsqueeze(3).broadcast_to([128, 4, 4, 4]), op=ALU.mult), ['lg4', 'r_oh'], ['r_prod'])
        V(lambda e: e.tensor_reduce(out=R4["les"][:], in_=prod[:].rearrange("p j g e -> p j e g"), axis=AXX, op=ALU.add), ['r_prod'], ['r_les'])
        V(lambda e: e.tensor_reduce(out=R1["m1"][:], in_=R4["les"][:], axis=AXX, op=ALU.max), ['r_les'], ['r_m1'])
        V(lambda e: e.tensor_tensor(out=R4["mk1"][:], in0=R4["les"][:], in1=bc3(R1["m1"][:, :]), op=ALU.is_ge), ['r_les', 'r_m1'], ['r_mk1'])
        V(lambda e: e.scalar_tensor_tensor(out=R4["le2"][:], in0=R4["mk1"][:], scalar=-1e30, in1=R4["les"][:], op0=ALU.mult, op1=ALU.add), ['r_mk1', 'r_les'], ['r_le2'])
        V(lambda e: e.tensor_reduce(out=R1["m2"][:], in_=R4["le2"][:], axis=AXX, op=ALU.max), ['r_le2'], ['r_m2'])
        V(lambda e: e.tensor_tensor(out=R4["mk2"][:], in0=R4["le2"][:], in1=bc3(R1["m2"][:, :]), op=ALU.is_ge), ['r_le2', 'r_m2'], ['r_mk2'])
        V(lambda e: e.tensor_tensor(out=R1["d"][:], in0=R1["m2"][:], in1=R1["m1"][:], op=ALU.subtract), ['r_m1', 'r_m2'], ['r_d'])
        A(lambda e: e.activation(out=R1["d"][:], in_=R1["d"][:], func=AF.Exp), ['r_d'], ['r_d'])
        V(lambda e: e.tensor_scalar(out=R1["d"][:], in0=R1["d"][:], scalar1=1.0, scalar2=None, op0=ALU.add), ['r_d'], ['r_d'])
        V(lambda e: e.reciprocal(out=R1["w1"][:], in_=R1["d"][:]), ['r_d'], ['r_w1'])
        V(lambda e: e.tensor_scalar(out=R1["w2"][:], in0=R1["w1"][:], scalar1=-1.0, scalar2=1.0, op0=ALU.mult, op1=ALU.add), ['r_w1'], ['r_w2'])
        V(lambda e: e.tensor_tensor(out=R1["w1"][:], in0=R1["w1"][:], in1=R1["psel"][:], op=ALU.mult), ['r_w1', 'r_psel', 'r_w2'], ['r_w1'])
        V(lambda e: e.tensor_tensor(out=R1["w2"][:], in0=R1["w2"][:], in1=R1["psel"][:], op=ALU.mult), ['r_w2', 'r_psel'], ['r_w2'])
        V(lambda e: e.tensor_tensor(out=R4["w12"][:], in0=R4["mk1"][:], in1=bc3(R1["w1"][:, :]), op=ALU.mult), ['r_mk1', 'r_w1'], ['r_w12'])
        V(lambda e: e.tensor_tensor(out=R4["tq"][:], in0=R4["mk2"][:], in1=bc3(R1["w2"][:, :]), op=ALU.mult), ['r_mk2', 'r_w2'], ['r_tq'])
        V(lambda e: e.tensor_tensor(out=R4["w12"][:], in0=R4["w12"][:], in1=R4["tq"][:], op=ALU.add), ['r_w12', 'r_tq'], ['r_w12'])
        V(lambda e: e.tensor_tensor(out=cw4[:].rearrange("p j (g e) -> p j g e", g=4), in0=R4["oh"][:, :, :].unsqueeze(3).broadcast_to([128, 4, 4, 4]),
                                    in1=R4["w12"][:, :, :].unsqueeze(2).broadcast_to([128, 4, 4, 4]), op=ALU.mult), ['r_oh', 'r_w12'], ['cw4'])
        V(lambda e: e.tensor_copy(out=cwb4[:], in_=cw4[:]), ['cw4'], ['cwb4'])
        for j in range(4):
            TR(pb[0][0:16, 0:128], cwb4[:, j, :], identb[:], ['cwb4', 'identb'], ['pb0'])
            V(lambda e, j=j: e.tensor_copy(out=cwTb[:, j * 128:(j + 1) * 128], in_=pb[0][0:16, 0:128]), ['pb0'], ['cwTb'])
        D(h2T_d.rearrange("(kc p) t -> p kc t", p=128)[:, :, t0:t0 + 512], h2Tb[:], r=['h2Tb'], w=['h2T_d'])
        D(cwT_d[:, t0:t0 + 512], cwTb[:], r=['cwTb'], w=['cwT_d'])
    c.barrier()
    c.release(base_mark)
    if 'dbg_x1' in dbg_out:
        tmpx = c.sb([128, TH // 128, 1024], F32, "tmpx")
        D(tmpx[:], x1_d.rearrange("(j p) d -> p j d", p=128), r=['x1_d'], w=['tmpx'])
        D(dbg_out['dbg_x1'].rearrange("(j p) d -> p j d", p=128), tmpx[:], r=['tmpx'])
        tmpc = c.sb([16, TH], BF16, "tmpc"); tmpcf = c.sb([16, TH], F32, "tmpcf")
        D(tmpc[:], cwT_d, r=['cwT_d'], w=['tmpc'])
        V(lambda e, tmpcf=tmpcf, tmpc=tmpc: e.tensor_copy(out=tmpcf[:], in_=tmpc[:]), ['tmpc'], ['tmpcf'])
        D(dbg_out['dbg_cw'], tmpcf[:], r=['tmpcf'])
        c.barrier()
        c.release(base_mark)

    if stop == 'D':
        c.emit()
        return nc
    ST = min(TH, 1024)
    NSB = ST // 128
    gf2 = c.sb([128, 8], F32, "gf2")
    D(gf2[:], gffnT, w=['gf2'])
    selb = c.sb([16, 2048], BF16, "selb")
    m_ = c.mark()
    selst = c.sb([16, 2048], F32, "selst")
    D(selst[:], sel_d, w=['selst'])
    V(lambda e: e.tensor_copy(out=selb[:], in_=selst[:]), ['selst'], ['selb'])
    gfin_t = c.sb([128, 1024], F32, "gfin")
    D(gfin_t[:], gfin, w=['gfin'])
    h2T = c.sb([128, 8, ST], BF16, "h2T")
    cwT = c.sb([16, ST], BF16, "cwT")
    acc = c.sb([128, NSB, 1024], F32, "acc")
    Wg_b = [c.sb([128, 8, 256], BF16, "Wg") for _ in range(2)]
    Wu_b = [c.sb([128, 8, 256], BF16, "Wu") for _ in range(2)]
    Wd_b = [c.sb([128, 2, 1024], BF16, "Wd") for _ in range(2)]
    wstg = [[c.sb([128, 8, 256], F32, "wstg") for _ in range(2)] for _ in range(2)]
    wstd = [c.sb([128, 2, 1024], F32, "wstd") for _ in range(2)]
    bcs_b = [c.sb([128, 512], BF16, "bcs") for _ in range(2)]
    sgl_b = [c.sb([128, 512], F32, "sgl") for _ in range(2)]
    tmu_b = [c.sb([128, 512], F32, "tmu") for _ in range(2)]
    actT_b = [c.sb([128, 2, 512], BF16, "actT") for _ in range(2)]
    x1f = c.sb([128, 1024], F32, "x1f")
    ssf = c.sb([128, 1], F32, "ssf")
    junk3 = c.sb([128, 1024], BF16, "junk3")
    outt = [c.sb([128, 1024], F32, "outt") for _ in range(2)]
    NST = TH // ST
    NSUB = ST // 512

    def load_expert(n):
        ex, sl = n % 16, n % 2
        Wg, Wu, Wd = Wg_b[sl], Wu_b[sl], Wd_b[sl]
        wk = f'We{sl}'
        D(wstg[sl][0][:], w_eg[ex].rearrange("(kc p) f -> p kc f", p=128), w=[f'wstg{sl}0'])
        D(wstg[sl][1][:], w_eu[ex].rearrange("(kc p) f -> p kc f", p=128), w=[f'wstg{sl}1'])
        D(wstd[sl][:], w_ed[ex].rearrange("(fc p) d -> p fc d", p=128), w=[f'wstd{sl}'])
        for kc in range(8):
            A(lambda e, kc=kc, Wg=Wg, sl=sl: e.activation(out=Wg[:, kc, :], in_=wstg[sl][0][:, kc, :], func=AF.Copy, scale=gf2[:, kc:kc + 1]), [f'wstg{sl}0', 'gf2'], [wk])
            A(lambda e, kc=kc, Wu=Wu, sl=sl: e.activation(out=Wu[:, kc, :], in_=wstg[sl][1][:, kc, :], func=AF.Copy, scale=gf2[:, kc:kc + 1]), [f'wstg{sl}1', 'gf2'], [wk])
        A(lambda e, Wd=Wd, sl=sl: e.activation(out=Wd[:, 0, :], in_=wstd[sl][:, 0, :], func=AF.Copy), [f'wstd{sl}'], [wk])
        V(lambda e, Wd=Wd, sl=sl: e.tensor_copy(out=Wd[:, 1, :], in_=wstd[sl][:, 1, :]), [f'wstd{sl}'], [wk])

    ucount = [0]

    def moe_stage1(n, sub):
        ex, sl = n % 16, n % 2
        Wg, Wu = Wg_b[sl], Wu_b[sl]
        wk = f'We{sl}'
        ub = ucount[0] % 2
        ucount[0] += 1
        bcs, sgl, tmu, actT = bcs_b[ub], sgl_b[ub], tmu_b[ub], actT_b[ub]
        q0 = sub * 512
        p = nps()
        MM(ps[p][:, :], selb[:, ex * 128:(ex + 1) * 128], cwT[:, q0:q0 + 512], True, True, ['selb', 'cwT'], [f'ps{p}'])
        A(lambda e, p=p, bcs=bcs: e.activation(out=bcs[:], in_=ps[p][:, :], func=AF.Copy), [f'ps{p}'], [f'bcs{ub}'])
        for fc in range(2):
            pg = nps()
            for kc in range(8):
                MM(ps[pg][:, :], Wg[:, kc, fc * 128:(fc + 1) * 128], h2T[:, kc, q0:q0 + 512], kc == 0, kc == 7, [wk, 'h2T'], [f'ps{pg}'])
            pu = nps()
            for kc in range(8):
                MM(ps[pu][:, :], Wu[:, kc, fc * 128:(fc + 1) * 128], h2T[:, kc, q0:q0 + 512], kc == 0, kc == 7, [wk, 'h2T'], [f'ps{pu}'])
            A(lambda e, pg=pg, sgl=sgl: e.activation(out=sgl[:], in_=ps[pg][:, :], func=AF.Silu), [f'ps{pg}'], [f'sgl{ub}'])
            V(lambda e, pu=pu, sgl=sgl, tmu=tmu: e.tensor_tensor(out=tmu[:], in0=sgl[:], in1=ps[pu][:, :], op=ALU.mult), [f'sgl{ub}', f'ps{pu}'], [f'tmu{ub}'])
            G(lambda e, fc=fc, tmu=tmu, bcs=bcs, actT=actT: e.tensor_tensor(out=actT[:, fc, :], in0=tmu[:], in1=bcs[:], op=ALU.mult), [f'tmu{ub}', f'bcs{ub}'], [f'actT{ub}'])
        return ub

    def moe_stage2(n, sub, ub):
        sl = n % 2
        Wd = Wd_b[sl]
        wk = f'We{sl}'
        actT = actT_b[ub]
        for j in range(4):
            blk = sub * 4 + j
            for hf in range(2):
                p = nps()
                for fc in range(2):
                    MM(ps[p][:, :], actT[:, fc, j * 128:(j + 1) * 128], Wd[:, fc, hf * 512:(hf + 1) * 512], fc == 0, fc == 1, [f'actT{ub}', wk], [f'ps{p}'])
                V(lambda e, p=p, blk=blk, hf=hf: e.tensor_tensor(out=acc[:, blk, hf * 512:(hf + 1) * 512], in0=acc[:, blk, hf * 512:(hf + 1) * 512], in1=ps[p][:, :], op=ALU.add), ['acc', f'ps{p}'], ['acc'])

    load_expert(0)
    for sti in range(NST):
        s0 = sti * ST
        D(h2T[:], h2T_d.rearrange("(kc p) t -> p kc t", p=128)[:, :, s0:s0 + ST], r=['h2T_d'], w=['h2T'])
        D(cwT[:], cwT_d[:, s0:s0 + ST], r=['cwT_d'], w=['cwT'])
        G(lambda e: e.memset(acc[:], 0.0), (), ['acc'])
        units = [(sti * 16 + ex, sub) for ex in range(16) for sub in range(NSUB)]
        prev = None
        for i in range(len(units) + 1):
            cur = None
            if i < len(units):
                n, sub = units[i]
                ub = moe_stage1(n, sub)
                cur = (n, sub, ub)
            if prev is not None:
                moe_stage2(*prev)
            if cur is not None and cur[1] == 0 and cur[0] + 1 < NST * 16:
                load_expert(cur[0] + 1)
            prev = cur
        for blk in range(NSB):
            r0 = s0 + blk * 128
            ot, otk = outt[blk % 2], f'outt{blk % 2}'
            D(x1f[:], x1_d[r0:r0 + 128, :], r=['x1_d'], w=['x1f'])
            V(lambda e, blk=blk: e.tensor_tensor(out=x1f[:], in0=x1f[:], in1=acc[:, blk, :], op=ALU.add), ['x1f', 'acc'], ['x1f'])
            A(lambda e: e.activation(out=junk3[:], in_=x1f[:], func=AF.Square), ['x1f'], ['junk3'])
            V(lambda e: e.tensor_reduce(out=ssf[:, 0:1], in_=junk3[:], axis=mybir.AxisListType.X, op=ALU.add), ['junk3'], ['Fss'])
            m0 = c.mark()
            rsf = rstd_of(ssf, 1, 'F')
            V(lambda e, ot=ot, rsf=rsf: e.scalar_tensor_tensor(out=ot[:], in0=x1f[:], scalar=rsf[:, 0:1], in1=gfin_t[:], op0=ALU.mult, op1=ALU.mult), ['x1f', 'Frs', 'gfin'], [otk])
            c.release(m0)
            D(out_d[r0:r0 + 128, :], ot[:], r=[otk], w=['out_d'])
    c.emit()
    return nc


def _ssm_layouts(lambda_re, lambda_im, log_step, b_re, b_im, c_re, c_im, ssm_d):
    f = np.float32
    o = {}
    r = np.arange(128)
    k4_r, mp_r, cp_r = r // 32, (r // 16) % 2, r % 16
    s_ = np.arange(4)
    q = np.arange(128)
    m_q, p_q = q // 64, q % 64
    g = 8 * s_[None, :, None] + 2 * k4_r[:, None, None] + m_q[None, None, :]
    P = np.broadcast_to(p_q[None, None, :], g.shape)
    o["LRX"] = lambda_re[g, P].reshape(128, 512).astype(f)
    o["LIX"] = lambda_im[g, P].reshape(128, 512).astype(f)
    o["LSX"] = log_step[g].reshape(128, 512).astype(f)
    msk = (mp_r[:, None, None] == m_q[None, None, :])
    CP = np.broadcast_to(cp_r[:, None, None], g.shape)
    o["BRX"] = np.where(msk, b_re[g, P, CP], 0).reshape(128, 512).astype(f)
    o["BIX"] = np.where(msk, b_im[g, P, CP], 0).reshape(128, 512).astype(f)
    k = np.arange(16)
    j = np.arange(32)
    mp_j, c_j = j // 16, j % 16
    g2 = 2 * k[None, :, None] + m_q[:, None, None] + 0 * j[None, None, :]
    P2 = np.broadcast_to(p_q[:, None, None], g2.shape)
    C2 = np.broadcast_to(c_j[None, None, :], g2.shape)
    msk2 = (mp_j[None, None, :] == m_q[:, None, None])
    o["LRY"] = lambda_re[g2, P2].reshape(128, 512).astype(f)
    o["LIY"] = lambda_im[g2, P2].reshape(128, 512).astype(f)
    o["LSY"] = log_step[g2].reshape(128, 512).astype(f)
    o["CRY"] = np.where(msk2, c_re[g2, C2, P2], 0).reshape(128, 512).astype(f)
    o["CIY"] = np.where(msk2, c_im[g2, C2, P2], 0).reshape(128, 512).astype(f)
    o["BRY"] = np.where(msk2, b_re[g2, P2, C2], 0).reshape(128, 512).astype(f)
    o["BIY"] = np.where(msk2, b_im[g2, P2, C2], 0).reshape(128, 512).astype(f)
    dd = np.zeros((128, 4, 128), f)
    for s in range(4):
        dd[r, s, r] = ssm_d[128 * s + r]
    o["DDG"] = dd.reshape(128, 512)
    return o


_CACHE = {}


def _prep_common(inp):
    f = np.float32
    A_ = lambda a: np.ascontiguousarray(a, dtype=f)
    d = {}
    d["w_in"] = A_(inp["w_in"][0])
    d["gmixT"] = A_(inp["g_mix"][0].reshape(8, 128).T)
    d["bforget"] = A_(inp["b_forget"][0].reshape(8, 1))
    d["bgate"] = A_(inp["b_gate"][0].reshape(16, 128).T)
    d["w_out_a"] = A_(inp["w_out_a"][0])
    d["w_glu"] = A_(inp["w_glu"][0])
    d["bglu"] = A_(inp["b_glu"][0].reshape(4, 128).T)
    d["w_out_b"] = A_(inp["w_out_b"][0])
    d["w_out"] = A_(inp["w_out"][0])
    d["gffnT"] = A_(inp["g_ffn"][0].reshape(8, 128).T)
    d["wr"] = A_(np.concatenate([inp["w_router_group"][0], inp["w_router_expert"][0]], axis=1))
    d["br"] = A_(np.broadcast_to(np.concatenate([inp["b_router_group"][0], inp["b_router_expert"][0]])[None, :], (128, 20)))
    d["w_eg"] = A_(inp["w_exp_gate"][0])
    d["w_eu"] = A_(inp["w_exp_up"][0])
    d["w_ed"] = A_(inp["w_exp_down"][0])
    d["gfin"] = A_(np.broadcast_to(inp["g_final"][None, :], (128, 1024)))
    d.update(_ssm_layouts(np.asarray(inp["lambda_re"][0]), np.asarray(inp["lambda_im"][0]), np.asarray(inp["log_step"][0]),
                          np.asarray(inp["ssm_b_re"][0]), np.asarray(inp["ssm_b_im"][0]), np.asarray(inp["ssm_c_re"][0]),
                          np.asarray(inp["ssm_c_im"][0]), np.asarray(inp["ssm_d"][0])))
    d["ident"] = np.eye(128, dtype=f)
    d["tri"] = np.triu(np.ones((128, 128), f))
    sel = np.zeros((16, 16, 128), f)
    for e in range(16):
        sel[e, e, :] = 1.0
    d["sel"] = sel.reshape(16, 2048)
    return d


def run(inputs, dbg=(), stop=None):
    inp = {k: np.asarray(v) for k, v in inputs.items()}
    x = inp["x"]
    B, S, _ = x.shape
    TH = S // 2
    key = (TH, tuple(dbg), stop)
    if key not in _CACHE:
        _CACHE[key] = build_program(TH, dbg, stop)
    nc = _CACHE[key]
    common = _prep_common(inp)
    in_maps = []
    for core in range(8):
        b, par = core // 2, core % 2
        xin = np.zeros((S, 1024), np.float32)
        if par == 1:
            xin[:TH] = x[b, :TH]
        xin[TH:] = x[b, par * TH:(par + 1) * TH]
        m = dict(common)
        m["xin"] = xin
        m["flag"] = np.full((128, 1), float(par), np.float32)
        in_maps.append(m)
    res = run_bass_kernel_spmd(nc, in_maps, core_ids=list(range(8)))
    return res, TH


def kernel(**inputs):
    res, TH = run(inputs)
    x = inputs["x"]
    B, S, Dm = x.shape
    out = np.zeros((B, S, Dm), np.float32)
    for core in range(8):
        b, par = core // 2, core % 2
        out[b, par * TH:(par + 1) * TH] = res.results[core]["out"]
    return out
```

```python
import contextlib
import numpy as np
import concourse.bass as bass
import concourse.mybir as mybir
from concourse.bass_utils import run_bass_kernel_spmd

F32 = mybir.dt.float32
BF16 = mybir.dt.bfloat16
I32 = mybir.dt.int32
AF = mybir.ActivationFunctionType
ALU = mybir.AluOpType

NDSEM = 48
SAME_SYNC = {'pe': False, 'act': True, 'dve': True, 'pool': True, 'sp': False}
EPS = 1e-6
L = 16
TWO_PI = 6.283185307179586


class Ctx:
    def __init__(self, nc):
        self.nc = nc
        self.names = ['pe', 'act', 'dve', 'pool', 'sp']
        self.ops = {e: [] for e in self.names}
        self.cnt = {e: 0 for e in self.names}
        self.seen = {e: {} for e in self.names}
        self.pending = {e: {} for e in self.names}
        self.lastw = {}
        self.readers = {}
        self.dval = [0] * NDSEM
        self.dnext = 0
        self.sb_off = 16640
        self.uid = 0
        self.sb_max = 0

    def sb(self, shape, dtype, name="t"):
        esz = {F32: 4, BF16: 2, I32: 4}[dtype]
        n = 1
        for s in shape[1:]:
            n *= s
        off = (self.sb_off + 63) // 64 * 64
        self.sb_off = off + n * esz
        self.sb_max = max(self.sb_max, self.sb_off)
        assert self.sb_off <= 229376, f"SBUF overflow {self.sb_off} ({name})"
        self.uid += 1
        return self.nc.alloc_sbuf_tensor_at(f"{name}_{self.uid}", list(shape), dtype, offset=off)

    def mark(self):
        return self.sb_off

    def release(self, m):
        self.sb_off = m

    def _deps(self, reads, writes):
        deps = {}
        for k in reads:
            t = self.lastw.get(k)
            if t and deps.get(t[0], 0) < t[1]:
                deps[t[0]] = t[1]
        for k in writes:
            t = self.lastw.get(k)
            if t and deps.get(t[0], 0) < t[1]:
                deps[t[0]] = t[1]
            for s, v in self.readers.get(k, {}).items():
                if deps.get(s, 0) < v:
                    deps[s] = v
        return deps

    def _waits(self, e, deps):
        for s, v in self.pending[e].items():
            if deps.get(s, 0) < v:
                deps[s] = v
        self.pending[e] = {}
        waits = []
        for s, v in deps.items():
            if s == e and not SAME_SYNC[e]:
                continue
            if self.seen[e].get(s, 0) >= v:
                continue
            self.seen[e][s] = v
            waits.append((s, v))
        return waits

    def _commit(self, tok, reads, writes):
        for k in reads:
            r = self.readers.setdefault(k, {})
            if r.get(tok[0], 0) < tok[1]:
                r[tok[0]] = tok[1]
        for k in writes:
            self.lastw[k] = tok
            self.readers[k] = {}

    def op(self, e, fn, reads=(), writes=()):
        deps = self._deps(reads, writes)
        waits = self._waits(e, deps)
        self.cnt[e] += 1
        tok = (e, self.cnt[e])
        self.ops[e].append((waits, fn, e))
        self._commit(tok, reads, writes)
        return tok

    def dma(self, e, out, in_, reads=(), writes=()):
        i = self.dnext
        self.dnext = (self.dnext + 1) % NDSEM
        deps = self._deps(reads, writes)
        if self.dval[i] > 0:
            s = ('d', i)
            if deps.get(s, 0) < self.dval[i]:
                deps[s] = self.dval[i]
        waits = self._waits(e, deps)
        self.dval[i] += 16
        tok = (('d', i), self.dval[i])
        self.ops[e].append((waits, lambda eng: eng.dma_start(out=out, in_=in_), ('d', i)))
        self._commit(tok, reads, writes)
        return tok

    def barrier(self):
        allt = {e: self.cnt[e] for e in self.names if self.cnt[e] > 0}
        for i in range(NDSEM):
            if self.dval[i] > 0:
                allt[('d', i)] = self.dval[i]
        for e in self.names:
            for s, v in allt.items():
                if self.pending[e].get(s, 0) < v:
                    self.pending[e][s] = v

    def emit(self):
        nc = self.nc
        self.barrier()
        self.op('sp', lambda eng: eng.nop(), (), ())
        with contextlib.ExitStack() as st:
            sems = {e: st.enter_context(nc.semaphore(f"s_{e}")) for e in self.names}
            for i in range(NDSEM):
                sems[('d', i)] = st.enter_context(nc.semaphore(f"d_{i}"))
            block = st.enter_context(nc.Block())

            def run(e, eng):
                for waits, fn, inc in self.ops[e]:
                    for s, v in waits:
                        eng.wait_ge(sems[s], v)
                    ins = fn(eng)
                    if isinstance(inc, tuple):
                        ins.then_inc(sems[inc], 16)
                    else:
                        ins.then_inc(sems[inc], 1)

            @block.sync
            def _(eng):
                run('sp', eng)

            @block.tensor
            def _(eng):
                run('pe', eng)

            @block.scalar
            def _(eng):
                run('act', eng)

            @block.vector
            def _(eng):
                run('dve', eng)

            @block.gpsimd
            def _(eng):
                run('pool', eng)


def build_program(TH, dbg=(), stop=None):
    nc = bass.Bass("TRN2", target_bir_lowering=False)
    T2 = 2 * TH
    NT = TH // 512
    NBH = TH // 128
    NCH = TH // L
    c = Ctx(nc)

    def din(name, shape, dt=F32):
        return nc.dram_tensor(name, list(shape), dt, kind="ExternalInput").ap()

    def dscr(name, shape, dt):
        return nc.dram_tensor(name, list(shape), dt).ap()

    xin = din("xin", [T2, 1024])
    flag_d = din("flag", [128, 1])
    w_in = din("w_in", [1024, 4104])
    gmixT = din("gmixT", [128, 8])
    bforget = din("bforget", [8, 1])
    bgate = din("bgate", [128, 16])
    w_out_a = din("w_out_a", [512, 1024])
    w_glu = din("w_glu", [512, 512])
    bglu = din("bglu", [128, 4])
    w_out_b = din("w_out_b", [512, 1024])
    w_out = din("w_out", [1024, 1024])
    gffnT = din("gffnT", [128, 8])
    wr = din("wr", [1024, 20])
    br = din("br", [128, 20])
    w_eg = din("w_eg", [16, 1024, 256])
    w_eu = din("w_eu", [16, 1024, 256])
    w_ed = din("w_ed", [16, 256, 1024])
    gfin = din("gfin", [128, 1024])
    ssm_names = ["LRX", "LIX", "LSX", "BRX", "BIX", "LRY", "LIY", "LSY", "CRY", "CIY", "BRY", "BIY"]
    ssm_in = {n: din(n, [128, 512]) for n in ssm_names}
    ddg = din("DDG", [128, 512])
    ident_d = din("ident", [128, 128])
    tri_d = din("tri", [128, 128])
    sel_d = din("sel", [16, 2048])
    out_d = nc.dram_tensor("out", [TH, 1024], F32, kind="ExternalOutput").ap()
    dbg_out = {}
    for name, shape in dbg:
        dbg_out[name] = nc.dram_tensor(name, list(shape), F32, kind="ExternalOutput").ap()

    kT_d = dscr("kT_d", [8, 70, T2], BF16)
    qT_d = dscr("qT_d", [8, 70, TH], BF16)
    v_d = dscr("v_d", [T2, 520], BF16)
    uT_d = dscr("uT_d", [512, T2], BF16)
    gT_d = dscr("gT_d", [2048, TH], BF16)
    oT_d = dscr("oT_d", [8, 64, TH], BF16)
    zT_d = dscr("zT_d", [512, TH], BF16)
    x1_d = dscr("x1_d", [TH, 1024], F32)
    h2T_d = dscr("h2T_d", [1024, TH], BF16)
    cwT_d = dscr("cwT_d", [16, TH], BF16)

    ps = [nc.alloc_psum_tensor(f"ps{i}", [128, 512], F32) for i in range(6)]
    pb = [nc.alloc_psum_tensor(f"pb{i}", [128, 1024], BF16) for i in range(2)]

    def V(fn, r=(), w=()):
        return c.op('dve', fn, r, w)

    def A(fn, r=(), w=()):
        return c.op('act', fn, r, w)

    def G(fn, r=(), w=()):
        return c.op('pool', fn, r, w)

    def MM(out, lhsT, rhs, st, sp_, r, w, tp=None):
        kw = dict(start=st, stop=sp_)
        if tp is not None:
            kw['tile_position'] = tp
        return c.op('pe', lambda e: e.matmul(out, lhsT=lhsT, rhs=rhs, **kw), r, w)

    def TR(out, in_, ident, r, w):
        return c.op('pe', lambda e: e.transpose(out, in_, ident), r, w)

    def D(out, in_, r=(), w=(), q='sp'):
        return c.dma(q, out, in_, r, w)


    dump_i = [0]
    dump_st = []

    def dump(name, ap2d, ncols):
        if name not in dbg_out:
            return
        for c0 in range(0, ncols, 2048):
            n = min(2048, ncols - c0)
            dump_i[0] += 1
            kx = 'dump0'
            if not dump_st:
                dump_st.append(c.sb([128, 2048], F32, "dumpst"))
            stt = dump_st[0]
            V(lambda e, stt=stt, c0=c0, n=n: e.tensor_copy(out=stt[:, 0:n], in_=ap2d[:, c0:c0 + n]), [], [kx])
            D(dbg_out[name][:, c0:c0 + n], stt[:, 0:n], r=[kx])
    identf = c.sb([128, 128], F32, "identf")
    identb = c.sb([128, 128], BF16, "identb")
    trib = c.sb([128, 128], BF16, "trib")
    onesf = c.sb([128, 512], F32, "onesf")
    flag = c.sb([128, 1], F32, "flag")
    stg = c.sb([128, 128], F32, "stg")
    D(identf[:], ident_d, w=['identf'])
    D(stg[:], tri_d, w=['stg'])
    D(flag[:], flag_d, w=['flag'])
    V(lambda e: e.tensor_copy(out=identb[:], in_=identf[:]), ['identf'], ['identb'])
    V(lambda e: e.tensor_copy(out=trib[:], in_=stg[:]), ['stg'], ['trib'])
    V(lambda e: e.memset(onesf[:], 1.0), (), ['onesf'])
    base_mark = c.mark()

    def rstd_of(ss, n, tag):
        ms = c.sb([128, n], F32, "ms")
        rs = c.sb([128, n], F32, "rs")
        V(lambda e: e.tensor_scalar(out=ms[:], in0=ss[:], scalar1=1.0 / 1024, scalar2=EPS, op0=ALU.mult, op1=ALU.add), [tag + 'ss'], [tag + 'ms'])
        A(lambda e: e.activation(out=ms[:], in_=ms[:], func=AF.Sqrt), [tag + 'ms'], [tag + 'ms'])
        V(lambda e: e.reciprocal(out=rs[:], in_=ms[:]), [tag + 'ms'], [tag + 'rs'])
        return rs

    Win = c.sb([128, 8, 4104], BF16, "Win")
    gm = c.sb([128, 8], F32, "gm")
    negb = c.sb([8, 1], F32, "negb")
    bg = c.sb([128, 16], F32, "bg")
    D(gm[:], gmixT, w=['gm'])
    D(negb[:], bforget, w=['negb'])
    D(bg[:], bgate, w=['bg'])
    V(lambda e: e.tensor_scalar(out=negb[:], in0=negb[:], scalar1=-1.0, scalar2=None, op0=ALU.mult), ['negb'], ['negb'])
    wst = [c.sb([128, 8, 256], F32, "wst")] * 2
    w_in_v = w_in.rearrange("(kc p) n -> p kc n", p=128)
    ei = 0
    for cc in range(17):
        c0 = cc * 256
        ncol = min(256, 4104 - c0)
        st = wst[cc % 2]
        sk = 'wst'
        D(st[:, :, 0:ncol], w_in_v[:, :, c0:c0 + ncol], w=[sk])
        for kc in range(8):
            if cc % 2 == 0:
                A(lambda e, st=st, kc=kc, c0=c0, ncol=ncol: e.activation(out=Win[:, kc, c0:c0 + ncol], in_=st[:, kc, 0:ncol], func=AF.Copy, scale=gm[:, kc:kc + 1]), [sk, 'gm'], ['Win0'])
            else:
                V(lambda e, st=st, kc=kc, c0=c0, ncol=ncol: e.tensor_scalar(out=Win[:, kc, c0:c0 + ncol], in0=st[:, kc, 0:ncol], scalar1=gm[:, kc:kc + 1], scalar2=None, op0=ALU.mult), [sk, 'gm'], ['Win1'])
            ei += 1

    CQ, CK, CV, CF, CU, CG = 0, 512, 1024, 1536, 1544, 2056
    xt_b = [c.sb([128, 4, 1024], F32, "xt") for _ in range(2)]
    xs = c.sb([128, 4, 1024], BF16, "xs")
    hT_b = [c.sb([128, 8, 512], BF16, "hT") for _ in range(2)]
    junk = c.sb([128, 1024], BF16, "junk")
    junk_b = [junk, c.sb([128, 1024], BF16, "junkb")]
    ss = c.sb([128, 4], F32, "ss")
    kT_s = [c.sb([64, 8, 512], BF16, "kTs")] * 2
    qT_s = [c.sb([64, 8, 512], BF16, "qTs")] * 2
    v_s = [c.sb([128, 4, 8, 65], BF16, "vs")] * 2
    uT_s = [c.sb([128, 4, 512], BF16, "uTs")] * 2
    gT_s = [c.sb([128, 16, 512], BF16, "gTs")] * 2
    CPK = c.sb([8, 6, 512], BF16, "CPK")
    CPQ = c.sb([8, 6, 512], BF16, "CPQ")
    e1 = c.sb([8, 512], F32, "e1")
    negc = c.sb([8, 512], F32, "negc")
    r1 = c.sb([8, 512], F32, "r1")
    carry = c.sb([8, 1], F32, "carry")
    ones_own = c.sb([128, 32], BF16, "ones_own")
    ones_ctx = c.sb([128, 32], BF16, "ones_ctx")
    V(lambda e: e.memset(ones_own[:], 1.0), (), ['ones_own'])
    V(lambda e: e.tensor_scalar(out=ones_ctx[:], in0=onesf[:, 0:32], scalar1=flag[:, 0:1], scalar2=None, op0=ALU.mult), ['onesf', 'flag'], ['ones_ctx'])
    V(lambda e: e.memset(CPK[:, 0:3, :], 1.0), (), ['CPK'])
    V(lambda e: e.memset(CPQ[:, 3:6, :], 1.0), (), ['CPQ'])
    V(lambda e: e.memset(carry[:], 0.0), (), ['carry'])

    xin_v = xin.rearrange("(t j p) d -> t p j d", j=4, p=128)
    D(xt_b[0][:], xin_v[0], w=['xt0'])
    pi = [0]

    def nps():
        pi[0] = (pi[0] + 1) % 6
        return pi[0]

    for i in range(2 * NT):
        own = i >= NT
        b = i % 2
        xt, xk = xt_b[b], f'xt{b}'
        hT, hk = hT_b[b], f'hT{b}'
        if i + 1 < 2 * NT:
            D(xt_b[1 - b][:], xin_v[i + 1], w=[f'xt{1 - b}'])
        for j in range(4):
            jk_, jkk_ = junk_b[j % 2], f'junk{j % 2}'
            A(lambda e, j=j, xt=xt, jk_=jk_: e.activation(out=jk_[:], in_=xt[:, j, :], func=AF.Square), [xk], [jkk_])
            V(lambda e, j=j, jk_=jk_: e.tensor_reduce(out=ss[:, j:j + 1], in_=jk_[:], axis=mybir.AxisListType.X, op=ALU.add), [jkk_], ['Ass'])
        m0 = c.mark()
        rs = rstd_of(ss, 4, 'A')
        for j in range(4):
            V(lambda e, j=j, xt=xt, rs=rs: e.tensor_scalar(out=xs[:, j, :], in0=xt[:, j, :], scalar1=rs[:, j:j + 1], scalar2=None, op0=ALU.mult), [xk, 'Ars'], [f'xs{j}'])
        c.release(m0)
        for j in range(4):
            pbk = j % 2
            for kc in range(8):
                TR(pb[pbk][:, kc * 128:(kc + 1) * 128], xs[:, j, kc * 128:(kc + 1) * 128], identb[:], [f'xs{j}', 'identb'], [f'pb{pbk}'])
            src = pb[pbk][:, :].rearrange("p (k t) -> p k t", k=8)
            if j % 2 == 0:
                A(lambda e, j=j, hT=hT, src=src: e.activation(out=hT[:, :, j * 128:(j + 1) * 128], in_=src, func=AF.Copy), [f'pb{pbk}'], [hk])
            else:
                V(lambda e, j=j, hT=hT, src=src: e.tensor_copy(out=hT[:, :, j * 128:(j + 1) * 128], in_=src), [f'pb{pbk}'], [hk])
        tok0 = i * 512
        p = nps()
        for kc in range(8):
            MM(ps[p][0:8, :], Win[:, kc, CF:CF + 8], hT[:, kc, :], kc == 0, kc == 7, ['Win0', 'Win1', hk], [f'ps{p}'])
        A(lambda e, p=p: e.activation(out=e1[:], in_=ps[p][0:8, :], func=AF.Exp, scale=-1.0, bias=negb[:, 0:1]), [f'ps{p}', 'negb'], ['e1'])
        A(lambda e: e.activation(out=e1[:], in_=e1[:], func=AF.Ln, bias=1.0), ['e1'], ['e1'])
        V(lambda e: e.tensor_tensor_scan(out=negc[:], data0=onesf[0:8, 0:512], data1=e1[:], initial=carry[:, 0:1], op0=ALU.mult, op1=ALU.add), ['e1', 'carry', 'onesf'], ['negc'])
        V(lambda e: e.tensor_copy(out=carry[:], in_=negc[:, 511:512]), ['negc'], ['carry'])
        V(lambda e: e.tensor_copy(out=CPK[:, 3, :], in_=negc[:]), ['negc'], ['CPK'])
        V(lambda e: e.tensor_tensor(out=r1[:], in0=negc[:], in1=CPK[:, 3, :], op=ALU.subtract), ['negc', 'CPK'], ['r1'])
        V(lambda e: e.tensor_copy(out=CPK[:, 4, :], in_=r1[:]), ['r1'], ['CPK'])
        V(lambda e: e.tensor_tensor(out=r1[:], in0=r1[:], in1=CPK[:, 4, :], op=ALU.subtract), ['r1', 'CPK'], ['r1'])
        V(lambda e: e.tensor_copy(out=CPK[:, 5, :], in_=r1[:]), ['r1'], ['CPK'])
        D(kT_d[:, 64:70, tok0:tok0 + 512], CPK[:, :, :], r=['CPK'], w=['kT_d'])
        if own:
            V(lambda e: e.tensor_scalar(out=CPQ[:, 0:3, :], in0=CPK[:, 3:6, :], scalar1=-1.0, scalar2=None, op0=ALU.mult), ['CPK'], ['CPQ'])
            D(qT_d[:, 64:70, tok0 - TH:tok0 - TH + 512], CPQ[:, :, :], r=['CPQ'], w=['qT_d'])
        kts, ktk = kT_s[b], 'kTs'
        for h in range(8):
            p = nps()
            for kc in range(8):
                MM(ps[p][0:64, :], Win[:, kc, CK + h * 64:CK + (h + 1) * 64], hT[:, kc, :], kc == 0, kc == 7, ['Win0', 'Win1', hk], [f'ps{p}'])
            if h % 2 == 0:
                A(lambda e, p=p, h=h, kts=kts: e.activation(out=kts[:, h, :], in_=ps[p][0:64, :], func=AF.Copy), [f'ps{p}'], [ktk])
            else:
                V(lambda e, p=p, h=h, kts=kts: e.tensor_copy(out=kts[:, h, :], in_=ps[p][0:64, :]), [f'ps{p}'], [ktk])
        D(kT_d[:, 0:64, tok0:tok0 + 512].rearrange("h r t -> r h t"), kts[:, :, :], r=[ktk], w=['kT_d'])
        vs, vk = v_s[b], 'vs'
        V(lambda e, vs=vs, own=own: e.tensor_copy(out=vs[:, :, :, 64:65], in_=(ones_own if own else ones_ctx)[:, :].rearrange("p (j h o) -> p j h o", j=4, o=1)), ['ones_own', 'ones_ctx'], [vk])
        for j in range(4):
            p = nps()
            for kc in range(8):
                MM(ps[p][:, :], hT[:, kc, j * 128:(j + 1) * 128], Win[:, kc, CV:CV + 512], kc == 0, kc == 7, ['Win0', 'Win1', hk], [f'ps{p}'])
            V(lambda e, p=p, j=j, vs=vs: e.tensor_copy(out=vs[:, j, :, 0:64], in_=ps[p][:, :].rearrange("p (h d) -> p h d", h=8)), [f'ps{p}'], [vk])
        D(v_d[tok0:tok0 + 512, :].rearrange("(j p) c -> p j c", p=128), vs[:, :, :, :].rearrange("p j h c -> p j (h c)"), r=[vk], w=['v_d'])
        us, uk = uT_s[b], 'uTs'
        for m in range(4):
            p = nps()
            for kc in range(8):
                MM(ps[p][:, :], Win[:, kc, CU + m * 128:CU + (m + 1) * 128], hT[:, kc, :], kc == 0, kc == 7, ['Win0', 'Win1', hk], [f'ps{p}'])
            V(lambda e, p=p, m=m, us=us: e.tensor_copy(out=us[:, m, :], in_=ps[p][:, :]), [f'ps{p}'], [uk])
        D(uT_d.rearrange("(s p) t -> p s t", p=128)[:, :, tok0:tok0 + 512], us[:, :, :], r=[uk], w=['uT_d'])
        if own:
            qts, qtk = qT_s[b], 'qTs'
            for h in range(8):
                p = nps()
                for kc in range(8):
                    MM(ps[p][0:64, :], Win[:, kc, CQ + h * 64:CQ + (h + 1) * 64], hT[:, kc, :], kc == 0, kc == 7, ['Win0', 'Win1', hk], [f'ps{p}'])
                A(lambda e, p=p, h=h, qts=qts: e.activation(out=qts[:, h, :], in_=ps[p][0:64, :], func=AF.Copy, scale=0.125), [f'ps{p}'], [qtk])
            D(qT_d[:, 0:64, tok0 - TH:tok0 - TH + 512].rearrange("h r t -> r h t"), qts[:, :, :], r=[qtk], w=['qT_d'])
            gs, gk = gT_s[b], 'gTs'
            for m in range(16):
                p = nps()
                for kc in range(8):
                    MM(ps[p][:, :], Win[:, kc, CG + m * 128:CG + (m + 1) * 128], hT[:, kc, :], kc == 0, kc == 7, ['Win0', 'Win1', hk], [f'ps{p}'])
                A(lambda e, p=p, m=m, gs=gs: e.activation(out=gs[:, m, :], in_=ps[p][:, :], func=AF.Sigmoid, bias=bg[:, m:m + 1]), [f'ps{p}', 'bg'], [gk])
            D(gT_d.rearrange("(m p) t -> p m t", p=128)[:, :, tok0 - TH:tok0 - TH + 512], gs[:, :, :], r=[gk], w=['gT_d'])

    c.barrier()
    c.release(base_mark)
    if 'dbg_k' in dbg_out:
        tmpk = c.sb([70, T2], BF16, "tmpk")
        tmpf = c.sb([70, T2], F32, "tmpf")
        D(tmpk[:], kT_d[0], r=['kT_d'], w=['tmpk'])
        V(lambda e, tmpf=tmpf, tmpk=tmpk: e.tensor_copy(out=tmpf[:], in_=tmpk[:]), ['tmpk'], ['tmpf'])
        D(dbg_out['dbg_k'], tmpf[:], r=['tmpf'])
        c.barrier()
        c.release(base_mark)

    if stop == 'A':
        c.emit()
        return nc
    NB2 = T2 // 128
    v_all = c.sb([128, NB2, 520], BF16, "v_all")
    for q4 in range(0, NB2, 8):
        n = min(8, NB2 - q4)
        D(v_all[:, q4:q4 + n, :], v_d[q4 * 128:(q4 + n) * 128, :].rearrange("(j p) c -> p j c", p=128), r=['v_d'], w=['v_all'])
    kT_h = [c.sb([70, T2], BF16, "kTh") for _ in range(2)]
    qT_h = [c.sb([70, TH], BF16, "qTh") for _ in range(2)]
    pT_b = [c.sb([128, 512], BF16, "pT") for _ in range(3)]
    rr = c.sb([128, 512], F32, "rr")
    rrh = c.sb([128, 512], BF16, "rrh")
    rrl = c.sb([128, 512], BF16, "rrl")
    onesb = c.sb([128, 64], BF16, "onesb")
    V(lambda e: e.memset(onesb[:], 1.0), (), ['onesb'])
    bc_sb = c.sb([64, 512], F32, "bc_sb")
    oT_s = [c.sb([64, 512], BF16, "oTs") for _ in range(2)]
    D(kT_h[0][:], kT_d[0], r=['kT_d'], w=['kTh0'])
    D(qT_h[0][:], qT_d[0], r=['qT_d'], w=['qTh0'])
    items = []
    gi = 0
    for h in range(8):
        for Gq in range(NT):
            kbs = list(range(NBH)) + [NBH + ob for ob in range(4 * Gq + 4)]
            for idx, gkb in enumerate(kbs):
                ob = gkb - NBH
                diag = ob >= 4 * Gq
                c0 = (ob - 4 * Gq) * 128 if diag else 0
                items.append(dict(h=h, Gq=Gq, gkb=gkb, c0=c0, diag=diag, first=(idx == 0), last=(idx == len(kbs) - 1), gi=gi,
                                  newhead=(Gq == 0 and idx == 0)))
            gi += 1
    DEPTH = 2

    def att_stage1(i, it):
        h, Gq, gkb, c0 = it['h'], it['Gq'], it['gkb'], it['c0']
        hb = h % 2
        if it['newhead'] and h + 1 < 8:
            D(kT_h[1 - hb][:], kT_d[h + 1], r=['kT_d'], w=[f'kTh{1 - hb}'])
            D(qT_h[1 - hb][:], qT_d[h + 1], r=['qT_d'], w=[f'qTh{1 - hb}'])
        kt, ktk = kT_h[hb], f'kTh{hb}'
        qt, qtk = qT_h[hb], f'qTh{hb}'
        p = i % 3
        pT, ptk = pT_b[i % 3], f'pT{i % 3}'
        MM(ps[p][:, c0:512], kt[:, gkb * 128:(gkb + 1) * 128], qt[:, Gq * 512 + c0:Gq * 512 + 512], True, True, [ktk, qtk], [f'ps{p}'])
        A(lambda e, p=p, pT=pT, c0=c0: e.activation(out=pT[:, c0:512], in_=ps[p][:, c0:512], func=AF.Exp), [f'ps{p}'], [ptk])
        if it['diag']:
            V(lambda e, pT=pT, c0=c0: e.tensor_tensor(out=pT[:, c0:c0 + 128], in0=pT[:, c0:c0 + 128], in1=trib[:, :], op=ALU.mult), [ptk, 'trib'], [ptk])

    def att_stage2(i, it):
        h, Gq, gkb, c0 = it['h'], it['Gq'], it['gkb'], it['c0']
        po = 3 + (it['gi'] % 2)
        pT, ptk = pT_b[i % 3], f'pT{i % 3}'
        MM(ps[po][0:65, c0:512], v_all[:, gkb, h * 65:(h + 1) * 65], pT[:, c0:512], it['first'], it['last'], ['v_all', ptk], [f'ps{po}'])
        if it['last']:
            V(lambda e, po=po: e.reciprocal(out=rr[64:65, :], in_=ps[po][64:65, :]), [f'ps{po}'], ['rr'])
            V(lambda e: e.tensor_copy(out=rrh[64:65, :], in_=rr[64:65, :]), ['rr'], ['rrh'])
            V(lambda e: e.tensor_tensor(out=rr[64:65, :], in0=rr[64:65, :], in1=rrh[64:65, :], op=ALU.subtract), ['rr', 'rrh'], ['rr'])
            V(lambda e: e.tensor_copy(out=rrl[64:65, :], in_=rr[64:65, :]), ['rr'], ['rrl'])
            def tail(po=po, h=h, Gq=Gq, gi_=it['gi']):
                MM(ps[5][0:64, :], onesb[64:65, 0:64], rrh[64:65, :], True, False, ['onesb', 'rrh'], ['ps5'])
                MM(ps[5][0:64, :], onesb[64:65, 0:64], rrl[64:65, :], False, True, ['onesb', 'rrl'], ['ps5'])
                A(lambda e: e.activation(out=bc_sb[:], in_=ps[5][0:64, :], func=AF.Copy), ['ps5'], ['bc_sb'])
                ots, otk = oT_s[gi_ % 2], f"oTs{gi_ % 2}"
                V(lambda e, po=po, ots=ots: e.tensor_tensor(out=ots[:], in0=ps[po][0:64, :], in1=bc_sb[:], op=ALU.mult), [f'ps{po}', 'bc_sb'], [otk])
                D(oT_d[h, :, Gq * 512:(Gq + 1) * 512], ots[:], r=[otk], w=['oT_d'])
            pending_tail.append([3, tail])

    pending_tail = []
    for i in range(len(items) + DEPTH):
        if i < len(items):
            att_stage1(i, items[i])
        if i - DEPTH >= 0:
            att_stage2(i - DEPTH, items[i - DEPTH])
        for pt_ in list(pending_tail):
            pt_[0] -= 1
            if pt_[0] <= 0:
                pt_[1]()
                pending_tail.remove(pt_)
    for pt_ in pending_tail:
        pt_[1]()
    c.barrier()
    c.release(base_mark)
    if 'dbg_o' in dbg_out:
        tmpk = c.sb([64, 8, TH], BF16, "tmpo")
        tmpf = c.sb([64, 8, TH], F32, "tmpof")
        D(tmpk[:], oT_d.rearrange("h d t -> d h t"), r=['oT_d'], w=['tmpk'])
        V(lambda e, tmpf=tmpf, tmpk=tmpk: e.tensor_copy(out=tmpf[:], in_=tmpk[:]), ['tmpk'], ['tmpf'])
        D(dbg_out['dbg_o'].rearrange("h d t -> d h t"), tmpf[:], r=['tmpf'])
        c.barrier()
        c.release(base_mark)

    if stop == 'B':
        c.emit()
        return nc
    def ssm_prep(sfx):
        k = 'pp' + sfx
        lr = c.sb([128, 512], F32, "lr"); li = c.sb([128, 512], F32, "li"); ls = c.sb([128, 512], F32, "ls")
        D(lr[:], ssm_in["LR" + sfx], w=[k + 'lr'])
        D(li[:], ssm_in["LI" + sfx], w=[k + 'li'])
        D(ls[:], ssm_in["LS" + sfx], w=[k + 'ls'])
        dt = c.sb([128, 512], F32, "dt"); mag = c.sb([128, 512], F32, "mag"); th = c.sb([128, 512], F32, "th")
        A(lambda e: e.activation(out=dt[:], in_=ls[:], func=AF.Exp), [k + 'ls'], [k + 'dt'])
        V(lambda e: e.tensor_tensor(out=mag[:], in0=lr[:], in1=dt[:], op=ALU.mult), [k + 'lr', k + 'dt'], [k + 'mag'])
        A(lambda e: e.activation(out=mag[:], in_=mag[:], func=AF.Exp), [k + 'mag'], [k + 'mag'])
        V(lambda e: e.tensor_tensor(out=th[:], in0=li[:], in1=dt[:], op=ALU.mult), [k + 'li', k + 'dt'], [k + 'th'])

        def sin_of(shift, outt, ok):
            t = c.sb([128, 512], F32, "t"); ni = c.sb([128, 512], I32, "ni"); nf = c.sb([128, 512], F32, "nf")
            a = c.sb([128, 512], F32, "a"); mk = c.sb([128, 512], F32, "mk")
            V(lambda e: e.tensor_scalar(out=t[:], in0=th[:], scalar1=1.0 / TWO_PI, scalar2=8.5 + shift, op0=ALU.mult, op1=ALU.add), [k + 'th'], [k + 't'])
            V(lambda e: e.tensor_copy(out=ni[:], in_=t[:]), [k + 't'], [k + 'ni'])
            V(lambda e: e.tensor_copy(out=nf[:], in_=ni[:]), [k + 'ni'], [k + 'nf'])
            V(lambda e: e.scalar_tensor_tensor(out=a[:], in0=t[:], scalar=-0.5, in1=nf[:], op0=ALU.add, op1=ALU.subtract), [k + 't', k + 'nf'], [k + 'a'])
            V(lambda e: e.tensor_single_scalar(out=mk[:], in_=a[:], scalar=-0.5, op=ALU.is_lt), [k + 'a'], [k + 'mk'])
            V(lambda e: e.tensor_tensor(out=a[:], in0=a[:], in1=mk[:], op=ALU.add), [k + 'a', k + 'mk'], [k + 'a'])
            A(lambda e: e.activation(out=outt[:], in_=a[:], func=AF.Sin, scale=TWO_PI), [k + 'a'], [ok])
        ar = c.sb([128, 512], F32, "ar"); ai = c.sb([128, 512], F32, "ai")
        sin_of(0.0, ai, k + 'ai')
        sin_of(0.25, ar, k + 'ar')
        V(lambda e: e.tensor_tensor(out=ai[:], in0=ai[:], in1=mag[:], op=ALU.mult), [k + 'ai', k + 'mag'], [k + 'ai'])
        V(lambda e: e.tensor_tensor(out=ar[:], in0=ar[:], in1=mag[:], op=ALU.mult), [k + 'ar', k + 'mag'], [k + 'ar'])
        zr = c.sb([128, 512], F32, "zr"); zi = c.sb([128, 512], F32, "zi")
        nr = c.sb([128, 512], F32, "nr"); den = c.sb([128, 512], F32, "den"); t2 = c.sb([128, 512], F32, "t2")
        V(lambda e: e.tensor_scalar(out=nr[:], in0=ar[:], scalar1=-1.0, scalar2=None, op0=ALU.add), [k + 'ar'], [k + 'nr'])
        V(lambda e: e.tensor_tensor(out=den[:], in0=lr[:], in1=lr[:], op=ALU.mult), [k + 'lr'], [k + 'den'])
        V(lambda e: e.tensor_tensor(out=t2[:], in0=li[:], in1=li[:], op=ALU.mult), [k + 'li'], [k + 't2'])
        V(lambda e: e.tensor_tensor(out=den[:], in0=den[:], in1=t2[:], op=ALU.add), [k + 'den', k + 't2'], [k + 'den'])
        V(lambda e: e.reciprocal(out=den[:], in_=den[:]), [k + 'den'], [k + 'den'])
        V(lambda e: e.tensor_tensor(out=zr[:], in0=nr[:], in1=lr[:], op=ALU.mult), [k + 'nr', k + 'lr'], [k + 'zr'])
        V(lambda e: e.tensor_tensor(out=t2[:], in0=ai[:], in1=li[:], op=ALU.mult), [k + 'ai', k + 'li'], [k + 't2'])
        V(lambda e: e.tensor_tensor(out=zr[:], in0=zr[:], in1=t2[:], op=ALU.add), [k + 'zr', k + 't2'], [k + 'zr'])
        V(lambda e: e.tensor_tensor(out=zr[:], in0=zr[:], in1=den[:], op=ALU.mult), [k + 'zr', k + 'den'], [k + 'zr'])
        V(lambda e: e.tensor_tensor(out=zi[:], in0=ai[:], in1=lr[:], op=ALU.mult), [k + 'ai', k + 'lr'], [k + 'zi'])
        V(lambda e: e.tensor_tensor(out=t2[:], in0=nr[:], in1=li[:], op=ALU.mult), [k + 'nr', k + 'li'], [k + 't2'])
        V(lambda e: e.tensor_tensor(out=zi[:], in0=zi[:], in1=t2[:], op=ALU.subtract), [k + 'zi', k + 't2'], [k + 'zi'])
        V(lambda e: e.tensor_tensor(out=zi[:], in0=zi[:], in1=den[:], op=ALU.mult), [k + 'zi', k + 'den'], [k + 'zi'])
        return ar, ai, zr, zi, k

    def cmul(outr, outi, xr, xi, yr, yi, keys_in, kor, koi, negate_im=False, view=None):
        ta_t, tb_t = cm_tmp
        ta = view(ta_t[:]) if view else ta_t[:]
        tb = view(tb_t[:]) if view else tb_t[:]
        V(lambda e: e.tensor_tensor(out=ta, in0=xr, in1=yr, op=ALU.mult), keys_in, ['cm_ta'])
        V(lambda e: e.tensor_tensor(out=tb, in0=xi, in1=yi, op=ALU.mult), keys_in, ['cm_tb'])
        V(lambda e: e.tensor_tensor(out=outr, in0=ta, in1=tb, op=ALU.subtract), ['cm_ta', 'cm_tb'], [kor])
        V(lambda e: e.tensor_tensor(out=ta, in0=xr, in1=yi, op=ALU.mult), keys_in + [kor], ['cm_ta'])
        V(lambda e: e.tensor_tensor(out=tb, in0=xi, in1=yr, op=ALU.mult), keys_in + [kor], ['cm_tb'])
        if negate_im:
            V(lambda e: e.scalar_tensor_tensor(out=outi, in0=ta, scalar=-1.0, in1=tb, op0=ALU.mult, op1=ALU.subtract), ['cm_ta', 'cm_tb'], [koi])
        else:
            V(lambda e: e.tensor_tensor(out=outi, in0=ta, in1=tb, op=ALU.add), ['cm_ta', 'cm_tb'], [koi])

    cm_tmp = []
    ZBT = c.sb([128, 4, L, 2, 128], BF16, "ZBT")
    CYT = c.sb([128, 16, L + 1, 2, 32], BF16, "CYT")
    TT = c.sb([128, 4, L, 128], BF16, "TT")
    BBY = c.sb([128, 2, 512], BF16, "BBY")
    AL = c.sb([128, 2, 2, 16], F32, "AL")
    tbl_mark = c.mark()
    ar, ai, zr, zi, k = ssm_prep('X')
    br_ = c.sb([128, 512], F32, "br"); bi_ = c.sb([128, 512], F32, "bi")
    D(br_[:], ssm_in["BRX"], w=['brx'])
    D(bi_[:], ssm_in["BIX"], w=['bix'])
    bbr = c.sb([128, 512], F32, "bbr"); bbi = c.sb([128, 512], F32, "bbi")
    cm_tmp[:] = [c.sb([128, 512], F32, "ta"), c.sb([128, 512], F32, "tb")]
    cmul(bbr[:], bbi[:], zr[:], zi[:], br_[:], bi_[:], [k + 'zr', k + 'zi', 'brx', 'bix'], 'bbr', 'bbi')
    pw = [c.sb([128, 2, 512], F32, "pw") for _ in range(2)]
    V(lambda e, pw=pw: e.memset(pw[0][:, 0, :], 1.0), (), ['pw0'])
    V(lambda e, pw=pw: e.memset(pw[0][:, 1, :], 0.0), (), ['pw0'])
    for n in range(L):
        cur, ck = pw[n % 2], f'pw{n % 2}'
        nxt, nk = pw[(n + 1) % 2], f'pw{(n + 1) % 2}'
        j = L - 1 - n
        cmul(ZBT[:, :, j, 0, :], ZBT[:, :, j, 1, :], cur[:, 0, :].rearrange("p (s q) -> p s q", s=4), cur[:, 1, :].rearrange("p (s q) -> p s q", s=4),
             bbr[:].rearrange("p (s q) -> p s q", s=4), bbi[:].rearrange("p (s q) -> p s q", s=4), [ck, 'bbr', 'bbi'], 'ZBT', 'ZBT',
             view=lambda ap: ap.rearrange("p (s q) -> p s q", s=4))
        if n < L - 1:
            cmul(nxt[:, 0, :], nxt[:, 1, :], cur[:, 0, :], cur[:, 1, :], ar[:], ai[:], [ck, k + 'ar', k + 'ai'], nk, nk)
    c.barrier()
    c.release(tbl_mark)
    ar, ai, zr, zi, k = ssm_prep('Y')
    br_ = c.sb([128, 512], F32, "br"); bi_ = c.sb([128, 512], F32, "bi")
    cr_ = c.sb([128, 512], F32, "cr"); ci_ = c.sb([128, 512], F32, "ci")
    D(br_[:], ssm_in["BRY"], w=['bry'])
    D(bi_[:], ssm_in["BIY"], w=['biy'])
    D(cr_[:], ssm_in["CRY"], w=['cry'])
    D(ci_[:], ssm_in["CIY"], w=['ciy'])
    cm_tmp[:] = [c.sb([128, 512], F32, "ta"), c.sb([128, 512], F32, "tb")]
    cmul(BBY[:, 0, :], BBY[:, 1, :], zr[:], zi[:], br_[:], bi_[:], [k + 'zr', k + 'zi', 'bry', 'biy'], 'BBY', 'BBY')
    pw = [c.sb([128, 2, 512], F32, "pw") for _ in range(2)]
    V(lambda e, pw=pw: e.memset(pw[0][:, 0, :], 1.0), (), ['pw0'])
    V(lambda e, pw=pw: e.memset(pw[0][:, 1, :], 0.0), (), ['pw0'])
    for n in range(L + 1):
        cur, ck = pw[n % 2], f'pw{n % 2}'
        nxt, nk = pw[(n + 1) % 2], f'pw{(n + 1) % 2}'
        cmul(CYT[:, :, n, 0, :], CYT[:, :, n, 1, :], cr_[:].rearrange("p (k j) -> p k j", k=16), ci_[:].rearrange("p (k j) -> p k j", k=16),
             cur[:, 0, :].rearrange("p (k j) -> p k j", k=16), cur[:, 1, :].rearrange("p (k j) -> p k j", k=16), [ck, 'cry', 'ciy'], 'CYT', 'CYT', negate_im=True,
             view=lambda ap: ap.rearrange("p (k j) -> p k j", k=16))
        if n < L:
            cmul(nxt[:, 0, :], nxt[:, 1, :], cur[:, 0, :], cur[:, 1, :], ar[:], ai[:], [ck, k + 'ar', k + 'ai'], nk, nk)
    pL, pLk = pw[L % 2], f'pw{L % 2}'
    pLv_r = pL[:, 0, :].rearrange("p (k j) -> p k j", k=16)[:, :, 0]
    pLv_i = pL[:, 1, :].rearrange("p (k j) -> p k j", k=16)[:, :, 0]
    V(lambda e: e.tensor_copy(out=AL[:, 0, 0, :], in_=pLv_r), [pLk], ['AL'])
    V(lambda e: e.tensor_copy(out=AL[:, 0, 1, :], in_=pLv_r), [pLk], ['AL'])
    V(lambda e: e.tensor_scalar(out=AL[:, 1, 0, :], in0=pLv_i, scalar1=-1.0, scalar2=None, op0=ALU.mult), [pLk], ['AL'])
    V(lambda e: e.tensor_copy(out=AL[:, 1, 1, :], in_=pLv_i), [pLk], ['AL'])
    V(lambda e: e.memset(TT[:], 0.0), (), ['TT'])
    ddt = c.sb([128, 512], F32, "ddt")
    D(ddt[:], ddg, w=['ddt'])
    for kk in range(16):
        s_, k4 = kk // 4, kk % 4
        p = nps()
        MM(ps[p][32 * k4:32 * k4 + 32, :], BBY[:, 0, kk * 32:(kk + 1) * 32], CYT[:, kk, 0:L, 0, :], True, False, ['BBY', 'CYT'], [f'ps{p}'], tp=(0, 32 * k4))
        MM(ps[p][32 * k4:32 * k4 + 32, :], BBY[:, 1, kk * 32:(kk + 1) * 32], CYT[:, kk, 0:L, 1, :], False, True, ['BBY', 'CYT'], [f'ps{p}'], tp=(0, 32 * k4))
        V(lambda e, p=p, s_=s_, k4=k4: e.tensor_copy(out=TT[32 * k4:32 * k4 + 32, s_, :, 32 * k4:32 * k4 + 32], in_=ps[p][32 * k4:32 * k4 + 32, :].rearrange("p (n j) -> p n j", n=L)), [f'ps{p}'], ['TT'])
    V(lambda e: e.tensor_tensor(out=TT[:, :, 0, :], in0=TT[:, :, 0, :], in1=ddt[:].rearrange("p (s q) -> p s q", s=4), op=ALU.add), ['TT', 'ddt'], ['TT'])
    c.barrier()
    c.release(tbl_mark)

    if stop == 'C1':
        c.barrier()
        dump('dbg_ZBT', ZBT[:].rearrange("p s j r q -> p (s j r q)"), 4 * L * 2 * 128)
        dump('dbg_CYT', CYT[:].rearrange("p k n r j -> p (k n r j)"), 16 * (L + 1) * 2 * 32)
        dump('dbg_TT', TT[:].rearrange("p s n q -> p (s n q)"), 4 * L * 128)
        dump('dbg_AL', AL[:].rearrange("p a r k -> p (a r k)"), 64)
        c.emit()
        return nc
    uT_all = c.sb([128, 4, TH], BF16, "uT_all")
    Zs = c.sb([128, 2, 16, NCH], BF16, "Zs")
    H = c.sb([128, 2, 16, NCH + 1], F32, "H")
    Hb = c.sb([128, 2, 16, NCH + 1], BF16, "Hb")
    t1 = c.sb([128, 2, 16], F32, "t1")
    t4 = c.sb([128, 2, 2, 16], F32, "t4")
    AL4 = c.sb([128, 2, 2, 16], F32, "AL4")
    V(lambda e: e.tensor_copy(out=AL4[:, 0, 0, :], in_=AL[:, 0, 0, :]), ['AL'], ['AL4'])
    V(lambda e: e.tensor_copy(out=AL4[:, 0, 1, :], in_=AL[:, 1, 0, :]), ['AL'], ['AL4'])
    V(lambda e: e.tensor_copy(out=AL4[:, 1, 0, :], in_=AL[:, 1, 1, :]), ['AL'], ['AL4'])
    V(lambda e: e.tensor_copy(out=AL4[:, 1, 1, :], in_=AL[:, 0, 1, :]), ['AL'], ['AL4'])
    t2_ = c.sb([128, 2, 16], F32, "t2_")
    G(lambda e: e.memset(H[:, :, :, 0], 0.0), (), ['H'])
    NZ = min(NCH, 512)
    for half in range(2):
        D(uT_all[:], uT_d.rearrange("(s p) t -> p s t", p=128)[:, :, half * TH:(half + 1) * TH], r=['uT_d'], w=['uT_all'])
        for kk in range(16):
            s_, k4 = kk // 4, kk % 4
            for ri in range(2):
                for z0 in range(0, NCH, NZ):
                    p = nps()
                    for j in range(L):
                        uview = uT_all[32 * k4:32 * k4 + 32, s_, :].rearrange("p (n j) -> p n j", j=L)[:, z0:z0 + NZ, j]
                        MM(ps[p][:, 0:NZ], ZBT[32 * k4:32 * k4 + 32, s_, j, ri, :], uview, j == 0, j == L - 1, ['ZBT', 'uT_all'], [f'ps{p}'], tp=(32 * k4, 0))
                    if (kk + ri) % 2 == 0:
                        A(lambda e, p=p, ri=ri, kk=kk, z0=z0: e.activation(out=Zs[:, ri, kk, z0:z0 + NZ], in_=ps[p][:, 0:NZ], func=AF.Copy), [f'ps{p}'], ['Zs'])
                    else:
                        V(lambda e, p=p, ri=ri, kk=kk, z0=z0: e.tensor_copy(out=Zs[:, ri, kk, z0:z0 + NZ], in_=ps[p][:, 0:NZ]), [f'ps{p}'], ['Zs'])
        for n in range(NCH):
            V(lambda e, n=n: e.tensor_tensor(out=t4[:], in0=AL4[:], in1=H[:, :, :, n].unsqueeze(1).broadcast_to([128, 2, 2, 16]), op=ALU.mult), ['AL4', 'H'], ['t4'])
            V(lambda e, n=n: e.tensor_tensor(out=t1[:], in0=t4[:, :, 0, :], in1=Zs[:, :, :, n], op=ALU.add), ['t4', 'Zs'], ['t1'])
            V(lambda e, n=n: e.tensor_tensor(out=H[:, :, :, n + 1], in0=t1[:], in1=t4[:, :, 1, :], op=ALU.add), ['t1', 't4'], ['H'])
        if half == 0:
            V(lambda e: e.tensor_copy(out=H[:, :, :, 0], in_=H[:, :, :, NCH]), ['H'], ['H'])
    V(lambda e: e.tensor_copy(out=Hb[:], in_=H[:]), ['H'], ['Hb'])

    if stop == 'C2':
        c.barrier()
        dump('dbg_H', H[:].rearrange("p a k n -> p (a k n)"), 2 * 16 * (NCH + 1))
        dump('dbg_Zs', Zs[:].rearrange("p a k n -> p (a k n)"), 2 * 16 * NCH)
        c.emit()
        return nc
    zT_s = [c.sb([128, 512], BF16, "zTs") for _ in range(2)]
    x2 = c.sb([128, 512], F32, "x2")
    wv = c.sb([128, 512], F32, "wv")
    zi_ = 0
    for s_ in range(4):
        for ti in range(NT):
            p = nps()
            tok0 = ti * 512
            yv = ps[p][:, :].rearrange("p (n j) -> p n j", j=L)
            uv = uT_all[:, s_, tok0:tok0 + 512].rearrange("p (n j) -> p n j", j=L)
            for n in range(L):
                MM(yv[:, :, n:L], TT[:, s_, n, :], uv[:, :, 0:L - n], n == 0, False, ['TT', 'uT_all'], [f'ps{p}'])
            cnt = 0
            for k4 in range(4):
                kk = 4 * s_ + k4
                for i in range(L):
                    for ri in range(2):
                        cnt += 1
                        MM(yv[32 * k4:32 * k4 + 32, :, i], CYT[:, kk, i + 1, ri, :], Hb[:, ri, kk, ti * 32:ti * 32 + 32], False, cnt == 4 * L * 2, ['CYT', 'Hb'], [f'ps{p}'], tp=(0, 32 * k4))
            zt, ztk = zT_s[zi_ % 2], f'zTs{zi_ % 2}'
            zi_ += 1
            A(lambda e, p=p: e.activation(out=x2[:], in_=ps[p][:, :], func=AF.Square), [f'ps{p}'], ['x2'])
            V(lambda e: e.tensor_scalar(out=wv[:], in0=x2[:], scalar1=0.044715, scalar2=1.0, op0=ALU.mult, op1=ALU.add), ['x2'], ['wv'])
            V(lambda e, p=p: e.tensor_tensor(out=wv[:], in0=wv[:], in1=ps[p][:, :], op=ALU.mult), ['wv', f'ps{p}'], ['wv'])
            A(lambda e: e.activation(out=wv[:], in_=wv[:], func=AF.Sigmoid, scale=1.5957691216057308), ['wv'], ['wv'])
            V(lambda e, p=p, zt=zt: e.tensor_tensor(out=zt[:], in0=wv[:], in1=ps[p][:, :], op=ALU.mult), ['wv', f'ps{p}'], [ztk])
            D(zT_d[s_ * 128:(s_ + 1) * 128, ti * 512:(ti + 1) * 512], zt[:], r=[ztk], w=['zT_d'])
    c.barrier()
    c.release(base_mark)

    if 'dbg_y' in dbg_out:
        tmpz = c.sb([128, 4, TH], BF16, "tmpz")
        tmpzf = c.sb([128, 4, TH], F32, "tmpzf")
        D(tmpz[:], zT_d.rearrange("(s p) t -> p s t", p=128), r=['zT_d'], w=['tmpz'])
        V(lambda e, tmpzf=tmpzf, tmpz=tmpz: e.tensor_copy(out=tmpzf[:], in_=tmpz[:]), ['tmpz'], ['tmpzf'])
        D(dbg_out['dbg_y'].rearrange("(s p) t -> p s t", p=128), tmpzf[:], r=['tmpzf'])
        c.barrier()
        c.release(base_mark)
    if stop == 'C':
        c.emit()
        return nc
    def load_w(dram_view, shape, key, scale_ap=None):
        wt = c.sb(shape, BF16, key)
        a_n, ncol = shape[1], shape[2]
        for a_ in range(a_n):
            lw_i[0] += 1
            lwk = 'lw_st0'
            st_ = lw_sts[lw_i[0] % 2][:, 0:ncol]
            D(st_, dram_view[:, a_, :], w=[lwk])
            if scale_ap is None:
                V(lambda e, a_=a_, st_=st_: e.tensor_copy(out=wt[:, a_, :], in_=st_), [lwk], [key])
            else:
                V(lambda e, a_=a_, st_=st_: e.tensor_scalar(out=wt[:, a_, :], in0=st_, scalar1=scale_ap[:, a_:a_ + 1], scalar2=None, op0=ALU.mult), [lwk, 'gf'], [key])
        return wt

    lw_sts = [c.sb([128, 1024], F32, "lw_st")] * 2
    lw_i = [0]
    gf = c.sb([128, 8], F32, "gf")
    D(gf[:], gffnT, w=['gf'])
    bgl = c.sb([128, 4], F32, "bgl")
    D(bgl[:], bglu, w=['bgl'])
    brt = c.sb([128, 20], F32, "brt")
    D(brt[:], br, w=['brt'])
    Wglu = load_w(w_glu.rearrange("(kc p) n -> p kc n", p=128), [128, 4, 512], 'Wglu')
    Wob = load_w(w_out_b.rearrange("(kc p) n -> p kc n", p=128), [128, 4, 1024], 'Wob')
    Woa = c.sb([64, 8, 1024], BF16, "Woa")
    woa_v = w_out_a.rearrange("(h p) n -> p h n", p=64)
    for h in range(8):
        lw_i[0] += 1
        lwk = 'lw_st0'
        lwt = lw_sts[lw_i[0] % 2]
        D(lwt[0:64, :], woa_v[:, h, :], w=[lwk])
        V(lambda e, h=h, lwt=lwt: e.tensor_copy(out=Woa[:, h, :], in_=lwt[0:64, :]), [lwk], ['Woa'])
    Wout = load_w(w_out.rearrange("(kc p) n -> p kc n", p=128), [128, 8, 1024], 'Wout')
    Wr = c.sb([128, 8, 20], F32, "Wr")
    D(Wr[:], wr.rearrange("(kc p) n -> p kc n", p=128), w=['Wr'])
    for kc in range(8):
        V(lambda e, kc=kc: e.tensor_scalar(out=Wr[:, kc, :], in0=Wr[:, kc, :], scalar1=gf[:, kc:kc + 1], scalar2=None, op0=ALU.mult), ['Wr', 'gf'], ['Wr'])

    if stop == 'D1':
        c.emit()
        return nc
    Wrhi = c.sb([128, 8, 20], BF16, "Wrhi")
    Wrlo = c.sb([128, 8, 20], BF16, "Wrlo")
    V(lambda e: e.tensor_copy(out=Wrhi[:], in_=Wr[:]), ['Wr'], ['Wrhi'])
    V(lambda e: e.tensor_tensor(out=Wr[:], in0=Wr[:], in1=Wrhi[:], op=ALU.subtract), ['Wr', 'Wrhi'], ['Wr'])
    V(lambda e: e.tensor_copy(out=Wrlo[:], in_=Wr[:]), ['Wr'], ['Wrlo'])
    h2hi = c.sb([128, 1024], BF16, "h2hi")
    h2lo = c.sb([128, 1024], BF16, "h2lo")
    hThi = c.sb([128, 8, 128], BF16, "hThi")
    hTlo = c.sb([128, 8, 128], BF16, "hTlo")
    cwb = c.sb([128, 16], BF16, "cwb")
    zT = c.sb([128, 4, 512], BF16, "zT")
    oT = c.sb([64, 8, 512], BF16, "oT")
    gT = c.sb([128, 16, 512], BF16, "gT")
    xt = c.sb([128, 4, 1024], F32, "xtD")
    zg = c.sb([128, 4, 512], BF16, "zg")
    sg = c.sb([128, 512], F32, "sg")
    mgd = c.sb([128, 8, 512], BF16, "mgd")
    ta_ = c.sb([128, 512], F32, "taD")
    tb_ = c.sb([128, 512], F32, "tbD")
    x1 = c.sb([128, 4, 1024], F32, "x1")
    h2f = c.sb([128, 1024], F32, "h2f")
    h2Tf = c.sb([128, 8, 128], F32, "h2Tf")
    h2Tb = c.sb([128, 8, 512], BF16, "h2Tb")
    cwTb = c.sb([16, 512], BF16, "cwTb")
    ss2 = c.sb([128, 4], F32, "ss2")
    junk2 = c.sb([128, 1024], BF16, "junk2")
    junk2_b = [junk2, c.sb([128, 1024], BF16, "junk2b")]
    lg = c.sb([128, 20], F32, "lg")
    cw = c.sb([128, 16], F32, "cw")
    sm = {n_: c.sb([128, 4], F32, n_) for n_ in ["gmx", "ge", "gsum", "oh", "les", "m1", "mk1", "le2", "m2", "mk2", "w12", "tmp4"]}
    one1 = {n_: c.sb([128, 1], F32, n_) for n_ in ["psel", "d21", "w1", "w2"]}
    xin_own = xin[TH:T2, :].rearrange("(t j p) d -> t p j d", j=4, p=128)
    zT_b2 = [zT, c.sb([128, 4, 512], BF16, "zT2")]
    oT_b2 = [oT, c.sb([64, 8, 512], BF16, "oT2")]
    gT_b2 = [gT, c.sb([128, 16, 512], BF16, "gT2")]
    xt_b2 = [xt, c.sb([128, 4, 1024], F32, "xtD2")]
    lg4 = c.sb([128, 4, 20], F32, "lg4")
    R4 = {n_: c.sb([128, 4, 4], F32, n_) for n_ in ["oh", "ge", "les", "mk1", "le2", "mk2", "w12", "tq"]}
    R1 = {n_: c.sb([128, 4], F32, n_) for n_ in ["mx", "gsum", "psel", "m1", "m2", "d", "w1", "w2"]}
    prod = c.sb([128, 4, 4, 4], F32, "prod")
    cw4 = c.sb([128, 4, 16], F32, "cw4")
    cwb4 = c.sb([128, 4, 16], BF16, "cwb4")
    AXX = mybir.AxisListType.X

    def bc3(ap2):
        return ap2.unsqueeze(2).broadcast_to([128, 4, 4])

    def load_tile(ti):
        b_ = ti % 2
        t0_ = ti * 512
        D(zT_b2[b_][:], zT_d.rearrange("(s p) t -> p s t", p=128)[:, :, t0_:t0_ + 512], r=['zT_d'], w=[f'zT{b_}'])
        D(oT_b2[b_][:], oT_d.rearrange("h d t -> d h t")[:, :, t0_:t0_ + 512], r=['oT_d'], w=[f'oT{b_}'])
        for hh in range(2):
            D(gT_b2[b_][:, hh * 8:(hh + 1) * 8, :], gT_d.rearrange("(m p) t -> p m t", p=128)[:, hh * 8:(hh + 1) * 8, t0_:t0_ + 512], r=['gT_d'], w=[f'gT{b_}'])
        D(xt_b2[b_][:], xin_own[ti], w=[f'xtD{b_}'])

    load_tile(0)
    for ti in range(NT):
        t0 = ti * 512
        b_ = ti % 2
        zT, oT, gT, xt = zT_b2[b_], oT_b2[b_], gT_b2[b_], xt_b2[b_]
        zTk, oTk, gTk, xtk = f'zT{b_}', f'oT{b_}', f'gT{b_}', f'xtD{b_}'
        if ti + 1 < NT:
            load_tile(ti + 1)
        for m in range(4):
            p = nps()
            for kc in range(4):
                MM(ps[p][:, :], Wglu[:, kc, m * 128:(m + 1) * 128], zT[:, kc, :], kc == 0, kc == 3, ['Wglu', zTk], [f'ps{p}'])
            A(lambda e, p=p, m=m: e.activation(out=sg[:], in_=ps[p][:, :], func=AF.Sigmoid, bias=bgl[:, m:m + 1]), [f'ps{p}', 'bgl'], ['sg'])
            V(lambda e, m=m, zT=zT: e.tensor_tensor(out=zg[:, m, :], in0=zT[:, m, :], in1=sg[:], op=ALU.mult), [zTk, 'sg'], ['zg'])
        for m in range(8):
            p = nps()
            for kc in range(4):
                MM(ps[p][:, :], Wob[:, kc, m * 128:(m + 1) * 128], zg[:, kc, :], kc == 0, kc == 3, ['Wob', 'zg'], [f'ps{p}'])
            p2 = nps()
            for h in range(8):
                MM(ps[p2][:, :], Woa[:, h, m * 128:(m + 1) * 128], oT[:, h, :], h == 0, h == 7, ['Woa', oTk], [f'ps{p2}'])
            V(lambda e, p=p, m=m, gT=gT: e.tensor_tensor(out=ta_[:], in0=gT[:, 8 + m, :], in1=ps[p][:, :], op=ALU.mult), [gTk, f'ps{p}'], ['taD'])
            V(lambda e, p2=p2, m=m, gT=gT: e.tensor_tensor(out=tb_[:], in0=gT[:, m, :], in1=ps[p2][:, :], op=ALU.mult), [gTk, f'ps{p2}'], ['tbD'])
            G(lambda e, m=m: e.tensor_tensor(out=mgd[:, m, :], in0=ta_[:], in1=tb_[:], op=ALU.add), ['taD', 'tbD'], ['mgd'])
        for j in range(4):
            for hf in range(2):
                p = nps()
                for kc in range(8):
                    MM(ps[p][:, :], mgd[:, kc, j * 128:(j + 1) * 128], Wout[:, kc, hf * 512:(hf + 1) * 512], kc == 0, kc == 7, ['mgd', 'Wout'], [f'ps{p}'])
                V(lambda e, p=p, j=j, hf=hf, xt=xt: e.tensor_tensor(out=x1[:, j, hf * 512:(hf + 1) * 512], in0=xt[:, j, hf * 512:(hf + 1) * 512], in1=ps[p][:, :], op=ALU.add), [xtk, f'ps{p}'], ['x1'])
        D(x1_d[t0:t0 + 512, :].rearrange("(j p) d -> p j d", p=128), x1[:], r=['x1'], w=['x1_d'])
        for j in range(4):
            jk_, jkk_ = junk2_b[j % 2], f'junk2{j % 2}'
            A(lambda e, j=j, jk_=jk_: e.activation(out=jk_[:], in_=x1[:, j, :], func=AF.Square), ['x1'], [jkk_])
            V(lambda e, j=j, jk_=jk_: e.tensor_reduce(out=ss2[:, j:j + 1], in_=jk_[:], axis=mybir.AxisListType.X, op=ALU.add), [jkk_], ['Ess'])
        m0 = c.mark()
        rs2 = rstd_of(ss2, 4, 'E')
        for j in range(4):
            A(lambda e, j=j, rs2=rs2: e.activation(out=h2f[:], in_=x1[:, j, :], func=AF.Copy, scale=rs2[:, j:j + 1]), ['x1', 'Ers'], ['h2f'])
            G(lambda e: e.tensor_copy(out=h2hi[:], in_=h2f[:]), ['h2f'], ['h2hi'])
            G(lambda e: e.tensor_tensor(out=h2lo[:], in0=h2f[:], in1=h2hi[:], op=ALU.subtract), ['h2f', 'h2hi'], ['h2lo'])
            for srct, srck, dst, dstk, pbk in ((h2hi, 'h2hi', hThi, 'hThi', 0), (h2lo, 'h2lo', hTlo, 'hTlo', 1)):
                for kc in range(8):
                    TR(pb[pbk][:, kc * 128:(kc + 1) * 128], srct[:, kc * 128:(kc + 1) * 128], identb[:], [srck, 'identb'], [f'pb{pbk}'])
                A(lambda e, dst=dst, pbk=pbk: e.activation(out=dst[:], in_=pb[pbk][:, :].rearrange("p (k t) -> p k t", k=8), func=AF.Copy), [f'pb{pbk}'], [dstk])
            G(lambda e, j=j: e.tensor_copy(out=h2Tb[:, :, j * 128:(j + 1) * 128], in_=hThi[:]), ['hThi'], ['h2Tb'])
            p = nps()
            nmm = 0
            for (lt, ltk, wt_, wtk) in ((hThi, 'hThi', Wrhi, 'Wrhi'), (hTlo, 'hTlo', Wrhi, 'Wrhi'), (hThi, 'hThi', Wrlo, 'Wrlo')):
                for kc in range(8):
                    MM(ps[p][:, 0:20], lt[:, kc, :], wt_[:, kc, :], nmm == 0, nmm == 23, [ltk, wtk], [f'ps{p}'])
                    nmm += 1
            V(lambda e, p=p, j=j: e.tensor_tensor(out=lg4[:, j, :], in0=ps[p][:, 0:20], in1=brt[:], op=ALU.add), [f'ps{p}', 'brt'], ['lg4'])
        c.release(m0)
        gl = lg4[:, :, 0:4]
        le4 = lg4[:, :, 4:20].rearrange("p j (g e) -> p j g e", g=4)
        V(lambda e: e.tensor_reduce(out=R1["mx"][:], in_=gl, axis=AXX, op=ALU.max), ['lg4'], ['r_mx'])
        V(lambda e: e.tensor_tensor(out=R4["oh"][:], in0=gl, in1=bc3(R1["mx"][:, :]), op=ALU.is_ge), ['lg4', 'r_mx'], ['r_oh'])
        V(lambda e: e.tensor_tensor(out=R4["ge"][:], in0=gl, in1=bc3(R1["mx"][:, :]), op=ALU.subtract), ['lg4', 'r_mx'], ['r_ge'])
        A(lambda e: e.activation(out=R4["ge"][:], in_=R4["ge"][:], func=AF.Exp), ['r_ge'], ['r_ge'])
        V(lambda e: e.tensor_reduce(out=R1["gsum"][:], in_=R4["ge"][:], axis=AXX, op=ALU.add), ['r_ge'], ['r_gsum'])
        V(lambda e: e.reciprocal(out=R1["psel"][:], in_=R1["gsum"][:]), ['r_gsum'], ['r_psel'])
        V(lambda e: e.tensor_tensor(out=prod[:], in0=le4, in1=R4["oh"][:, :, :].unsqueeze(3).broadcast_to([128, 4, 4, 4]), op=ALU.mult), ['lg4', 'r_oh'], ['r_prod'])
        V(lambda e: e.tensor_reduce(out=R4["les"][:], in_=prod[:].rearrange("p j g e -> p j e g"), axis=AXX, op=ALU.add), ['r_prod'], ['r_les'])
        V(lambda e: e.tensor_reduce(out=R1["m1"][:], in_=R4["les"][:], axis=AXX, op=ALU.max), ['r_les'], ['r_m1'])
        V(lambda e: e.tensor_tensor(out=R4["mk1"][:], in0=R4["les"][:], in1=bc3(R1["m1"][:, :]), op=ALU.is_ge), ['r_les', 'r_m1'], ['r_mk1'])
        V(lambda e: e.scalar_tensor_tensor(out=R4["le2"][:], in0=R4["mk1"][:], scalar=-1e30, in1=R4["les"][:], op0=ALU.mult, op1=ALU.add), ['r_mk1', 'r_les'], ['r_le2'])
        V(lambda e: e.tensor_reduce(out=R1["m2"][:], in_=R4["le2"][:], axis=AXX, op=ALU.max), ['r_le2'], ['r_m2'])
        V(lambda e: e.tensor_tensor(out=R4["mk2"][:], in0=R4["le2"][:], in1=bc3(R1["m2"][:, :]), op=ALU.is_ge), ['r_le2', 'r_m2'], ['r_mk2'])
        V(lambda e: e.tensor_tensor(out=R1["d"][:], in0=R1["m2"][:], in1=R1["m1"][:], op=ALU.subtract), ['r_m1', 'r_m2'], ['r_d'])
        A(lambda e: e.activation(out=R1["d"][:], in_=R1["d"][:], func=AF.Exp), ['r_d'], ['r_d'])
        V(lambda e: e.tensor_scalar(out=R1["d"][:], in0=R1["d"][:], scalar1=1.0, scalar2=None, op0=ALU.add), ['r_d'], ['r_d'])
        V(lambda e: e.reciprocal(out=R1["w1"][:], in_=R1["d"][:]), ['r_d'], ['r_w1'])
        V(lambda e: e.tensor_scalar(out=R1["w2"][:], in0=R1["w1"][:], scalar1=-1.0, scalar2=1.0, op0=ALU.mult, op1=ALU.add), ['r_w1'], ['r_w2'])
        V(lambda e: e.tensor_tensor(out=R1["w1"][:], in0=R1["w1"][:], in1=R1["psel"][:], op=ALU.mult), ['r_w1', 'r_psel', 'r_w2'], ['r_w1'])
        V(lambda e: e.tensor_tensor(out=R1["w2"][:], in0=R1["w2"][:], in1=R1["psel"][:], op=ALU.mult), ['r_w2', 'r_psel'], ['r_w2'])
        V(lambda e: e.tensor_tensor(out=R4["w12"][:], in0=R4["mk1"][:], in1=bc3(R1["w1"][:, :]), op=ALU.mult), ['r_mk1', 'r_w1'], ['r_w12'])
        V(lambda e: e.tensor_tensor(out=R4["tq"][:], in0=R4["mk2"][:], in1=bc3(R1["w2"][:, :]), op=ALU.mult), ['r_mk2', 'r_w2'], ['r_tq'])
        V(lambda e: e.tensor_tensor(out=R4["w12"][:], in0=R4["w12"][:], in1=R4["tq"][:], op=ALU.add), ['r_w12', 'r_tq'], ['r_w12'])
        V(lambda e: e.tensor_tensor(out=cw4[:].rearrange("p j (g e) -> p j g e", g=4), in0=R4["oh"][:, :, :].unsqueeze(3).broadcast_to([128, 4, 4, 4]),
                                    in1=R4["w12"][:, :, :].unsqueeze(2).broadcast_to([128, 4, 4, 4]), op=ALU.mult), ['r_oh', 'r_w12'], ['cw4'])
        V(lambda e: e.tensor_copy(out=cwb4[:], in_=cw4[:]), ['cw4'], ['cwb4'])
        for j in range(4):
            TR(pb[0][0:16, 0:128], cwb4[:, j, :], identb[:], ['cwb4', 'identb'], ['pb0'])
            V(lambda e, j=j: e.tensor_copy(out=cwTb[:, j * 128:(j + 1) * 128], in_=pb[0][0:16, 0:128]), ['pb0'], ['cwTb'])
        D(h2T_d.rearrange("(kc p) t -> p kc t", p=128)[:, :, t0:t0 + 512], h2Tb[:], r=['h2Tb'], w=['h2T_d'])
        D(cwT_d[:, t0:t0 + 512], cwTb[:], r=['cwTb'], w=['cwT_d'])
    c.barrier()
    c.release(base_mark)
    if 'dbg_x1' in dbg_out:
        tmpx = c.sb([128, TH // 128, 1024], F32, "tmpx")
        D(tmpx[:], x1_d.rearrange("(j p) d -> p j d", p=128), r=['x1_d'], w=['tmpx'])
        D(dbg_out['dbg_x1'].rearrange("(j p) d -> p j d", p=128), tmpx[:], r=['tmpx'])
        tmpc = c.sb([16, TH], BF16, "tmpc"); tmpcf = c.sb([16, TH], F32, "tmpcf")
        D(tmpc[:], cwT_d, r=['cwT_d'], w=['tmpc'])
        V(lambda e, tmpcf=tmpcf, tmpc=tmpc: e.tensor_copy(out=tmpcf[:], in_=tmpc[:]), ['tmpc'], ['tmpcf'])
        D(dbg_out['dbg_cw'], tmpcf[:], r=['tmpcf'])
        c.barrier()
        c.release(base_mark)

    if stop == 'D':
        c.emit()
        return nc
    ST = min(TH, 1024)
    NSB = ST // 128
    gf2 = c.sb([128, 8], F32, "gf2")
    D(gf2[:], gffnT, w=['gf2'])
    selb = c.sb([16, 2048], BF16, "selb")
    m_ = c.mark()
    selst = c.sb([16, 2048], F32, "selst")
    D(selst[:], sel_d, w=['selst'])
    V(lambda e: e.tensor_copy(out=selb[:], in_=selst[:]), ['selst'], ['selb'])
    gfin_t = c.sb([128, 1024], F32, "gfin")
    D(gfin_t[:], gfin, w=['gfin'])
    h2T = c.sb([128, 8, ST], BF16, "h2T")
    cwT = c.sb([16, ST], BF16, "cwT")
    acc = c.sb([128, NSB, 1024], F32, "acc")
    Wg_b = [c.sb([128, 8, 256], BF16, "Wg") for _ in range(2)]
    Wu_b = [c.sb([128, 8, 256], BF16, "Wu") for _ in range(2)]
    Wd_b = [c.sb([128, 2, 1024], BF16, "Wd") for _ in range(2)]
    wstg = [[c.sb([128, 8, 256], F32, "wstg") for _ in range(2)] for _ in range(2)]
    wstd = [c.sb([128, 2, 1024], F32, "wstd") for _ in range(2)]
    bcs_b = [c.sb([128, 512], BF16, "bcs") for _ in range(2)]
    sgl_b = [c.sb([128, 512], F32, "sgl") for _ in range(2)]
    tmu_b = [c.sb([128, 512], F32, "tmu") for _ in range(2)]
    actT_b = [c.sb([128, 2, 512], BF16, "actT") for _ in range(2)]
    x1f = c.sb([128, 1024], F32, "x1f")
    ssf = c.sb([128, 1], F32, "ssf")
    junk3 = c.sb([128, 1024], BF16, "junk3")
    outt = [c.sb([128, 1024], F32, "outt") for _ in range(2)]
    NST = TH // ST
    NSUB = ST // 512

    def load_expert(n):
        ex, sl = n % 16, n % 2
        Wg, Wu, Wd = Wg_b[sl], Wu_b[sl], Wd_b[sl]
        wk = f'We{sl}'
        D(wstg[sl][0][:], w_eg[ex].rearrange("(kc p) f -> p kc f", p=128), w=[f'wstg{sl}0'])
        D(wstg[sl][1][:], w_eu[ex].rearrange("(kc p) f -> p kc f", p=128), w=[f'wstg{sl}1'])
        D(wstd[sl][:], w_ed[ex].rearrange("(fc p) d -> p fc d", p=128), w=[f'wstd{sl}'])
        for kc in range(8):
            A(lambda e, kc=kc, Wg=Wg, sl=sl: e.activation(out=Wg[:, kc, :], in_=wstg[sl][0][:, kc, :], func=AF.Copy, scale=gf2[:, kc:kc + 1]), [f'wstg{sl}0', 'gf2'], [wk])
            A(lambda e, kc=kc, Wu=Wu, sl=sl: e.activation(out=Wu[:, kc, :], in_=wstg[sl][1][:, kc, :], func=AF.Copy, scale=gf2[:, kc:kc + 1]), [f'wstg{sl}1', 'gf2'], [wk])
        A(lambda e, Wd=Wd, sl=sl: e.activation(out=Wd[:, 0, :], in_=wstd[sl][:, 0, :], func=AF.Copy), [f'wstd{sl}'], [wk])
        V(lambda e, Wd=Wd, sl=sl: e.tensor_copy(out=Wd[:, 1, :], in_=wstd[sl][:, 1, :]), [f'wstd{sl}'], [wk])

    ucount = [0]

    def moe_stage1(n, sub):
        ex, sl = n % 16, n % 2
        Wg, Wu = Wg_b[sl], Wu_b[sl]
        wk = f'We{sl}'
        ub = ucount[0] % 2
        ucount[0] += 1
        bcs, sgl, tmu, actT = bcs_b[ub], sgl_b[ub], tmu_b[ub], actT_b[ub]
        q0 = sub * 512
        p = nps()
        MM(ps[p][:, :], selb[:, ex * 128:(ex + 1) * 128], cwT[:, q0:q0 + 512], True, True, ['selb', 'cwT'], [f'ps{p}'])
        A(lambda e, p=p, bcs=bcs: e.activation(out=bcs[:], in_=ps[p][:, :], func=AF.Copy), [f'ps{p}'], [f'bcs{ub}'])
        for fc in range(2):
            pg = nps()
            for kc in range(8):
                MM(ps[pg][:, :], Wg[:, kc, fc * 128:(fc + 1) * 128], h2T[:, kc, q0:q0 + 512], kc == 0, kc == 7, [wk, 'h2T'], [f'ps{pg}'])
            pu = nps()
            for kc in range(8):
                MM(ps[pu][:, :], Wu[:, kc, fc * 128:(fc + 1) * 128], h2T[:, kc, q0:q0 + 512], kc == 0, kc == 7, [wk, 'h2T'], [f'ps{pu}'])
            A(lambda e, pg=pg, sgl=sgl: e.activation(out=sgl[:], in_=ps[pg][:, :], func=AF.Silu), [f'ps{pg}'], [f'sgl{ub}'])
            V(lambda e, pu=pu, sgl=sgl, tmu=tmu: e.tensor_tensor(out=tmu[:], in0=sgl[:], in1=ps[pu][:, :], op=ALU.mult), [f'sgl{ub}', f'ps{pu}'], [f'tmu{ub}'])
            G(lambda e, fc=fc, tmu=tmu, bcs=bcs, actT=actT: e.tensor_tensor(out=actT[:, fc, :], in0=tmu[:], in1=bcs[:], op=ALU.mult), [f'tmu{ub}', f'bcs{ub}'], [f'actT{ub}'])
        return ub

    def moe_stage2(n, sub, ub):
        sl = n % 2
        Wd = Wd_b[sl]
        wk = f'We{sl}'
        actT = actT_b[ub]
        for j in range(4):
            blk = sub * 4 + j
            for hf in range(2):
                p = nps()
                for fc in range(2):
                    MM(ps[p][:, :], actT[:, fc, j * 128:(j + 1) * 128], Wd[:, fc, hf * 512:(hf + 1) * 512], fc == 0, fc == 1, [f'actT{ub}', wk], [f'ps{p}'])
                V(lambda e, p=p, blk=blk, hf=hf: e.tensor_tensor(out=acc[:, blk, hf * 512:(hf + 1) * 512], in0=acc[:, blk, hf * 512:(hf + 1) * 512], in1=ps[p][:, :], op=ALU.add), ['acc', f'ps{p}'], ['acc'])

    load_expert(0)
    for sti in range(NST):
        s0 = sti * ST
        D(h2T[:], h2T_d.rearrange("(kc p) t -> p kc t", p=128)[:, :, s0:s0 + ST], r=['h2T_d'], w=['h2T'])
        D(cwT[:], cwT_d[:, s0:s0 + ST], r=['cwT_d'], w=['cwT'])
        G(lambda e: e.memset(acc[:], 0.0), (), ['acc'])
        units = [(sti * 16 + ex, sub) for ex in range(16) for sub in range(NSUB)]
        prev = None
        for i in range(len(units) + 1):
            cur = None
            if i < len(units):
                n, sub = units[i]
                ub = moe_stage1(n, sub)
                cur = (n, sub, ub)
            if prev is not None:
                moe_stage2(*prev)
            if cur is not None and cur[1] == 0 and cur[0] + 1 < NST * 16:
                load_expert(cur[0] + 1)
            prev = cur
        for blk in range(NSB):
            r0 = s0 + blk * 128
            ot, otk = outt[blk % 2], f'outt{blk % 2}'
            D(x1f[:], x1_d[r0:r0 + 128, :], r=['x1_d'], w=['x1f'])
            V(lambda e, blk=blk: e.tensor_tensor(out=x1f[:], in0=x1f[:], in1=acc[:, blk, :], op=ALU.add), ['x1f', 'acc'], ['x1f'])
            A(lambda e: e.activation(out=junk3[:], in_=x1f[:], func=AF.Square), ['x1f'], ['junk3'])
            V(lambda e: e.tensor_reduce(out=ssf[:, 0:1], in_=junk3[:], axis=mybir.AxisListType.X, op=ALU.add), ['junk3'], ['Fss'])
            m0 = c.mark()
            rsf = rstd_of(ssf, 1, 'F')
            V(lambda e, ot=ot, rsf=rsf: e.scalar_tensor_tensor(out=ot[:], in0=x1f[:], scalar=rsf[:, 0:1], in1=gfin_t[:], op0=ALU.mult, op1=ALU.mult), ['x1f', 'Frs', 'gfin'], [otk])
            c.release(m0)
            D(out_d[r0:r0 + 128, :], ot[:], r=[otk], w=['out_d'])
    c.emit()
    return nc


def _ssm_layouts(lambda_re, lambda_im, log_step, b_re, b_im, c_re, c_im, ssm_d):
    f = np.float32
    o = {}
    r = np.arange(128)
    k4_r, mp_r, cp_r = r // 32, (r // 16) % 2, r % 16
    s_ = np.arange(4)
    q = np.arange(128)
    m_q, p_q = q // 64, q % 64
    g = 8 * s_[None, :, None] + 2 * k4_r[:, None, None] + m_q[None, None, :]
    P = np.broadcast_to(p_q[None, None, :], g.shape)
    o["LRX"] = lambda_re[g, P].reshape(128, 512).astype(f)
    o["LIX"] = lambda_im[g, P].reshape(128, 512).astype(f)
    o["LSX"] = log_step[g].reshape(128, 512).astype(f)
    msk = (mp_r[:, None, None] == m_q[None, None, :])
    CP = np.broadcast_to(cp_r[:, None, None], g.shape)
    o["BRX"] = np.where(msk, b_re[g, P, CP], 0).reshape(128, 512).astype(f)
    o["BIX"] = np.where(msk, b_im[g, P, CP], 0).reshape(128, 512).astype(f)
    k = np.arange(16)
    j = np.arange(32)
    mp_j, c_j = j // 16, j % 16
    g2 = 2 * k[None, :, None] + m_q[:, None, None] + 0 * j[None, None, :]
    P2 = np.broadcast_to(p_q[:, None, None], g2.shape)
    C2 = np.broadcast_to(c_j[None, None, :], g2.shape)
    msk2 = (mp_j[None, None, :] == m_q[:, None, None])
    o["LRY"] = lambda_re[g2, P2].reshape(128, 512).astype(f)
    o["LIY"] = lambda_im[g2, P2].reshape(128, 512).astype(f)
    o["LSY"] = log_step[g2].reshape(128, 512).astype(f)
    o["CRY"] = np.where(msk2, c_re[g2, C2, P2], 0).reshape(128, 512).astype(f)
    o["CIY"] = np.where(msk2, c_im[g2, C2, P2], 0).reshape(128, 512).astype(f)
    o["BRY"] = np.where(msk2, b_re[g2, P2, C2], 0).reshape(128, 512).astype(f)
    o["BIY"] = np.where(msk2, b_im[g2, P2, C2], 0).reshape(128, 512).astype(f)
    dd = np.zeros((128, 4, 128), f)
    for s in range(4):
        dd[r, s, r] = ssm_d[128 * s + r]
    o["DDG"] = dd.reshape(128, 512)
    return o


_CACHE = {}


def _prep_common(inp):
    f = np.float32
    A_ = lambda a: np.ascontiguousarray(a, dtype=f)
    d = {}
    d["w_in"] = A_(inp["w_in"][0])
    d["gmixT"] = A_(inp["g_mix"][0].reshape(8, 128).T)
    d["bforget"] = A_(inp["b_forget"][0].reshape(8, 1))
    d["bgate"] = A_(inp["b_gate"][0].reshape(16, 128).T)
    d["w_out_a"] = A_(inp["w_out_a"][0])
    d["w_glu"] = A_(inp["w_glu"][0])
    d["bglu"] = A_(inp["b_glu"][0].reshape(4, 128).T)
    d["w_out_b"] = A_(inp["w_out_b"][0])
    d["w_out"] = A_(inp["w_out"][0])
    d["gffnT"] = A_(inp["g_ffn"][0].reshape(8, 128).T)
    d["wr"] = A_(np.concatenate([inp["w_router_group"][0], inp["w_router_expert"][0]], axis=1))
    d["br"] = A_(np.broadcast_to(np.concatenate([inp["b_router_group"][0], inp["b_router_expert"][0]])[None, :], (128, 20)))
    d["w_eg"] = A_(inp["w_exp_gate"][0])
    d["w_eu"] = A_(inp["w_exp_up"][0])
    d["w_ed"] = A_(inp["w_exp_down"][0])
    d["gfin"] = A_(np.broadcast_to(inp["g_final"][None, :], (128, 1024)))
    d.update(_ssm_layouts(np.asarray(inp["lambda_re"][0]), np.asarray(inp["lambda_im"][0]), np.asarray(inp["log_step"][0]),
                          np.asarray(inp["ssm_b_re"][0]), np.asarray(inp["ssm_b_im"][0]), np.asarray(inp["ssm_c_re"][0]),
                          np.asarray(inp["ssm_c_im"][0]), np.asarray(inp["ssm_d"][0])))
    d["ident"] = np.eye(128, dtype=f)
    d["tri"] = np.triu(np.ones((128, 128), f))
    sel = np.zeros((16, 16, 128), f)
    for e in range(16):
        sel[e, e, :] = 1.0
    d["sel"] = sel.reshape(16, 2048)
    return d


def run(inputs, dbg=(), stop=None):
    inp = {k: np.asarray(v) for k, v in inputs.items()}
    x = inp["x"]
    B, S, _ = x.shape
    TH = S // 2
    key = (TH, tuple(dbg), stop)
    if key not in _CACHE:
        _CACHE[key] = build_program(TH, dbg, stop)
    nc = _CACHE[key]
    common = _prep_common(inp)
    in_maps = []
    for core in range(8):
        b, par = core // 2, core % 2
        xin = np.zeros((S, 1024), np.float32)
        if par == 1:
            xin[:TH] = x[b, :TH]
        xin[TH:] = x[b, par * TH:(par + 1) * TH]
        m = dict(common)
        m["xin"] = xin
        m["flag"] = np.full((128, 1), float(par), np.float32)
        in_maps.append(m)
    res = run_bass_kernel_spmd(nc, in_maps, core_ids=list(range(8)))
    return res, TH


def kernel(**inputs):
    res, TH = run(inputs)
    x = inputs["x"]
    B, S, Dm = x.shape
    out = np.zeros((B, S, Dm), np.float32)
    for core in range(8):
        b, par = core // 2, core % 2
        out[b, par * TH:(par + 1) * TH] = res.results[core]["out"]
    return out
```

```python
import contextlib
import numpy as np
import concourse.bass as bass
import concourse.mybir as mybir
from concourse.bass_utils import run_bass_kernel_spmd

F32 = mybir.dt.float32
BF16 = mybir.dt.bfloat16
I32 = mybir.dt.int32
AF = mybir.ActivationFunctionType
ALU = mybir.AluOpType

NDSEM = 48
SAME_SYNC = {'pe': False, 'act': True, 'dve': True, 'pool': True, 'sp': False}
EPS = 1e-6
L = 16
TWO_PI = 6.283185307179586


class Ctx:
    def __init__(self, nc):
        self.nc = nc
        self.names = ['pe', 'act', 'dve', 'pool', 'sp']
        self.ops = {e: [] for e in self.names}
        self.cnt = {e: 0 for e in self.names}
        self.seen = {e: {} for e in self.names}
        self.pending = {e: {} for e in self.names}
        self.lastw = {}
        self.readers = {}
        self.dval = [0] * NDSEM
        self.dnext = 0
        self.sb_off = 16640
        self.uid = 0
        self.sb_max = 0

    def sb(self, shape, dtype, name="t"):
        esz = {F32: 4, BF16: 2, I32: 4}[dtype]
        n = 1
        for s in shape[1:]:
            n *= s
        off = (self.sb_off + 63) // 64 * 64
        self.sb_off = off + n * esz
        self.sb_max = max(self.sb_max, self.sb_off)
        assert self.sb_off <= 229376, f"SBUF overflow {self.sb_off} ({name})"
        self.uid += 1
        return self.nc.alloc_sbuf_tensor_at(f"{name}_{self.uid}", list(shape), dtype, offset=off)

    def mark(self):
        return self.sb_off

    def release(self, m):
        self.sb_off = m

    def _deps(self, reads, writes):
        deps = {}
        for k in reads:
            t = self.lastw.get(k)
            if t and deps.get(t[0], 0) < t[1]:
                deps[t[0]] = t[1]
        for k in writes:
            t = self.lastw.get(k)
            if t and deps.get(t[0], 0) < t[1]:
                deps[t[0]] = t[1]
            for s, v in self.readers.get(k, {}).items():
                if deps.get(s, 0) < v:
                    deps[s] = v
        return deps

    def _waits(self, e, deps):
        for s, v in self.pending[e].items():
            if deps.get(s, 0) < v:
                deps[s] = v
        self.pending[e] = {}
        waits = []
        for s, v in deps.items():
            if s == e and not SAME_SYNC[e]:
                continue
            if self.seen[e].get(s, 0) >= v:
                continue
            self.seen[e][s] = v
            waits.append((s, v))
        return waits

    def _commit(self, tok, reads, writes):
        for k in reads:
            r = self.readers.setdefault(k, {})
            if r.get(tok[0], 0) < tok[1]:
                r[tok[0]] = tok[1]
        for k in writes:
            self.lastw[k] = tok
            self.readers[k] = {}

    def op(self, e, fn, reads=(), writes=()):
        deps = self._deps(reads, writes)
        waits = self._waits(e, deps)
        self.cnt[e] += 1
        tok = (e, self.cnt[e])
        self.ops[e].append((waits, fn, e))
        self._commit(tok, reads, writes)
        return tok

    def dma(self, e, out, in_, reads=(), writes=()):
        i = self.dnext
        self.dnext = (self.dnext + 1) % NDSEM
        deps = self._deps(reads, writes)
        if self.dval[i] > 0:
            s = ('d', i)
            if deps.get(s, 0) < self.dval[i]:
                deps[s] = self.dval[i]
        waits = self._waits(e, deps)
        self.dval[i] += 16
        tok = (('d', i), self.dval[i])
        self.ops[e].append((waits, lambda eng: eng.dma_start(out=out, in_=in_), ('d', i)))
        self._commit(tok, reads, writes)
        return tok

    def barrier(self):
        allt = {e: self.cnt[e] for e in self.names if self.cnt[e] > 0}
        for i in range(NDSEM):
            if self.dval[i] > 0:
                allt[('d', i)] = self.dval[i]
        for e in self.names:
            for s, v in allt.items():
                if self.pending[e].get(s, 0) < v:
                    self.pending[e][s] = v

    def emit(self):
        nc = self.nc
        self.barrier()
        self.op('sp', lambda eng: eng.nop(), (), ())
        with contextlib.ExitStack() as st:
            sems = {e: st.enter_context(nc.semaphore(f"s_{e}")) for e in self.names}
            for i in range(NDSEM):
                sems[('d', i)] = st.enter_context(nc.semaphore(f"d_{i}"))
            block = st.enter_context(nc.Block())

            def run(e, eng):
                for waits, fn, inc in self.ops[e]:
                    for s, v in waits:
                        eng.wait_ge(sems[s], v)
                    ins = fn(eng)
                    if isinstance(inc, tuple):
                        ins.then_inc(sems[inc], 16)
                    else:
                        ins.then_inc(sems[inc], 1)

            @block.sync
            def _(eng):
                run('sp', eng)

            @block.tensor
            def _(eng):
                run('pe', eng)

            @block.scalar
            def _(eng):
                run('act', eng)

            @block.vector
            def _(eng):
                run('dve', eng)

            @block.gpsimd
            def _(eng):
                run('pool', eng)


def build_program(TH, dbg=(), stop=None):
    nc = bass.Bass("TRN2", target_bir_lowering=False)
    T2 = 2 * TH
    NT = TH // 512
    NBH = TH // 128
    NCH = TH // L
    c = Ctx(nc)

    def din(name, shape, dt=F32):
        return nc.dram_tensor(name, list(shape), dt, kind="ExternalInput").ap()

    def dscr(name, shape, dt):
        return nc.dram_tensor(name, list(shape), dt).ap()

    xin = din("xin", [T2, 1024])
    flag_d = din("flag", [128, 1])
    w_in = din("w_in", [1024, 4104])
    gmixT = din("gmixT", [128, 8])
    bforget = din("bforget", [8, 1])
    bgate = din("bgate", [128, 16])
    w_out_a = din("w_out_a", [512, 1024])
    w_glu = din("w_glu", [512, 512])
    bglu = din("bglu", [128, 4])
    w_out_b = din("w_out_b", [512, 1024])
    w_out = din("w_out", [1024, 1024])
    gffnT = din("gffnT", [128, 8])
    wr = din("wr", [1024, 20])
    br = din("br", [128, 20])
    w_eg = din("w_eg", [16, 1024, 256])
    w_eu = din("w_eu", [16, 1024, 256])
    w_ed = din("w_ed", [16, 256, 1024])
    gfin = din("gfin", [128, 1024])
    ssm_names = ["LRX", "LIX", "LSX", "BRX", "BIX", "LRY", "LIY", "LSY", "CRY", "CIY", "BRY", "BIY"]
    ssm_in = {n: din(n, [128, 512]) for n in ssm_names}
    ddg = din("DDG", [128, 512])
    ident_d = din("ident", [128, 128])
    tri_d = din("tri", [128, 128])
    sel_d = din("sel", [16, 2048])
    out_d = nc.dram_tensor("out", [TH, 1024], F32, kind="ExternalOutput").ap()
    dbg_out = {}
    for name, shape in dbg:
        dbg_out[name] = nc.dram_tensor(name, list(shape), F32, kind="ExternalOutput").ap()

    kT_d = dscr("kT_d", [8, 70, T2], BF16)
    qT_d = dscr("qT_d", [8, 70, TH], BF16)
    v_d = dscr("v_d", [T2, 520], BF16)
    uT_d = dscr("uT_d", [512, T2], BF16)
    gT_d = dscr("gT_d", [2048, TH], BF16)
    oT_d = dscr("oT_d", [8, 64, TH], BF16)
    zT_d = dscr("zT_d", [512, TH], BF16)
    x1_d = dscr("x1_d", [TH, 1024], F32)
    h2T_d = dscr("h2T_d", [1024, TH], BF16)
    cwT_d = dscr("cwT_d", [16, TH], BF16)

    ps = [nc.alloc_psum_tensor(f"ps{i}", [128, 512], F32) for i in range(6)]
    pb = [nc.alloc_psum_tensor(f"pb{i}", [128, 1024], BF16) for i in range(2)]

    def V(fn, r=(), w=()):
        return c.op('dve', fn, r, w)

    def A(fn, r=(), w=()):
        return c.op('act', fn, r, w)

    def G(fn, r=(), w=()):
        return c.op('pool', fn, r, w)

    def MM(out, lhsT, rhs, st, sp_, r, w, tp=None):
        kw = dict(start=st, stop=sp_)
        if tp is not None:
            kw['tile_position'] = tp
        return c.op('pe', lambda e: e.matmul(out, lhsT=lhsT, rhs=rhs, **kw), r, w)

    def TR(out, in_, ident, r, w):
        return c.op('pe', lambda e: e.transpose(out, in_, ident), r, w)

    def D(out, in_, r=(), w=(), q='sp'):
        return c.dma(q, out, in_, r, w)


    dump_i = [0]
    dump_st = []

    def dump(name, ap2d, ncols):
        if name not in dbg_out:
            return
        for c0 in range(0, ncols, 2048):
            n = min(2048, ncols - c0)
            dump_i[0] += 1
            kx = 'dump0'
            if not dump_st:
                dump_st.append(c.sb([128, 2048], F32, "dumpst"))
            stt = dump_st[0]
            V(lambda e, stt=stt, c0=c0, n=n: e.tensor_copy(out=stt[:, 0:n], in_=ap2d[:, c0:c0 + n]), [], [kx])
            D(dbg_out[name][:, c0:c0 + n], stt[:, 0:n], r=[kx])
    identf = c.sb([128, 128], F32, "identf")
    identb = c.sb([128, 128], BF16, "identb")
    trib = c.sb([128, 128], BF16, "trib")
    onesf = c.sb([128, 512], F32, "onesf")
    flag = c.sb([128, 1], F32, "flag")
    stg = c.sb([128, 128], F32, "stg")
    D(identf[:], ident_d, w=['identf'])
    D(stg[:], tri_d, w=['stg'])
    D(flag[:], flag_d, w=['flag'])
    V(lambda e: e.tensor_copy(out=identb[:], in_=identf[:]), ['identf'], ['identb'])
    V(lambda e: e.tensor_copy(out=trib[:], in_=stg[:]), ['stg'], ['trib'])
    V(lambda e: e.memset(onesf[:], 1.0), (), ['onesf'])
    base_mark = c.mark()

    def rstd_of(ss, n, tag):
        ms = c.sb([128, n], F32, "ms")
        rs = c.sb([128, n], F32, "rs")
        V(lambda e: e.tensor_scalar(out=ms[:], in0=ss[:], scalar1=1.0 / 1024, scalar2=EPS, op0=ALU.mult, op1=ALU.add), [tag + 'ss'], [tag + 'ms'])
        A(lambda e: e.activation(out=ms[:], in_=ms[:], func=AF.Sqrt), [tag + 'ms'], [tag + 'ms'])
        V(lambda e: e.reciprocal(out=rs[:], in_=ms[:]), [tag + 'ms'], [tag + 'rs'])
        return rs

    Win = c.sb([128, 8, 4104], BF16, "Win")
    gm = c.sb([128, 8], F32, "gm")
    negb = c.sb([8, 1], F32, "negb")
    bg = c.sb([128, 16], F32, "bg")
    D(gm[:], gmixT, w=['gm'])
    D(negb[:], bforget, w=['negb'])
    D(bg[:], bgate, w=['bg'])
    V(lambda e: e.tensor_scalar(out=negb[:], in0=negb[:], scalar1=-1.0, scalar2=None, op0=ALU.mult), ['negb'], ['negb'])
    wst = [c.sb([128, 8, 256], F32, "wst")] * 2
    w_in_v = w_in.rearrange("(kc p) n -> p kc n", p=128)
    ei = 0
    for cc in range(17):
        c0 = cc * 256
        ncol = min(256, 4104 - c0)
        st = wst[cc % 2]
        sk = 'wst'
        D(st[:, :, 0:ncol], w_in_v[:, :, c0:c0 + ncol], w=[sk])
        for kc in range(8):
            if cc % 2 == 0:
                A(lambda e, st=st, kc=kc, c0=c0, ncol=ncol: e.activation(out=Win[:, kc, c0:c0 + ncol], in_=st[:, kc, 0:ncol], func=AF.Copy, scale=gm[:, kc:kc + 1]), [sk, 'gm'], ['Win0'])
            else:
                V(lambda e, st=st, kc=kc, c0=c0, ncol=ncol: e.tensor_scalar(out=Win[:, kc, c0:c0 + ncol], in0=st[:, kc, 0:ncol], scalar1=gm[:, kc:kc + 1], scalar2=None, op0=ALU.mult), [sk, 'gm'], ['Win1'])
            ei += 1

    CQ, CK, CV, CF, CU, CG = 0, 512, 1024, 1536, 1544, 2056
    xt_b = [c.sb([128, 4, 1024], F32, "xt") for _ in range(2)]
    xs = c.sb([128, 4, 1024], BF16, "xs")
    hT_b = [c.sb([128, 8, 512], BF16, "hT") for _ in range(2)]
    junk = c.sb([128, 1024], BF16, "junk")
    junk_b = [junk, c.sb([128, 1024], BF16, "junkb")]
    ss = c.sb([128, 4], F32, "ss")
    kT_s = [c.sb([64, 8, 512], BF16, "kTs")] * 2
    qT_s = [c.sb([64, 8, 512], BF16, "qTs")] * 2
    v_s = [c.sb([128, 4, 8, 65], BF16, "vs")] * 2
    uT_s = [c.sb([128, 4, 512], BF16, "uTs")] * 2
    gT_s = [c.sb([128, 16, 512], BF16, "gTs")] * 2
    CPK = c.sb([8, 6, 512], BF16, "CPK")
    CPQ = c.sb([8, 6, 512], BF16, "CPQ")
    e1 = c.sb([8, 512], F32, "e1")
    negc = c.sb([8, 512], F32, "negc")
    r1 = c.sb([8, 512], F32, "r1")
    carry = c.sb([8, 1], F32, "carry")
    ones_own = c.sb([128, 32], BF16, "ones_own")
    ones_ctx = c.sb([128, 32], BF16, "ones_ctx")
    V(lambda e: e.memset(ones_own[:], 1.0), (), ['ones_own'])
    V(lambda e: e.tensor_scalar(out=ones_ctx[:], in0=onesf[:, 0:32], scalar1=flag[:, 0:1], scalar2=None, op0=ALU.mult), ['onesf', 'flag'], ['ones_ctx'])
    V(lambda e: e.memset(CPK[:, 0:3, :], 1.0), (), ['CPK'])
    V(lambda e: e.memset(CPQ[:, 3:6, :], 1.0), (), ['CPQ'])
    V(lambda e: e.memset(carry[:], 0.0), (), ['carry'])

    xin_v = xin.rearrange("(t j p) d -> t p j d", j=4, p=128)
    D(xt_b[0][:], xin_v[0], w=['xt0'])
    pi = [0]

    def nps():
        pi[0] = (pi[0] + 1) % 6
        return pi[0]

    for i in range(2 * NT):
        own = i >= NT
        b = i % 2
        xt, xk = xt_b[b], f'xt{b}'
        hT, hk = hT_b[b], f'hT{b}'
        if i + 1 < 2 * NT:
            D(xt_b[1 - b][:], xin_v[i + 1], w=[f'xt{1 - b}'])
        for j in range(4):
            jk_, jkk_ = junk_b[j % 2], f'junk{j % 2}'
            A(lambda e, j=j, xt=xt, jk_=jk_: e.activation(out=jk_[:], in_=xt[:, j, :], func=AF.Square), [xk], [jkk_])
            V(lambda e, j=j, jk_=jk_: e.tensor_reduce(out=ss[:, j:j + 1], in_=jk_[:], axis=mybir.AxisListType.X, op=ALU.add), [jkk_], ['Ass'])
        m0 = c.mark()
        rs = rstd_of(ss, 4, 'A')
        for j in range(4):
            V(lambda e, j=j, xt=xt, rs=rs: e.tensor_scalar(out=xs[:, j, :], in0=xt[:, j, :], scalar1=rs[:, j:j + 1], scalar2=None, op0=ALU.mult), [xk, 'Ars'], [f'xs{j}'])
        c.release(m0)
        for j in range(4):
            pbk = j % 2
            for kc in range(8):
                TR(pb[pbk][:, kc * 128:(kc + 1) * 128], xs[:, j, kc * 128:(kc + 1) * 128], identb[:], [f'xs{j}', 'identb'], [f'pb{pbk}'])
            src = pb[pbk][:, :].rearrange("p (k t) -> p k t", k=8)
            if j % 2 == 0:
                A(lambda e, j=j, hT=hT, src=src: e.activation(out=hT[:, :, j * 128:(j + 1) * 128], in_=src, func=AF.Copy), [f'pb{pbk}'], [hk])
            else:
                V(lambda e, j=j, hT=hT, src=src: e.tensor_copy(out=hT[:, :, j * 128:(j + 1) * 128], in_=src), [f'pb{pbk}'], [hk])
        tok0 = i * 512
        p = nps()
        for kc in range(8):
            MM(ps[p][0:8, :], Win[:, kc, CF:CF + 8], hT[:, kc, :], kc == 0, kc == 7, ['Win0', 'Win1', hk], [f'ps{p}'])
        A(lambda e, p=p: e.activation(out=e1[:], in_=ps[p][0:8, :], func=AF.Exp, scale=-1.0, bias=negb[:, 0:1]), [f'ps{p}', 'negb'], ['e1'])
        A(lambda e: e.activation(out=e1[:], in_=e1[:], func=AF.Ln, bias=1.0), ['e1'], ['e1'])
        V(lambda e: e.tensor_tensor_scan(out=negc[:], data0=onesf[0:8, 0:512], data1=e1[:], initial=carry[:, 0:1], op0=ALU.mult, op1=ALU.add), ['e1', 'carry', 'onesf'], ['negc'])
        V(lambda e: e.tensor_copy(out=carry[:], in_=negc[:, 511:512]), ['negc'], ['carry'])
        V(lambda e: e.tensor_copy(out=CPK[:, 3, :], in_=negc[:]), ['negc'], ['CPK'])
        V(lambda e: e.tensor_tensor(out=r1[:], in0=negc[:], in1=CPK[:, 3, :], op=ALU.subtract), ['negc', 'CPK'], ['r1'])
        V(lambda e: e.tensor_copy(out=CPK[:, 4, :], in_=r1[:]), ['r1'], ['CPK'])
        V(lambda e: e.tensor_tensor(out=r1[:], in0=r1[:], in1=CPK[:, 4, :], op=ALU.subtract), ['r1', 'CPK'], ['r1'])
        V(lambda e: e.tensor_copy(out=CPK[:, 5, :], in_=r1[:]), ['r1'], ['CPK'])
        D(kT_d[:, 64:70, tok0:tok0 + 512], CPK[:, :, :], r=['CPK'], w=['kT_d'])
        if own:
            V(lambda e: e.tensor_scalar(out=CPQ[:, 0:3, :], in0=CPK[:, 3:6, :], scalar1=-1.0, scalar2=None, op0=ALU.mult), ['CPK'], ['CPQ'])
            D(qT_d[:, 64:70, tok0 - TH:tok0 - TH + 512], CPQ[:, :, :], r=['CPQ'], w=['qT_d'])
        kts, ktk = kT_s[b], 'kTs'
        for h in range(8):
            p = nps()
            for kc in range(8):
                MM(ps[p][0:64, :], Win[:, kc, CK + h * 64:CK + (h + 1) * 64], hT[:, kc, :], kc == 0, kc == 7, ['Win0', 'Win1', hk], [f'ps{p}'])
            if h % 2 == 0:
                A(lambda e, p=p, h=h, kts=kts: e.activation(out=kts[:, h, :], in_=ps[p][0:64, :], func=AF.Copy), [f'ps{p}'], [ktk])
            else:
                V(lambda e, p=p, h=h, kts=kts: e.tensor_copy(out=kts[:, h, :], in_=ps[p][0:64, :]), [f'ps{p}'], [ktk])
        D(kT_d[:, 0:64, tok0:tok0 + 512].rearrange("h r t -> r h t"), kts[:, :, :], r=[ktk], w=['kT_d'])
        vs, vk = v_s[b], 'vs'
        V(lambda e, vs=vs, own=own: e.tensor_copy(out=vs[:, :, :, 64:65], in_=(ones_own if own else ones_ctx)[:, :].rearrange("p (j h o) -> p j h o", j=4, o=1)), ['ones_own', 'ones_ctx'], [vk])
        for j in range(4):
            p = nps()
            for kc in range(8):
                MM(ps[p][:, :], hT[:, kc, j * 128:(j + 1) * 128], Win[:, kc, CV:CV + 512], kc == 0, kc == 7, ['Win0', 'Win1', hk], [f'ps{p}'])
            V(lambda e, p=p, j=j, vs=vs: e.tensor_copy(out=vs[:, j, :, 0:64], in_=ps[p][:, :].rearrange("p (h d) -> p h d", h=8)), [f'ps{p}'], [vk])
        D(v_d[tok0:tok0 + 512, :].rearrange("(j p) c -> p j c", p=128), vs[:, :, :, :].rearrange("p j h c -> p j (h c)"), r=[vk], w=['v_d'])
        us, uk = uT_s[b], 'uTs'
        for m in range(4):
            p = nps()
            for kc in range(8):
                MM(ps[p][:, :], Win[:, kc, CU + m * 128:CU + (m + 1) * 128], hT[:, kc, :], kc == 0, kc == 7, ['Win0', 'Win1', hk], [f'ps{p}'])
            V(lambda e, p=p, m=m, us=us: e.tensor_copy(out=us[:, m, :], in_=ps[p][:, :]), [f'ps{p}'], [uk])
        D(uT_d.rearrange("(s p) t -> p s t", p=128)[:, :, tok0:tok0 + 512], us[:, :, :], r=[uk], w=['uT_d'])
        if own:
            qts, qtk = qT_s[b], 'qTs'
            for h in range(8):
                p = nps()
                for kc in range(8):
                    MM(ps[p][0:64, :], Win[:, kc, CQ + h * 64:CQ + (h + 1) * 64], hT[:, kc, :], kc == 0, kc == 7, ['Win0', 'Win1', hk], [f'ps{p}'])
                A(lambda e, p=p, h=h, qts=qts: e.activation(out=qts[:, h, :], in_=ps[p][0:64, :], func=AF.Copy, scale=0.125), [f'ps{p}'], [qtk])
            D(qT_d[:, 0:64, tok0 - TH:tok0 - TH + 512].rearrange("h r t -> r h t"), qts[:, :, :], r=[qtk], w=['qT_d'])
            gs, gk = gT_s[b], 'gTs'
            for m in range(16):
                p = nps()
                for kc in range(8):
                    MM(ps[p][:, :], Win[:, kc, CG + m * 128:CG + (m + 1) * 128], hT[:, kc, :], kc == 0, kc == 7, ['Win0', 'Win1', hk], [f'ps{p}'])
                A(lambda e, p=p, m=m, gs=gs: e.activation(out=gs[:, m, :], in_=ps[p][:, :], func=AF.Sigmoid, bias=bg[:, m:m + 1]), [f'ps{p}', 'bg'], [gk])
            D(gT_d.rearrange("(m p) t -> p m t", p=128)[:, :, tok0 - TH:tok0 - TH + 512], gs[:, :, :], r=[gk], w=['gT_d'])

    c.barrier()
    c.release(base_mark)
    if 'dbg_k' in dbg_out:
        tmpk = c.sb([70, T2], BF16, "tmpk")
        tmpf = c.sb([70, T2], F32, "tmpf")
        D(tmpk[:], kT_d[0], r=['kT_d'], w=['tmpk'])
        V(lambda e, tmpf=tmpf, tmpk=tmpk: e.tensor_copy(out=tmpf[:], in_=tmpk[:]), ['tmpk'], ['tmpf'])
        D(dbg_out['dbg_k'], tmpf[:], r=['tmpf'])
        c.barrier()
        c.release(base_mark)

    if stop == 'A':
        c.emit()
        return nc
    NB2 = T2 // 128
    v_all = c.sb([128, NB2, 520], BF16, "v_all")
    for q4 in range(0, NB2, 8):
        n = min(8, NB2 - q4)
        D(v_all[:, q4:q4 + n, :], v_d[q4 * 128:(q4 + n) * 128, :].rearrange("(j p) c -> p j c", p=128), r=['v_d'], w=[f'v_all{q4 // 8}'])
    kT_h = [c.sb([70, T2], BF16, "kTh") for _ in range(2)]
    qT_h = [c.sb([70, TH], BF16, "qTh") for _ in range(2)]
    pT_b = [c.sb([128, 512], BF16, "pT") for _ in range(3)]
    rr = c.sb([128, 512], F32, "rr")
    rrh = c.sb([128, 512], BF16, "rrh")
    rrl = c.sb([128, 512], BF16, "rrl")
    onesb = c.sb([128, 64], BF16, "onesb")
    V(lambda e: e.memset(onesb[:], 1.0), (), ['onesb'])
    bc_sb = c.sb([64, 512], F32, "bc_sb")
    oT_s = [c.sb([64, 512], BF16, "oTs") for _ in range(2)]
    D(kT_h[0][:], kT_d[0], r=['kT_d'], w=['kTh0'])
    D(qT_h[0][:], qT_d[0], r=['qT_d'], w=['qTh0'])
    items = []
    gi = 0
    for h in range(8):
        for Gq in range(NT):
            kbs = list(range(NBH)) + [NBH + ob for ob in range(4 * Gq + 4)]
            for idx, gkb in enumerate(kbs):
                ob = gkb - NBH
                diag = ob >= 4 * Gq
                c0 = (ob - 4 * Gq) * 128 if diag else 0
                items.append(dict(h=h, Gq=Gq, gkb=gkb, c0=c0, diag=diag, first=(idx == 0), last=(idx == len(kbs) - 1), gi=gi,
                                  newhead=(Gq == 0 and idx == 0)))
            gi += 1
    DEPTH = 2

    def att_stage1(i, it):
        h, Gq, gkb, c0 = it['h'], it['Gq'], it['gkb'], it['c0']
        hb = h % 2
        if it['newhead'] and h + 1 < 8:
            D(kT_h[1 - hb][:], kT_d[h + 1], r=['kT_d'], w=[f'kTh{1 - hb}'])
            D(qT_h[1 - hb][:], qT_d[h + 1], r=['qT_d'], w=[f'qTh{1 - hb}'])
        kt, ktk = kT_h[hb], f'kTh{hb}'
        qt, qtk = qT_h[hb], f'qTh{hb}'
        p = i % 3
        pT, ptk = pT_b[i % 3], f'pT{i % 3}'
        MM(ps[p][:, c0:512], kt[:, gkb * 128:(gkb + 1) * 128], qt[:, Gq * 512 + c0:Gq * 512 + 512], True, True, [ktk, qtk], [f'ps{p}'])
        A(lambda e, p=p, pT=pT, c0=c0: e.activation(out=pT[:, c0:512], in_=ps[p][:, c0:512], func=AF.Exp), [f'ps{p}'], [ptk])
        if it['diag']:
            V(lambda e, pT=pT, c0=c0: e.tensor_tensor(out=pT[:, c0:c0 + 128], in0=pT[:, c0:c0 + 128], in1=trib[:, :], op=ALU.mult), [ptk, 'trib'], [ptk])

    def att_stage2(i, it):
        h, Gq, gkb, c0 = it['h'], it['Gq'], it['gkb'], it['c0']
        po = 3 + (it['gi'] % 2)
        pT, ptk = pT_b[i % 3], f'pT{i % 3}'
        MM(ps[po][0:65, c0:512], v_all[:, gkb, h * 65:(h + 1) * 65], pT[:, c0:512], it['first'], it['last'], [f'v_all{gkb // 8}', ptk], [f'ps{po}'])
        if it['last']:
            V(lambda e, po=po: e.reciprocal(out=rr[64:65, :], in_=ps[po][64:65, :]), [f'ps{po}'], ['rr'])
            V(lambda e: e.tensor_copy(out=rrh[64:65, :], in_=rr[64:65, :]), ['rr'], ['rrh'])
            V(lambda e: e.tensor_tensor(out=rr[64:65, :], in0=rr[64:65, :], in1=rrh[64:65, :], op=ALU.subtract), ['rr', 'rrh'], ['rr'])
            V(lambda e: e.tensor_copy(out=rrl[64:65, :], in_=rr[64:65, :]), ['rr'], ['rrl'])
            def tail(po=po, h=h, Gq=Gq, gi_=it['gi']):
                MM(ps[5][0:64, :], onesb[64:65, 0:64], rrh[64:65, :], True, False, ['onesb', 'rrh'], ['ps5'])
                MM(ps[5][0:64, :], onesb[64:65, 0:64], rrl[64:65, :], False, True, ['onesb', 'rrl'], ['ps5'])
                A(lambda e: e.activation(out=bc_sb[:], in_=ps[5][0:64, :], func=AF.Copy), ['ps5'], ['bc_sb'])
                ots, otk = oT_s[gi_ % 2], f"oTs{gi_ % 2}"
                V(lambda e, po=po, ots=ots: e.tensor_tensor(out=ots[:], in0=ps[po][0:64, :], in1=bc_sb[:], op=ALU.mult), [f'ps{po}', 'bc_sb'], [otk])
                D(oT_d[h, :, Gq * 512:(Gq + 1) * 512], ots[:], r=[otk], w=['oT_d'])
            pending_tail.append([3, tail])

    pending_tail = []
    for i in range(len(items) + DEPTH):
        if i < len(items):
            att_stage1(i, items[i])
        if i - DEPTH >= 0:
            att_stage2(i - DEPTH, items[i - DEPTH])
        for pt_ in list(pending_tail):
            pt_[0] -= 1
            if pt_[0] <= 0:
                pt_[1]()
                pending_tail.remove(pt_)
    for pt_ in pending_tail:
        pt_[1]()
    c.barrier()
    c.release(base_mark)
    if 'dbg_o' in dbg_out:
        tmpk = c.sb([64, 8, TH], BF16, "tmpo")
        tmpf = c.sb([64, 8, TH], F32, "tmpof")
        D(tmpk[:], oT_d.rearrange("h d t -> d h t"), r=['oT_d'], w=['tmpk'])
        V(lambda e, tmpf=tmpf, tmpk=tmpk: e.tensor_copy(out=tmpf[:], in_=tmpk[:]), ['tmpk'], ['tmpf'])
        D(dbg_out['dbg_o'].rearrange("h d t -> d h t"), tmpf[:], r=['tmpf'])
        c.barrier()
        c.release(base_mark)

    if stop == 'B':
        c.emit()
        return nc
    def ssm_prep(sfx):
        k = 'pp' + sfx
        lr = c.sb([128, 512], F32, "lr"); li = c.sb([128, 512], F32, "li"); ls = c.sb([128, 512], F32, "ls")
        D(lr[:], ssm_in["LR" + sfx], w=[k + 'lr'])
        D(li[:], ssm_in["LI" + sfx], w=[k + 'li'])
        D(ls[:], ssm_in["LS" + sfx], w=[k + 'ls'])
        dt = c.sb([128, 512], F32, "dt"); mag = c.sb([128, 512], F32, "mag"); th = c.sb([128, 512], F32, "th")
        A(lambda e: e.activation(out=dt[:], in_=ls[:], func=AF.Exp), [k + 'ls'], [k + 'dt'])
        V(lambda e: e.tensor_tensor(out=mag[:], in0=lr[:], in1=dt[:], op=ALU.mult), [k + 'lr', k + 'dt'], [k + 'mag'])
        A(lambda e: e.activation(out=mag[:], in_=mag[:], func=AF.Exp), [k + 'mag'], [k + 'mag'])
        V(lambda e: e.tensor_tensor(out=th[:], in0=li[:], in1=dt[:], op=ALU.mult), [k + 'li', k + 'dt'], [k + 'th'])

        def sin_of(shift, outt, ok):
            t = c.sb([128, 512], F32, "t"); ni = c.sb([128, 512], I32, "ni"); nf = c.sb([128, 512], F32, "nf")
            a = c.sb([128, 512], F32, "a"); mk = c.sb([128, 512], F32, "mk")
            V(lambda e: e.tensor_scalar(out=t[:], in0=th[:], scalar1=1.0 / TWO_PI, scalar2=8.5 + shift, op0=ALU.mult, op1=ALU.add), [k + 'th'], [k + 't'])
            V(lambda e: e.tensor_copy(out=ni[:], in_=t[:]), [k + 't'], [k + 'ni'])
            V(lambda e: e.tensor_copy(out=nf[:], in_=ni[:]), [k + 'ni'], [k + 'nf'])
            V(lambda e: e.scalar_tensor_tensor(out=a[:], in0=t[:], scalar=-0.5, in1=nf[:], op0=ALU.add, op1=ALU.subtract), [k + 't', k + 'nf'], [k + 'a'])
            V(lambda e: e.tensor_single_scalar(out=mk[:], in_=a[:], scalar=-0.5, op=ALU.is_lt), [k + 'a'], [k + 'mk'])
            V(lambda e: e.tensor_tensor(out=a[:], in0=a[:], in1=mk[:], op=ALU.add), [k + 'a', k + 'mk'], [k + 'a'])
            A(lambda e: e.activation(out=outt[:], in_=a[:], func=AF.Sin, scale=TWO_PI), [k + 'a'], [ok])
        ar = c.sb([128, 512], F32, "ar"); ai = c.sb([128, 512], F32, "ai")
        sin_of(0.0, ai, k + 'ai')
        sin_of(0.25, ar, k + 'ar')
        V(lambda e: e.tensor_tensor(out=ai[:], in0=ai[:], in1=mag[:], op=ALU.mult), [k + 'ai', k + 'mag'], [k + 'ai'])
        V(lambda e: e.tensor_tensor(out=ar[:], in0=ar[:], in1=mag[:], op=ALU.mult), [k + 'ar', k + 'mag'], [k + 'ar'])
        zr = c.sb([128, 512], F32, "zr"); zi = c.sb([128, 512], F32, "zi")
        nr = c.sb([128, 512], F32, "nr"); den = c.sb([128, 512], F32, "den"); t2 = c.sb([128, 512], F32, "t2")
        V(lambda e: e.tensor_scalar(out=nr[:], in0=ar[:], scalar1=-1.0, scalar2=None, op0=ALU.add), [k + 'ar'], [k + 'nr'])
        V(lambda e: e.tensor_tensor(out=den[:], in0=lr[:], in1=lr[:], op=ALU.mult), [k + 'lr'], [k + 'den'])
        V(lambda e: e.tensor_tensor(out=t2[:], in0=li[:], in1=li[:], op=ALU.mult), [k + 'li'], [k + 't2'])
        V(lambda e: e.tensor_tensor(out=den[:], in0=den[:], in1=t2[:], op=ALU.add), [k + 'den', k + 't2'], [k + 'den'])
        V(lambda e: e.reciprocal(out=den[:], in_=den[:]), [k + 'den'], [k + 'den'])
        V(lambda e: e.tensor_tensor(out=zr[:], in0=nr[:], in1=lr[:], op=ALU.mult), [k + 'nr', k + 'lr'], [k + 'zr'])
        V(lambda e: e.tensor_tensor(out=t2[:], in0=ai[:], in1=li[:], op=ALU.mult), [k + 'ai', k + 'li'], [k + 't2'])
        V(lambda e: e.tensor_tensor(out=zr[:], in0=zr[:], in1=t2[:], op=ALU.add), [k + 'zr', k + 't2'], [k + 'zr'])
        V(lambda e: e.tensor_tensor(out=zr[:], in0=zr[:], in1=den[:], op=ALU.mult), [k + 'zr', k + 'den'], [k + 'zr'])
        V(lambda e: e.tensor_tensor(out=zi[:], in0=ai[:], in1=lr[:], op=ALU.mult), [k + 'ai', k + 'lr'], [k + 'zi'])
        V(lambda e: e.tensor_tensor(out=t2[:], in0=nr[:], in1=li[:], op=ALU.mult), [k + 'nr', k + 'li'], [k + 't2'])
        V(lambda e: e.tensor_tensor(out=zi[:], in0=zi[:], in1=t2[:], op=ALU.subtract), [k + 'zi', k + 't2'], [k + 'zi'])
        V(lambda e: e.tensor_tensor(out=zi[:], in0=zi[:], in1=den[:], op=ALU.mult), [k + 'zi', k + 'den'], [k + 'zi'])
        return ar, ai, zr, zi, k

    def cmul(outr, outi, xr, xi, yr, yi, keys_in, kor, koi, negate_im=False, view=None):
        ta_t, tb_t = cm_tmp
        ta = view(ta_t[:]) if view else ta_t[:]
        tb = view(tb_t[:]) if view else tb_t[:]
        V(lambda e: e.tensor_tensor(out=ta, in0=xr, in1=yr, op=ALU.mult), keys_in, ['cm_ta'])
        V(lambda e: e.tensor_tensor(out=tb, in0=xi, in1=yi, op=ALU.mult), keys_in, ['cm_tb'])
        V(lambda e: e.tensor_tensor(out=outr, in0=ta, in1=tb, op=ALU.subtract), ['cm_ta', 'cm_tb'], [kor])
        V(lambda e: e.tensor_tensor(out=ta, in0=xr, in1=yi, op=ALU.mult), keys_in + [kor], ['cm_ta'])
        V(lambda e: e.tensor_tensor(out=tb, in0=xi, in1=yr, op=ALU.mult), keys_in + [kor], ['cm_tb'])
        if negate_im:
            V(lambda e: e.scalar_tensor_tensor(out=outi, in0=ta, scalar=-1.0, in1=tb, op0=ALU.mult, op1=ALU.subtract), ['cm_ta', 'cm_tb'], [koi])
        else:
            V(lambda e: e.tensor_tensor(out=outi, in0=ta, in1=tb, op=ALU.add), ['cm_ta', 'cm_tb'], [koi])

    cm_tmp = []
    ZBT = c.sb([128, 4, L, 2, 128], BF16, "ZBT")
    CYT = c.sb([128, 16, L + 1, 2, 32], BF16, "CYT")
    TT = c.sb([128, 4, L, 128], BF16, "TT")
    BBY = c.sb([128, 2, 512], BF16, "BBY")
    AL = c.sb([128, 2, 2, 16], F32, "AL")
    tbl_mark = c.mark()
    ar, ai, zr, zi, k = ssm_prep('X')
    br_ = c.sb([128, 512], F32, "br"); bi_ = c.sb([128, 512], F32, "bi")
    D(br_[:], ssm_in["BRX"], w=['brx'])
    D(bi_[:], ssm_in["BIX"], w=['bix'])
    bbr = c.sb([128, 512], F32, "bbr"); bbi = c.sb([128, 512], F32, "bbi")
    cm_tmp[:] = [c.sb([128, 512], F32, "ta"), c.sb([128, 512], F32, "tb")]
    cmul(bbr[:], bbi[:], zr[:], zi[:], br_[:], bi_[:], [k + 'zr', k + 'zi', 'brx', 'bix'], 'bbr', 'bbi')
    pw = [c.sb([128, 2, 512], F32, "pw") for _ in range(2)]
    V(lambda e, pw=pw: e.memset(pw[0][:, 0, :], 1.0), (), ['pw0'])
    V(lambda e, pw=pw: e.memset(pw[0][:, 1, :], 0.0), (), ['pw0'])
    for n in range(L):
        cur, ck = pw[n % 2], f'pw{n % 2}'
        nxt, nk = pw[(n + 1) % 2], f'pw{(n + 1) % 2}'
        j = L - 1 - n
        cmul(ZBT[:, :, j, 0, :], ZBT[:, :, j, 1, :], cur[:, 0, :].rearrange("p (s q) -> p s q", s=4), cur[:, 1, :].rearrange("p (s q) -> p s q", s=4),
             bbr[:].rearrange("p (s q) -> p s q", s=4), bbi[:].rearrange("p (s q) -> p s q", s=4), [ck, 'bbr', 'bbi'], 'ZBT', 'ZBT',
             view=lambda ap: ap.rearrange("p (s q) -> p s q", s=4))
        if n < L - 1:
            cmul(nxt[:, 0, :], nxt[:, 1, :], cur[:, 0, :], cur[:, 1, :], ar[:], ai[:], [ck, k + 'ar', k + 'ai'], nk, nk)
    c.barrier()
    c.release(tbl_mark)
    ar, ai, zr, zi, k = ssm_prep('Y')
    br_ = c.sb([128, 512], F32, "br"); bi_ = c.sb([128, 512], F32, "bi")
    cr_ = c.sb([128, 512], F32, "cr"); ci_ = c.sb([128, 512], F32, "ci")
    D(br_[:], ssm_in["BRY"], w=['bry'])
    D(bi_[:], ssm_in["BIY"], w=['biy'])
    D(cr_[:], ssm_in["CRY"], w=['cry'])
    D(ci_[:], ssm_in["CIY"], w=['ciy'])
    cm_tmp[:] = [c.sb([128, 512], F32, "ta"), c.sb([128, 512], F32, "tb")]
    cmul(BBY[:, 0, :], BBY[:, 1, :], zr[:], zi[:], br_[:], bi_[:], [k + 'zr', k + 'zi', 'bry', 'biy'], 'BBY', 'BBY')
    pw = [c.sb([128, 2, 512], F32, "pw") for _ in range(2)]
    V(lambda e, pw=pw: e.memset(pw[0][:, 0, :], 1.0), (), ['pw0'])
    V(lambda e, pw=pw: e.memset(pw[0][:, 1, :], 0.0), (), ['pw0'])
    for n in range(L + 1):
        cur, ck = pw[n % 2], f'pw{n % 2}'
        nxt, nk = pw[(n + 1) % 2], f'pw{(n + 1) % 2}'
        cmul(CYT[:, :, n, 0, :], CYT[:, :, n, 1, :], cr_[:].rearrange("p (k j) -> p k j", k=16), ci_[:].rearrange("p (k j) -> p k j", k=16),
             cur[:, 0, :].rearrange("p (k j) -> p k j", k=16), cur[:, 1, :].rearrange("p (k j) -> p k j", k=16), [ck, 'cry', 'ciy'], 'CYT', 'CYT', negate_im=True,
             view=lambda ap: ap.rearrange("p (k j) -> p k j", k=16))
        if n < L:
            cmul(nxt[:, 0, :], nxt[:, 1, :], cur[:, 0, :], cur[:, 1, :], ar[:], ai[:], [ck, k + 'ar', k + 'ai'], nk, nk)
    pL, pLk = pw[L % 2], f'pw{L % 2}'
    pLv_r = pL[:, 0, :].rearrange("p (k j) -> p k j", k=16)[:, :, 0]
    pLv_i = pL[:, 1, :].rearrange("p (k j) -> p k j", k=16)[:, :, 0]
    V(lambda e: e.tensor_copy(out=AL[:, 0, 0, :], in_=pLv_r), [pLk], ['AL'])
    V(lambda e: e.tensor_copy(out=AL[:, 0, 1, :], in_=pLv_r), [pLk], ['AL'])
    V(lambda e: e.tensor_scalar(out=AL[:, 1, 0, :], in0=pLv_i, scalar1=-1.0, scalar2=None, op0=ALU.mult), [pLk], ['AL'])
    V(lambda e: e.tensor_copy(out=AL[:, 1, 1, :], in_=pLv_i), [pLk], ['AL'])
    V(lambda e: e.memset(TT[:], 0.0), (), ['TT'])
    ddt = c.sb([128, 512], F32, "ddt")
    D(ddt[:], ddg, w=['ddt'])
    for kk in range(16):
        s_, k4 = kk // 4, kk % 4
        p = nps()
        MM(ps[p][32 * k4:32 * k4 + 32, :], BBY[:, 0, kk * 32:(kk + 1) * 32], CYT[:, kk, 0:L, 0, :], True, False, ['BBY', 'CYT'], [f'ps{p}'], tp=(0, 32 * k4))
        MM(ps[p][32 * k4:32 * k4 + 32, :], BBY[:, 1, kk * 32:(kk + 1) * 32], CYT[:, kk, 0:L, 1, :], False, True, ['BBY', 'CYT'], [f'ps{p}'], tp=(0, 32 * k4))
        V(lambda e, p=p, s_=s_, k4=k4: e.tensor_copy(out=TT[32 * k4:32 * k4 + 32, s_, :, 32 * k4:32 * k4 + 32], in_=ps[p][32 * k4:32 * k4 + 32, :].rearrange("p (n j) -> p n j", n=L)), [f'ps{p}'], ['TT'])
    V(lambda e: e.tensor_tensor(out=TT[:, :, 0, :], in0=TT[:, :, 0, :], in1=ddt[:].rearrange("p (s q) -> p s q", s=4), op=ALU.add), ['TT', 'ddt'], ['TT'])
    c.barrier()
    c.release(tbl_mark)

    if stop == 'C1':
        c.barrier()
        dump('dbg_ZBT', ZBT[:].rearrange("p s j r q -> p (s j r q)"), 4 * L * 2 * 128)
        dump('dbg_CYT', CYT[:].rearrange("p k n r j -> p (k n r j)"), 16 * (L + 1) * 2 * 32)
        dump('dbg_TT', TT[:].rearrange("p s n q -> p (s n q)"), 4 * L * 128)
        dump('dbg_AL', AL[:].rearrange("p a r k -> p (a r k)"), 64)
        c.emit()
        return nc
    uT_all = c.sb([128, 4, TH], BF16, "uT_all")
    Zs = c.sb([128, 2, 16, NCH], BF16, "Zs")
    H = c.sb([128, 2, 16, NCH + 1], F32, "H")
    Hb = c.sb([128, 2, 16, NCH + 1], BF16, "Hb")
    t1 = c.sb([128, 2, 16], F32, "t1")
    t4 = c.sb([128, 2, 2, 16], F32, "t4")
    AL4 = c.sb([128, 2, 2, 16], F32, "AL4")
    V(lambda e: e.tensor_copy(out=AL4[:, 0, 0, :], in_=AL[:, 0, 0, :]), ['AL'], ['AL4'])
    V(lambda e: e.tensor_copy(out=AL4[:, 0, 1, :], in_=AL[:, 1, 0, :]), ['AL'], ['AL4'])
    V(lambda e: e.tensor_copy(out=AL4[:, 1, 0, :], in_=AL[:, 1, 1, :]), ['AL'], ['AL4'])
    V(lambda e: e.tensor_copy(out=AL4[:, 1, 1, :], in_=AL[:, 0, 1, :]), ['AL'], ['AL4'])
    t2_ = c.sb([128, 2, 16], F32, "t2_")
    G(lambda e: e.memset(H[:, :, :, 0], 0.0), (), ['H'])
    NZ = min(NCH, 512)
    for half in range(2):
        D(uT_all[:], uT_d.rearrange("(s p) t -> p s t", p=128)[:, :, half * TH:(half + 1) * TH], r=['uT_d'], w=['uT_all'])
        for kk in range(16):
            s_, k4 = kk // 4, kk % 4
            for ri in range(2):
                for z0 in range(0, NCH, NZ):
                    p = nps()
                    for j in range(L):
                        uview = uT_all[32 * k4:32 * k4 + 32, s_, :].rearrange("p (n j) -> p n j", j=L)[:, z0:z0 + NZ, j]
                        MM(ps[p][:, 0:NZ], ZBT[32 * k4:32 * k4 + 32, s_, j, ri, :], uview, j == 0, j == L - 1, ['ZBT', 'uT_all'], [f'ps{p}'], tp=(32 * k4, 0))
                    if (kk + ri) % 2 == 0:
                        A(lambda e, p=p, ri=ri, kk=kk, z0=z0: e.activation(out=Zs[:, ri, kk, z0:z0 + NZ], in_=ps[p][:, 0:NZ], func=AF.Copy), [f'ps{p}'], ['Zs'])
                    else:
                        V(lambda e, p=p, ri=ri, kk=kk, z0=z0: e.tensor_copy(out=Zs[:, ri, kk, z0:z0 + NZ], in_=ps[p][:, 0:NZ]), [f'ps{p}'], ['Zs'])
        for n in range(NCH):
            V(lambda e, n=n: e.tensor_tensor(out=t4[:], in0=AL4[:], in1=H[:, :, :, n].unsqueeze(1).broadcast_to([128, 2, 2, 16]), op=ALU.mult), ['AL4', 'H'], ['t4'])
            V(lambda e, n=n: e.tensor_tensor(out=t1[:], in0=t4[:, :, 0, :], in1=Zs[:, :, :, n], op=ALU.add), ['t4', 'Zs'], ['t1'])
            V(lambda e, n=n: e.tensor_tensor(out=H[:, :, :, n + 1], in0=t1[:], in1=t4[:, :, 1, :], op=ALU.add), ['t1', 't4'], ['H'])
        if half == 0:
            V(lambda e: e.tensor_copy(out=H[:, :, :, 0], in_=H[:, :, :, NCH]), ['H'], ['H'])
    V(lambda e: e.tensor_copy(out=Hb[:], in_=H[:]), ['H'], ['Hb'])

    if stop == 'C2':
        c.barrier()
        dump('dbg_H', H[:].rearrange("p a k n -> p (a k n)"), 2 * 16 * (NCH + 1))
        dump('dbg_Zs', Zs[:].rearrange("p a k n -> p (a k n)"), 2 * 16 * NCH)
        c.emit()
        return nc
    zT_s = [c.sb([128, 512], BF16, "zTs") for _ in range(2)]
    x2 = c.sb([128, 512], F32, "x2")
    wv = c.sb([128, 512], F32, "wv")
    zi_ = 0
    for s_ in range(4):
        for ti in range(NT):
            p = nps()
            tok0 = ti * 512
            yv = ps[p][:, :].rearrange("p (n j) -> p n j", j=L)
            uv = uT_all[:, s_, tok0:tok0 + 512].rearrange("p (n j) -> p n j", j=L)
            for n in range(L):
                MM(yv[:, :, n:L], TT[:, s_, n, :], uv[:, :, 0:L - n], n == 0, False, ['TT', 'uT_all'], [f'ps{p}'])
            cnt = 0
            for k4 in range(4):
                kk = 4 * s_ + k4
                for i in range(L):
                    for ri in range(2):
                        cnt += 1
                        MM(yv[32 * k4:32 * k4 + 32, :, i], CYT[:, kk, i + 1, ri, :], Hb[:, ri, kk, ti * 32:ti * 32 + 32], False, cnt == 4 * L * 2, ['CYT', 'Hb'], [f'ps{p}'], tp=(0, 32 * k4))
            zt, ztk = zT_s[zi_ % 2], f'zTs{zi_ % 2}'
            zi_ += 1
            A(lambda e, p=p: e.activation(out=x2[:], in_=ps[p][:, :], func=AF.Square), [f'ps{p}'], ['x2'])
            V(lambda e: e.tensor_scalar(out=wv[:], in0=x2[:], scalar1=0.044715, scalar2=1.0, op0=ALU.mult, op1=ALU.add), ['x2'], ['wv'])
            V(lambda e, p=p: e.tensor_tensor(out=wv[:], in0=wv[:], in1=ps[p][:, :], op=ALU.mult), ['wv', f'ps{p}'], ['wv'])
            A(lambda e: e.activation(out=wv[:], in_=wv[:], func=AF.Sigmoid, scale=1.5957691216057308), ['wv'], ['wv'])
            V(lambda e, p=p, zt=zt: e.tensor_tensor(out=zt[:], in0=wv[:], in1=ps[p][:, :], op=ALU.mult), ['wv', f'ps{p}'], [ztk])
            D(zT_d[s_ * 128:(s_ + 1) * 128, ti * 512:(ti + 1) * 512], zt[:], r=[ztk], w=['zT_d'])
    c.barrier()
    c.release(base_mark)

    if 'dbg_y' in dbg_out:
        tmpz = c.sb([128, 4, TH], BF16, "tmpz")
        tmpzf = c.sb([128, 4, TH], F32, "tmpzf")
        D(tmpz[:], zT_d.rearrange("(s p) t -> p s t", p=128), r=['zT_d'], w=['tmpz'])
        V(lambda e, tmpzf=tmpzf, tmpz=tmpz: e.tensor_copy(out=tmpzf[:], in_=tmpz[:]), ['tmpz'], ['tmpzf'])
        D(dbg_out['dbg_y'].rearrange("(s p) t -> p s t", p=128), tmpzf[:], r=['tmpzf'])
        c.barrier()
        c.release(base_mark)
    if stop == 'C':
        c.emit()
        return nc
    def load_w(dram_view, shape, key, scale_ap=None):
        wt = c.sb(shape, BF16, key)
        a_n, ncol = shape[1], shape[2]
        for a_ in range(a_n):
            lw_i[0] += 1
            lwk = 'lw_st0'
            st_ = lw_sts[lw_i[0] % 2][:, 0:ncol]
            D(st_, dram_view[:, a_, :], w=[lwk])
            if scale_ap is None:
                V(lambda e, a_=a_, st_=st_: e.tensor_copy(out=wt[:, a_, :], in_=st_), [lwk], [key])
            else:
                V(lambda e, a_=a_, st_=st_: e.tensor_scalar(out=wt[:, a_, :], in0=st_, scalar1=scale_ap[:, a_:a_ + 1], scalar2=None, op0=ALU.mult), [lwk, 'gf'], [key])
        return wt

    lw_sts = [c.sb([128, 1024], F32, "lw_st")] * 2
    lw_i = [0]
    gf = c.sb([128, 8], F32, "gf")
    D(gf[:], gffnT, w=['gf'])
    bgl = c.sb([128, 4], F32, "bgl")
    D(bgl[:], bglu, w=['bgl'])
    brt = c.sb([128, 20], F32, "brt")
    D(brt[:], br, w=['brt'])
    Wglu = load_w(w_glu.rearrange("(kc p) n -> p kc n", p=128), [128, 4, 512], 'Wglu')
    Wob = load_w(w_out_b.rearrange("(kc p) n -> p kc n", p=128), [128, 4, 1024], 'Wob')
    Woa = c.sb([64, 8, 1024], BF16, "Woa")
    woa_v = w_out_a.rearrange("(h p) n -> p h n", p=64)
    for h in range(8):
        lw_i[0] += 1
        lwk = 'lw_st0'
        lwt = lw_sts[lw_i[0] % 2]
        D(lwt[0:64, :], woa_v[:, h, :], w=[lwk])
        V(lambda e, h=h, lwt=lwt: e.tensor_copy(out=Woa[:, h, :], in_=lwt[0:64, :]), [lwk], ['Woa'])
    Wout = load_w(w_out.rearrange("(kc p) n -> p kc n", p=128), [128, 8, 1024], 'Wout')
    Wr = c.sb([128, 8, 20], F32, "Wr")
    D(Wr[:], wr.rearrange("(kc p) n -> p kc n", p=128), w=['Wr'])
    for kc in range(8):
        V(lambda e, kc=kc: e.tensor_scalar(out=Wr[:, kc, :], in0=Wr[:, kc, :], scalar1=gf[:, kc:kc + 1], scalar2=None, op0=ALU.mult), ['Wr', 'gf'], ['Wr'])

    if stop == 'D1':
        c.emit()
        return nc
    Wrhi = c.sb([128, 8, 20], BF16, "Wrhi")
    Wrlo = c.sb([128, 8, 20], BF16, "Wrlo")
    V(lambda e: e.tensor_copy(out=Wrhi[:], in_=Wr[:]), ['Wr'], ['Wrhi'])
    V(lambda e: e.tensor_tensor(out=Wr[:], in0=Wr[:], in1=Wrhi[:], op=ALU.subtract), ['Wr', 'Wrhi'], ['Wr'])
    V(lambda e: e.tensor_copy(out=Wrlo[:], in_=Wr[:]), ['Wr'], ['Wrlo'])
    h2hi = c.sb([128, 1024], BF16, "h2hi")
    h2lo = c.sb([128, 1024], BF16, "h2lo")
    hThi = c.sb([128, 8, 128], BF16, "hThi")
    hTlo = c.sb([128, 8, 128], BF16, "hTlo")
    cwb = c.sb([128, 16], BF16, "cwb")
    zT = c.sb([128, 4, 512], BF16, "zT")
    oT = c.sb([64, 8, 512], BF16, "oT")
    gT = c.sb([128, 16, 512], BF16, "gT")
    xt = c.sb([128, 4, 1024], F32, "xtD")
    zg = c.sb([128, 4, 512], BF16, "zg")
    sg = c.sb([128, 512], F32, "sg")
    mgd = c.sb([128, 8, 512], BF16, "mgd")
    ta_ = c.sb([128, 512], F32, "taD")
    tb_ = c.sb([128, 512], F32, "tbD")
    x1 = c.sb([128, 4, 1024], F32, "x1")
    h2f = c.sb([128, 1024], F32, "h2f")
    h2Tf = c.sb([128, 8, 128], F32, "h2Tf")
    h2Tb = c.sb([128, 8, 512], BF16, "h2Tb")
    cwTb = c.sb([16, 512], BF16, "cwTb")
    ss2 = c.sb([128, 4], F32, "ss2")
    junk2 = c.sb([128, 1024], BF16, "junk2")
    junk2_b = [junk2, c.sb([128, 1024], BF16, "junk2b")]
    lg = c.sb([128, 20], F32, "lg")
    cw = c.sb([128, 16], F32, "cw")
    sm = {n_: c.sb([128, 4], F32, n_) for n_ in ["gmx", "ge", "gsum", "oh", "les", "m1", "mk1", "le2", "m2", "mk2", "w12", "tmp4"]}
    one1 = {n_: c.sb([128, 1], F32, n_) for n_ in ["psel", "d21", "w1", "w2"]}
    xin_own = xin[TH:T2, :].rearrange("(t j p) d -> t p j d", j=4, p=128)
    zT_b2 = [zT, c.sb([128, 4, 512], BF16, "zT2")]
    oT_b2 = [oT, c.sb([64, 8, 512], BF16, "oT2")]
    gT_b2 = [gT, c.sb([128, 16, 512], BF16, "gT2")]
    xt_b2 = [xt, c.sb([128, 4, 1024], F32, "xtD2")]
    lg4 = c.sb([128, 4, 20], F32, "lg4")
    R4 = {n_: c.sb([128, 4, 4], F32, n_) for n_ in ["oh", "ge", "les", "mk1", "le2", "mk2", "w12", "tq"]}
    R1 = {n_: c.sb([128, 4], F32, n_) for n_ in ["mx", "gsum", "psel", "m1", "m2", "d", "w1", "w2"]}
    prod = c.sb([128, 4, 4, 4], F32, "prod")
    cw4 = c.sb([128, 4, 16], F32, "cw4")
    cwb4 = c.sb([128, 4, 16], BF16, "cwb4")
    AXX = mybir.AxisListType.X

    def bc3(ap2):
        return ap2.unsqueeze(2).broadcast_to([128, 4, 4])

    def load_tile(ti):
        b_ = ti % 2
        t0_ = ti * 512
        D(zT_b2[b_][:], zT_d.rearrange("(s p) t -> p s t", p=128)[:, :, t0_:t0_ + 512], r=['zT_d'], w=[f'zT{b_}'])
        D(oT_b2[b_][:], oT_d.rearrange("h d t -> d h t")[:, :, t0_:t0_ + 512], r=['oT_d'], w=[f'oT{b_}'])
        for hh in range(2):
            D(gT_b2[b_][:, hh * 8:(hh + 1) * 8, :], gT_d.rearrange("(m p) t -> p m t", p=128)[:, hh * 8:(hh + 1) * 8, t0_:t0_ + 512], r=['gT_d'], w=[f'gT{b_}'])
        D(xt_b2[b_][:], xin_own[ti], w=[f'xtD{b_}'])

    load_tile(0)
    for ti in range(NT):
        t0 = ti * 512
        b_ = ti % 2
        zT, oT, gT, xt = zT_b2[b_], oT_b2[b_], gT_b2[b_], xt_b2[b_]
        zTk, oTk, gTk, xtk = f'zT{b_}', f'oT{b_}', f'gT{b_}', f'xtD{b_}'
        if ti + 1 < NT:
            load_tile(ti + 1)
        for m in range(4):
            p = nps()
            for kc in range(4):
                MM(ps[p][:, :], Wglu[:, kc, m * 128:(m + 1) * 128], zT[:, kc, :], kc == 0, kc == 3, ['Wglu', zTk], [f'ps{p}'])
            A(lambda e, p=p, m=m: e.activation(out=sg[:], in_=ps[p][:, :], func=AF.Sigmoid, bias=bgl[:, m:m + 1]), [f'ps{p}', 'bgl'], ['sg'])
            V(lambda e, m=m, zT=zT: e.tensor_tensor(out=zg[:, m, :], in0=zT[:, m, :], in1=sg[:], op=ALU.mult), [zTk, 'sg'], ['zg'])
        for m in range(8):
            p = nps()
            for kc in range(4):
                MM(ps[p][:, :], Wob[:, kc, m * 128:(m + 1) * 128], zg[:, kc, :], kc == 0, kc == 3, ['Wob', 'zg'], [f'ps{p}'])
            p2 = nps()
            for h in range(8):
                MM(ps[p2][:, :], Woa[:, h, m * 128:(m + 1) * 128], oT[:, h, :], h == 0, h == 7, ['Woa', oTk], [f'ps{p2}'])
            V(lambda e, p=p, m=m, gT=gT: e.tensor_tensor(out=ta_[:], in0=gT[:, 8 + m, :], in1=ps[p][:, :], op=ALU.mult), [gTk, f'ps{p}'], ['taD'])
            V(lambda e, p2=p2, m=m, gT=gT: e.tensor_tensor(out=tb_[:], in0=gT[:, m, :], in1=ps[p2][:, :], op=ALU.mult), [gTk, f'ps{p2}'], ['tbD'])
            G(lambda e, m=m: e.tensor_tensor(out=mgd[:, m, :], in0=ta_[:], in1=tb_[:], op=ALU.add), ['taD', 'tbD'], ['mgd'])
        for j in range(4):
            for hf in range(2):
                p = nps()
                for kc in range(8):
                    MM(ps[p][:, :], mgd[:, kc, j * 128:(j + 1) * 128], Wout[:, kc, hf * 512:(hf + 1) * 512], kc == 0, kc == 7, ['mgd', 'Wout'], [f'ps{p}'])
                V(lambda e, p=p, j=j, hf=hf, xt=xt: e.tensor_tensor(out=x1[:, j, hf * 512:(hf + 1) * 512], in0=xt[:, j, hf * 512:(hf + 1) * 512], in1=ps[p][:, :], op=ALU.add), [xtk, f'ps{p}'], ['x1'])
        D(x1_d[t0:t0 + 512, :].rearrange("(j p) d -> p j d", p=128), x1[:], r=['x1'], w=['x1_d'])
        for j in range(4):
            jk_, jkk_ = junk2_b[j % 2], f'junk2{j % 2}'
            A(lambda e, j=j, jk_=jk_: e.activation(out=jk_[:], in_=x1[:, j, :], func=AF.Square), ['x1'], [jkk_])
            V(lambda e, j=j, jk_=jk_: e.tensor_reduce(out=ss2[:, j:j + 1], in_=jk_[:], axis=mybir.AxisListType.X, op=ALU.add), [jkk_], ['Ess'])
        m0 = c.mark()
        rs2 = rstd_of(ss2, 4, 'E')
        for j in range(4):
            A(lambda e, j=j, rs2=rs2: e.activation(out=h2f[:], in_=x1[:, j, :], func=AF.Copy, scale=rs2[:, j:j + 1]), ['x1', 'Ers'], ['h2f'])
            G(lambda e: e.tensor_copy(out=h2hi[:], in_=h2f[:]), ['h2f'], ['h2hi'])
            G(lambda e: e.tensor_tensor(out=h2lo[:], in0=h2f[:], in1=h2hi[:], op=ALU.subtract), ['h2f', 'h2hi'], ['h2lo'])
            for srct, srck, dst, dstk, pbk in ((h2hi, 'h2hi', hThi, 'hThi', 0), (h2lo, 'h2lo', hTlo, 'hTlo', 1)):
                for kc in range(8):
                    TR(pb[pbk][:, kc * 128:(kc + 1) * 128], srct[:, kc * 128:(kc + 1) * 128], identb[:], [srck, 'identb'], [f'pb{pbk}'])
                A(lambda e, dst=dst, pbk=pbk: e.activation(out=dst[:], in_=pb[pbk][:, :].rearrange("p (k t) -> p k t", k=8), func=AF.Copy), [f'pb{pbk}'], [dstk])
            G(lambda e, j=j: e.tensor_copy(out=h2Tb[:, :, j * 128:(j + 1) * 128], in_=hThi[:]), ['hThi'], ['h2Tb'])
            p = nps()
            nmm = 0
            for (lt, ltk, wt_, wtk) in ((hThi, 'hThi', Wrhi, 'Wrhi'), (hTlo, 'hTlo', Wrhi, 'Wrhi'), (hThi, 'hThi', Wrlo, 'Wrlo')):
                for kc in range(8):
                    MM(ps[p][:, 0:20], lt[:, kc, :], wt_[:, kc, :], nmm == 0, nmm == 23, [ltk, wtk], [f'ps{p}'])
                    nmm += 1
            V(lambda e, p=p, j=j: e.tensor_tensor(out=lg4[:, j, :], in0=ps[p][:, 0:20], in1=brt[:], op=ALU.add), [f'ps{p}', 'brt'], ['lg4'])
        c.release(m0)
        gl = lg4[:, :, 0:4]
        le4 = lg4[:, :, 4:20].rearrange("p j (g e) -> p j g e", g=4)
        V(lambda e: e.tensor_reduce(out=R1["mx"][:], in_=gl, axis=AXX, op=ALU.max), ['lg4'], ['r_mx'])
        V(lambda e: e.tensor_tensor(out=R4["oh"][:], in0=gl, in1=bc3(R1["mx"][:, :]), op=ALU.is_ge), ['lg4', 'r_mx'], ['r_oh'])
        V(lambda e: e.tensor_tensor(out=R4["ge"][:], in0=gl, in1=bc3(R1["mx"][:, :]), op=ALU.subtract), ['lg4', 'r_mx'], ['r_ge'])
        A(lambda e: e.activation(out=R4["ge"][:], in_=R4["ge"][:], func=AF.Exp), ['r_ge'], ['r_ge'])
        V(lambda e: e.tensor_reduce(out=R1["gsum"][:], in_=R4["ge"][:], axis=AXX, op=ALU.add), ['r_ge'], ['r_gsum'])
        V(lambda e: e.reciprocal(out=R1["psel"][:], in_=R1["gsum"][:]), ['r_gsum'], ['r_psel'])
        V(lambda e: e.tensor_tensor(out=prod[:], in0=le4, in1=R4["oh"][:, :, :].unsqueeze(3).broadcast_to([128, 4, 4, 4]), op=ALU.mult), ['lg4', 'r_oh'], ['r_prod'])
        V(lambda e: e.tensor_reduce(out=R4["les"][:], in_=prod[:].rearrange("p j g e -> p j e g"), axis=AXX, op=ALU.add), ['r_prod'], ['r_les'])
        V(lambda e: e.tensor_reduce(out=R1["m1"][:], in_=R4["les"][:], axis=AXX, op=ALU.max), ['r_les'], ['r_m1'])
        V(lambda e: e.tensor_tensor(out=R4["mk1"][:], in0=R4["les"][:], in1=bc3(R1["m1"][:, :]), op=ALU.is_ge), ['r_les', 'r_m1'], ['r_mk1'])
        V(lambda e: e.scalar_tensor_tensor(out=R4["le2"][:], in0=R4["mk1"][:], scalar=-1e30, in1=R4["les"][:], op0=ALU.mult, op1=ALU.add), ['r_mk1', 'r_les'], ['r_le2'])
        V(lambda e: e.tensor_reduce(out=R1["m2"][:], in_=R4["le2"][:], axis=AXX, op=ALU.max), ['r_le2'], ['r_m2'])
        V(lambda e: e.tensor_tensor(out=R4["mk2"][:], in0=R4["le2"][:], in1=bc3(R1["m2"][:, :]), op=ALU.is_ge), ['r_le2', 'r_m2'], ['r_mk2'])
        V(lambda e: e.tensor_tensor(out=R1["d"][:], in0=R1["m2"][:], in1=R1["m1"][:], op=ALU.subtract), ['r_m1', 'r_m2'], ['r_d'])
        A(lambda e: e.activation(out=R1["d"][:], in_=R1["d"][:], func=AF.Exp), ['r_d'], ['r_d'])
        V(lambda e: e.tensor_scalar(out=R1["d"][:], in0=R1["d"][:], scalar1=1.0, scalar2=None, op0=ALU.add), ['r_d'], ['r_d'])
        V(lambda e: e.reciprocal(out=R1["w1"][:], in_=R1["d"][:]), ['r_d'], ['r_w1'])
        V(lambda e: e.tensor_scalar(out=R1["w2"][:], in0=R1["w1"][:], scalar1=-1.0, scalar2=1.0, op0=ALU.mult, op1=ALU.add), ['r_w1'], ['r_w2'])
        V(lambda e: e.tensor_tensor(out=R1["w1"][:], in0=R1["w1"][:], in1=R1["psel"][:], op=ALU.mult), ['r_w1', 'r_psel', 'r_w2'], ['r_w1'])
        V(lambda e: e.tensor_tensor(out=R1["w2"][:], in0=R1["w2"][:], in1=R1["psel"][:], op=ALU.mult), ['r_w2', 'r_psel'], ['r_w2'])
        V(lambda e: e.tensor_tensor(out=R4["w12"][:], in0=R4["mk1"][:], in1=bc3(R1["w1"][:, :]), op=ALU.mult), ['r_mk1', 'r_w1'], ['r_w12'])
        V(lambda e: e.tensor_tensor(out=R4["tq"][:], in0=R4["mk2"][:], in1=bc3(R1["w2"][:, :]), op=ALU.mult), ['r_mk2', 'r_w2'], ['r_tq'])
        V(lambda e: e.tensor_tensor(out=R4["w12"][:], in0=R4["w12"][:], in1=R4["tq"][:], op=ALU.add), ['r_w12', 'r_tq'], ['r_w12'])
        V(lambda e: e.tensor_tensor(out=cw4[:].rearrange("p j (g e) -> p j g e", g=4), in0=R4["oh"][:, :, :].unsqueeze(3).broadcast_to([128, 4, 4, 4]),
                                    in1=R4["w12"][:, :, :].unsqueeze(2).broadcast_to([128, 4, 4, 4]), op=ALU.mult), ['r_oh', 'r_w12'], ['cw4'])
        V(lambda e: e.tensor_copy(out=cwb4[:], in_=cw4[:]), ['cw4'], ['cwb4'])
        for j in range(4):
            TR(pb[0][0:16, 0:128], cwb4[:, j, :], identb[:], ['cwb4', 'identb'], ['pb0'])
            V(lambda e, j=j: e.tensor_copy(out=cwTb[:, j * 128:(j + 1) * 128], in_=pb[0][0:16, 0:128]), ['pb0'], ['cwTb'])
        D(h2T_d.rearrange("(kc p) t -> p kc t", p=128)[:, :, t0:t0 + 512], h2Tb[:], r=['h2Tb'], w=['h2T_d'])
        D(cwT_d[:, t0:t0 + 512], cwTb[:], r=['cwTb'], w=['cwT_d'])
    c.barrier()
    c.release(base_mark)
    if 'dbg_x1' in dbg_out:
        tmpx = c.sb([128, TH // 128, 1024], F32, "tmpx")
        D(tmpx[:], x1_d.rearrange("(j p) d -> p j d", p=128), r=['x1_d'], w=['tmpx'])
        D(dbg_out['dbg_x1'].rearrange("(j p) d -> p j d", p=128), tmpx[:], r=['tmpx'])
        tmpc = c.sb([16, TH], BF16, "tmpc"); tmpcf = c.sb([16, TH], F32, "tmpcf")
        D(tmpc[:], cwT_d, r=['cwT_d'], w=['tmpc'])
        V(lambda e, tmpcf=tmpcf, tmpc=tmpc: e.tensor_copy(out=tmpcf[:], in_=tmpc[:]), ['tmpc'], ['tmpcf'])
        D(dbg_out['dbg_cw'], tmpcf[:], r=['tmpcf'])
        c.barrier()
        c.release(base_mark)

    if stop == 'D':
        c.emit()
        return nc
    ST = min(TH, 1024)
    NSB = ST // 128
    gf2 = c.sb([128, 8], F32, "gf2")
    D(gf2[:], gffnT, w=['gf2'])
    selb = c.sb([16, 2048], BF16, "selb")
    m_ = c.mark()
    selst = c.sb([16, 2048], F32, "selst")
    D(selst[:], sel_d, w=['selst'])
    V(lambda e: e.tensor_copy(out=selb[:], in_=selst[:]), ['selst'], ['selb'])
    gfin_t = c.sb([128, 1024], F32, "gfin")
    D(gfin_t[:], gfin, w=['gfin'])
    h2T = c.sb([128, 8, ST], BF16, "h2T")
    cwT = c.sb([16, ST], BF16, "cwT")
    acc = c.sb([128, NSB, 1024], F32, "acc")
    Wg_b = [c.sb([128, 8, 256], BF16, "Wg") for _ in range(2)]
    Wu_b = [c.sb([128, 8, 256], BF16, "Wu") for _ in range(2)]
    Wd_b = [c.sb([128, 2, 1024], BF16, "Wd") for _ in range(2)]
    wstg = [[c.sb([128, 8, 256], F32, "wstg") for _ in range(2)] for _ in range(2)]
    wstd = [c.sb([128, 2, 1024], F32, "wstd") for _ in range(2)]
    bcs_b = [c.sb([128, 512], BF16, "bcs") for _ in range(2)]
    sgl_b = [c.sb([128, 512], F32, "sgl") for _ in range(2)]
    tmu_b = [c.sb([128, 512], F32, "tmu") for _ in range(2)]
    actT_b = [c.sb([128, 2, 512], BF16, "actT") for _ in range(2)]
    x1f = c.sb([128, 1024], F32, "x1f")
    ssf = c.sb([128, 1], F32, "ssf")
    junk3 = c.sb([128, 1024], BF16, "junk3")
    outt = [c.sb([128, 1024], F32, "outt") for _ in range(2)]
    NST = TH // ST
    NSUB = ST // 512

    def load_expert(n):
        ex, sl = n % 16, n % 2
        Wg, Wu, Wd = Wg_b[sl], Wu_b[sl], Wd_b[sl]
        wk = f'We{sl}'
        D(wstg[sl][0][:], w_eg[ex].rearrange("(kc p) f -> p kc f", p=128), w=[f'wstg{sl}0'])
        D(wstg[sl][1][:], w_eu[ex].rearrange("(kc p) f -> p kc f", p=128), w=[f'wstg{sl}1'])
        D(wstd[sl][:], w_ed[ex].rearrange("(fc p) d -> p fc d", p=128), w=[f'wstd{sl}'])
        for kc in range(8):
            A(lambda e, kc=kc, Wg=Wg, sl=sl: e.activation(out=Wg[:, kc, :], in_=wstg[sl][0][:, kc, :], func=AF.Copy, scale=gf2[:, kc:kc + 1]), [f'wstg{sl}0', 'gf2'], [wk])
            A(lambda e, kc=kc, Wu=Wu, sl=sl: e.activation(out=Wu[:, kc, :], in_=wstg[sl][1][:, kc, :], func=AF.Copy, scale=gf2[:, kc:kc + 1]), [f'wstg{sl}1', 'gf2'], [wk])
        A(lambda e, Wd=Wd, sl=sl: e.activation(out=Wd[:, 0, :], in_=wstd[sl][:, 0, :], func=AF.Copy), [f'wstd{sl}'], [wk])
        V(lambda e, Wd=Wd, sl=sl: e.tensor_copy(out=Wd[:, 1, :], in_=wstd[sl][:, 1, :]), [f'wstd{sl}'], [wk])

    ucount = [0]

    def moe_stage1(n, sub):
        ex, sl = n % 16, n % 2
        Wg, Wu = Wg_b[sl], Wu_b[sl]
        wk = f'We{sl}'
        ub = ucount[0] % 2
        ucount[0] += 1
        bcs, sgl, tmu, actT = bcs_b[ub], sgl_b[ub], tmu_b[ub], actT_b[ub]
        q0 = sub * 512
        p = nps()
        MM(ps[p][:, :], selb[:, ex * 128:(ex + 1) * 128], cwT[:, q0:q0 + 512], True, True, ['selb', 'cwT'], [f'ps{p}'])
        A(lambda e, p=p, bcs=bcs: e.activation(out=bcs[:], in_=ps[p][:, :], func=AF.Copy), [f'ps{p}'], [f'bcs{ub}'])
        for fc in range(2):
            pg = nps()
            for kc in range(8):
                MM(ps[pg][:, :], Wg[:, kc, fc * 128:(fc + 1) * 128], h2T[:, kc, q0:q0 + 512], kc == 0, kc == 7, [wk, 'h2T'], [f'ps{pg}'])
            pu = nps()
            for kc in range(8):
                MM(ps[pu][:, :], Wu[:, kc, fc * 128:(fc + 1) * 128], h2T[:, kc, q0:q0 + 512], kc == 0, kc == 7, [wk, 'h2T'], [f'ps{pu}'])
            A(lambda e, pg=pg, sgl=sgl: e.activation(out=sgl[:], in_=ps[pg][:, :], func=AF.Silu), [f'ps{pg}'], [f'sgl{ub}'])
            V(lambda e, pu=pu, sgl=sgl, tmu=tmu: e.tensor_tensor(out=tmu[:], in0=sgl[:], in1=ps[pu][:, :], op=ALU.mult), [f'sgl{ub}', f'ps{pu}'], [f'tmu{ub}'])
            G(lambda e, fc=fc, tmu=tmu, bcs=bcs, actT=actT: e.tensor_tensor(out=actT[:, fc, :], in0=tmu[:], in1=bcs[:], op=ALU.mult), [f'tmu{ub}', f'bcs{ub}'], [f'actT{ub}'])
        return ub

    def moe_stage2(n, sub, ub):
        sl = n % 2
        Wd = Wd_b[sl]
        wk = f'We{sl}'
        actT = actT_b[ub]
        for j in range(4):
            blk = sub * 4 + j
            for hf in range(2):
                p = nps()
                for fc in range(2):
                    MM(ps[p][:, :], actT[:, fc, j * 128:(j + 1) * 128], Wd[:, fc, hf * 512:(hf + 1) * 512], fc == 0, fc == 1, [f'actT{ub}', wk], [f'ps{p}'])
                V(lambda e, p=p, blk=blk, hf=hf: e.tensor_tensor(out=acc[:, blk, hf * 512:(hf + 1) * 512], in0=acc[:, blk, hf * 512:(hf + 1) * 512], in1=ps[p][:, :], op=ALU.add), ['acc', f'ps{p}'], ['acc'])

    load_expert(0)
    for sti in range(NST):
        s0 = sti * ST
        D(h2T[:], h2T_d.rearrange("(kc p) t -> p kc t", p=128)[:, :, s0:s0 + ST], r=['h2T_d'], w=['h2T'])
        D(cwT[:], cwT_d[:, s0:s0 + ST], r=['cwT_d'], w=['cwT'])
        G(lambda e: e.memset(acc[:], 0.0), (), ['acc'])
        units = [(sti * 16 + ex, sub) for ex in range(16) for sub in range(NSUB)]
        prev = None
        for i in range(len(units) + 1):
            cur = None
            if i < len(units):
                n, sub = units[i]
                ub = moe_stage1(n, sub)
                cur = (n, sub, ub)
            if prev is not None:
                moe_stage2(*prev)
            if cur is not None and cur[1] == 0 and cur[0] + 1 < NST * 16:
                load_expert(cur[0] + 1)
            prev = cur
        for blk in range(NSB):
            r0 = s0 + blk * 128
            ot, otk = outt[blk % 2], f'outt{blk % 2}'
            D(x1f[:], x1_d[r0:r0 + 128, :], r=['x1_d'], w=['x1f'])
            V(lambda e, blk=blk: e.tensor_tensor(out=x1f[:], in0=x1f[:], in1=acc[:, blk, :], op=ALU.add), ['x1f', 'acc'], ['x1f'])
            A(lambda e: e.activation(out=junk3[:], in_=x1f[:], func=AF.Square), ['x1f'], ['junk3'])
            V(lambda e: e.tensor_reduce(out=ssf[:, 0:1], in_=junk3[:], axis=mybir.AxisListType.X, op=ALU.add), ['junk3'], ['Fss'])
            m0 = c.mark()
            rsf = rstd_of(ssf, 1, 'F')
            V(lambda e, ot=ot, rsf=rsf: e.scalar_tensor_tensor(out=ot[:], in0=x1f[:], scalar=rsf[:, 0:1], in1=gfin_t[:], op0=ALU.mult, op1=ALU.mult), ['x1f', 'Frs', 'gfin'], [otk])
            c.release(m0)
            D(out_d[r0:r0 + 128, :], ot[:], r=[otk], w=['out_d'])
    c.emit()
    return nc


def _ssm_layouts(lambda_re, lambda_im, log_step, b_re, b_im, c_re, c_im, ssm_d):
    f = np.float32
    o = {}
    r = np.arange(128)
    k4_r, mp_r, cp_r = r // 32, (r // 16) % 2, r % 16
    s_ = np.arange(4)
    q = np.arange(128)
    m_q, p_q = q // 64, q % 64
    g = 8 * s_[None, :, None] + 2 * k4_r[:, None, None] + m_q[None, None, :]
    P = np.broadcast_to(p_q[None, None, :], g.shape)
    o["LRX"] = lambda_re[g, P].reshape(128, 512).astype(f)
    o["LIX"] = lambda_im[g, P].reshape(128, 512).astype(f)
    o["LSX"] = log_step[g].reshape(128, 512).astype(f)
    msk = (mp_r[:, None, None] == m_q[None, None, :])
    CP = np.broadcast_to(cp_r[:, None, None], g.shape)
    o["BRX"] = np.where(msk, b_re[g, P, CP], 0).reshape(128, 512).astype(f)
    o["BIX"] = np.where(msk, b_im[g, P, CP], 0).reshape(128, 512).astype(f)
    k = np.arange(16)
    j = np.arange(32)
    mp_j, c_j = j // 16, j % 16
    g2 = 2 * k[None, :, None] + m_q[:, None, None] + 0 * j[None, None, :]
    P2 = np.broadcast_to(p_q[:, None, None], g2.shape)
    C2 = np.broadcast_to(c_j[None, None, :], g2.shape)
    msk2 = (mp_j[None, None, :] == m_q[:, None, None])
    o["LRY"] = lambda_re[g2, P2].reshape(128, 512).astype(f)
    o["LIY"] = lambda_im[g2, P2].reshape(128, 512).astype(f)
    o["LSY"] = log_step[g2].reshape(128, 512).astype(f)
    o["CRY"] = np.where(msk2, c_re[g2, C2, P2], 0).reshape(128, 512).astype(f)
    o["CIY"] = np.where(msk2, c_im[g2, C2, P2], 0).reshape(128, 512).astype(f)
    o["BRY"] = np.where(msk2, b_re[g2, P2, C2], 0).reshape(128, 512).astype(f)
    o["BIY"] = np.where(msk2, b_im[g2, P2, C2], 0).reshape(128, 512).astype(f)
    dd = np.zeros((128, 4, 128), f)
    for s in range(4):
        dd[r, s, r] = ssm_d[128 * s + r]
    o["DDG"] = dd.reshape(128, 512)
    return o


_CACHE = {}


def _prep_common(inp):
    f = np.float32
    A_ = lambda a: np.ascontiguousarray(a, dtype=f)
    d = {}
    d["w_in"] = A_(inp["w_in"][0])
    d["gmixT"] = A_(inp["g_mix"][0].reshape(8, 128).T)
    d["bforget"] = A_(inp["b_forget"][0].reshape(8, 1))
    d["bgate"] = A_(inp["b_gate"][0].reshape(16, 128).T)
    d["w_out_a"] = A_(inp["w_out_a"][0])
    d["w_glu"] = A_(inp["w_glu"][0])
    d["bglu"] = A_(inp["b_glu"][0].reshape(4, 128).T)
    d["w_out_b"] = A_(inp["w_out_b"][0])
    d["w_out"] = A_(inp["w_out"][0])
    d["gffnT"] = A_(inp["g_ffn"][0].reshape(8, 128).T)
    d["wr"] = A_(np.concatenate([inp["w_router_group"][0], inp["w_router_expert"][0]], axis=1))
    d["br"] = A_(np.broadcast_to(np.concatenate([inp["b_router_group"][0], inp["b_router_expert"][0]])[None, :], (128, 20)))
    d["w_eg"] = A_(inp["w_exp_gate"][0])
    d["w_eu"] = A_(inp["w_exp_up"][0])
    d["w_ed"] = A_(inp["w_exp_down"][0])
    d["gfin"] = A_(np.broadcast_to(inp["g_final"][None, :], (128, 1024)))
    d.update(_ssm_layouts(np.asarray(inp["lambda_re"][0]), np.asarray(inp["lambda_im"][0]), np.asarray(inp["log_step"][0]),
                          np.asarray(inp["ssm_b_re"][0]), np.asarray(inp["ssm_b_im"][0]), np.asarray(inp["ssm_c_re"][0]),
                          np.asarray(inp["ssm_c_im"][0]), np.asarray(inp["ssm_d"][0])))
    d["ident"] = np.eye(128, dtype=f)
    d["tri"] = np.triu(np.ones((128, 128), f))
    sel = np.zeros((16, 16, 128), f)
    for e in range(16):
        sel[e, e, :] = 1.0
    d["sel"] = sel.reshape(16, 2048)
    return d


def run(inputs, dbg=(), stop=None):
    inp = {k: np.asarray(v) for k, v in inputs.items()}
    x = inp["x"]
    B, S, _ = x.shape
    TH = S // 2
    key = (TH, tuple(dbg), stop)
    if key not in _CACHE:
        _CACHE[key] = build_program(TH, dbg, stop)
    nc = _CACHE[key]
    common = _prep_common(inp)
    in_maps = []
    for core in range(8):
        b, par = core // 2, core % 2
        xin = np.zeros((S, 1024), np.float32)
        if par == 1:
            xin[:TH] = x[b, :TH]
        xin[TH:] = x[b, par * TH:(par + 1) * TH]
        m = dict(common)
        m["xin"] = xin
        m["flag"] = np.full((128, 1), float(par), np.float32)
        in_maps.append(m)
    res = run_bass_kernel_spmd(nc, in_maps, core_ids=list(range(8)))
    return res, TH


def kernel(**inputs):
    res, TH = run(inputs)
    x = inputs["x"]
    B, S, Dm = x.shape
    out = np.zeros((B, S, Dm), np.float32)
    for core in range(8):
        b, par = core // 2, core % 2
        out[b, par * TH:(par + 1) * TH] = res.results[core]["out"]
    return out
```

```python
import contextlib
import numpy as np
import concourse.bass as bass
import concourse.mybir as mybir
from concourse.bass_utils import run_bass_kernel_spmd

F32 = mybir.dt.float32
BF16 = mybir.dt.bfloat16
I32 = mybir.dt.int32
AF = mybir.ActivationFunctionType
ALU = mybir.AluOpType

NDSEM = 48
SAME_SYNC = {'pe': False, 'act': True, 'dve': True, 'pool': True, 'sp': False}
EPS = 1e-6
L = 16
TWO_PI = 6.283185307179586


class Ctx:
    def __init__(self, nc):
        self.nc = nc
        self.names = ['pe', 'act', 'dve', 'pool', 'sp']
        self.ops = {e: [] for e in self.names}
        self.cnt = {e: 0 for e in self.names}
        self.seen = {e: {} for e in self.names}
        self.pending = {e: {} for e in self.names}
        self.lastw = {}
        self.readers = {}
        self.dval = [0] * NDSEM
        self.dnext = 0
        self.sb_off = 16640
        self.uid = 0
        self.sb_max = 0

    def sb(self, shape, dtype, name="t"):
        esz = {F32: 4, BF16: 2, I32: 4}[dtype]
        n = 1
        for s in shape[1:]:
            n *= s
        off = (self.sb_off + 63) // 64 * 64
        self.sb_off = off + n * esz
        self.sb_max = max(self.sb_max, self.sb_off)
        assert self.sb_off <= 229376, f"SBUF overflow {self.sb_off} ({name})"
        self.uid += 1
        return self.nc.alloc_sbuf_tensor_at(f"{name}_{self.uid}", list(shape), dtype, offset=off)

    def mark(self):
        return self.sb_off

    def release(self, m):
        self.sb_off = m

    def _deps(self, reads, writes):
        deps = {}
        for k in reads:
            t = self.lastw.get(k)
            if t and deps.get(t[0], 0) < t[1]:
                deps[t[0]] = t[1]
        for k in writes:
            t = self.lastw.get(k)
            if t and deps.get(t[0], 0) < t[1]:
                deps[t[0]] = t[1]
            for s, v in self.readers.get(k, {}).items():
                if deps.get(s, 0) < v:
                    deps[s] = v
        return deps

    def _waits(self, e, deps):
        for s, v in self.pending[e].items():
            if deps.get(s, 0) < v:
                deps[s] = v
        self.pending[e] = {}
        waits = []
        for s, v in deps.items():
            if s == e and not SAME_SYNC[e]:
                continue
            if self.seen[e].get(s, 0) >= v:
                continue
            self.seen[e][s] = v
            waits.append((s, v))
        return waits

    def _commit(self, tok, reads, writes):
        for k in reads:
            r = self.readers.setdefault(k, {})
            if r.get(tok[0], 0) < tok[1]:
                r[tok[0]] = tok[1]
        for k in writes:
            self.lastw[k] = tok
            self.readers[k] = {}

    def op(self, e, fn, reads=(), writes=()):
        deps = self._deps(reads, writes)
        waits = self._waits(e, deps)
        self.cnt[e] += 1
        tok = (e, self.cnt[e])
        self.ops[e].append((waits, fn, e))
        self._commit(tok, reads, writes)
        return tok

    def dma(self, e, out, in_, reads=(), writes=()):
        i = self.dnext
        self.dnext = (self.dnext + 1) % NDSEM
        deps = self._deps(reads, writes)
        if self.dval[i] > 0:
            s = ('d', i)
            if deps.get(s, 0) < self.dval[i]:
                deps[s] = self.dval[i]
        waits = self._waits(e, deps)
        self.dval[i] += 16
        tok = (('d', i), self.dval[i])
        self.ops[e].append((waits, lambda eng: eng.dma_start(out=out, in_=in_), ('d', i)))
        self._commit(tok, reads, writes)
        return tok

    def barrier(self):
        allt = {e: self.cnt[e] for e in self.names if self.cnt[e] > 0}
        for i in range(NDSEM):
            if self.dval[i] > 0:
                allt[('d', i)] = self.dval[i]
        for e in self.names:
            for s, v in allt.items():
                if self.pending[e].get(s, 0) < v:
                    self.pending[e][s] = v

    def emit(self):
        nc = self.nc
        self.barrier()
        self.op('sp', lambda eng: eng.nop(), (), ())
        with contextlib.ExitStack() as st:
            sems = {e: st.enter_context(nc.semaphore(f"s_{e}")) for e in self.names}
            for i in range(NDSEM):
                sems[('d', i)] = st.enter_context(nc.semaphore(f"d_{i}"))
            block = st.enter_context(nc.Block())

            def run(e, eng):
                for waits, fn, inc in self.ops[e]:
                    for s, v in waits:
                        eng.wait_ge(sems[s], v)
                    ins = fn(eng)
                    if isinstance(inc, tuple):
                        ins.then_inc(sems[inc], 16)
                    else:
                        ins.then_inc(sems[inc], 1)

            @block.sync
            def _(eng):
                run('sp', eng)

            @block.tensor
            def _(eng):
                run('pe', eng)

            @block.scalar
            def _(eng):
                run('act', eng)

            @block.vector
            def _(eng):
                run('dve', eng)

            @block.gpsimd
            def _(eng):
                run('pool', eng)


def build_program(TH, dbg=(), stop=None):
    nc = bass.Bass("TRN2", target_bir_lowering=False)
    T2 = 2 * TH
    NT = TH // 512
    NBH = TH // 128
    NCH = TH // L
    c = Ctx(nc)

    def din(name, shape, dt=F32):
        return nc.dram_tensor(name, list(shape), dt, kind="ExternalInput").ap()

    def dscr(name, shape, dt):
        return nc.dram_tensor(name, list(shape), dt).ap()

    xin = din("xin", [T2, 1024])
    flag_d = din("flag", [128, 1])
    w_in = din("w_in", [1024, 4104])
    gmixT = din("gmixT", [128, 8])
    bforget = din("bforget", [8, 1])
    bgate = din("bgate", [128, 16])
    w_out_a = din("w_out_a", [512, 1024])
    w_glu = din("w_glu", [512, 512])
    bglu = din("bglu", [128, 4])
    w_out_b = din("w_out_b", [512, 1024])
    w_out = din("w_out", [1024, 1024])
    gffnT = din("gffnT", [128, 8])
    wr = din("wr", [1024, 20])
    br = din("br", [128, 20])
    w_eg = din("w_eg", [16, 1024, 256])
    w_eu = din("w_eu", [16, 1024, 256])
    w_ed = din("w_ed", [16, 256, 1024])
    gfin = din("gfin", [128, 1024])
    ssm_names = ["LRX", "LIX", "LSX", "BRX", "BIX", "LRY", "LIY", "LSY", "CRY", "CIY", "BRY", "BIY"]
    ssm_in = {n: din(n, [128, 512]) for n in ssm_names}
    ddg = din("DDG", [128, 512])
    ident_d = din("ident", [128, 128])
    tri_d = din("tri", [128, 128])
    sel_d = din("sel", [16, 2048])
    out_d = nc.dram_tensor("out", [TH, 1024], F32, kind="ExternalOutput").ap()
    dbg_out = {}
    for name, shape in dbg:
        dbg_out[name] = nc.dram_tensor(name, list(shape), F32, kind="ExternalOutput").ap()

    kT_d = dscr("kT_d", [8, 70, T2], BF16)
    qT_d = dscr("qT_d", [8, 70, TH], BF16)
    v_d = dscr("v_d", [T2, 520], BF16)
    uT_d = dscr("uT_d", [512, T2], BF16)
    gT_d = dscr("gT_d", [2048, TH], BF16)
    oT_d = dscr("oT_d", [8, 64, TH], BF16)
    zT_d = dscr("zT_d", [512, TH], BF16)
    x1_d = dscr("x1_d", [TH, 1024], F32)
    h2T_d = dscr("h2T_d", [1024, TH], BF16)
    cwT_d = dscr("cwT_d", [16, TH], BF16)

    ps = [nc.alloc_psum_tensor(f"ps{i}", [128, 512], F32) for i in range(6)]
    pb = [nc.alloc_psum_tensor(f"pb{i}", [128, 1024], BF16) for i in range(2)]

    def V(fn, r=(), w=()):
        return c.op('dve', fn, r, w)

    def A(fn, r=(), w=()):
        return c.op('act', fn, r, w)

    def G(fn, r=(), w=()):
        return c.op('pool', fn, r, w)

    def MM(out, lhsT, rhs, st, sp_, r, w, tp=None):
        kw = dict(start=st, stop=sp_)
        if tp is not None:
            kw['tile_position'] = tp
        return c.op('pe', lambda e: e.matmul(out, lhsT=lhsT, rhs=rhs, **kw), r, w)

    def TR(out, in_, ident, r, w):
        return c.op('pe', lambda e: e.transpose(out, in_, ident), r, w)

    def D(out, in_, r=(), w=(), q='sp'):
        return c.dma(q, out, in_, r, w)


    dump_i = [0]
    dump_st = []

    def dump(name, ap2d, ncols):
        if name not in dbg_out:
            return
        for c0 in range(0, ncols, 2048):
            n = min(2048, ncols - c0)
            dump_i[0] += 1
            kx = 'dump0'
            if not dump_st:
                dump_st.append(c.sb([128, 2048], F32, "dumpst"))
            stt = dump_st[0]
            V(lambda e, stt=stt, c0=c0, n=n: e.tensor_copy(out=stt[:, 0:n], in_=ap2d[:, c0:c0 + n]), [], [kx])
            D(dbg_out[name][:, c0:c0 + n], stt[:, 0:n], r=[kx])
    identf = c.sb([128, 128], F32, "identf")
    identb = c.sb([128, 128], BF16, "identb")
    trib = c.sb([128, 128], BF16, "trib")
    onesf = c.sb([128, 512], F32, "onesf")
    flag = c.sb([128, 1], F32, "flag")
    stg = c.sb([128, 128], F32, "stg")
    D(identf[:], ident_d, w=['identf'])
    D(stg[:], tri_d, w=['stg'])
    D(flag[:], flag_d, w=['flag'])
    V(lambda e: e.tensor_copy(out=identb[:], in_=identf[:]), ['identf'], ['identb'])
    V(lambda e: e.tensor_copy(out=trib[:], in_=stg[:]), ['stg'], ['trib'])
    V(lambda e: e.memset(onesf[:], 1.0), (), ['onesf'])
    base_mark = c.mark()

    def rstd_of(ss, n, tag):
        ms = c.sb([128, n], F32, "ms")
        rs = c.sb([128, n], F32, "rs")
        V(lambda e: e.tensor_scalar(out=ms[:], in0=ss[:], scalar1=1.0 / 1024, scalar2=EPS, op0=ALU.mult, op1=ALU.add), [tag + 'ss'], [tag + 'ms'])
        A(lambda e: e.activation(out=ms[:], in_=ms[:], func=AF.Sqrt), [tag + 'ms'], [tag + 'ms'])
        V(lambda e: e.reciprocal(out=rs[:], in_=ms[:]), [tag + 'ms'], [tag + 'rs'])
        return rs

    Win = c.sb([128, 8, 4104], BF16, "Win")
    gm = c.sb([128, 8], F32, "gm")
    negb = c.sb([8, 1], F32, "negb")
    bg = c.sb([128, 16], F32, "bg")
    D(gm[:], gmixT, w=['gm'])
    D(negb[:], bforget, w=['negb'])
    D(bg[:], bgate, w=['bg'])
    V(lambda e: e.tensor_scalar(out=negb[:], in0=negb[:], scalar1=-1.0, scalar2=None, op0=ALU.mult), ['negb'], ['negb'])
    wst = [c.sb([128, 8, 256], F32, "wst")] * 2
    w_in_v = w_in.rearrange("(kc p) n -> p kc n", p=128)
    ei = 0
    for cc in range(17):
        c0 = cc * 256
        ncol = min(256, 4104 - c0)
        st = wst[cc % 2]
        sk = 'wst'
        D(st[:, :, 0:ncol], w_in_v[:, :, c0:c0 + ncol], w=[sk])
        for kc in range(8):
            if cc % 2 == 0:
                A(lambda e, st=st, kc=kc, c0=c0, ncol=ncol: e.activation(out=Win[:, kc, c0:c0 + ncol], in_=st[:, kc, 0:ncol], func=AF.Copy, scale=gm[:, kc:kc + 1]), [sk, 'gm'], ['Win0'])
            else:
                V(lambda e, st=st, kc=kc, c0=c0, ncol=ncol: e.tensor_scalar(out=Win[:, kc, c0:c0 + ncol], in0=st[:, kc, 0:ncol], scalar1=gm[:, kc:kc + 1], scalar2=None, op0=ALU.mult), [sk, 'gm'], ['Win1'])
            ei += 1

    CQ, CK, CV, CF, CU, CG = 0, 512, 1024, 1536, 1544, 2056
    xt_b = [c.sb([128, 4, 1024], F32, "xt") for _ in range(2)]
    xs = c.sb([128, 4, 1024], BF16, "xs")
    hT_b = [c.sb([128, 8, 512], BF16, "hT") for _ in range(2)]
    junk = c.sb([128, 1024], BF16, "junk")
    junk_b = [junk, c.sb([128, 1024], BF16, "junkb")]
    ss = c.sb([128, 4], F32, "ss")
    kT_s = [c.sb([128, 4, 512], BF16, "kTs")] * 2
    qT_s = [c.sb([128, 4, 512], BF16, "qTs")] * 2
    v_s = [c.sb([128, 4, 8, 65], BF16, "vs")] * 2
    uT_s = [c.sb([128, 4, 512], BF16, "uTs")] * 2
    gT_s = [c.sb([128, 16, 512], BF16, "gTs")] * 2
    CPK = c.sb([8, 6, 512], BF16, "CPK")
    CPQ = c.sb([8, 6, 512], BF16, "CPQ")
    e1 = c.sb([8, 512], F32, "e1")
    negc = c.sb([8, 512], F32, "negc")
    r1 = c.sb([8, 512], F32, "r1")
    carry = c.sb([8, 1], F32, "carry")
    ones_own = c.sb([128, 32], BF16, "ones_own")
    ones_ctx = c.sb([128, 32], BF16, "ones_ctx")
    V(lambda e: e.memset(ones_own[:], 1.0), (), ['ones_own'])
    V(lambda e: e.tensor_scalar(out=ones_ctx[:], in0=onesf[:, 0:32], scalar1=flag[:, 0:1], scalar2=None, op0=ALU.mult), ['onesf', 'flag'], ['ones_ctx'])
    V(lambda e: e.memset(CPK[:, 0:3, :], 1.0), (), ['CPK'])
    V(lambda e: e.memset(CPQ[:, 3:6, :], 1.0), (), ['CPQ'])
    V(lambda e: e.memset(carry[:], 0.0), (), ['carry'])

    xin_v = xin.rearrange("(t j p) d -> t p j d", j=4, p=128)
    D(xt_b[0][:], xin_v[0], w=['xt0'])
    pi = [0]

    def nps():
        pi[0] = (pi[0] + 1) % 6
        return pi[0]

    for i in range(2 * NT):
        own = i >= NT
        b = i % 2
        xt, xk = xt_b[b], f'xt{b}'
        hT, hk = hT_b[b], f'hT{b}'
        if i + 1 < 2 * NT:
            D(xt_b[1 - b][:], xin_v[i + 1], w=[f'xt{1 - b}'])
        for j in range(4):
            jk_, jkk_ = junk_b[j % 2], f'junk{j % 2}'
            A(lambda e, j=j, xt=xt, jk_=jk_: e.activation(out=jk_[:], in_=xt[:, j, :], func=AF.Square), [xk], [jkk_])
            V(lambda e, j=j, jk_=jk_: e.tensor_reduce(out=ss[:, j:j + 1], in_=jk_[:], axis=mybir.AxisListType.X, op=ALU.add), [jkk_], ['Ass'])
        m0 = c.mark()
        rs = rstd_of(ss, 4, 'A')
        for j in range(4):
            V(lambda e, j=j, xt=xt, rs=rs: e.tensor_scalar(out=xs[:, j, :], in0=xt[:, j, :], scalar1=rs[:, j:j + 1], scalar2=None, op0=ALU.mult), [xk, 'Ars'], [f'xs{j}'])
        c.release(m0)
        for j in range(4):
            pbk = j % 2
            for kc in range(8):
                TR(pb[pbk][:, kc * 128:(kc + 1) * 128], xs[:, j, kc * 128:(kc + 1) * 128], identb[:], [f'xs{j}', 'identb'], [f'pb{pbk}'])
            src = pb[pbk][:, :].rearrange("p (k t) -> p k t", k=8)
            if j % 2 == 0:
                A(lambda e, j=j, hT=hT, src=src: e.activation(out=hT[:, :, j * 128:(j + 1) * 128], in_=src, func=AF.Copy), [f'pb{pbk}'], [hk])
            else:
                V(lambda e, j=j, hT=hT, src=src: e.tensor_copy(out=hT[:, :, j * 128:(j + 1) * 128], in_=src), [f'pb{pbk}'], [hk])
        tok0 = i * 512
        p = nps()
        for kc in range(8):
            MM(ps[p][0:8, :], Win[:, kc, CF:CF + 8], hT[:, kc, :], kc == 0, kc == 7, ['Win0', 'Win1', hk], [f'ps{p}'])
        A(lambda e, p=p: e.activation(out=e1[:], in_=ps[p][0:8, :], func=AF.Exp, scale=-1.0, bias=negb[:, 0:1]), [f'ps{p}', 'negb'], ['e1'])
        A(lambda e: e.activation(out=e1[:], in_=e1[:], func=AF.Ln, bias=1.0), ['e1'], ['e1'])
        V(lambda e: e.tensor_tensor_scan(out=negc[:], data0=onesf[0:8, 0:512], data1=e1[:], initial=carry[:, 0:1], op0=ALU.mult, op1=ALU.add), ['e1', 'carry', 'onesf'], ['negc'])
        V(lambda e: e.tensor_copy(out=carry[:], in_=negc[:, 511:512]), ['negc'], ['carry'])
        V(lambda e: e.tensor_copy(out=CPK[:, 3, :], in_=negc[:]), ['negc'], ['CPK'])
        V(lambda e: e.tensor_tensor(out=r1[:], in0=negc[:], in1=CPK[:, 3, :], op=ALU.subtract), ['negc', 'CPK'], ['r1'])
        V(lambda e: e.tensor_copy(out=CPK[:, 4, :], in_=r1[:]), ['r1'], ['CPK'])
        V(lambda e: e.tensor_tensor(out=r1[:], in0=r1[:], in1=CPK[:, 4, :], op=ALU.subtract), ['r1', 'CPK'], ['r1'])
        V(lambda e: e.tensor_copy(out=CPK[:, 5, :], in_=r1[:]), ['r1'], ['CPK'])
        D(kT_d[:, 64:70, tok0:tok0 + 512], CPK[:, :, :], r=['CPK'], w=['kT_d'])
        if own:
            V(lambda e: e.tensor_scalar(out=CPQ[:, 0:3, :], in0=CPK[:, 3:6, :], scalar1=-1.0, scalar2=None, op0=ALU.mult), ['CPK'], ['CPQ'])
            D(qT_d[:, 64:70, tok0 - TH:tok0 - TH + 512], CPQ[:, :, :], r=['CPQ'], w=['qT_d'])
        kts, ktk = kT_s[b], 'kTs'
        for hp in range(4):
            p = nps()
            for kc in range(8):
                MM(ps[p][:, :], Win[:, kc, CK + hp * 128:CK + (hp + 1) * 128], hT[:, kc, :], kc == 0, kc == 7, ['Win0', 'Win1', hk], [f'ps{p}'])
            if hp % 2 == 0:
                A(lambda e, p=p, hp=hp, kts=kts: e.activation(out=kts[:, hp, :], in_=ps[p][:, :], func=AF.Copy), [f'ps{p}'], [ktk])
            else:
                V(lambda e, p=p, hp=hp, kts=kts: e.tensor_copy(out=kts[:, hp, :], in_=ps[p][:, :]), [f'ps{p}'], [ktk])
        kv_ = kT_d[:, 0:64, tok0:tok0 + 512].rearrange("(hp two) r t -> two r hp t", two=2)
        for two in range(2):
            D(kv_[two], kts[two * 64:(two + 1) * 64, :, :], r=[ktk], w=['kT_d'])
        vs, vk = v_s[b], 'vs'
        V(lambda e, vs=vs, own=own: e.tensor_copy(out=vs[:, :, :, 64:65], in_=(ones_own if own else ones_ctx)[:, :].rearrange("p (j h o) -> p j h o", j=4, o=1)), ['ones_own', 'ones_ctx'], [vk])
        for j in range(4):
            p = nps()
            for kc in range(8):
                MM(ps[p][:, :], hT[:, kc, j * 128:(j + 1) * 128], Win[:, kc, CV:CV + 512], kc == 0, kc == 7, ['Win0', 'Win1', hk], [f'ps{p}'])
            V(lambda e, p=p, j=j, vs=vs: e.tensor_copy(out=vs[:, j, :, 0:64], in_=ps[p][:, :].rearrange("p (h d) -> p h d", h=8)), [f'ps{p}'], [vk])
        D(v_d[tok0:tok0 + 512, :].rearrange("(j p) c -> p j c", p=128), vs[:, :, :, :].rearrange("p j h c -> p j (h c)"), r=[vk], w=['v_d'])
        us, uk = uT_s[b], 'uTs'
        for m in range(4):
            p = nps()
            for kc in range(8):
                MM(ps[p][:, :], Win[:, kc, CU + m * 128:CU + (m + 1) * 128], hT[:, kc, :], kc == 0, kc == 7, ['Win0', 'Win1', hk], [f'ps{p}'])
            V(lambda e, p=p, m=m, us=us: e.tensor_copy(out=us[:, m, :], in_=ps[p][:, :]), [f'ps{p}'], [uk])
        D(uT_d.rearrange("(s p) t -> p s t", p=128)[:, :, tok0:tok0 + 512], us[:, :, :], r=[uk], w=['uT_d'])
        if own:
            qts, qtk = qT_s[b], 'qTs'
            for hp in range(4):
                p = nps()
                for kc in range(8):
                    MM(ps[p][:, :], Win[:, kc, CQ + hp * 128:CQ + (hp + 1) * 128], hT[:, kc, :], kc == 0, kc == 7, ['Win0', 'Win1', hk], [f'ps{p}'])
                A(lambda e, p=p, hp=hp, qts=qts: e.activation(out=qts[:, hp, :], in_=ps[p][:, :], func=AF.Copy, scale=0.125), [f'ps{p}'], [qtk])
            qv_ = qT_d[:, 0:64, tok0 - TH:tok0 - TH + 512].rearrange("(hp two) r t -> two r hp t", two=2)
            for two in range(2):
                D(qv_[two], qts[two * 64:(two + 1) * 64, :, :], r=[qtk], w=['qT_d'])
            gs, gk = gT_s[b], 'gTs'
            for m in range(16):
                p = nps()
                for kc in range(8):
                    MM(ps[p][:, :], Win[:, kc, CG + m * 128:CG + (m + 1) * 128], hT[:, kc, :], kc == 0, kc == 7, ['Win0', 'Win1', hk], [f'ps{p}'])
                A(lambda e, p=p, m=m, gs=gs: e.activation(out=gs[:, m, :], in_=ps[p][:, :], func=AF.Sigmoid, bias=bg[:, m:m + 1]), [f'ps{p}', 'bg'], [gk])
            D(gT_d.rearrange("(m p) t -> p m t", p=128)[:, :, tok0 - TH:tok0 - TH + 512], gs[:, :, :], r=[gk], w=['gT_d'])

    c.barrier()
    c.release(base_mark)
    if 'dbg_k' in dbg_out:
        tmpk = c.sb([70, T2], BF16, "tmpk")
        tmpf = c.sb([70, T2], F32, "tmpf")
        D(tmpk[:], kT_d[0], r=['kT_d'], w=['tmpk'])
        V(lambda e, tmpf=tmpf, tmpk=tmpk: e.tensor_copy(out=tmpf[:], in_=tmpk[:]), ['tmpk'], ['tmpf'])
        D(dbg_out['dbg_k'], tmpf[:], r=['tmpf'])
        c.barrier()
        c.release(base_mark)

    if stop == 'A':
        c.emit()
        return nc
    NB2 = T2 // 128
    v_all = c.sb([128, NB2, 520], BF16, "v_all")
    for q4 in range(0, NB2, 8):
        n = min(8, NB2 - q4)
        D(v_all[:, q4:q4 + n, :], v_d[q4 * 128:(q4 + n) * 128, :].rearrange("(j p) c -> p j c", p=128), r=['v_d'], w=[f'v_all{q4 // 8}'])
    kT_h = [c.sb([70, T2], BF16, "kTh") for _ in range(2)]
    qT_h = [c.sb([70, TH], BF16, "qTh") for _ in range(2)]
    pT_b = [c.sb([128, 512], BF16, "pT") for _ in range(3)]
    rr = c.sb([128, 512], F32, "rr")
    rrh = c.sb([128, 512], BF16, "rrh")
    rrl = c.sb([128, 512], BF16, "rrl")
    onesb = c.sb([128, 64], BF16, "onesb")
    V(lambda e: e.memset(onesb[:], 1.0), (), ['onesb'])
    bc_sb = c.sb([64, 512], F32, "bc_sb")
    oT_s = [c.sb([64, 512], BF16, "oTs") for _ in range(2)]
    D(kT_h[0][:], kT_d[0], r=['kT_d'], w=['kTh0'])
    D(qT_h[0][:], qT_d[0], r=['qT_d'], w=['qTh0'])
    items = []
    gi = 0
    for h in range(8):
        for Gq in range(NT):
            kbs = list(range(NBH)) + [NBH + ob for ob in range(4 * Gq + 4)]
            for idx, gkb in enumerate(kbs):
                ob = gkb - NBH
                diag = ob >= 4 * Gq
                c0 = (ob - 4 * Gq) * 128 if diag else 0
                items.append(dict(h=h, Gq=Gq, gkb=gkb, c0=c0, diag=diag, first=(idx == 0), last=(idx == len(kbs) - 1), gi=gi,
                                  newhead=(Gq == 0 and idx == 0)))
            gi += 1
    DEPTH = 2

    def att_stage1(i, it):
        h, Gq, gkb, c0 = it['h'], it['Gq'], it['gkb'], it['c0']
        hb = h % 2
        if it['newhead'] and h + 1 < 8:
            D(kT_h[1 - hb][:], kT_d[h + 1], r=['kT_d'], w=[f'kTh{1 - hb}'])
            D(qT_h[1 - hb][:], qT_d[h + 1], r=['qT_d'], w=[f'qTh{1 - hb}'])
        kt, ktk = kT_h[hb], f'kTh{hb}'
        qt, qtk = qT_h[hb], f'qTh{hb}'
        p = i % 3
        pT, ptk = pT_b[i % 3], f'pT{i % 3}'
        MM(ps[p][:, c0:512], kt[:, gkb * 128:(gkb + 1) * 128], qt[:, Gq * 512 + c0:Gq * 512 + 512], True, True, [ktk, qtk], [f'ps{p}'])
        A(lambda e, p=p, pT=pT, c0=c0: e.activation(out=pT[:, c0:512], in_=ps[p][:, c0:512], func=AF.Exp), [f'ps{p}'], [ptk])
        if it['diag']:
            V(lambda e, pT=pT, c0=c0: e.tensor_tensor(out=pT[:, c0:c0 + 128], in0=pT[:, c0:c0 + 128], in1=trib[:, :], op=ALU.mult), [ptk, 'trib'], [ptk])

    def att_stage2(i, it):
        h, Gq, gkb, c0 = it['h'], it['Gq'], it['gkb'], it['c0']
        po = 3 + (it['gi'] % 2)
        pT, ptk = pT_b[i % 3], f'pT{i % 3}'
        MM(ps[po][0:65, c0:512], v_all[:, gkb, h * 65:(h + 1) * 65], pT[:, c0:512], it['first'], it['last'], [f'v_all{gkb // 8}', ptk], [f'ps{po}'])
        if it['last']:
            V(lambda e, po=po: e.reciprocal(out=rr[64:65, :], in_=ps[po][64:65, :]), [f'ps{po}'], ['rr'])
            V(lambda e: e.tensor_copy(out=rrh[64:65, :], in_=rr[64:65, :]), ['rr'], ['rrh'])
            V(lambda e: e.tensor_tensor(out=rr[64:65, :], in0=rr[64:65, :], in1=rrh[64:65, :], op=ALU.subtract), ['rr', 'rrh'], ['rr'])
            V(lambda e: e.tensor_copy(out=rrl[64:65, :], in_=rr[64:65, :]), ['rr'], ['rrl'])
            def tail(po=po, h=h, Gq=Gq, gi_=it['gi']):
                MM(ps[5][0:64, :], onesb[64:65, 0:64], rrh[64:65, :], True, False, ['onesb', 'rrh'], ['ps5'])
                MM(ps[5][0:64, :], onesb[64:65, 0:64], rrl[64:65, :], False, True, ['onesb', 'rrl'], ['ps5'])
                A(lambda e: e.activation(out=bc_sb[:], in_=ps[5][0:64, :], func=AF.Copy), ['ps5'], ['bc_sb'])
                ots, otk = oT_s[gi_ % 2], f"oTs{gi_ % 2}"
                V(lambda e, po=po, ots=ots: e.tensor_tensor(out=ots[:], in0=ps[po][0:64, :], in1=bc_sb[:], op=ALU.mult), [f'ps{po}', 'bc_sb'], [otk])
                D(oT_d[h, :, Gq * 512:(Gq + 1) * 512], ots[:], r=[otk], w=['oT_d'])
            pending_tail.append([3, tail])

    pending_tail = []
    for i in range(len(items) + DEPTH):
        if i < len(items):
            att_stage1(i, items[i])
        if i - DEPTH >= 0:
            att_stage2(i - DEPTH, items[i - DEPTH])
        for pt_ in list(pending_tail):
            pt_[0] -= 1
            if pt_[0] <= 0:
                pt_[1]()
                pending_tail.remove(pt_)
    for pt_ in pending_tail:
        pt_[1]()
    c.barrier()
    c.release(base_mark)
    if 'dbg_o' in dbg_out:
        tmpk = c.sb([64, 8, TH], BF16, "tmpo")
        tmpf = c.sb([64, 8, TH], F32, "tmpof")
        D(tmpk[:], oT_d.rearrange("h d t -> d h t"), r=['oT_d'], w=['tmpk'])
        V(lambda e, tmpf=tmpf, tmpk=tmpk: e.tensor_copy(out=tmpf[:], in_=tmpk[:]), ['tmpk'], ['tmpf'])
        D(dbg_out['dbg_o'].rearrange("h d t -> d h t"), tmpf[:], r=['tmpf'])
        c.barrier()
        c.release(base_mark)

    if stop == 'B':
        c.emit()
        return nc
    def ssm_prep(sfx):
        k = 'pp' + sfx
        lr = c.sb([128, 512], F32, "lr"); li = c.sb([128, 512], F32, "li"); ls = c.sb([128, 512], F32, "ls")
        D(lr[:], ssm_in["LR" + sfx], w=[k + 'lr'])
        D(li[:], ssm_in["LI" + sfx], w=[k + 'li'])
        D(ls[:], ssm_in["LS" + sfx], w=[k + 'ls'])
        dt = c.sb([128, 512], F32, "dt"); mag = c.sb([128, 512], F32, "mag"); th = c.sb([128, 512], F32, "th")
        A(lambda e: e.activation(out=dt[:], in_=ls[:], func=AF.Exp), [k + 'ls'], [k + 'dt'])
        V(lambda e: e.tensor_tensor(out=mag[:], in0=lr[:], in1=dt[:], op=ALU.mult), [k + 'lr', k + 'dt'], [k + 'mag'])
        A(lambda e: e.activation(out=mag[:], in_=mag[:], func=AF.Exp), [k + 'mag'], [k + 'mag'])
        V(lambda e: e.tensor_tensor(out=th[:], in0=li[:], in1=dt[:], op=ALU.mult), [k + 'li', k + 'dt'], [k + 'th'])

        def sin_of(shift, outt, ok):
            t = c.sb([128, 512], F32, "t"); ni = c.sb([128, 512], I32, "ni"); nf = c.sb([128, 512], F32, "nf")
            a = c.sb([128, 512], F32, "a"); mk = c.sb([128, 512], F32, "mk")
            V(lambda e: e.tensor_scalar(out=t[:], in0=th[:], scalar1=1.0 / TWO_PI, scalar2=8.5 + shift, op0=ALU.mult, op1=ALU.add), [k + 'th'], [k + 't'])
            V(lambda e: e.tensor_copy(out=ni[:], in_=t[:]), [k + 't'], [k + 'ni'])
            V(lambda e: e.tensor_copy(out=nf[:], in_=ni[:]), [k + 'ni'], [k + 'nf'])
            V(lambda e: e.scalar_tensor_tensor(out=a[:], in0=t[:], scalar=-0.5, in1=nf[:], op0=ALU.add, op1=ALU.subtract), [k + 't', k + 'nf'], [k + 'a'])
            V(lambda e: e.tensor_single_scalar(out=mk[:], in_=a[:], scalar=-0.5, op=ALU.is_lt), [k + 'a'], [k + 'mk'])
            V(lambda e: e.tensor_tensor(out=a[:], in0=a[:], in1=mk[:], op=ALU.add), [k + 'a', k + 'mk'], [k + 'a'])
            A(lambda e: e.activation(out=outt[:], in_=a[:], func=AF.Sin, scale=TWO_PI), [k + 'a'], [ok])
        ar = c.sb([128, 512], F32, "ar"); ai = c.sb([128, 512], F32, "ai")
        sin_of(0.0, ai, k + 'ai')
        sin_of(0.25, ar, k + 'ar')
        V(lambda e: e.tensor_tensor(out=ai[:], in0=ai[:], in1=mag[:], op=ALU.mult), [k + 'ai', k + 'mag'], [k + 'ai'])
        V(lambda e: e.tensor_tensor(out=ar[:], in0=ar[:], in1=mag[:], op=ALU.mult), [k + 'ar', k + 'mag'], [k + 'ar'])
        zr = c.sb([128, 512], F32, "zr"); zi = c.sb([128, 512], F32, "zi")
        nr = c.sb([128, 512], F32, "nr"); den = c.sb([128, 512], F32, "den"); t2 = c.sb([128, 512], F32, "t2")
        V(lambda e: e.tensor_scalar(out=nr[:], in0=ar[:], scalar1=-1.0, scalar2=None, op0=ALU.add), [k + 'ar'], [k + 'nr'])
        V(lambda e: e.tensor_tensor(out=den[:], in0=lr[:], in1=lr[:], op=ALU.mult), [k + 'lr'], [k + 'den'])
        V(lambda e: e.tensor_tensor(out=t2[:], in0=li[:], in1=li[:], op=ALU.mult), [k + 'li'], [k + 't2'])
        V(lambda e: e.tensor_tensor(out=den[:], in0=den[:], in1=t2[:], op=ALU.add), [k + 'den', k + 't2'], [k + 'den'])
        V(lambda e: e.reciprocal(out=den[:], in_=den[:]), [k + 'den'], [k + 'den'])
        V(lambda e: e.tensor_tensor(out=zr[:], in0=nr[:], in1=lr[:], op=ALU.mult), [k + 'nr', k + 'lr'], [k + 'zr'])
        V(lambda e: e.tensor_tensor(out=t2[:], in0=ai[:], in1=li[:], op=ALU.mult), [k + 'ai', k + 'li'], [k + 't2'])
        V(lambda e: e.tensor_tensor(out=zr[:], in0=zr[:], in1=t2[:], op=ALU.add), [k + 'zr', k + 't2'], [k + 'zr'])
        V(lambda e: e.tensor_tensor(out=zr[:], in0=zr[:], in1=den[:], op=ALU.mult), [k + 'zr', k + 'den'], [k + 'zr'])
        V(lambda e: e.tensor_tensor(out=zi[:], in0=ai[:], in1=lr[:], op=ALU.mult), [k + 'ai', k + 'lr'], [k + 'zi'])
        V(lambda e: e.tensor_tensor(out=t2[:], in0=nr[:], in1=li[:], op=ALU.mult), [k + 'nr', k + 'li'], [k + 't2'])
        V(lambda e: e.tensor_tensor(out=zi[:], in0=zi[:], in1=t2[:], op=ALU.subtract), [k + 'zi', k + 't2'], [k + 'zi'])
        V(lambda e: e.tensor_tensor(out=zi[:], in0=zi[:], in1=den[:], op=ALU.mult), [k + 'zi', k + 'den'], [k + 'zi'])
        return ar, ai, zr, zi, k

    def cmul(outr, outi, xr, xi, yr, yi, keys_in, kor, koi, negate_im=False, view=None):
        ta_t, tb_t = cm_tmp
        ta = view(ta_t[:]) if view else ta_t[:]
        tb = view(tb_t[:]) if view else tb_t[:]
        V(lambda e: e.tensor_tensor(out=ta, in0=xr, in1=yr, op=ALU.mult), keys_in, ['cm_ta'])
        V(lambda e: e.tensor_tensor(out=tb, in0=xi, in1=yi, op=ALU.mult), keys_in, ['cm_tb'])
        V(lambda e: e.tensor_tensor(out=outr, in0=ta, in1=tb, op=ALU.subtract), ['cm_ta', 'cm_tb'], [kor])
        V(lambda e: e.tensor_tensor(out=ta, in0=xr, in1=yi, op=ALU.mult), keys_in + [kor], ['cm_ta'])
        V(lambda e: e.tensor_tensor(out=tb, in0=xi, in1=yr, op=ALU.mult), keys_in + [kor], ['cm_tb'])
        if negate_im:
            V(lambda e: e.scalar_tensor_tensor(out=outi, in0=ta, scalar=-1.0, in1=tb, op0=ALU.mult, op1=ALU.subtract), ['cm_ta', 'cm_tb'], [koi])
        else:
            V(lambda e: e.tensor_tensor(out=outi, in0=ta, in1=tb, op=ALU.add), ['cm_ta', 'cm_tb'], [koi])

    cm_tmp = []
    ZBT = c.sb([128, 4, L, 2, 128], BF16, "ZBT")
    CYT = c.sb([128, 16, L + 1, 2, 32], BF16, "CYT")
    TT = c.sb([128, 4, L, 128], BF16, "TT")
    BBY = c.sb([128, 2, 512], BF16, "BBY")
    AL = c.sb([128, 2, 2, 16], F32, "AL")
    tbl_mark = c.mark()
    ar, ai, zr, zi, k = ssm_prep('X')
    br_ = c.sb([128, 512], F32, "br"); bi_ = c.sb([128, 512], F32, "bi")
    D(br_[:], ssm_in["BRX"], w=['brx'])
    D(bi_[:], ssm_in["BIX"], w=['bix'])
    bbr = c.sb([128, 512], F32, "bbr"); bbi = c.sb([128, 512], F32, "bbi")
    cm_tmp[:] = [c.sb([128, 512], F32, "ta"), c.sb([128, 512], F32, "tb")]
    cmul(bbr[:], bbi[:], zr[:], zi[:], br_[:], bi_[:], [k + 'zr', k + 'zi', 'brx', 'bix'], 'bbr', 'bbi')
    pw = [c.sb([128, 2, 512], F32, "pw") for _ in range(2)]
    V(lambda e, pw=pw: e.memset(pw[0][:, 0, :], 1.0), (), ['pw0'])
    V(lambda e, pw=pw: e.memset(pw[0][:, 1, :], 0.0), (), ['pw0'])
    for n in range(L):
        cur, ck = pw[n % 2], f'pw{n % 2}'
        nxt, nk = pw[(n + 1) % 2], f'pw{(n + 1) % 2}'
        j = L - 1 - n
        cmul(ZBT[:, :, j, 0, :], ZBT[:, :, j, 1, :], cur[:, 0, :].rearrange("p (s q) -> p s q", s=4), cur[:, 1, :].rearrange("p (s q) -> p s q", s=4),
             bbr[:].rearrange("p (s q) -> p s q", s=4), bbi[:].rearrange("p (s q) -> p s q", s=4), [ck, 'bbr', 'bbi'], 'ZBT', 'ZBT',
             view=lambda ap: ap.rearrange("p (s q) -> p s q", s=4))
        if n < L - 1:
            cmul(nxt[:, 0, :], nxt[:, 1, :], cur[:, 0, :], cur[:, 1, :], ar[:], ai[:], [ck, k + 'ar', k + 'ai'], nk, nk)
    c.barrier()
    c.release(tbl_mark)
    ar, ai, zr, zi, k = ssm_prep('Y')
    br_ = c.sb([128, 512], F32, "br"); bi_ = c.sb([128, 512], F32, "bi")
    cr_ = c.sb([128, 512], F32, "cr"); ci_ = c.sb([128, 512], F32, "ci")
    D(br_[:], ssm_in["BRY"], w=['bry'])
    D(bi_[:], ssm_in["BIY"], w=['biy'])
    D(cr_[:], ssm_in["CRY"], w=['cry'])
    D(ci_[:], ssm_in["CIY"], w=['ciy'])
    cm_tmp[:] = [c.sb([128, 512], F32, "ta"), c.sb([128, 512], F32, "tb")]
    cmul(BBY[:, 0, :], BBY[:, 1, :], zr[:], zi[:], br_[:], bi_[:], [k + 'zr', k + 'zi', 'bry', 'biy'], 'BBY', 'BBY')
    pw = [c.sb([128, 2, 512], F32, "pw") for _ in range(2)]
    V(lambda e, pw=pw: e.memset(pw[0][:, 0, :], 1.0), (), ['pw0'])
    V(lambda e, pw=pw: e.memset(pw[0][:, 1, :], 0.0), (), ['pw0'])
    for n in range(L + 1):
        cur, ck = pw[n % 2], f'pw{n % 2}'
        nxt, nk = pw[(n + 1) % 2], f'pw{(n + 1) % 2}'
        cmul(CYT[:, :, n, 0, :], CYT[:, :, n, 1, :], cr_[:].rearrange("p (k j) -> p k j", k=16), ci_[:].rearrange("p (k j) -> p k j", k=16),
             cur[:, 0, :].rearrange("p (k j) -> p k j", k=16), cur[:, 1, :].rearrange("p (k j) -> p k j", k=16), [ck, 'cry', 'ciy'], 'CYT', 'CYT', negate_im=True,
             view=lambda ap: ap.rearrange("p (k j) -> p k j", k=16))
        if n < L:
            cmul(nxt[:, 0, :], nxt[:, 1, :], cur[:, 0, :], cur[:, 1, :], ar[:], ai[:], [ck, k + 'ar', k + 'ai'], nk, nk)
    pL, pLk = pw[L % 2], f'pw{L % 2}'
    pLv_r = pL[:, 0, :].rearrange("p (k j) -> p k j", k=16)[:, :, 0]
    pLv_i = pL[:, 1, :].rearrange("p (k j) -> p k j", k=16)[:, :, 0]
    V(lambda e: e.tensor_copy(out=AL[:, 0, 0, :], in_=pLv_r), [pLk], ['AL'])
    V(lambda e: e.tensor_copy(out=AL[:, 0, 1, :], in_=pLv_r), [pLk], ['AL'])
    V(lambda e: e.tensor_scalar(out=AL[:, 1, 0, :], in0=pLv_i, scalar1=-1.0, scalar2=None, op0=ALU.mult), [pLk], ['AL'])
    V(lambda e: e.tensor_copy(out=AL[:, 1, 1, :], in_=pLv_i), [pLk], ['AL'])
    V(lambda e: e.memset(TT[:], 0.0), (), ['TT'])
    ddt = c.sb([128, 512], F32, "ddt")
    D(ddt[:], ddg, w=['ddt'])
    for kk in range(16):
        s_, k4 = kk // 4, kk % 4
        p = nps()
        MM(ps[p][32 * k4:32 * k4 + 32, :], BBY[:, 0, kk * 32:(kk + 1) * 32], CYT[:, kk, 0:L, 0, :], True, False, ['BBY', 'CYT'], [f'ps{p}'], tp=(0, 32 * k4))
        MM(ps[p][32 * k4:32 * k4 + 32, :], BBY[:, 1, kk * 32:(kk + 1) * 32], CYT[:, kk, 0:L, 1, :], False, True, ['BBY', 'CYT'], [f'ps{p}'], tp=(0, 32 * k4))
        V(lambda e, p=p, s_=s_, k4=k4: e.tensor_copy(out=TT[32 * k4:32 * k4 + 32, s_, :, 32 * k4:32 * k4 + 32], in_=ps[p][32 * k4:32 * k4 + 32, :].rearrange("p (n j) -> p n j", n=L)), [f'ps{p}'], ['TT'])
    V(lambda e: e.tensor_tensor(out=TT[:, :, 0, :], in0=TT[:, :, 0, :], in1=ddt[:].rearrange("p (s q) -> p s q", s=4), op=ALU.add), ['TT', 'ddt'], ['TT'])
    c.barrier()
    c.release(tbl_mark)

    if stop == 'C1':
        c.barrier()
        dump('dbg_ZBT', ZBT[:].rearrange("p s j r q -> p (s j r q)"), 4 * L * 2 * 128)
        dump('dbg_CYT', CYT[:].rearrange("p k n r j -> p (k n r j)"), 16 * (L + 1) * 2 * 32)
        dump('dbg_TT', TT[:].rearrange("p s n q -> p (s n q)"), 4 * L * 128)
        dump('dbg_AL', AL[:].rearrange("p a r k -> p (a r k)"), 64)
        c.emit()
        return nc
    uT_all = c.sb([128, 4, TH], BF16, "uT_all")
    Zs = c.sb([128, 2, 16, NCH], BF16, "Zs")
    H = c.sb([128, 2, 16, NCH + 1], F32, "H")
    Hb = c.sb([128, 2, 16, NCH + 1], BF16, "Hb")
    t1 = c.sb([128, 2, 16], F32, "t1")
    t4 = c.sb([128, 2, 2, 16], F32, "t4")
    AL4 = c.sb([128, 2, 2, 16], F32, "AL4")
    V(lambda e: e.tensor_copy(out=AL4[:, 0, 0, :], in_=AL[:, 0, 0, :]), ['AL'], ['AL4'])
    V(lambda e: e.tensor_copy(out=AL4[:, 0, 1, :], in_=AL[:, 1, 0, :]), ['AL'], ['AL4'])
    V(lambda e: e.tensor_copy(out=AL4[:, 1, 0, :], in_=AL[:, 1, 1, :]), ['AL'], ['AL4'])
    V(lambda e: e.tensor_copy(out=AL4[:, 1, 1, :], in_=AL[:, 0, 1, :]), ['AL'], ['AL4'])
    t2_ = c.sb([128, 2, 16], F32, "t2_")
    G(lambda e: e.memset(H[:, :, :, 0], 0.0), (), ['H'])
    NZ = min(NCH, 512)
    for half in range(2):
        D(uT_all[:], uT_d.rearrange("(s p) t -> p s t", p=128)[:, :, half * TH:(half + 1) * TH], r=['uT_d'], w=['uT_all'])
        for kk in range(16):
            s_, k4 = kk // 4, kk % 4
            for ri in range(2):
                for z0 in range(0, NCH, NZ):
                    p = nps()
                    for j in range(L):
                        uview = uT_all[32 * k4:32 * k4 + 32, s_, :].rearrange("p (n j) -> p n j", j=L)[:, z0:z0 + NZ, j]
                        MM(ps[p][:, 0:NZ], ZBT[32 * k4:32 * k4 + 32, s_, j, ri, :], uview, j == 0, j == L - 1, ['ZBT', 'uT_all'], [f'ps{p}'], tp=(32 * k4, 0))
                    if (kk + ri) % 2 == 0:
                        A(lambda e, p=p, ri=ri, kk=kk, z0=z0: e.activation(out=Zs[:, ri, kk, z0:z0 + NZ], in_=ps[p][:, 0:NZ], func=AF.Copy), [f'ps{p}'], ['Zs'])
                    else:
                        V(lambda e, p=p, ri=ri, kk=kk, z0=z0: e.tensor_copy(out=Zs[:, ri, kk, z0:z0 + NZ], in_=ps[p][:, 0:NZ]), [f'ps{p}'], ['Zs'])
        for n in range(NCH):
            V(lambda e, n=n: e.tensor_tensor(out=t4[:], in0=AL4[:], in1=H[:, :, :, n].unsqueeze(1).broadcast_to([128, 2, 2, 16]), op=ALU.mult), ['AL4', 'H'], ['t4'])
            V(lambda e, n=n: e.tensor_tensor(out=t1[:], in0=t4[:, :, 0, :], in1=Zs[:, :, :, n], op=ALU.add), ['t4', 'Zs'], ['t1'])
            V(lambda e, n=n: e.tensor_tensor(out=H[:, :, :, n + 1], in0=t1[:], in1=t4[:, :, 1, :], op=ALU.add), ['t1', 't4'], ['H'])
        if half == 0:
            V(lambda e: e.tensor_copy(out=H[:, :, :, 0], in_=H[:, :, :, NCH]), ['H'], ['H'])
    V(lambda e: e.tensor_copy(out=Hb[:], in_=H[:]), ['H'], ['Hb'])

    if stop == 'C2':
        c.barrier()
        dump('dbg_H', H[:].rearrange("p a k n -> p (a k n)"), 2 * 16 * (NCH + 1))
        dump('dbg_Zs', Zs[:].rearrange("p a k n -> p (a k n)"), 2 * 16 * NCH)
        c.emit()
        return nc
    zT_s = [c.sb([128, 512], BF16, "zTs") for _ in range(2)]
    x2 = c.sb([128, 512], F32, "x2")
    wv = c.sb([128, 512], F32, "wv")
    zi_ = 0
    for s_ in range(4):
        for ti in range(NT):
            p = nps()
            tok0 = ti * 512
            yv = ps[p][:, :].rearrange("p (n j) -> p n j", j=L)
            uv = uT_all[:, s_, tok0:tok0 + 512].rearrange("p (n j) -> p n j", j=L)
            for n in range(L):
                MM(yv[:, :, n:L], TT[:, s_, n, :], uv[:, :, 0:L - n], n == 0, False, ['TT', 'uT_all'], [f'ps{p}'])
            cnt = 0
            for k4 in range(4):
                kk = 4 * s_ + k4
                for i in range(L):
                    for ri in range(2):
                        cnt += 1
                        MM(yv[32 * k4:32 * k4 + 32, :, i], CYT[:, kk, i + 1, ri, :], Hb[:, ri, kk, ti * 32:ti * 32 + 32], False, cnt == 4 * L * 2, ['CYT', 'Hb'], [f'ps{p}'], tp=(0, 32 * k4))
            zt, ztk = zT_s[zi_ % 2], f'zTs{zi_ % 2}'
            zi_ += 1
            A(lambda e, p=p: e.activation(out=x2[:], in_=ps[p][:, :], func=AF.Square), [f'ps{p}'], ['x2'])
            V(lambda e: e.tensor_scalar(out=wv[:], in0=x2[:], scalar1=0.044715, scalar2=1.0, op0=ALU.mult, op1=ALU.add), ['x2'], ['wv'])
            V(lambda e, p=p: e.tensor_tensor(out=wv[:], in0=wv[:], in1=ps[p][:, :], op=ALU.mult), ['wv', f'ps{p}'], ['wv'])
            A(lambda e: e.activation(out=wv[:], in_=wv[:], func=AF.Sigmoid, scale=1.5957691216057308), ['wv'], ['wv'])
            V(lambda e, p=p, zt=zt: e.tensor_tensor(out=zt[:], in0=wv[:], in1=ps[p][:, :], op=ALU.mult), ['wv', f'ps{p}'], [ztk])
            D(zT_d[s_ * 128:(s_ + 1) * 128, ti * 512:(ti + 1) * 512], zt[:], r=[ztk], w=['zT_d'])
    c.barrier()
    c.release(base_mark)

    if 'dbg_y' in dbg_out:
        tmpz = c.sb([128, 4, TH], BF16, "tmpz")
        tmpzf = c.sb([128, 4, TH], F32, "tmpzf")
        D(tmpz[:], zT_d.rearrange("(s p) t -> p s t", p=128), r=['zT_d'], w=['tmpz'])
        V(lambda e, tmpzf=tmpzf, tmpz=tmpz: e.tensor_copy(out=tmpzf[:], in_=tmpz[:]), ['tmpz'], ['tmpzf'])
        D(dbg_out['dbg_y'].rearrange("(s p) t -> p s t", p=128), tmpzf[:], r=['tmpzf'])
        c.barrier()
        c.release(base_mark)
    if stop == 'C':
        c.emit()
        return nc
    def load_w(dram_view, shape, key, scale_ap=None):
        wt = c.sb(shape, BF16, key)
        a_n, ncol = shape[1], shape[2]
        for a_ in range(a_n):
            lw_i[0] += 1
            lwk = 'lw_st0'
            st_ = lw_sts[lw_i[0] % 2][:, 0:ncol]
            D(st_, dram_view[:, a_, :], w=[lwk])
            if scale_ap is None:
                V(lambda e, a_=a_, st_=st_: e.tensor_copy(out=wt[:, a_, :], in_=st_), [lwk], [key])
            else:
                V(lambda e, a_=a_, st_=st_: e.tensor_scalar(out=wt[:, a_, :], in0=st_, scalar1=scale_ap[:, a_:a_ + 1], scalar2=None, op0=ALU.mult), [lwk, 'gf'], [key])
        return wt

    lw_sts = [c.sb([128, 1024], F32, "lw_st")] * 2
    lw_i = [0]
    gf = c.sb([128, 8], F32, "gf")
    D(gf[:], gffnT, w=['gf'])
    bgl = c.sb([128, 4], F32, "bgl")
    D(bgl[:], bglu, w=['bgl'])
    brt = c.sb([128, 20], F32, "brt")
    D(brt[:], br, w=['brt'])
    Wglu = load_w(w_glu.rearrange("(kc p) n -> p kc n", p=128), [128, 4, 512], 'Wglu')
    Wob = load_w(w_out_b.rearrange("(kc p) n -> p kc n", p=128), [128, 4, 1024], 'Wob')
    Woa = c.sb([64, 8, 1024], BF16, "Woa")
    woa_v = w_out_a.rearrange("(h p) n -> p h n", p=64)
    for h in range(8):
        lw_i[0] += 1
        lwk = 'lw_st0'
        lwt = lw_sts[lw_i[0] % 2]
        D(lwt[0:64, :], woa_v[:, h, :], w=[lwk])
        V(lambda e, h=h, lwt=lwt: e.tensor_copy(out=Woa[:, h, :], in_=lwt[0:64, :]), [lwk], ['Woa'])
    Wout = load_w(w_out.rearrange("(kc p) n -> p kc n", p=128), [128, 8, 1024], 'Wout')
    Wr = c.sb([128, 8, 20], F32, "Wr")
    D(Wr[:], wr.rearrange("(kc p) n -> p kc n", p=128), w=['Wr'])
    for kc in range(8):
        V(lambda e, kc=kc: e.tensor_scalar(out=Wr[:, kc, :], in0=Wr[:, kc, :], scalar1=gf[:, kc:kc + 1], scalar2=None, op0=ALU.mult), ['Wr', 'gf'], ['Wr'])

    if stop == 'D1':
        c.emit()
        return nc
    Wrhi = c.sb([128, 8, 20], BF16, "Wrhi")
    Wrlo = c.sb([128, 8, 20], BF16, "Wrlo")
    V(lambda e: e.tensor_copy(out=Wrhi[:], in_=Wr[:]), ['Wr'], ['Wrhi'])
    V(lambda e: e.tensor_tensor(out=Wr[:], in0=Wr[:], in1=Wrhi[:], op=ALU.subtract), ['Wr', 'Wrhi'], ['Wr'])
    V(lambda e: e.tensor_copy(out=Wrlo[:], in_=Wr[:]), ['Wr'], ['Wrlo'])
    h2hi = c.sb([128, 1024], BF16, "h2hi")
    h2lo = c.sb([128, 1024], BF16, "h2lo")
    hThi = c.sb([128, 8, 128], BF16, "hThi")
    hTlo = c.sb([128, 8, 128], BF16, "hTlo")
    cwb = c.sb([128, 16], BF16, "cwb")
    zT = c.sb([128, 4, 512], BF16, "zT")
    oT = c.sb([64, 8, 512], BF16, "oT")
    gT = c.sb([128, 16, 512], BF16, "gT")
    xt = c.sb([128, 4, 1024], F32, "xtD")
    zg = c.sb([128, 4, 512], BF16, "zg")
    sg = c.sb([128, 512], F32, "sg")
    mgd = c.sb([128, 8, 512], BF16, "mgd")
    ta_ = c.sb([128, 512], F32, "taD")
    tb_ = c.sb([128, 512], F32, "tbD")
    x1 = c.sb([128, 4, 1024], F32, "x1")
    h2f = c.sb([128, 1024], F32, "h2f")
    h2Tf = c.sb([128, 8, 128], F32, "h2Tf")
    h2Tb = c.sb([128, 8, 512], BF16, "h2Tb")
    cwTb = c.sb([16, 512], BF16, "cwTb")
    ss2 = c.sb([128, 4], F32, "ss2")
    junk2 = c.sb([128, 1024], BF16, "junk2")
    junk2_b = [junk2, c.sb([128, 1024], BF16, "junk2b")]
    lg = c.sb([128, 20], F32, "lg")
    cw = c.sb([128, 16], F32, "cw")
    sm = {n_: c.sb([128, 4], F32, n_) for n_ in ["gmx", "ge", "gsum", "oh", "les", "m1", "mk1", "le2", "m2", "mk2", "w12", "tmp4"]}
    one1 = {n_: c.sb([128, 1], F32, n_) for n_ in ["psel", "d21", "w1", "w2"]}
    xin_own = xin[TH:T2, :].rearrange("(t j p) d -> t p j d", j=4, p=128)
    zT_b2 = [zT, c.sb([128, 4, 512], BF16, "zT2")]
    oT_b2 = [oT, c.sb([64, 8, 512], BF16, "oT2")]
    gT_b2 = [gT, c.sb([128, 16, 512], BF16, "gT2")]
    xt_b2 = [xt, c.sb([128, 4, 1024], F32, "xtD2")]
    lg4 = c.sb([128, 4, 20], F32, "lg4")
    R4 = {n_: c.sb([128, 4, 4], F32, n_) for n_ in ["oh", "ge", "les", "mk1", "le2", "mk2", "w12", "tq"]}
    R1 = {n_: c.sb([128, 4], F32, n_) for n_ in ["mx", "gsum", "psel", "m1", "m2", "d", "w1", "w2"]}
    prod = c.sb([128, 4, 4, 4], F32, "prod")
    cw4 = c.sb([128, 4, 16], F32, "cw4")
    cwb4 = c.sb([128, 4, 16], BF16, "cwb4")
    AXX = mybir.AxisListType.X

    def bc3(ap2):
        return ap2.unsqueeze(2).broadcast_to([128, 4, 4])

    def load_tile(ti):
        b_ = ti % 2
        t0_ = ti * 512
        D(zT_b2[b_][:], zT_d.rearrange("(s p) t -> p s t", p=128)[:, :, t0_:t0_ + 512], r=['zT_d'], w=[f'zT{b_}'])
        D(oT_b2[b_][:], oT_d.rearrange("h d t -> d h t")[:, :, t0_:t0_ + 512], r=['oT_d'], w=[f'oT{b_}'])
        for hh in range(2):
            D(gT_b2[b_][:, hh * 8:(hh + 1) * 8, :], gT_d.rearrange("(m p) t -> p m t", p=128)[:, hh * 8:(hh + 1) * 8, t0_:t0_ + 512], r=['gT_d'], w=[f'gT{b_}'])
        D(xt_b2[b_][:], xin_own[ti], w=[f'xtD{b_}'])

    load_tile(0)
    for ti in range(NT):
        t0 = ti * 512
        b_ = ti % 2
        zT, oT, gT, xt = zT_b2[b_], oT_b2[b_], gT_b2[b_], xt_b2[b_]
        zTk, oTk, gTk, xtk = f'zT{b_}', f'oT{b_}', f'gT{b_}', f'xtD{b_}'
        if ti + 1 < NT:
            load_tile(ti + 1)
        for m in range(4):
            p = nps()
            for kc in range(4):
                MM(ps[p][:, :], Wglu[:, kc, m * 128:(m + 1) * 128], zT[:, kc, :], kc == 0, kc == 3, ['Wglu', zTk], [f'ps{p}'])
            A(lambda e, p=p, m=m: e.activation(out=sg[:], in_=ps[p][:, :], func=AF.Sigmoid, bias=bgl[:, m:m + 1]), [f'ps{p}', 'bgl'], ['sg'])
            V(lambda e, m=m, zT=zT: e.tensor_tensor(out=zg[:, m, :], in0=zT[:, m, :], in1=sg[:], op=ALU.mult), [zTk, 'sg'], ['zg'])
        for m in range(8):
            p = nps()
            for kc in range(4):
                MM(ps[p][:, :], Wob[:, kc, m * 128:(m + 1) * 128], zg[:, kc, :], kc == 0, kc == 3, ['Wob', 'zg'], [f'ps{p}'])
            p2 = nps()
            for h in range(8):
                MM(ps[p2][:, :], Woa[:, h, m * 128:(m + 1) * 128], oT[:, h, :], h == 0, h == 7, ['Woa', oTk], [f'ps{p2}'])
            V(lambda e, p=p, m=m, gT=gT: e.tensor_tensor(out=ta_[:], in0=gT[:, 8 + m, :], in1=ps[p][:, :], op=ALU.mult), [gTk, f'ps{p}'], ['taD'])
            V(lambda e, p2=p2, m=m, gT=gT: e.tensor_tensor(out=tb_[:], in0=gT[:, m, :], in1=ps[p2][:, :], op=ALU.mult), [gTk, f'ps{p2}'], ['tbD'])
            G(lambda e, m=m: e.tensor_tensor(out=mgd[:, m, :], in0=ta_[:], in1=tb_[:], op=ALU.add), ['taD', 'tbD'], ['mgd'])
        for j in range(4):
            for hf in range(2):
                p = nps()
                for kc in range(8):
                    MM(ps[p][:, :], mgd[:, kc, j * 128:(j + 1) * 128], Wout[:, kc, hf * 512:(hf + 1) * 512], kc == 0, kc == 7, ['mgd', 'Wout'], [f'ps{p}'])
                V(lambda e, p=p, j=j, hf=hf, xt=xt: e.tensor_tensor(out=x1[:, j, hf * 512:(hf + 1) * 512], in0=xt[:, j, hf * 512:(hf + 1) * 512], in1=ps[p][:, :], op=ALU.add), [xtk, f'ps{p}'], ['x1'])
        D(x1_d[t0:t0 + 512, :].rearrange("(j p) d -> p j d", p=128), x1[:], r=['x1'], w=['x1_d'])
        for j in range(4):
            jk_, jkk_ = junk2_b[j % 2], f'junk2{j % 2}'
            A(lambda e, j=j, jk_=jk_: e.activation(out=jk_[:], in_=x1[:, j, :], func=AF.Square), ['x1'], [jkk_])
            V(lambda e, j=j, jk_=jk_: e.tensor_reduce(out=ss2[:, j:j + 1], in_=jk_[:], axis=mybir.AxisListType.X, op=ALU.add), [jkk_], ['Ess'])
        m0 = c.mark()
        rs2 = rstd_of(ss2, 4, 'E')
        for j in range(4):
            A(lambda e, j=j, rs2=rs2: e.activation(out=h2f[:], in_=x1[:, j, :], func=AF.Copy, scale=rs2[:, j:j + 1]), ['x1', 'Ers'], ['h2f'])
            G(lambda e: e.tensor_copy(out=h2hi[:], in_=h2f[:]), ['h2f'], ['h2hi'])
            G(lambda e: e.tensor_tensor(out=h2lo[:], in0=h2f[:], in1=h2hi[:], op=ALU.subtract), ['h2f', 'h2hi'], ['h2lo'])
            for srct, srck, dst, dstk, pbk in ((h2hi, 'h2hi', hThi, 'hThi', 0), (h2lo, 'h2lo', hTlo, 'hTlo', 1)):
                for kc in range(8):
                    TR(pb[pbk][:, kc * 128:(kc + 1) * 128], srct[:, kc * 128:(kc + 1) * 128], identb[:], [srck, 'identb'], [f'pb{pbk}'])
                A(lambda e, dst=dst, pbk=pbk: e.activation(out=dst[:], in_=pb[pbk][:, :].rearrange("p (k t) -> p k t", k=8), func=AF.Copy), [f'pb{pbk}'], [dstk])
            G(lambda e, j=j: e.tensor_copy(out=h2Tb[:, :, j * 128:(j + 1) * 128], in_=hThi[:]), ['hThi'], ['h2Tb'])
            p = nps()
            nmm = 0
            for (lt, ltk, wt_, wtk) in ((hThi, 'hThi', Wrhi, 'Wrhi'), (hTlo, 'hTlo', Wrhi, 'Wrhi'), (hThi, 'hThi', Wrlo, 'Wrlo')):
                for kc in range(8):
                    MM(ps[p][:, 0:20], lt[:, kc, :], wt_[:, kc, :], nmm == 0, nmm == 23, [ltk, wtk], [f'ps{p}'])
                    nmm += 1
            V(lambda e, p=p, j=j: e.tensor_tensor(out=lg4[:, j, :], in0=ps[p][:, 0:20], in1=brt[:], op=ALU.add), [f'ps{p}', 'brt'], ['lg4'])
        c.release(m0)
        gl = lg4[:, :, 0:4]
        le4 = lg4[:, :, 4:20].rearrange("p j (g e) -> p j g e", g=4)
        V(lambda e: e.tensor_reduce(out=R1["mx"][:], in_=gl, axis=AXX, op=ALU.max), ['lg4'], ['r_mx'])
        V(lambda e: e.tensor_tensor(out=R4["oh"][:], in0=gl, in1=bc3(R1["mx"][:, :]), op=ALU.is_ge), ['lg4', 'r_mx'], ['r_oh'])
        V(lambda e: e.tensor_tensor(out=R4["ge"][:], in0=gl, in1=bc3(R1["mx"][:, :]), op=ALU.subtract), ['lg4', 'r_mx'], ['r_ge'])
        A(lambda e: e.activation(out=R4["ge"][:], in_=R4["ge"][:], func=AF.Exp), ['r_ge'], ['r_ge'])
        V(lambda e: e.tensor_reduce(out=R1["gsum"][:], in_=R4["ge"][:], axis=AXX, op=ALU.add), ['r_ge'], ['r_gsum'])
        V(lambda e: e.reciprocal(out=R1["psel"][:], in_=R1["gsum"][:]), ['r_gsum'], ['r_psel'])
        V(lambda e: e.tensor_tensor(out=prod[:], in0=le4, in1=R4["oh"][:, :, :].unsqueeze(3).broadcast_to([128, 4, 4, 4]), op=ALU.mult), ['lg4', 'r_oh'], ['r_prod'])
        V(lambda e: e.tensor_reduce(out=R4["les"][:], in_=prod[:].rearrange("p j g e -> p j e g"), axis=AXX, op=ALU.add), ['r_prod'], ['r_les'])
        V(lambda e: e.tensor_reduce(out=R1["m1"][:], in_=R4["les"][:], axis=AXX, op=ALU.max), ['r_les'], ['r_m1'])
        V(lambda e: e.tensor_tensor(out=R4["mk1"][:], in0=R4["les"][:], in1=bc3(R1["m1"][:, :]), op=ALU.is_ge), ['r_les', 'r_m1'], ['r_mk1'])
        V(lambda e: e.scalar_tensor_tensor(out=R4["le2"][:], in0=R4["mk1"][:], scalar=-1e30, in1=R4["les"][:], op0=ALU.mult, op1=ALU.add), ['r_mk1', 'r_les'], ['r_le2'])
        V(lambda e: e.tensor_reduce(out=R1["m2"][:], in_=R4["le2"][:], axis=AXX, op=ALU.max), ['r_le2'], ['r_m2'])
        V(lambda e: e.tensor_tensor(out=R4["mk2"][:], in0=R4["le2"][:], in1=bc3(R1["m2"][:, :]), op=ALU.is_ge), ['r_le2', 'r_m2'], ['r_mk2'])
        V(lambda e: e.tensor_tensor(out=R1["d"][:], in0=R1["m2"][:], in1=R1["m1"][:], op=ALU.subtract), ['r_m1', 'r_m2'], ['r_d'])
        A(lambda e: e.activation(out=R1["d"][:], in_=R1["d"][:], func=AF.Exp), ['r_d'], ['r_d'])
        V(lambda e: e.tensor_scalar(out=R1["d"][:], in0=R1["d"][:], scalar1=1.0, scalar2=None, op0=ALU.add), ['r_d'], ['r_d'])
        V(lambda e: e.reciprocal(out=R1["w1"][:], in_=R1["d"][:]), ['r_d'], ['r_w1'])
        V(lambda e: e.tensor_scalar(out=R1["w2"][:], in0=R1["w1"][:], scalar1=-1.0, scalar2=1.0, op0=ALU.mult, op1=ALU.add), ['r_w1'], ['r_w2'])
        V(lambda e: e.tensor_tensor(out=R1["w1"][:], in0=R1["w1"][:], in1=R1["psel"][:], op=ALU.mult), ['r_w1', 'r_psel', 'r_w2'], ['r_w1'])
        V(lambda e: e.tensor_tensor(out=R1["w2"][:], in0=R1["w2"][:], in1=R1["psel"][:], op=ALU.mult), ['r_w2', 'r_psel'], ['r_w2'])
        V(lambda e: e.tensor_tensor(out=R4["w12"][:], in0=R4["mk1"][:], in1=bc3(R1["w1"][:, :]), op=ALU.mult), ['r_mk1', 'r_w1'], ['r_w12'])
        V(lambda e: e.tensor_tensor(out=R4["tq"][:], in0=R4["mk2"][:], in1=bc3(R1["w2"][:, :]), op=ALU.mult), ['r_mk2', 'r_w2'], ['r_tq'])
        V(lambda e: e.tensor_tensor(out=R4["w12"][:], in0=R4["w12"][:], in1=R4["tq"][:], op=ALU.add), ['r_w12', 'r_tq'], ['r_w12'])
        V(lambda e: e.tensor_tensor(out=cw4[:].rearrange("p j (g e) -> p j g e", g=4), in0=R4["oh"][:, :, :].unsqueeze(3).broadcast_to([128, 4, 4, 4]),
                                    in1=R4["w12"][:, :, :].unsqueeze(2).broadcast_to([128, 4, 4, 4]), op=ALU.mult), ['r_oh', 'r_w12'], ['cw4'])
        V(lambda e: e.tensor_copy(out=cwb4[:], in_=cw4[:]), ['cw4'], ['cwb4'])
        for j in range(4):
            TR(pb[0][0:16, 0:128], cwb4[:, j, :], identb[:], ['cwb4', 'identb'], ['pb0'])
            V(lambda e, j=j: e.tensor_copy(out=cwTb[:, j * 128:(j + 1) * 128], in_=pb[0][0:16, 0:128]), ['pb0'], ['cwTb'])
        D(h2T_d.rearrange("(kc p) t -> p kc t", p=128)[:, :, t0:t0 + 512], h2Tb[:], r=['h2Tb'], w=['h2T_d'])
        D(cwT_d[:, t0:t0 + 512], cwTb[:], r=['cwTb'], w=['cwT_d'])
    c.barrier()
    c.release(base_mark)
    if 'dbg_x1' in dbg_out:
        tmpx = c.sb([128, TH // 128, 1024], F32, "tmpx")
        D(tmpx[:], x1_d.rearrange("(j p) d -> p j d", p=128), r=['x1_d'], w=['tmpx'])
        D(dbg_out['dbg_x1'].rearrange("(j p) d -> p j d", p=128), tmpx[:], r=['tmpx'])
        tmpc = c.sb([16, TH], BF16, "tmpc"); tmpcf = c.sb([16, TH], F32, "tmpcf")
        D(tmpc[:], cwT_d, r=['cwT_d'], w=['tmpc'])
        V(lambda e, tmpcf=tmpcf, tmpc=tmpc: e.tensor_copy(out=tmpcf[:], in_=tmpc[:]), ['tmpc'], ['tmpcf'])
        D(dbg_out['dbg_cw'], tmpcf[:], r=['tmpcf'])
        c.barrier()
        c.release(base_mark)

    if stop == 'D':
        c.emit()
        return nc
    ST = min(TH, 1024)
    NSB = ST // 128
    gf2 = c.sb([128, 8], F32, "gf2")
    D(gf2[:], gffnT, w=['gf2'])
    selb = c.sb([16, 2048], BF16, "selb")
    m_ = c.mark()
    selst = c.sb([16, 2048], F32, "selst")
    D(selst[:], sel_d, w=['selst'])
    V(lambda e: e.tensor_copy(out=selb[:], in_=selst[:]), ['selst'], ['selb'])
    gfin_t = c.sb([128, 1024], F32, "gfin")
    D(gfin_t[:], gfin, w=['gfin'])
    h2T = c.sb([128, 8, ST], BF16, "h2T")
    cwT = c.sb([16, ST], BF16, "cwT")
    acc = c.sb([128, NSB, 1024], F32, "acc")
    Wg_b = [c.sb([128, 8, 256], BF16, "Wg") for _ in range(2)]
    Wu_b = [c.sb([128, 8, 256], BF16, "Wu") for _ in range(2)]
    Wd_b = [c.sb([128, 2, 1024], BF16, "Wd") for _ in range(2)]
    wstg = [[c.sb([128, 8, 256], F32, "wstg") for _ in range(2)] for _ in range(2)]
    wstd = [c.sb([128, 2, 1024], F32, "wstd") for _ in range(2)]
    bcs_b = [c.sb([128, 512], BF16, "bcs") for _ in range(2)]
    sgl_b = [c.sb([128, 512], F32, "sgl") for _ in range(2)]
    tmu_b = [c.sb([128, 512], F32, "tmu") for _ in range(2)]
    actT_b = [c.sb([128, 2, 512], BF16, "actT") for _ in range(2)]
    x1f = c.sb([128, 1024], F32, "x1f")
    ssf = c.sb([128, 1], F32, "ssf")
    junk3 = c.sb([128, 1024], BF16, "junk3")
    outt = [c.sb([128, 1024], F32, "outt") for _ in range(2)]
    NST = TH // ST
    NSUB = ST // 512

    def load_expert(n):
        ex, sl = n % 16, n % 2
        Wg, Wu, Wd = Wg_b[sl], Wu_b[sl], Wd_b[sl]
        wk = f'We{sl}'
        D(wstg[sl][0][:], w_eg[ex].rearrange("(kc p) f -> p kc f", p=128), w=[f'wstg{sl}0'])
        D(wstg[sl][1][:], w_eu[ex].rearrange("(kc p) f -> p kc f", p=128), w=[f'wstg{sl}1'])
        D(wstd[sl][:], w_ed[ex].rearrange("(fc p) d -> p fc d", p=128), w=[f'wstd{sl}'])
        for kc in range(8):
            A(lambda e, kc=kc, Wg=Wg, sl=sl: e.activation(out=Wg[:, kc, :], in_=wstg[sl][0][:, kc, :], func=AF.Copy, scale=gf2[:, kc:kc + 1]), [f'wstg{sl}0', 'gf2'], [wk])
            A(lambda e, kc=kc, Wu=Wu, sl=sl: e.activation(out=Wu[:, kc, :], in_=wstg[sl][1][:, kc, :], func=AF.Copy, scale=gf2[:, kc:kc + 1]), [f'wstg{sl}1', 'gf2'], [wk])
        A(lambda e, Wd=Wd, sl=sl: e.activation(out=Wd[:, 0, :], in_=wstd[sl][:, 0, :], func=AF.Copy), [f'wstd{sl}'], [wk])
        V(lambda e, Wd=Wd, sl=sl: e.tensor_copy(out=Wd[:, 1, :], in_=wstd[sl][:, 1, :]), [f'wstd{sl}'], [wk])

    ucount = [0]

    def moe_stage1(n, sub):
        ex, sl = n % 16, n % 2
        Wg, Wu = Wg_b[sl], Wu_b[sl]
        wk = f'We{sl}'
        ub = ucount[0] % 2
        ucount[0] += 1
        bcs, sgl, tmu, actT = bcs_b[ub], sgl_b[ub], tmu_b[ub], actT_b[ub]
        q0 = sub * 512
        p = nps()
        MM(ps[p][:, :], selb[:, ex * 128:(ex + 1) * 128], cwT[:, q0:q0 + 512], True, True, ['selb', 'cwT'], [f'ps{p}'])
        A(lambda e, p=p, bcs=bcs: e.activation(out=bcs[:], in_=ps[p][:, :], func=AF.Copy), [f'ps{p}'], [f'bcs{ub}'])
        for fc in range(2):
            pg = nps()
            for kc in range(8):
                MM(ps[pg][:, :], Wg[:, kc, fc * 128:(fc + 1) * 128], h2T[:, kc, q0:q0 + 512], kc == 0, kc == 7, [wk, 'h2T'], [f'ps{pg}'])
            pu = nps()
            for kc in range(8):
                MM(ps[pu][:, :], Wu[:, kc, fc * 128:(fc + 1) * 128], h2T[:, kc, q0:q0 + 512], kc == 0, kc == 7, [wk, 'h2T'], [f'ps{pu}'])
            A(lambda e, pg=pg, sgl=sgl: e.activation(out=sgl[:], in_=ps[pg][:, :], func=AF.Silu), [f'ps{pg}'], [f'sgl{ub}'])
            V(lambda e, pu=pu, sgl=sgl, tmu=tmu: e.tensor_tensor(out=tmu[:], in0=sgl[:], in1=ps[pu][:, :], op=ALU.mult), [f'sgl{ub}', f'ps{pu}'], [f'tmu{ub}'])
            G(lambda e, fc=fc, tmu=tmu, bcs=bcs, actT=actT: e.tensor_tensor(out=actT[:, fc, :], in0=tmu[:], in1=bcs[:], op=ALU.mult), [f'tmu{ub}', f'bcs{ub}'], [f'actT{ub}'])
        return ub

    def moe_stage2(n, sub, ub):
        sl = n % 2
        Wd = Wd_b[sl]
        wk = f'We{sl}'
        actT = actT_b[ub]
        for j in range(4):
            blk = sub * 4 + j
            for hf in range(2):
                p = nps()
                for fc in range(2):
                    MM(ps[p][:, :], actT[:, fc, j * 128:(j + 1) * 128], Wd[:, fc, hf * 512:(hf + 1) * 512], fc == 0, fc == 1, [f'actT{ub}', wk], [f'ps{p}'])
                V(lambda e, p=p, blk=blk, hf=hf: e.tensor_tensor(out=acc[:, blk, hf * 512:(hf + 1) * 512], in0=acc[:, blk, hf * 512:(hf + 1) * 512], in1=ps[p][:, :], op=ALU.add), ['acc', f'ps{p}'], ['acc'])

    load_expert(0)
    for sti in range(NST):
        s0 = sti * ST
        D(h2T[:], h2T_d.rearrange("(kc p) t -> p kc t", p=128)[:, :, s0:s0 + ST], r=['h2T_d'], w=['h2T'])
        D(cwT[:], cwT_d[:, s0:s0 + ST], r=['cwT_d'], w=['cwT'])
        G(lambda e: e.memset(acc[:], 0.0), (), ['acc'])
        units = [(sti * 16 + ex, sub) for ex in range(16) for sub in range(NSUB)]
        prev = None
        for i in range(len(units) + 1):
            cur = None
            if i < len(units):
                n, sub = units[i]
                ub = moe_stage1(n, sub)
                cur = (n, sub, ub)
            if prev is not None:
                moe_stage2(*prev)
            if cur is not None and cur[1] == 0 and cur[0] + 1 < NST * 16:
                load_expert(cur[0] + 1)
            prev = cur
        for blk in range(NSB):
            r0 = s0 + blk * 128
            ot, otk = outt[blk % 2], f'outt{blk % 2}'
            D(x1f[:], x1_d[r0:r0 + 128, :], r=['x1_d'], w=['x1f'])
            V(lambda e, blk=blk: e.tensor_tensor(out=x1f[:], in0=x1f[:], in1=acc[:, blk, :], op=ALU.add), ['x1f', 'acc'], ['x1f'])
            A(lambda e: e.activation(out=junk3[:], in_=x1f[:], func=AF.Square), ['x1f'], ['junk3'])
            V(lambda e: e.tensor_reduce(out=ssf[:, 0:1], in_=junk3[:], axis=mybir.AxisListType.X, op=ALU.add), ['junk3'], ['Fss'])
            m0 = c.mark()
            rsf = rstd_of(ssf, 1, 'F')
            V(lambda e, ot=ot, rsf=rsf: e.scalar_tensor_tensor(out=ot[:], in0=x1f[:], scalar=rsf[:, 0:1], in1=gfin_t[:], op0=ALU.mult, op1=ALU.mult), ['x1f', 'Frs', 'gfin'], [otk])
            c.release(m0)
            D(out_d[r0:r0 + 128, :], ot[:], r=[otk], w=['out_d'])
    c.emit()
    return nc


def _ssm_layouts(lambda_re, lambda_im, log_step, b_re, b_im, c_re, c_im, ssm_d):
    f = np.float32
    o = {}
    r = np.arange(128)
    k4_r, mp_r, cp_r = r // 32, (r // 16) % 2, r % 16
    s_ = np.arange(4)
    q = np.arange(128)
    m_q, p_q = q // 64, q % 64
    g = 8 * s_[None, :, None] + 2 * k4_r[:, None, None] + m_q[None, None, :]
    P = np.broadcast_to(p_q[None, None, :], g.shape)
    o["LRX"] = lambda_re[g, P].reshape(128, 512).astype(f)
    o["LIX"] = lambda_im[g, P].reshape(128, 512).astype(f)
    o["LSX"] = log_step[g].reshape(128, 512).astype(f)
    msk = (mp_r[:, None, None] == m_q[None, None, :])
    CP = np.broadcast_to(cp_r[:, None, None], g.shape)
    o["BRX"] = np.where(msk, b_re[g, P, CP], 0).reshape(128, 512).astype(f)
    o["BIX"] = np.where(msk, b_im[g, P, CP], 0).reshape(128, 512).astype(f)
    k = np.arange(16)
    j = np.arange(32)
    mp_j, c_j = j // 16, j % 16
    g2 = 2 * k[None, :, None] + m_q[:, None, None] + 0 * j[None, None, :]
    P2 = np.broadcast_to(p_q[:, None, None], g2.shape)
    C2 = np.broadcast_to(c_j[None, None, :], g2.shape)
    msk2 = (mp_j[None, None, :] == m_q[:, None, None])
    o["LRY"] = lambda_re[g2, P2].reshape(128, 512).astype(f)
    o["LIY"] = lambda_im[g2, P2].reshape(128, 512).astype(f)
    o["LSY"] = log_step[g2].reshape(128, 512).astype(f)
    o["CRY"] = np.where(msk2, c_re[g2, C2, P2], 0).reshape(128, 512).astype(f)
    o["CIY"] = np.where(msk2, c_im[g2, C2, P2], 0).reshape(128, 512).astype(f)
    o["BRY"] = np.where(msk2, b_re[g2, P2, C2], 0).reshape(128, 512).astype(f)
    o["BIY"] = np.where(msk2, b_im[g2, P2, C2], 0).reshape(128, 512).astype(f)
    dd = np.zeros((128, 4, 128), f)
    for s in range(4):
        dd[r, s, r] = ssm_d[128 * s + r]
    o["DDG"] = dd.reshape(128, 512)
    return o


_CACHE = {}


def _prep_common(inp):
    f = np.float32
    A_ = lambda a: np.ascontiguousarray(a, dtype=f)
    d = {}
    d["w_in"] = A_(inp["w_in"][0])
    d["gmixT"] = A_(inp["g_mix"][0].reshape(8, 128).T)
    d["bforget"] = A_(inp["b_forget"][0].reshape(8, 1))
    d["bgate"] = A_(inp["b_gate"][0].reshape(16, 128).T)
    d["w_out_a"] = A_(inp["w_out_a"][0])
    d["w_glu"] = A_(inp["w_glu"][0])
    d["bglu"] = A_(inp["b_glu"][0].reshape(4, 128).T)
    d["w_out_b"] = A_(inp["w_out_b"][0])
    d["w_out"] = A_(inp["w_out"][0])
    d["gffnT"] = A_(inp["g_ffn"][0].reshape(8, 128).T)
    d["wr"] = A_(np.concatenate([inp["w_router_group"][0], inp["w_router_expert"][0]], axis=1))
    d["br"] = A_(np.broadcast_to(np.concatenate([inp["b_router_group"][0], inp["b_router_expert"][0]])[None, :], (128, 20)))
    d["w_eg"] = A_(inp["w_exp_gate"][0])
    d["w_eu"] = A_(inp["w_exp_up"][0])
    d["w_ed"] = A_(inp["w_exp_down"][0])
    d["gfin"] = A_(np.broadcast_to(inp["g_final"][None, :], (128, 1024)))
    d.update(_ssm_layouts(np.asarray(inp["lambda_re"][0]), np.asarray(inp["lambda_im"][0]), np.asarray(inp["log_step"][0]),
                          np.asarray(inp["ssm_b_re"][0]), np.asarray(inp["ssm_b_im"][0]), np.asarray(inp["ssm_c_re"][0]),
                          np.asarray(inp["ssm_c_im"][0]), np.asarray(inp["ssm_d"][0])))
    d["ident"] = np.eye(128, dtype=f)
    d["tri"] = np.triu(np.ones((128, 128), f))
    sel = np.zeros((16, 16, 128), f)
    for e in range(16):
        sel[e, e, :] = 1.0
    d["sel"] = sel.reshape(16, 2048)
    return d


def run(inputs, dbg=(), stop=None):
    inp = {k: np.asarray(v) for k, v in inputs.items()}
    x = inp["x"]
    B, S, _ = x.shape
    TH = S // 2
    key = (TH, tuple(dbg), stop)
    if key not in _CACHE:
        _CACHE[key] = build_program(TH, dbg, stop)
    nc = _CACHE[key]
    common = _prep_common(inp)
    in_maps = []
    for core in range(8):
        b, par = core // 2, core % 2
        xin = np.zeros((S, 1024), np.float32)
        if par == 1:
            xin[:TH] = x[b, :TH]
        xin[TH:] = x[b, par * TH:(par + 1) * TH]
        m = dict(common)
        m["xin"] = xin
        m["flag"] = np.full((128, 1), float(par), np.float32)
        in_maps.append(m)
    res = run_bass_kernel_spmd(nc, in_maps, core_ids=list(range(8)))
    return res, TH


def kernel(**inputs):
    res, TH = run(inputs)
    x = inputs["x"]
    B, S, Dm = x.shape
    out = np.zeros((B, S, Dm), np.float32)
    for core in range(8):
        b, par = core // 2, core % 2
        out[b, par * TH:(par + 1) * TH] = res.results[core]["out"]
    return out
```
